# Optimizing a Trainium2 kernel written in Bass

```python
import math
import jax, jax.numpy as jnp
from jax import lax
import numpy as np

D_MODEL = 1024
BATCH = 2
SEQ = 16384
DEPTH = 2

CHUNK = 64
Q_BLOCK = 128
DA_HEADS = 6
DA_HEAD_DIM = 32
DA_V_DIM = 2 * DA_HEAD_DIM
DA_QK = DA_HEADS * 2 * DA_HEAD_DIM
DA_WIDTH = DA_HEADS * DA_V_DIM
GDN_HEADS = 6
GDN_HEAD_DIM = 64
GDN_WIDTH = GDN_HEADS * GDN_HEAD_DIM
CONV_K = 4
S5_GROUP_DIM = 16
S5_GROUPS = 16
S5_WIDTH = S5_GROUPS * S5_GROUP_DIM
S5_STATE = 64
D_MIX = DA_WIDTH + GDN_WIDTH + S5_WIDTH
N_IN = 2 * DA_QK + DA_WIDTH + 4 * GDN_WIDTH + 2 * GDN_HEADS + S5_WIDTH
N_EXPERT_GROUPS = 4
EXPERTS_PER_GROUP = 4
N_EXPERTS = N_EXPERT_GROUPS * EXPERTS_PER_GROUP
TOP_K_INNER = 2
D_EXPERT = 512
ALPHA = (2 * DEPTH) ** 0.25
DEEPNORM_BETA = (8 * DEPTH) ** -0.25
LN_EPS = 1e-5
RMS_EPS = 1e-6

kernel_name = 'hybrid_diffattn_gdn_s5_hmoe_deepnorm'


def split_cols(t, sizes):
    outs, start = [], 0
    for s in sizes:
        outs.append(t[..., start:start + s])
        start += s
    return outs


def layer_norm(x, g, b):
    xf = x.astype(jnp.float32)
    mu = jnp.mean(xf, -1, keepdims=True)
    var = jnp.mean(jnp.square(xf - mu), -1, keepdims=True)
    y = (xf - mu) * lax.rsqrt(var + LN_EPS) * g.astype(jnp.float32) + b.astype(jnp.float32)
    return y.astype(x.dtype)


def rms_norm(x, g):
    xf = x.astype(jnp.float32)
    y = xf * lax.rsqrt(jnp.mean(jnp.square(xf), -1, keepdims=True) + RMS_EPS)
    return (y * g.astype(jnp.float32)).astype(x.dtype)


def l2_normalize(x):
    return x * lax.rsqrt(jnp.sum(jnp.square(x), -1, keepdims=True) + RMS_EPS)


def causal_depthwise_conv(x, w):
    K, C = w.shape
    return lax.conv_general_dilated(x, w[:, None, :], window_strides=(1,), padding=[(K - 1, 0)],
                                    dimension_numbers=('NWC', 'WIO', 'NWC'), feature_group_count=C)


def diff_attention(q, k, v, lam, norm_g, lam_init):
    Bsz, S, H, _, dh = q.shape
    dv = v.shape[-1]
    nb = S // Q_BLOCK
    qb = jnp.swapaxes(q.reshape(Bsz, nb, Q_BLOCK, H, 2, dh), 0, 1)
    k_chunk = jnp.arange(S) // CHUNK
    vf = v.astype(jnp.float32)
    scale = dh ** -0.5

    def block(args):
        qi, i = args
        s = jnp.einsum('bqhcd,bkhcd->bhcqk', qi, k).astype(jnp.float32) * scale
        q_chunk = (i * Q_BLOCK + jnp.arange(Q_BLOCK)) // CHUNK
        mask = k_chunk[None, :] <= q_chunk[:, None]
        p = jax.nn.softmax(jnp.where(mask, s, -jnp.inf), axis=-1)
        a = p[:, :, 0] - lam * p[:, :, 1]
        return jnp.einsum('bhqk,bkhe->bqhe', a, vf)

    o = lax.map(block, (qb, jnp.arange(nb)))
    o = jnp.swapaxes(o, 0, 1).reshape(Bsz, S, H, dv)
    o = rms_norm(o, norm_g) * (1.0 - lam_init)
    return o.reshape(Bsz, S, H * dv).astype(v.dtype)


def chunked_gated_delta_rule(q, k, v, beta, g):
    Bsz, S, H, dk = q.shape
    dv = v.shape[-1]
    n, C = S // CHUNK, CHUNK

    def chunks(t):
        return t.reshape(Bsz, n, C, H, -1).transpose(0, 3, 1, 2, 4)

    q = chunks(q) * dk ** -0.5
    k = chunks(k)
    v = chunks(v)
    beta = beta.reshape(Bsz, n, C, H).transpose(0, 3, 1, 2)
    gc = jnp.cumsum(g.reshape(Bsz, n, C, H).transpose(0, 3, 1, 2), axis=-1)
    tri = jnp.tril(jnp.ones((C, C), bool))
    strict = jnp.tril(jnp.ones((C, C), bool), -1)
    decay = jnp.exp(jnp.where(tri, gc[..., :, None] - gc[..., None, :], -jnp.inf))
    kb = k * beta[..., None]
    l_mat = jnp.where(strict, jnp.einsum('bhnid,bhnjd->bhnij', kb, k) * decay, 0.0)
    rhs = jnp.concatenate([v * beta[..., None], kb * jnp.exp(gc)[..., None]], -1)
    sol = lax.linalg.triangular_solve(l_mat + jnp.eye(C, dtype=l_mat.dtype), rhs,
                                      left_side=True, lower=True)
    u, w = sol[..., :dv], sol[..., dv:]
    a_intra = jnp.einsum('bhnid,bhnjd->bhnij', q, k) * decay
    q_dec = q * jnp.exp(gc)[..., None]
    g_last = gc[..., -1]
    k_dec = k * jnp.exp(g_last[..., None] - gc)[..., None]

    def step(state, inp):
        qd, a, uu, ww, kd, gl = inp
        v_new = uu - jnp.einsum('bhcd,bhde->bhce', ww, state)
        o = jnp.einsum('bhcd,bhde->bhce', qd, state) + jnp.einsum('bhij,bhje->bhie', a, v_new)
        state = state * jnp.exp(gl)[..., None, None] + jnp.einsum('bhcd,bhce->bhde', kd, v_new)
        return state, o

    xs = tuple(jnp.moveaxis(t, 2, 0) for t in (q_dec, a_intra, u, w, k_dec, g_last))
    s0 = jnp.zeros((Bsz, H, dk, dv), jnp.float32)
    _, o = lax.scan(step, s0, xs)
    return o.transpose(1, 0, 3, 2, 4).reshape(Bsz, S, H, dv)


def gdn_mixer(qkv, gate, beta_raw, a_raw, conv_w, a_log, dt_bias, norm_g):
    Bsz, S, _ = qkv.shape
    qkv = jax.nn.silu(causal_depthwise_conv(qkv, conv_w)).astype(jnp.float32)
    q, k, v = split_cols(qkv, (GDN_WIDTH, GDN_WIDTH, GDN_WIDTH))
    hs = (Bsz, S, GDN_HEADS, GDN_HEAD_DIM)
    q = l2_normalize(q.reshape(hs))
    k = l2_normalize(k.reshape(hs))
    v = v.reshape(hs)
    beta = jax.nn.sigmoid(beta_raw.astype(jnp.float32))
    g = -jnp.exp(a_log.astype(jnp.float32)) * jax.nn.softplus(
        a_raw.astype(jnp.float32) + dt_bias.astype(jnp.float32))
    o = chunked_gated_delta_rule(q, k, v, beta, g)
    o = rms_norm(o, norm_g) * jax.nn.silu(gate.astype(jnp.float32)).reshape(hs)
    return o.reshape(Bsz, S, GDN_WIDTH).astype(gate.dtype)


def _complex_affine_combine(e1, e2):
    a1r, a1i, b1r, b1i = e1
    a2r, a2i, b2r, b2i = e2
    return (a2r * a1r - a2i * a1i,
            a2r * a1i + a2i * a1r,
            a2r * b1r - a2i * b1i + b2r,
            a2r * b1i + a2i * b1r + b2i)


def s5_mixer(u, lam_re, lam_im, log_dt, b_re, b_im, c_re, c_im, d, w_glu):
    f32 = jnp.float32
    Bsz, S, _ = u.shape
    uf = u.astype(f32)
    ug = uf.reshape(Bsz, S, S5_GROUPS, S5_GROUP_DIM)
    lre, lim = lam_re.astype(f32), lam_im.astype(f32)
    dt = jnp.exp(log_dt.astype(f32))[:, None]
    mag = jnp.exp(lre * dt)
    ab_re, ab_im = mag * jnp.cos(lim * dt), mag * jnp.sin(lim * dt)
    num_re, num_im = ab_re - 1.0, ab_im
    den = lre * lre + lim * lim
    coef_re = (num_re * lre + num_im * lim) / den
    coef_im = (num_im * lre - num_re * lim) / den
    br, bi = b_re.astype(f32), b_im.astype(f32)
    bb_re = coef_re[..., None] * br - coef_im[..., None] * bi
    bb_im = coef_re[..., None] * bi + coef_im[..., None] * br
    bu_re = jnp.einsum('bsgh,gph->bsgp', ug, bb_re)
    bu_im = jnp.einsum('bsgh,gph->bsgp', ug, bb_im)
    a_re = jnp.broadcast_to(ab_re, bu_re.shape)
    a_im = jnp.broadcast_to(ab_im, bu_re.shape)
    _, _, x_re, x_im = lax.associative_scan(_complex_affine_combine, (a_re, a_im, bu_re, bu_im), axis=1)
    y = (jnp.einsum('ghp,bsgp->bsgh', c_re.astype(f32), x_re)
         - jnp.einsum('ghp,bsgp->bsgh', c_im.astype(f32), x_im))
    y = y.reshape(Bsz, S, S5_WIDTH) + d.astype(f32) * uf
    y = jax.nn.gelu(y)
    y = y * jax.nn.sigmoid(y @ w_glu.astype(f32))
    return y.astype(u.dtype)


def hier_moe(h, w_grp, b_grp, w_exp, b_exp, w1, w3, w2):
    Bsz, S, D = h.shape
    hf = h.reshape(-1, D)
    t = hf.shape[0]
    g_prob = jax.nn.softmax((hf @ w_grp + b_grp).astype(jnp.float32), axis=-1)
    g_p, g_idx = lax.top_k(g_prob, 1)
    g_onehot = jax.nn.one_hot(g_idx[:, 0], N_EXPERT_GROUPS, dtype=jnp.float32)
    e_logits = (hf @ w_exp + b_exp).astype(jnp.float32).reshape(t, N_EXPERT_GROUPS, EXPERTS_PER_GROUP)
    e_sel = jnp.sum(e_logits * g_onehot[:, :, None], axis=1)
    e_val, e_idx = lax.top_k(e_sel, TOP_K_INNER)
    e_w = jax.nn.softmax(e_val, axis=-1) * g_p
    glob = g_idx * EXPERTS_PER_GROUP + e_idx
    comb = jnp.sum(jax.nn.one_hot(glob, N_EXPERTS, dtype=jnp.float32) * e_w[..., None], axis=1)
    out = jnp.zeros((t, D), jnp.float32)
    for e in range(N_EXPERTS):
        hid = jax.nn.silu(hf @ w1[e]) * (hf @ w3[e])
        out = out + comb[:, e:e + 1] * (hid @ w2[e]).astype(jnp.float32)
    return out.reshape(Bsz, S, D).astype(h.dtype)


def setup_inputs(seed: int = 0) -> dict:
    key = jax.random.key(seed)
    ks = iter(jax.random.split(key, 48))
    f32 = jnp.float32
    L = DEPTH

    def nrm(shape, scale):
        return jax.random.normal(next(ks), shape, f32) * scale

    def unif(shape, lo, hi):
        return jax.random.uniform(next(ks), shape, f32, lo, hi)

    x = nrm((BATCH, SEQ, D_MODEL), 1.0)
    ln_in_g = 1.0 + nrm((D_MODEL,), 0.02)
    ln_in_b = nrm((D_MODEL,), 0.02)
    w_in = nrm((L, D_MODEL, N_IN), D_MODEL ** -0.5)
    w_out = nrm((L, D_MIX, D_MODEL), D_MIX ** -0.5 * DEEPNORM_BETA)
    lam_q1 = nrm((L, DA_HEAD_DIM), 0.1)
    lam_k1 = nrm((L, DA_HEAD_DIM), 0.1)
    lam_q2 = nrm((L, DA_HEAD_DIM), 0.1)
    lam_k2 = nrm((L, DA_HEAD_DIM), 0.1)
    diff_norm_g = 1.0 + nrm((L, DA_V_DIM), 0.02)
    dn_conv_w = nrm((L, CONV_K, 3 * GDN_WIDTH), CONV_K ** -0.5)
    dn_a_log = jnp.log(unif((L, GDN_HEADS), 1.0, 16.0))
    dt0 = jnp.exp(unif((L, GDN_HEADS), math.log(1e-3), math.log(1e-1)))
    dn_dt_bias = dt0 + jnp.log(-jnp.expm1(-dt0))
    dn_norm_g = 1.0 + nrm((L, GDN_HEAD_DIM), 0.02)
    n_idx = jnp.arange(S5_STATE, dtype=f32)
    s5_lambda_re = -0.5 + nrm((L, S5_GROUPS, S5_STATE), 0.01)
    s5_lambda_im = math.pi * n_idx + nrm((L, S5_GROUPS, S5_STATE), 0.01)
    s5_log_dt = unif((L, S5_GROUPS), math.log(1e-3), math.log(1e-1))
    s5_b_re = nrm((L, S5_GROUPS, S5_STATE, S5_GROUP_DIM), (2 * S5_GROUP_DIM) ** -0.5)
    s5_b_im = nrm((L, S5_GROUPS, S5_STATE, S5_GROUP_DIM), (2 * S5_GROUP_DIM) ** -0.5)
    s5_c_re = nrm((L, S5_GROUPS, S5_GROUP_DIM, S5_STATE), S5_STATE ** -0.5)
    s5_c_im = nrm((L, S5_GROUPS, S5_GROUP_DIM, S5_STATE), S5_STATE ** -0.5)
    s5_d = nrm((L, S5_WIDTH), 1.0)
    s5_w_glu = nrm((L, S5_WIDTH, S5_WIDTH), S5_WIDTH ** -0.5)
    ln1_g = 1.0 + nrm((L, D_MODEL), 0.02)
    ln1_b = nrm((L, D_MODEL), 0.02)
    moe_w_grp = nrm((L, D_MODEL, N_EXPERT_GROUPS), D_MODEL ** -0.5)
    moe_b_grp = nrm((L, N_EXPERT_GROUPS), 0.01)
    moe_w_exp = nrm((L, D_MODEL, N_EXPERTS), D_MODEL ** -0.5)
    moe_b_exp = nrm((L, N_EXPERTS), 0.01)
    moe_w1 = nrm((L, N_EXPERTS, D_MODEL, D_EXPERT), D_MODEL ** -0.5)
    moe_w3 = nrm((L, N_EXPERTS, D_MODEL, D_EXPERT), D_MODEL ** -0.5)
    moe_w2 = nrm((L, N_EXPERTS, D_EXPERT, D_MODEL), D_EXPERT ** -0.5 * DEEPNORM_BETA)
    ln2_g = 1.0 + nrm((L, D_MODEL), 0.02)
    ln2_b = nrm((L, D_MODEL), 0.02)
    return {'x': x, 'ln_in_g': ln_in_g, 'ln_in_b': ln_in_b, 'w_in': w_in, 'w_out': w_out,
            'lam_q1': lam_q1, 'lam_k1': lam_k1, 'lam_q2': lam_q2, 'lam_k2': lam_k2,
            'diff_norm_g': diff_norm_g, 'dn_conv_w': dn_conv_w, 'dn_a_log': dn_a_log,
            'dn_dt_bias': dn_dt_bias, 'dn_norm_g': dn_norm_g,
            's5_lambda_re': s5_lambda_re, 's5_lambda_im': s5_lambda_im, 's5_log_dt': s5_log_dt,
            's5_b_re': s5_b_re, 's5_b_im': s5_b_im, 's5_c_re': s5_c_re, 's5_c_im': s5_c_im,
            's5_d': s5_d, 's5_w_glu': s5_w_glu, 'ln1_g': ln1_g, 'ln1_b': ln1_b,
            'moe_w_grp': moe_w_grp, 'moe_b_grp': moe_b_grp, 'moe_w_exp': moe_w_exp,
            'moe_b_exp': moe_b_exp, 'moe_w1': moe_w1, 'moe_w3': moe_w3, 'moe_w2': moe_w2,
            'ln2_g': ln2_g, 'ln2_b': ln2_b}


def reference(x, ln_in_g, ln_in_b, w_in, w_out, lam_q1, lam_k1, lam_q2, lam_k2,
              diff_norm_g, dn_conv_w, dn_a_log, dn_dt_bias, dn_norm_g,
              s5_lambda_re, s5_lambda_im, s5_log_dt, s5_b_re, s5_b_im, s5_c_re, s5_c_im,
              s5_d, s5_w_glu, ln1_g, ln1_b, moe_w_grp, moe_b_grp, moe_w_exp, moe_b_exp,
              moe_w1, moe_w3, moe_w2, ln2_g, ln2_b):
    Bsz, S, _ = x.shape
    h = layer_norm(x, ln_in_g, ln_in_b)
    for l in range(DEPTH):
        lam_init = 0.8 - 0.6 * math.exp(-0.3 * l)
        proj = h @ w_in[l]
        a_q, a_k, a_v, b_qkv, b_gate, b_beta, b_a, c_u = split_cols(
            proj, (DA_QK, DA_QK, DA_WIDTH, 3 * GDN_WIDTH, GDN_WIDTH, GDN_HEADS, GDN_HEADS, S5_WIDTH))
        lam = (jnp.exp(jnp.sum(lam_q1[l] * lam_k1[l])) - jnp.exp(jnp.sum(lam_q2[l] * lam_k2[l]))).astype(jnp.float32) + lam_init
        y_a = diff_attention(a_q.reshape(Bsz, S, DA_HEADS, 2, DA_HEAD_DIM),
                             a_k.reshape(Bsz, S, DA_HEADS, 2, DA_HEAD_DIM),
                             a_v.reshape(Bsz, S, DA_HEADS, DA_V_DIM), lam, diff_norm_g[l], lam_init)
        y_b = gdn_mixer(b_qkv, b_gate, b_beta, b_a, dn_conv_w[l], dn_a_log[l], dn_dt_bias[l], dn_norm_g[l])
        y_c = s5_mixer(c_u, s5_lambda_re[l], s5_lambda_im[l], s5_log_dt[l], s5_b_re[l], s5_b_im[l],
                       s5_c_re[l], s5_c_im[l], s5_d[l], s5_w_glu[l])
        mix = jnp.concatenate([y_a.astype(h.dtype), y_b.astype(h.dtype), y_c.astype(h.dtype)], -1) @ w_out[l]
        h = layer_norm(ALPHA * h + mix, ln1_g[l], ln1_b[l])
        ffn = hier_moe(h, moe_w_grp[l], moe_b_grp[l], moe_w_exp[l], moe_b_exp[l],
                       moe_w1[l], moe_w3[l], moe_w2[l])
        h = layer_norm(ALPHA * h + ffn, ln2_g[l], ln2_b[l])
    return h
```

```python
import math
from contextlib import ExitStack

import numpy as np
import ml_dtypes
import concourse.bass as bass
import concourse.mybir as mybir
from concourse.bass_utils import run_bass_kernel_spmd

F32 = mybir.dt.float32
BF16 = mybir.dt.bfloat16
I32 = mybir.dt.int32
ALU = mybir.AluOpType
AF = mybir.ActivationFunctionType
AX = mybir.AxisListType

NCORES = 8


class Prog:
    COMPUTE = ("pe", "act", "dve", "pool")
    NDMASEM = 6

    _uid = 0
    _phase = 0

    def __init__(self, nc):
        Prog._phase += 1
        self.ph = Prog._phase
        self.nc = nc
        self.ops = []
        self.last_w = {}
        self.readers = {}
        self.dma_count = {"sp": 0, "actq": 0, "poolq": 0}
        self.stack = ExitStack()
        self.nt = 0
        self.excl = set()
        self.sp_wrap = None

    def sb(self, shape, dtype, name=None):
        Prog._uid += 1
        return self.stack.enter_context(self.nc.sbuf_tensor(f"{name or 't'}_{Prog._uid}", list(shape), dtype))

    def ps(self, shape, dtype=F32, name=None):
        Prog._uid += 1
        return self.stack.enter_context(self.nc.psum_tensor(f"{name or 'p'}_{Prog._uid}", list(shape), dtype))

    def op(self, eng, fn, reads=(), writes=()):
        idx = len(self.ops)
        isdma = eng in self.dma_count
        issue = {"sp": "sp", "actq": "act", "poolq": "pool"}.get(eng, eng)
        if self.excl:
            writes = list(writes) + [k for k in reads if k in self.excl]
            reads = [k for k in reads if k not in self.excl]
        deps = set()
        for k in reads:
            w = self.last_w.get(k)
            if w is not None:
                deps.add(w)
        for k in writes:
            w = self.last_w.get(k)
            if w is not None:
                deps.add(w)
            for r in self.readers.get(k, ()):
                deps.add(r)
        o = dict(idx=idx, eng=eng, issue=issue, fn=fn, deps=deps, isdma=isdma, needed=False)
        if isdma:
            n = self.dma_count[eng]
            self.dma_count[eng] = n + 1
            o["dsem"] = n % self.NDMASEM
            o["dtarget"] = 16 * (n // self.NDMASEM + 1)
            o["dprev"] = 16 * (n // self.NDMASEM)
        self.ops.append(o)
        for k in writes:
            self.last_w[k] = idx
            self.readers[k] = []
        for k in reads:
            lst = self.readers.setdefault(k, [])
            if not isdma:
                lst[:] = [r for r in lst if self.ops[r]["isdma"] or self.ops[r]["eng"] != eng]
            lst.append(idx)
        return idx

    def emit(self, final_wait_keys=()):
        nc = self.nc
        ops = self.ops
        for o in ops:
            nd = set()
            for d in o["deps"]:
                p = ops[d]
                if (not p["isdma"]) and (not o["isdma"]) and p["eng"] == o["eng"]:
                    if o["eng"] == "pe":
                        continue
                nd.add(d)
            o["deps"] = nd
            for d in nd:
                ops[d]["needed"] = True
        final = [self.last_w[k] for k in final_wait_keys if k in self.last_w]
        for d in final:
            ops[d]["needed"] = True
        tick = {e: 0 for e in self.COMPUTE}
        for o in ops:
            if not o["isdma"] and o["needed"]:
                tick[o["eng"]] += 1
                o["tick"] = tick[o["eng"]]
        sems = {e: self.stack.enter_context(nc.semaphore(f"s_{e}_{self.ph}")) for e in self.COMPUTE}
        dsems = {q: [self.stack.enter_context(nc.semaphore(f"d_{q}{i}_{self.ph}")) for i in range(self.NDMASEM)]
                 for q in self.dma_count}
        per = {e: [] for e in ("pe", "act", "dve", "pool", "sp")}
        for o in ops:
            per[o["issue"]].append(o)
        engobj = {"pe": nc.tensor, "act": nc.scalar, "dve": nc.vector, "pool": nc.gpsimd, "sp": nc.sync}

        def run(ename, extra_final=False):
            eng = engobj[ename]
            waited = {}

            def wait_for(p):
                if p["isdma"]:
                    key = (p["eng"], p["dsem"])
                    val = p["dtarget"]
                    s = dsems[p["eng"]][p["dsem"]]
                else:
                    key = p["eng"]
                    val = p["tick"]
                    s = sems[p["eng"]]
                if waited.get(key, 0) >= val:
                    return
                waited[key] = val
                eng.wait_ge(s, val)

            for o in per[ename]:
                for d in sorted(o["deps"]):
                    wait_for(ops[d])
                if o["isdma"] and o["dprev"] > 0:
                    key = (o["eng"], o["dsem"])
                    if waited.get(key, 0) < o["dprev"]:
                        waited[key] = o["dprev"]
                        eng.wait_ge(dsems[o["eng"]][o["dsem"]], o["dprev"])
                ins = o["fn"]()
                if o["isdma"]:
                    ins.then_inc(dsems[o["eng"]][o["dsem"]], 16)
                elif o["needed"]:
                    ins.then_inc(sems[o["eng"]], 1)
            if extra_final:
                for d in final:
                    wait_for(ops[d])

        allsems = list(sems.values()) + [s for q in dsems.values() for s in q]
        with nc.Block() as block:
            @block.gpsimd
            def _(e):
                for s in allsems:
                    e.sem_clear(s)

        with nc.Block() as block:
            @block.sync
            def _(e):
                if self.sp_wrap is not None:
                    with self.sp_wrap(e):
                        run("sp", extra_final=True)
                else:
                    run("sp", extra_final=True)

            @block.tensor
            def _(e):
                run("pe")

            @block.scalar
            def _(e):
                run("act")

            @block.vector
            def _(e):
                run("dve")

            @block.gpsimd
            def _(e):
                run("pool")


D = 1024
SEQ = 16384
BATCH = 2
DEPTH = 2
TOK_CORE = 4096
ALPHA = (2 * DEPTH) ** 0.25
LN_EPS = 1e-5
RMS_EPS = 1e-6
NEXP = 16
DEXP = 512


def bf(a):
    return np.ascontiguousarray(np.asarray(a, np.float32).astype(ml_dtypes.bfloat16))


def f32c(a):
    return np.ascontiguousarray(np.asarray(a, np.float32))


class Ctx:
    def __init__(self, nc):
        self.nc = nc
        self.P = Prog(nc)
        self.banks = None

    def alloc_banks(self):
        self.banks = [self.P.ps([128, 512], F32, name=f"bank{i}") for i in range(8)]
        self.P.excl |= {f"b{i}" for i in range(8)}


class Glob:
    def __init__(self, nc):
        self.nc = nc
        self.t = {}

    def din(self, name, shape, dt=F32):
        if name not in self.t:
            self.t[name] = self.nc.dram_tensor(name, list(shape), dt, kind="ExternalInput").ap()
        return self.t[name]

    def internal(self, name, shape, dt=F32):
        if name not in self.t:
            self.t[name] = self.nc.dram_tensor(name, list(shape), dt).ap()
        return self.t[name]


GROUPS = [[0, 1, 2, 3], [4, 5, 6, 7]]


def allgather(nc, pairs):
    Prog._uid += 1
    with nc.semaphore(f"cc_{Prog._uid}") as cc:
        with nc.Block() as block:
            @block.gpsimd
            def _(g):
                g.sem_clear(cc)
        with nc.Block() as block:
            @block.gpsimd
            def _(g):
                for src, dst in pairs:
                    g.collective_compute("AllGather", ALU.bypass, replica_groups=GROUPS, ins=[src], outs=[dst]).then_inc(cc, 1)
                g.wait_ge(cc, len(pairs))


def dma(P, q, out, in_, reads=(), writes=()):
    eng = {"sp": P.nc.sync, "actq": P.nc.scalar, "poolq": P.nc.gpsimd}[q]
    return P.op(q, lambda: eng.dma_start(out=out, in_=in_), reads=reads, writes=writes)


def layer_norm_tile(P, nc, src, srck, dst, dstk, g_t, b_t, gk, bk, scr, tag):
    st, mv, rstd, xn = scr["st"], scr["mv"], scr["rstd"], scr["xn"]

    def bs():
        nc.vector.bn_stats(out=st[:, 0, :], in_=src[:, 0:512])
        return nc.vector.bn_stats(out=st[:, 1, :], in_=src[:, 512:1024])
    P.op("dve", bs, reads=[srck], writes=[tag + "st"])
    P.op("dve", lambda: nc.vector.bn_aggr(out=mv[:], in_=st[:].rearrange("p a s -> p (a s)")),
         reads=[tag + "st"], writes=[tag + "mv"])
    P.op("act", lambda: nc.scalar.activation(out=rstd[:], in_=mv[:, 1:2], func=AF.Sqrt, bias=scr["eps_ln"][:, 0:1], scale=1.0),
         reads=[tag + "mv"], writes=[tag + "rstd"])
    P.op("dve", lambda: nc.vector.reciprocal(out=rstd[:], in_=rstd[:]), reads=[tag + "rstd"], writes=[tag + "rstd"])
    P.op("dve", lambda: nc.vector.tensor_scalar(out=xn[:], in0=src[:], scalar1=mv[:, 0:1], scalar2=rstd[:, 0:1],
                                                op0=ALU.subtract, op1=ALU.mult),
         reads=[srck, tag + "mv", tag + "rstd"], writes=[tag + "xn"])
    P.op("pool", lambda: nc.gpsimd.tensor_tensor(out=xn[:], in0=xn[:], in1=g_t[:], op=ALU.mult),
         reads=[tag + "xn", gk], writes=[tag + "xn"])
    P.op("pool", lambda: nc.gpsimd.tensor_tensor(out=dst[:], in0=xn[:], in1=b_t[:], op=ALU.add),
         reads=[tag + "xn", bk], writes=[dstk])


def ln_scratch(P, tag):
    return dict(st=P.sb([128, 2, 6], F32), mv=P.sb([128, 2], F32), rstd=P.sb([128, 1], F32),
                xn=P.sb([128, 1024], F32))


def const_col(P, nc, val, key):
    t = P.sb([128, 1], F32)
    P.op("pool", lambda: nc.gpsimd.memset(t[:], val), writes=[key])
    return t


def to_featmajor_bf16(P, nc, src, srck, hb, hbk, bank, bankk, dstT, dstk, ident, cast_eng="act"):
    if cast_eng == "act":
        P.op("act", lambda: nc.scalar.copy(out=hb[:], in_=src[:]), reads=[srck], writes=[hbk])
    else:
        P.op("pool", lambda: nc.gpsimd.tensor_copy(out=hb[:], in_=src[:]), reads=[srck], writes=[hbk])
    pv = bank[:].bitcast(BF16).rearrange("p (k t) -> p k t", k=8)

    def tr():
        for kc in range(8):
            ins = nc.tensor.transpose(out=pv[:, kc, :], in_=hb[:, kc * 128:(kc + 1) * 128], identity=ident[:])
        return ins
    P.op("pe", tr, reads=[hbk, "ident"], writes=[bankk])
    P.op("dve", lambda: nc.vector.tensor_copy(out=dstT, in_=pv), reads=[bankk], writes=[dstk])


def phase_pre(nc, G, h, hT_loc, ntok=TOK_CORE):
    C = Ctx(nc)
    P = C.P
    nt = ntok // 128
    x = G.din("x", [ntok, D])
    g = G.din("g", [1, D])
    b = G.din("b", [1, D])
    idn = G.din("idn", [128, 128], BF16)
    hT = hT_loc.rearrange("(k p) t -> k p t", k=8)
    with P.stack:
        C.alloc_banks()
        gt = P.sb([128, D], F32)
        bt = P.sb([128, D], F32)
        ident = P.sb([128, 128], BF16)
        eps = const_col(P, nc, LN_EPS, "eps_ln")
        xt = [P.sb([128, D], F32) for _ in range(2)]
        ht = [P.sb([128, D], F32) for _ in range(2)]
        hb = P.sb([128, D], BF16)
        hTt = [P.sb([128, 8, 128], BF16) for _ in range(2)]
        scr = ln_scratch(P, "ln")
        scr["eps_ln"] = eps
        dma(P, "sp", gt[:], g.partition_broadcast(128), writes=["g"])
        dma(P, "sp", bt[:], b.partition_broadcast(128), writes=["b"])
        dma(P, "sp", ident[:], idn, writes=["ident"])
        outs = []
        for t in range(nt):
            s = t % 2
            dma(P, "sp", xt[s][:], x[t * 128:(t + 1) * 128, :], writes=[("xt", s)])
            layer_norm_tile(P, nc, xt[s], ("xt", s), ht[s], ("ht", s), gt, bt, "g", "b", scr, "ln")
            dma(P, "poolq", h[t * 128:(t + 1) * 128, :], ht[s][:], reads=[("ht", s)], writes=[("h", t)])
            to_featmajor_bf16(P, nc, ht[s], ("ht", s), hb, "hb", C.banks[s], f"b{s}", hTt[s][:], ("hTt", s), ident)
            dma(P, "sp", hT[:, :, t * 128:(t + 1) * 128].rearrange("k p t -> p k t"), hTt[s][:],
                reads=[("hTt", s)], writes=[("hT", t)])
            outs += [("h", t), ("hT", t)]
        P.emit(final_wait_keys=outs)


BIG = 1.0e4


def phase_post(nc, G, l, h_in, h_out, hT_loc, y_all, ntok=TOK_CORE):
    C = Ctx(nc)
    P = C.P
    nsup = ntok // 512
    pf = f"L{l}_"

    def din(name, shape, dt=F32):
        return G.din(pf + name, shape, dt)
    wout = din("wout", [128, 8, D], BF16)
    wglu = din("wglu", [128, 2, 256], BF16)
    ln1g = din("ln1g", [1, D]); ln1b = din("ln1b", [1, D]); ln2g = din("ln2g", [1, D]); ln2b = din("ln2b", [1, D])
    dng = din("dng", [1, 64]); lamv = din("lamv", [1, 130])
    wr = din("wr", [128, 8, 20]); br = din("br", [1, 20])
    w1 = din("w1", [NEXP, 128, 8, DEXP], BF16); w3 = din("w3", [NEXP, 128, 8, DEXP], BF16)
    w2 = din("w2", [NEXP, 128, 4, D], BF16)
    idn = G.din("idn", [128, 128], BF16); idn32 = G.din("idn32", [128, 128])
    rofs = G.din("rofs", [1, 1], I32)
    hT_out = hT_loc.rearrange("(k p) t -> k p t", k=8) if hT_loc is not None else None
    ymj = G.internal("y_mine", [4, ntok, 384])
    ym = ymj.rearrange("j s c -> s j c")
    offh = {}

    from contextlib import contextmanager

    @contextmanager
    def sp_wrap(sp):
        with sp.register(f"rofs{l}") as reg:
            sp.reg_load(reg, rofs[0:1, 0:1])
            offh["v"] = sp.snap(reg)
            yield
    P.sp_wrap = sp_wrap
    nch = ntok // YCH
    yj = y_all.rearrange("i (j s) c -> j i s c", j=4)
    for j in range(4):
        P.op("sp", lambda j=j: nc.sync.dma_start(out=ymj[j].rearrange("(i a b) c -> i a (b c)", i=nch, a=16),
                                                 in_=yj[j][bass.ds(offh["v"], nch)].rearrange("i (a b) c -> i a (b c)", a=16)),
             writes=[("ymine", j)])

    with P.stack:
        C.alloc_banks()
        B = C.banks
        sb = P.sb
        ident = sb([128, 128], BF16); ident32 = sb([128, 128], F32)
        g1 = sb([128, D], F32); b1 = sb([128, D], F32); g2 = sb([128, D], F32); b2 = sb([128, D], F32)
        wout_t = sb([128, 8, D], BF16); wglu_t = sb([128, 2, 256], BF16)
        wr_t = sb([128, 8, 20], F32); br_t = sb([128, 20], F32)
        gA = sb([128, 64], F32); lam_t = sb([128, 130], F32); lprod = sb([128, 2, 32], F32)
        lsum = sb([128, 2], F32); nlam = sb([128, 1], F32)
        eps_ln = const_col(P, nc, LN_EPS, "eps_ln"); eps_rms = const_col(P, nc, RMS_EPS, "eps_rms")
        scr = ln_scratch(P, "ln"); scr["eps_ln"] = eps_ln
        ht = [sb([128, D], F32) for _ in range(2)]
        ya_t = [sb([128, 768], F32) for _ in range(2)]
        yc_t = [sb([128, 256], F32) for _ in range(2)]
        ymix = [sb([128, D], F32) for _ in range(2)]
        dd = sb([128, 6, 64], F32); sq = sb([128, 6, 64], F32); ss = sb([128, 6], F32)
        c2 = sb([128, 256], F32); c3 = sb([128, 256], F32); ygb = sb([128, 256], BF16)
        ygT = sb([128, 2, 128], BF16); sig = sb([128, 256], F32)
        ymb = sb([128, D], BF16); ymT = sb([128, 8, 128], BF16)
        z = sb([128, D], F32)
        h1 = [sb([128, D], F32) for _ in range(4)]
        h1T32 = sb([128, 8, 128], F32)
        h1T = sb([128, 8, 512], BF16)
        lg = sb([128, 20], F32)
        r = {n: sb([128, s], F32) for n, s in dict(gmax=1, goh=4, ngmax=1, gexp=4, gsum=1, gp=1, em=16, pen=4, m1=1, oh1=16,
                                                   em2=16, m2=1, oh2=16, dl=1, ex=1, den=1, w1=1, w2=1).items()}
        comb = sb([128, 4, 16], F32)
        wA = [sb([128, 8, DEXP], BF16) for _ in range(2)]
        wB = [sb([128, 8, DEXP], BF16) for _ in range(2)]
        wC = [sb([128, 4, D], BF16) for _ in range(2)]
        sil = [sb([128, 512], F32) for _ in range(2)]
        hid = [sb([128, 512], BF16) for _ in range(4)]
        acc = [sb([128, D], F32) for _ in range(4)]
        z2 = sb([128, D], F32)
        ho = [sb([128, D], F32) for _ in range(2)]
        hob = sb([128, D], BF16)
        hoT = [sb([128, 8, 128], BF16) for _ in range(2)]

        dma(P, "sp", ident[:], idn, writes=["ident"]); dma(P, "sp", ident32[:], idn32, writes=["ident32"])
        for t_, s_, k_ in ((g1, ln1g, "g1"), (b1, ln1b, "b1"), (g2, ln2g, "g2"), (b2, ln2b, "b2")):
            dma(P, "sp", t_[:], s_.partition_broadcast(128), writes=[k_])
        dma(P, "sp", wout_t[:], wout, writes=["wout"]); dma(P, "sp", wglu_t[:], wglu, writes=["wglu"])
        dma(P, "sp", wr_t[:], wr, writes=["wr"]); dma(P, "sp", br_t[:], br.partition_broadcast(128), writes=["br"])
        dma(P, "sp", gA[:], dng.partition_broadcast(128), writes=["gA"])
        dma(P, "sp", lam_t[:], lamv.partition_broadcast(128), writes=["lamt"])
        P.op("dve", lambda: nc.vector.tensor_scalar(out=gA[:], in0=gA[:], scalar1=lam_t[:, 128:129], scalar2=None, op0=ALU.mult),
             reads=["gA", "lamt"], writes=["gA"])
        lv = lam_t[:, 0:128].rearrange("p (a b c) -> p a b c", a=2, b=2)
        P.op("dve", lambda: nc.vector.tensor_tensor(out=lprod[:], in0=lv[:, :, 0, :], in1=lv[:, :, 1, :], op=ALU.mult),
             reads=["lamt"], writes=["lprod"])
        P.op("dve", lambda: nc.vector.tensor_reduce(out=lsum[:], in_=lprod[:], axis=AX.X, op=ALU.add), reads=["lprod"], writes=["lsum"])
        P.op("act", lambda: nc.scalar.activation(out=lsum[:], in_=lsum[:], func=AF.Exp), reads=["lsum"], writes=["lsum"])
        P.op("dve", lambda: nc.vector.tensor_tensor(out=nlam[:], in0=lsum[:, 1:2], in1=lsum[:, 0:1], op=ALU.subtract),
             reads=["lsum"], writes=["nlam"])
        P.op("dve", lambda: nc.vector.tensor_scalar(out=nlam[:], in0=nlam[:], scalar1=lam_t[:, 129:130], scalar2=None, op0=ALU.add),
             reads=["nlam", "lamt"], writes=["nlam"])

        outs = []
        for su in range(nsup):
            for tt in range(4):
                t = su * 4 + tt
                s = t % 2
                rows = slice(t * 128, (t + 1) * 128)
                dma(P, "sp", ht[s][:], h_in[rows, :], writes=[("ht", s)])
                ymk_ = [("ymine", j_) for j_ in range(4)]
                dma(P, "sp", ya_t[s][:].rearrange("p (j c) -> p j c", j=4), ym[rows, :, 0:192], reads=ymk_, writes=[("ya", s)])
                dma(P, "sp", ymix[s][:, 384:768].rearrange("p (j c) -> p j c", j=3), ym[rows, 0:3, 192:320], reads=ymk_, writes=[("ymix", s, 1)])
                dma(P, "sp", yc_t[s][:].rearrange("p (j c) -> p j c", j=4), ym[rows, :, 320:384], reads=ymk_, writes=[("yc", s)])
                yav = ya_t[s][:].rearrange("p (h m d) -> p h m d", h=6, m=2)
                P.op("dve", lambda yav=yav: nc.vector.scalar_tensor_tensor(out=dd[:], in0=yav[:, :, 1, :], scalar=nlam[:, 0:1],
                                                                          in1=yav[:, :, 0, :], op0=ALU.mult, op1=ALU.add),
                     reads=[("ya", s), "nlam"], writes=["dd"])
                P.op("pool", lambda: nc.gpsimd.tensor_tensor(out=sq[:], in0=dd[:], in1=dd[:], op=ALU.mult), reads=["dd"], writes=["sq"])
                P.op("dve", lambda: nc.vector.tensor_reduce(out=ss[:], in_=sq[:], axis=AX.X, op=ALU.add), reads=["sq"], writes=["ss"])
                P.op("act", lambda: nc.scalar.activation(out=ss[:], in_=ss[:], func=AF.Sqrt, bias=eps_rms[:, 0:1], scale=1.0 / 64),
                     reads=["ss", "eps_rms"], writes=["ss"])
                P.op("dve", lambda: nc.vector.reciprocal(out=ss[:], in_=ss[:]), reads=["ss"], writes=["ss"])
                P.op("dve", lambda: nc.vector.tensor_tensor(out=dd[:], in0=dd[:], in1=ss[:].unsqueeze(2).to_broadcast([128, 6, 64]), op=ALU.mult),
                     reads=["dd", "ss"], writes=["dd"])
                ym0 = ymix[s][:, 0:384].rearrange("p (h d) -> p h d", h=6)
                P.op("pool", lambda ym0=ym0: nc.gpsimd.tensor_tensor(out=ym0, in0=dd[:], in1=gA[:].unsqueeze(1).to_broadcast([128, 6, 64]), op=ALU.mult),
                     reads=["dd", "gA"], writes=[("ymix", s, 0)])
                yct = yc_t[s]
                P.op("pool", lambda yct=yct: nc.gpsimd.tensor_tensor(out=c2[:], in0=yct[:], in1=yct[:], op=ALU.mult), reads=[("yc", s)], writes=["c2"])
                P.op("dve", lambda: nc.vector.tensor_scalar(out=c2[:], in0=c2[:], scalar1=0.044715, scalar2=1.0, op0=ALU.mult, op1=ALU.add),
                     reads=["c2"], writes=["c2"])
                P.op("dve", lambda yct=yct: nc.vector.tensor_tensor(out=c2[:], in0=c2[:], in1=yct[:], op=ALU.mult), reads=["c2", ("yc", s)], writes=["c2"])
                P.op("act", lambda: nc.scalar.activation(out=c2[:], in_=c2[:], func=AF.Sigmoid, scale=1.5957691216057308),
                     reads=["c2"], writes=["c2"])
                P.op("dve", lambda yct=yct: nc.vector.tensor_tensor(out=c3[:], in0=c2[:], in1=yct[:], op=ALU.mult), reads=["c2", ("yc", s)], writes=["c3"])
                P.op("act", lambda: nc.scalar.copy(out=ygb[:], in_=c3[:]), reads=["c3"], writes=["ygb"])
                pv0 = B[0][:].bitcast(BF16)

                def trg(pv0=pv0):
                    for k in range(2):
                        ins = nc.tensor.transpose(out=pv0[:, k * 128:(k + 1) * 128], in_=ygb[:, k * 128:(k + 1) * 128], identity=ident[:])
                    return ins
                P.op("pe", trg, reads=["ygb", "ident"], writes=["b0"])
                P.op("dve", lambda pv0=pv0: nc.vector.tensor_copy(out=ygT[:].rearrange("p k t -> p (k t)"), in_=pv0[:, 0:256]), reads=["b0"], writes=["ygT"])

                def mmg():
                    for k in range(2):
                        ins = nc.tensor.matmul(B[1][:, 0:256], lhsT=ygT[:, k, :], rhs=wglu_t[:, k, :], start=(k == 0), stop=(k == 1))
                    return ins
                P.op("pe", mmg, reads=["ygT", "wglu"], writes=["b1"])
                P.op("act", lambda: nc.scalar.activation(out=sig[:], in_=B[1][:, 0:256], func=AF.Sigmoid), reads=["b1"], writes=["sig"])
                P.op("dve", lambda s=s: nc.vector.tensor_tensor(out=ymix[s][:, 768:1024], in0=c3[:], in1=sig[:], op=ALU.mult),
                     reads=["c3", "sig"], writes=[("ymix", s, 2)])
                ymk = [("ymix", s, 0), ("ymix", s, 1), ("ymix", s, 2)]
                P.op("act", lambda s=s: nc.scalar.copy(out=ymb[:], in_=ymix[s][:]), reads=ymk, writes=["ymb"])
                pvb = B[0][:].bitcast(BF16).rearrange("p (k t) -> p k t", k=8)

                def try_(pvb=pvb):
                    for kc in range(8):
                        ins = nc.tensor.transpose(out=pvb[:, kc, :], in_=ymb[:, kc * 128:(kc + 1) * 128], identity=ident[:])
                    return ins
                P.op("pe", try_, reads=["ymb", "ident"], writes=["b0"])
                P.op("dve", lambda pvb=pvb: nc.vector.tensor_copy(out=ymT[:], in_=pvb), reads=["b0"], writes=["ymT"])
                for half in range(2):
                    def mmo(half=half):
                        for kc in range(8):
                            ins = nc.tensor.matmul(B[2 + half][:], lhsT=ymT[:, kc, :], rhs=wout_t[:, kc, half * 512:(half + 1) * 512],
                                                   start=(kc == 0), stop=(kc == 7))
                        return ins
                    P.op("pe", mmo, reads=["ymT", "wout"], writes=[f"b{2 + half}"])
                    P.op("dve", lambda half=half, s=s: nc.vector.scalar_tensor_tensor(
                        out=z[:, half * 512:(half + 1) * 512], in0=ht[s][:, half * 512:(half + 1) * 512], scalar=float(ALPHA),
                        in1=B[2 + half][:], op0=ALU.mult, op1=ALU.add), reads=[f"b{2 + half}", ("ht", s)], writes=[("z", half)])
                P.op("pool", lambda: nc.gpsimd.tensor_copy(out=z[:, 0:1], in_=z[:, 0:1]), reads=[("z", 0), ("z", 1)], writes=["zz"])
                layer_norm_tile(P, nc, z, "zz", h1[tt], ("h1", tt), g1, b1, "g1", "b1", scr, "ln")
                for half in range(2):
                    pv32 = B[2 + half][:].rearrange("p (k t) -> p k t", k=4)

                    def trh(half=half, pv32=pv32, tt=tt):
                        for k in range(4):
                            kc = half * 4 + k
                            ins = nc.tensor.transpose(out=pv32[:, k, :], in_=h1[tt][:, kc * 128:(kc + 1) * 128], identity=ident32[:])
                        return ins
                    P.op("pe", trh, reads=[("h1", tt), "ident32"], writes=[f"b{2 + half}"])
                    P.op("dve", lambda half=half, pv32=pv32: nc.vector.tensor_copy(out=h1T32[:, half * 4:(half + 1) * 4, :], in_=pv32),
                         reads=[f"b{2 + half}"], writes=[("h1T32", half)])
                    P.op("act", lambda half=half, pv32=pv32, tt=tt: nc.scalar.copy(
                        out=h1T[:, half * 4:(half + 1) * 4, tt * 128:(tt + 1) * 128], in_=pv32),
                        reads=[f"b{2 + half}"], writes=[("h1T", tt, half)])

                def mmr():
                    for kc in range(8):
                        ins = nc.tensor.matmul(B[1][:, 0:20], lhsT=h1T32[:, kc, :], rhs=wr_t[:, kc, :], start=(kc == 0), stop=(kc == 7))
                    return ins
                P.op("pe", mmr, reads=[("h1T32", 0), ("h1T32", 1), "wr"], writes=["b1"])
                P.op("dve", lambda: nc.vector.tensor_tensor(out=lg[:], in0=B[1][:, 0:20], in1=br_t[:], op=ALU.add), reads=["b1", "br"], writes=["lg"])
                V = nc.vector
                glog = lg[:, 0:4]
                elog = lg[:, 4:20]
                seq = [
                    lambda: V.tensor_reduce(out=r["gmax"][:], in_=glog, axis=AX.X, op=ALU.max),
                    lambda: V.tensor_scalar(out=r["goh"][:], in0=glog, scalar1=r["gmax"][:, 0:1], scalar2=None, op0=ALU.is_ge),
                    lambda: V.tensor_scalar(out=r["ngmax"][:], in0=r["gmax"][:], scalar1=-1.0, scalar2=None, op0=ALU.mult),
                ]
                for f_ in seq:
                    P.op("dve", f_, reads=["lg", "rt"], writes=["rt"])
                P.op("act", lambda: nc.scalar.activation(out=r["gexp"][:], in_=glog, func=AF.Exp, bias=r["ngmax"][:, 0:1], scale=1.0),
                     reads=["lg", "rt"], writes=["rt2"])
                seq = [
                    lambda: V.tensor_reduce(out=r["gsum"][:], in_=r["gexp"][:], axis=AX.X, op=ALU.add),
                    lambda: V.reciprocal(out=r["gp"][:], in_=r["gsum"][:]),
                    lambda: V.tensor_tensor(out=r["em"][:].rearrange("p (g e) -> p g e", g=4), in0=elog.rearrange("p (g e) -> p g e", g=4),
                                            in1=r["goh"][:].unsqueeze(2).to_broadcast([128, 4, 4]), op=ALU.mult),
                    lambda: V.tensor_scalar(out=r["pen"][:], in0=r["goh"][:], scalar1=-1.0, scalar2=BIG, op0=ALU.add, op1=ALU.mult),
                    lambda: V.tensor_tensor(out=r["em"][:].rearrange("p (g e) -> p g e", g=4), in0=r["em"][:].rearrange("p (g e) -> p g e", g=4),
                                            in1=r["pen"][:].unsqueeze(2).to_broadcast([128, 4, 4]), op=ALU.add),
                    lambda: V.tensor_reduce(out=r["m1"][:], in_=r["em"][:], axis=AX.X, op=ALU.max),
                    lambda: V.tensor_scalar(out=r["oh1"][:], in0=r["em"][:], scalar1=r["m1"][:, 0:1], scalar2=None, op0=ALU.is_ge),
                    lambda: V.scalar_tensor_tensor(out=r["em2"][:], in0=r["oh1"][:], scalar=-BIG, in1=r["em"][:], op0=ALU.mult, op1=ALU.add),
                    lambda: V.tensor_reduce(out=r["m2"][:], in_=r["em2"][:], axis=AX.X, op=ALU.max),
                    lambda: V.tensor_scalar(out=r["oh2"][:], in0=r["em2"][:], scalar1=r["m2"][:, 0:1], scalar2=None, op0=ALU.is_ge),
                    lambda: V.tensor_tensor(out=r["dl"][:], in0=r["m2"][:], in1=r["m1"][:], op=ALU.subtract),
                ]
                for f_ in seq:
                    P.op("dve", f_, reads=["lg", "rt", "rt2"], writes=["rt"])
                P.op("act", lambda: nc.scalar.activation(out=r["ex"][:], in_=r["dl"][:], func=AF.Exp), reads=["rt"], writes=["rt2"])
                seq = [
                    lambda: V.tensor_scalar(out=r["den"][:], in0=r["ex"][:], scalar1=1.0, scalar2=None, op0=ALU.add),
                    lambda: V.reciprocal(out=r["w1"][:], in_=r["den"][:]),
                    lambda: V.tensor_tensor(out=r["w1"][:], in0=r["w1"][:], in1=r["gp"][:], op=ALU.mult),
                    lambda: V.tensor_tensor(out=r["w2"][:], in0=r["w1"][:], in1=r["ex"][:], op=ALU.mult),
                    lambda tt=tt: V.tensor_scalar(out=comb[:, tt, :], in0=r["oh1"][:], scalar1=r["w1"][:, 0:1], scalar2=None, op0=ALU.mult),
                    lambda tt=tt: V.scalar_tensor_tensor(out=comb[:, tt, :], in0=r["oh2"][:], scalar=r["w2"][:, 0:1], in1=comb[:, tt, :],
                                                         op0=ALU.mult, op1=ALU.add),
                ]
                for i_, f_ in enumerate(seq):
                    P.op("dve", f_, reads=["rt", "rt2"] + ([("comb", tt)] if i_ == 5 else []), writes=["rt"] if i_ < 4 else [("comb", tt)])
            h1Tk = [("h1T", tt, half) for tt in range(4) for half in range(2)]
            for e in range(NEXP):
                ws = e % 2
                dma(P, "sp", wA[ws][:], w1[e], writes=[("wA", ws)])
                dma(P, "poolq", wB[ws][:], w3[e], writes=[("wB", ws)])
                dma(P, "sp", wC[ws][:], w2[e], writes=[("wC", ws)])
                for hc in range(4):
                    ba, bb = 4 + hc % 2, 6 + hc % 2

                    def mma(hc=hc, ba=ba, ws=ws):
                        for kc in range(8):
                            ins = nc.tensor.matmul(B[ba][:], lhsT=wA[ws][:, kc, hc * 128:(hc + 1) * 128], rhs=h1T[:, kc, :],
                                                   start=(kc == 0), stop=(kc == 7))
                        return ins

                    def mmb(hc=hc, bb=bb, ws=ws):
                        for kc in range(8):
                            ins = nc.tensor.matmul(B[bb][:], lhsT=wB[ws][:, kc, hc * 128:(hc + 1) * 128], rhs=h1T[:, kc, :],
                                                   start=(kc == 0), stop=(kc == 7))
                        return ins
                    P.op("pe", mma, reads=h1Tk + [("wA", ws)], writes=[f"b{ba}"])
                    P.op("pe", mmb, reads=h1Tk + [("wB", ws)], writes=[f"b{bb}"])
                    P.op("act", lambda hc=hc, ba=ba: nc.scalar.activation(out=sil[hc % 2][:], in_=B[ba][:], func=AF.Silu),
                         reads=[f"b{ba}"], writes=[("sil", hc % 2)])
                    P.op("dve", lambda hc=hc, bb=bb: nc.vector.tensor_tensor(out=hid[hc][:], in0=sil[hc % 2][:], in1=B[bb][:], op=ALU.mult),
                         reads=[f"b{bb}", ("sil", hc % 2)], writes=[("hid", hc)])
                for tt in range(4):
                    for half in range(2):
                        bo = 2 + half

                        def mm2(tt=tt, half=half, bo=bo, ws=ws):
                            for hc in range(4):
                                ins = nc.tensor.matmul(B[bo][:], lhsT=hid[hc][:, tt * 128:(tt + 1) * 128],
                                                       rhs=wC[ws][:, hc, half * 512:(half + 1) * 512], start=(hc == 0), stop=(hc == 3))
                            return ins
                        P.op("pe", mm2, reads=[("hid", hc) for hc in range(4)] + [("wC", ws)], writes=[f"b{bo}"])
                        av = acc[tt][:, half * 512:(half + 1) * 512]
                        if e == 0:
                            P.op("dve", lambda av=av, bo=bo, tt=tt, e=e: nc.vector.tensor_scalar(
                                out=av, in0=B[bo][:], scalar1=comb[:, tt, e:e + 1], scalar2=None, op0=ALU.mult),
                                reads=[f"b{bo}", ("comb", tt)], writes=[("acc", tt, half)])
                        else:
                            P.op("dve", lambda av=av, bo=bo, tt=tt, e=e: nc.vector.scalar_tensor_tensor(
                                out=av, in0=B[bo][:], scalar=comb[:, tt, e:e + 1], in1=av, op0=ALU.mult, op1=ALU.add),
                                reads=[f"b{bo}", ("comb", tt), ("acc", tt, half)], writes=[("acc", tt, half)])
            for tt in range(4):
                t = su * 4 + tt
                s = t % 2
                rows = slice(t * 128, (t + 1) * 128)
                P.op("dve", lambda tt=tt: nc.vector.scalar_tensor_tensor(out=z2[:], in0=h1[tt][:], scalar=float(ALPHA), in1=acc[tt][:],
                                                                          op0=ALU.mult, op1=ALU.add),
                     reads=[("h1", tt), ("acc", tt, 0), ("acc", tt, 1)], writes=["z2"])
                layer_norm_tile(P, nc, z2, "z2", ho[s], ("ho", s), g2, b2, "g2", "b2", scr, "ln")
                dma(P, "poolq", h_out[rows, :], ho[s][:], reads=[("ho", s)], writes=[("h_out", t)])
                outs.append(("h_out", t))
                if hT_out is not None:
                    to_featmajor_bf16(P, nc, ho[s], ("ho", s), hob, "hob", B[0], "b0", hoT[s][:], ("hoT", s), ident)
                    dma(P, "poolq", hT_out[:, :, rows].rearrange("k p t -> p k t"), hoT[s][:], reads=[("hoT", s)], writes=[("hT_out", t)])
                    outs.append(("hT_out", t))
        P.emit(final_wait_keys=outs)


def post_inputs(l, p, lam_init):
    d = {}
    d["wout"] = bf(p["w_out"][l].reshape(8, 128, D).transpose(1, 0, 2))
    d["wglu"] = bf(p["s5_w_glu"][l].reshape(2, 128, 256).transpose(1, 0, 2))
    for n in ("ln1_g", "ln1_b", "ln2_g", "ln2_b"):
        d[n.replace("_", "")] = f32c(p[n][l].reshape(1, D))
    d["dng"] = f32c(p["diff_norm_g"][l].reshape(1, 64))
    d["lamv"] = f32c(np.concatenate([p["lam_q1"][l], p["lam_k1"][l], p["lam_q2"][l], p["lam_k2"][l],
                                     np.array([1.0 - lam_init, -lam_init], np.float32)]).reshape(1, 130))
    wr = np.concatenate([p["moe_w_grp"][l], p["moe_w_exp"][l]], axis=1)
    d["wr"] = f32c(wr.reshape(8, 128, 20).transpose(1, 0, 2))
    d["br"] = f32c(np.concatenate([p["moe_b_grp"][l], p["moe_b_exp"][l]]).reshape(1, 20))
    d["w1"] = bf(p["moe_w1"][l].reshape(NEXP, 8, 128, DEXP).transpose(0, 2, 1, 3))
    d["w3"] = bf(p["moe_w3"][l].reshape(NEXP, 8, 128, DEXP).transpose(0, 2, 1, 3))
    d["w2"] = bf(p["moe_w2"][l].reshape(NEXP, 4, 128, D).transpose(0, 2, 1, 3))
    d["idn"] = bf(np.eye(128)); d["idn32"] = f32c(np.eye(128))
    return d


TWO_PI = 2.0 * math.pi
MAGIC = 12582912.0


def phase_mix(nc, G, l, hT_all, y_o, S=SEQ, do_attn=True, do_s5=True, do_gdn=True):
    debug = False
    C = Ctx(nc)
    P = C.P
    nst = S // 512
    nblk = S // 128
    pf = f"L{l}_"

    def din(name, shape, dt=F32):
        return G.din((pf + name) if name not in ("amask", "idn32", "srow", "cTri", "cSL", "cMask2", "cBones") else name, shape, dt)
    hT4 = hT_all.rearrange("k (r p) t -> r p k t", r=4)
    wq = din("wq", [128, 8, 96], BF16); wk = din("wk", [128, 8, 96], BF16); wv = din("wv", [128, 8, 192], BF16)
    amask = din("amask", [128, 4, 512], BF16)
    idn32 = din("idn32", [128, 128])
    wu = din("wu", [128, 8, 64], BF16)
    s5row = din("s5row", [2, 3, 128])
    s5col = din("s5col", [2, 128, 3])
    s5bT = din("s5bT", [2, 2, 2, 16, 64])
    s5cT = din("s5cT", [2, 2, 2, 64, 16])
    s5d = din("s5d", [64, 1])
    srow = din("srow", [1, 512])
    wg = din("wg", [128, 8, 384], BF16); wt = din("wt", [128, 8, 132], BF16)
    cvw = din("cvw", [128, 3, 4])
    galog = din("galog", [1, 2]); gdtb = din("gdtb", [1, 2]); gng = din("gng", [1, 64])
    cTri = din("cTri", [64, 64]); cSL = din("cSL", [64, 64]); cMask2 = din("cMask2", [64, 2, 64]); cBones = din("cBones", [128, 128])
    ya_o = y_o[:, 0:192].rearrange("s (u d) -> s u d", u=3)
    yb_o = y_o[:, 192:320].rearrange("s (h d) -> s h d", h=2)
    yc_o = y_o[:, 320:384]
    outs = []
    dbg = []
    V = nc.vector
    G = nc.gpsimd
    A = nc.scalar
    T = nc.tensor

    with P.stack:
        C.alloc_banks()
        B = C.banks
        sb = P.sb
        ident32 = sb([128, 128], F32)
        dma(P, "sp", ident32[:], idn32, writes=["ident32"])
        hTt = [sb([128, 8, 512], BF16)] * 2
        eps_rms = const_col(P, nc, RMS_EPS, "eps_rms")
        if do_attn:
            wq_t = sb([128, 8, 96], BF16); wk_t = sb([128, 8, 96], BF16); wv_t = sb([128, 8, 192], BF16)
            QT = sb([96, S], BF16); KT = sb([96, S], BF16)
            Vall = sb([128, nblk, 3, 65], BF16)
            am_t = sb([128, 4, 512], BF16)
            PT = [sb([128, 512], BF16) for _ in range(4)]
            osb = sb([65, 512], F32); rec = sb([128, 4], F32)
            oT = [sb([128, 4, 64], F32) for _ in range(2)]
            dma(P, "sp", wq_t[:], wq, writes=["wq"]); dma(P, "sp", wk_t[:], wk, writes=["wk"]); dma(P, "sp", wv_t[:], wv, writes=["wv"])
            dma(P, "sp", am_t[:], amask, writes=["amask"])
            P.op("pool", lambda: G.memset(Vall[:, :, :, 64:65], 1.0), writes=["Vones"])
        if do_s5:
            wu_t = sb([128, 8, 64], BF16)
            dma(P, "sp", wu_t[:], wu, writes=["wu"])
            uT = [sb([32, 512], F32) for _ in range(2)]
            srow_t = sb([128, 512], F32)
            dma(P, "sp", srow_t[:], srow.partition_broadcast(128), writes=["srow"])
            d_col = [sb([32, 1], F32) for _ in range(2)]
            for pr_ in range(2):
                dma(P, "sp", d_col[pr_][:], s5d[pr_ * 32:(pr_ + 1) * 32, :], writes=[("dcol", pr_)])
            s5 = []
            for pr in range(2):
                t = dict(row=sb([32, 3, 128], F32), col=sb([128, 3], F32),
                         BrBD=sb([32, 128], F32), BiBD=sb([32, 128], F32), CrBD=sb([128, 32], F32), CiBD=sb([128, 32], F32),
                         bbr=sb([32, 128], F32), bbi=sb([32, 128], F32),
                         w=[sb([32, 128], F32) for _ in range(8)],
                         cw=[sb([128, 1], F32) for _ in range(8)],
                         RHO=sb([128, 512], F32), CS=sb([128, 512], F32), SN=sb([128, 512], F32),
                         zi=sb([128, 2], F32), zt=sb([128, 2], F32))
                if pr == 0:
                    for nm_ in ("bre", "bim", "t1", "t2", "zre", "zim", "xre", "xim", "ang", "tmp"):
                        t[nm_] = sb([128, 512], F32)
                if pr == 1:
                    for nm_ in ("bre", "bim", "t1", "t2", "zre", "zim", "xre", "xim", "ang", "tmp"):
                        t[nm_] = s5[0][nm_]
                s5.append(t)
            yT = [sb([32, 512], F32) for _ in range(2)]
            yc_tm = [sb([128, 4, 64], F32) for _ in range(2)]

            def range_reduce(eng_name, x, tmp, key_x, key_t, shape_all=True):
                P.op("dve", lambda: V.tensor_scalar(out=tmp, in0=x, scalar1=1.0 / TWO_PI, scalar2=MAGIC, op0=ALU.mult, op1=ALU.add),
                     reads=[key_x], writes=[key_t])
                P.op("dve", lambda: V.tensor_scalar(out=tmp, in0=tmp, scalar1=-MAGIC, scalar2=-TWO_PI, op0=ALU.add, op1=ALU.mult),
                     reads=[key_t], writes=[key_t])
                P.op("dve", lambda: V.tensor_tensor(out=x, in0=x, in1=tmp, op=ALU.add), reads=[key_x, key_t], writes=[key_x])

            def s5_setup(pr):
                t = s5[pr]
                k = lambda n, pr=pr: ("s5", "sh" if n in ("bre", "bim", "t1", "t2", "zre", "zim", "xre", "xim", "tab", "roww_scratch") else pr, n)
                dma(P, "sp", t["row"][:], s5row[pr:pr + 1].partition_broadcast(32), writes=[k("row")])
                dma(P, "sp", t["col"][:], s5col[pr], writes=[k("col")])
                for nm in ("BrBD", "BiBD", "CrBD", "CiBD"):
                    P.op("pool", lambda nm=nm, t=t: G.memset(t[nm][:], 0.0), writes=[k(nm)])
                for g in range(2):
                    dma(P, "sp", t["BrBD"][g * 16:(g + 1) * 16, g * 64:(g + 1) * 64], s5bT[pr, g, 0], reads=[k("BrBD")], writes=[k("BrBD")])
                    dma(P, "sp", t["BiBD"][g * 16:(g + 1) * 16, g * 64:(g + 1) * 64], s5bT[pr, g, 1], reads=[k("BiBD")], writes=[k("BiBD")])
                    dma(P, "sp", t["CrBD"][g * 64:(g + 1) * 64, g * 16:(g + 1) * 16], s5cT[pr, g, 0], reads=[k("CrBD")], writes=[k("CrBD")])
                    dma(P, "sp", t["CiBD"][g * 64:(g + 1) * 64, g * 16:(g + 1) * 16], s5cT[pr, g, 1], reads=[k("CiBD")], writes=[k("CiBD")])
                P.op("dve", lambda t=t: V.tensor_scalar(out=t["CiBD"][:], in0=t["CiBD"][:], scalar1=-1.0, scalar2=None, op0=ALU.mult),
                     reads=[k("CiBD")], writes=[k("CiBD")])
                lre, lim, ldt = t["row"][:, 0, :], t["row"][:, 1, :], t["row"][:, 2, :]
                dt_, lr_, mag, ang, tmp_, sn, cs, den = [t["w"][i][:] for i in range(8)]
                rk = k("roww")
                steps = [
                    ("act", lambda: A.activation(out=dt_, in_=ldt, func=AF.Exp)),
                    ("dve", lambda: V.tensor_tensor(out=lr_, in0=lre, in1=dt_, op=ALU.mult)),
                    ("act", lambda: A.activation(out=mag, in_=lr_, func=AF.Exp)),
                    ("dve", lambda: V.tensor_tensor(out=ang, in0=lim, in1=dt_, op=ALU.mult)),
                ]
                for e_, f_ in steps:
                    P.op(e_, f_, reads=[k("row"), rk], writes=[rk])
                range_reduce("dve", ang, tmp_, rk, rk)
                P.op("act", lambda: A.activation(out=sn, in_=ang, func=AF.Sin), reads=[rk], writes=[rk])
                P.op("dve", lambda: V.tensor_scalar(out=ang, in0=ang, scalar1=math.pi / 2, scalar2=None, op0=ALU.add), reads=[rk], writes=[rk])
                range_reduce("dve", ang, tmp_, rk, rk)
                P.op("act", lambda: A.activation(out=cs, in_=ang, func=AF.Sin), reads=[rk], writes=[rk])
                steps = [
                    lambda: V.tensor_tensor(out=cs, in0=cs, in1=mag, op=ALU.mult),
                    lambda: V.tensor_scalar(out=cs, in0=cs, scalar1=-1.0, scalar2=None, op0=ALU.add),
                    lambda: V.tensor_tensor(out=sn, in0=sn, in1=mag, op=ALU.mult),
                    lambda: V.tensor_tensor(out=den, in0=lre, in1=lre, op=ALU.mult),
                    lambda: V.tensor_tensor(out=tmp_, in0=lim, in1=lim, op=ALU.mult),
                    lambda: V.tensor_tensor(out=den, in0=den, in1=tmp_, op=ALU.add),
                    lambda: V.reciprocal(out=den, in_=den),
                    lambda: V.tensor_tensor(out=dt_, in0=cs, in1=lre, op=ALU.mult),
                    lambda: V.tensor_tensor(out=tmp_, in0=sn, in1=lim, op=ALU.mult),
                    lambda: V.tensor_tensor(out=dt_, in0=dt_, in1=tmp_, op=ALU.add),
                    lambda: V.tensor_tensor(out=dt_, in0=dt_, in1=den, op=ALU.mult),
                    lambda: V.tensor_tensor(out=lr_, in0=sn, in1=lre, op=ALU.mult),
                    lambda: V.tensor_tensor(out=tmp_, in0=cs, in1=lim, op=ALU.mult),
                    lambda: V.tensor_tensor(out=lr_, in0=lr_, in1=tmp_, op=ALU.subtract),
                    lambda: V.tensor_tensor(out=lr_, in0=lr_, in1=den, op=ALU.mult),
                    lambda t=t: V.tensor_tensor(out=t["bbr"][:], in0=dt_, in1=t["BrBD"][:], op=ALU.mult),
                    lambda t=t: V.tensor_tensor(out=tmp_, in0=lr_, in1=t["BiBD"][:], op=ALU.mult),
                    lambda t=t: V.tensor_tensor(out=t["bbr"][:], in0=t["bbr"][:], in1=tmp_, op=ALU.subtract),
                    lambda t=t: V.tensor_tensor(out=t["bbi"][:], in0=dt_, in1=t["BiBD"][:], op=ALU.mult),
                    lambda t=t: V.tensor_tensor(out=tmp_, in0=lr_, in1=t["BrBD"][:], op=ALU.mult),
                    lambda t=t: V.tensor_tensor(out=t["bbi"][:], in0=t["bbi"][:], in1=tmp_, op=ALU.add),
                ]
                for f_ in steps:
                    P.op("dve", f_, reads=[k("row"), rk, k("BrBD"), k("BiBD")], writes=[rk])
                cdt, cth, crho, ca, ctmp, c512s, c512c, cx = [t["cw"][i][:] for i in range(8)]
                ck = k("colw")
                steps = [
                    ("act", lambda t=t: A.activation(out=cdt, in_=t["col"][:, 2:3], func=AF.Exp)),
                    ("dve", lambda t=t: V.tensor_tensor(out=cth, in0=t["col"][:, 1:2], in1=cdt, op=ALU.mult)),
                    ("dve", lambda t=t: V.tensor_tensor(out=crho, in0=t["col"][:, 0:1], in1=cdt, op=ALU.mult)),
                    ("act", lambda: A.activation(out=crho, in_=crho, func=AF.Exp)),
                    ("dve", lambda: V.tensor_scalar(out=ca, in0=cth, scalar1=512.0, scalar2=None, op0=ALU.mult)),
                ]
                for e_, f_ in steps:
                    P.op(e_, f_, reads=[k("col"), ck], writes=[ck])
                range_reduce("dve", ca, ctmp, ck, ck)
                P.op("act", lambda: A.activation(out=c512s, in_=ca, func=AF.Sin), reads=[ck], writes=[ck])
                P.op("dve", lambda: V.tensor_scalar(out=ca, in0=ca, scalar1=math.pi / 2, scalar2=None, op0=ALU.add), reads=[ck], writes=[ck])
                range_reduce("dve", ca, ctmp, ck, ck)
                P.op("act", lambda: A.activation(out=c512c, in_=ca, func=AF.Sin), reads=[ck], writes=[ck])
                tk = k("tab")
                P.op("dve", lambda t=t: V.tensor_scalar(out=t["ang"][:], in0=srow_t[:], scalar1=cth, scalar2=None, op0=ALU.mult),
                     reads=["srow", ck], writes=[tk])
                range_reduce("dve", t["ang"][:], t["tmp"][:], tk, tk)
                P.op("act", lambda t=t: A.activation(out=t["SN"][:], in_=t["ang"][:], func=AF.Sin), reads=[tk], writes=[tk])
                P.op("dve", lambda t=t: V.tensor_scalar(out=t["ang"][:], in0=t["ang"][:], scalar1=math.pi / 2, scalar2=None, op0=ALU.add), reads=[tk], writes=[tk])
                range_reduce("dve", t["ang"][:], t["tmp"][:], tk, tk)
                P.op("act", lambda t=t: A.activation(out=t["CS"][:], in_=t["ang"][:], func=AF.Sin), reads=[tk], writes=[tk])
                P.op("pool", lambda t=t: G.memset(t["RHO"][:], 1.0), writes=[k("rho")])
                P.op("dve", lambda t=t: V.tensor_scalar(out=t["RHO"][:], in0=t["RHO"][:], scalar1=crho, scalar2=None, op0=ALU.mult),
                     reads=[k("rho"), ck], writes=[k("rho")])
                P.op("pool", lambda t=t: G.memset(t["zi"][:], 0.0), writes=[k("zi")])
                if pr == 0:
                    dbg.extend([("CS", t["CS"][:], [128, 512], [k("tab")]), ("SN", t["SN"][:], [128, 512], [k("tab")]),
                            ("RHO", t["RHO"][:], [128, 512], [k("rho")]), ("bbr", t["bbr"][:], [32, 128], [k("roww")]),
                            ("bbi", t["bbi"][:], [32, 128], [k("roww")]), ("cr", t["w"][0][:], [32, 128], [k("roww")]),
                            ("ci", t["w"][1][:], [32, 128], [k("roww")]), ("c512", t["cw"][5][:], [128, 1], [k("colw")]),
                            ("row", t["row"][:], [32, 3, 128], [k("row")]), ("col", t["col"][:], [128, 3], [k("col")])])
            for pr_ in range(2):
                s5_setup(pr_)
        if do_gdn:
            wg_t = sb([128, 8, 384], BF16); wt_t = sb([128, 8, 132], BF16)
            dma(P, "sp", wg_t[:], wg, writes=["wg"]); dma(P, "sp", wt_t[:], wt, writes=["wt"])
            cvw_t = sb([128, 3, 4], F32); dma(P, "sp", cvw_t[:], cvw, writes=["cvw"])
            alog_t = sb([64, 2], F32); dtb_t = sb([64, 2], F32); ng_t = sb([64, 64], F32)
            dma(P, "sp", alog_t[:], galog.partition_broadcast(64), writes=["alog"])
            dma(P, "sp", dtb_t[:], gdtb.partition_broadcast(64), writes=["dtb"])
            dma(P, "sp", ng_t[:], gng.partition_broadcast(64), writes=["ngt"])
            Tri = sb([64, 64], F32); SL = sb([64, 64], F32); Mask2 = sb([64, 2, 64], F32); Bones = sb([128, 128], F32); ones64 = sb([64, 64], F32)
            dma(P, "sp", Tri[:], cTri, writes=["Tri"]); dma(P, "sp", SL[:], cSL, writes=["SL"])
            dma(P, "sp", Mask2[:], cMask2, writes=["Mask2"]); dma(P, "sp", Bones[:], cBones, writes=["Bones"])
            P.op("pool", lambda: G.memset(ones64[:], 1.0), writes=["ones64"])
            P.op("act", lambda: A.activation(out=alog_t[:], in_=alog_t[:], func=AF.Exp), reads=["alog"], writes=["alog"])
            P.op("dve", lambda: V.tensor_scalar(out=alog_t[:], in0=alog_t[:], scalar1=-1.0, scalar2=None, op0=ALU.mult), reads=["alog"], writes=["alog"])
            xraw = [sb([128, 515], F32) for _ in range(3)]
            for c_ in range(3):
                P.op("pool", lambda c_=c_: G.memset(xraw[c_][:], 0.0), writes=[("xraw", c_)])
            cvt = sb([128, 512], F32)
            qkv = [sb([128, 512], F32) for _ in range(3)]
            sqn = sb([128, 512], F32); rn_ = sb([128, 512], F32)
            Sst = [sb([64, 64], F32) for _ in range(2)]
            for h_ in range(2):
                P.op("pool", lambda h_=h_: G.memset(Sst[h_][:], 0.0), writes=[("S", h_)])
            gd = dict(ch=[], hd=[])
            for sl in range(2):
                gd["ch"].append(dict(gs=sb([64, 128], F32), bg=sb([64, 4], F32), nbeta=sb([64, 2], F32)))
            for sl in range(4):
                gd["hd"].append(dict(qkv_tm=sb([64, 3, 64], F32), gcl=sb([64, 2], F32), ex3=sb([64, 3], F32), Gm=sb([64, 64], F32),
                                     EE=sb([64, 2, 64], F32), Pm=sb([64, 64], F32), PTm=sb([64, 64], F32), AT=sb([64, 64], F32),
                                     PP=[sb([64, 2, 64], F32) for _ in range(2)], X=sb([64, 128], F32), tb=sb([64, 1], F32),
                                     kdec=sb([64, 64], F32), qdec=sb([64, 64], F32), wqT=sb([64, 2, 64], F32), vnew=sb([64, 64], F32),
                                     osb=sb([64, 64], F32), osq=sb([64, 64], F32), oss=sb([64, 1], F32), ngate=sb([64, 64], F32)))
            ybuf = [sb([64, 8, 2, 64], F32) for _ in range(2)]

        for st in range(nst):
            hs = 0
            cols = slice(st * 512, (st + 1) * 512)
            dma(P, "sp", hTt[hs][:], hT4[st // (nst // 4)][:, :, (st % (nst // 4)) * 512:(st % (nst // 4) + 1) * 512], writes=[("hTt", hs)])
            hk = ("hTt", hs)
            if do_attn:
                for (w_t, wkey, dst, dk_, bank) in ((wq_t, "wq", QT, "QT", 0), (wk_t, "wk", KT, "KT", 1)):
                    def mmqk(w_t=w_t, bank=bank, hs=hs):
                        for kc in range(8):
                            ins = T.matmul(B[bank][0:96, :], lhsT=w_t[:, kc, :], rhs=hTt[hs][:, kc, :], start=(kc == 0), stop=(kc == 7))
                        return ins
                    P.op("pe", mmqk, reads=[hk, wkey], writes=[f"b{bank}"])
                    P.op("act", lambda dst=dst, bank=bank, cols=cols: A.copy(out=dst[:, cols], in_=B[bank][0:96, :]),
                         reads=[f"b{bank}"], writes=[(dk_, st)])
                for pair in range(2):
                    bank = 2 + pair
                    pv = B[bank][:, 0:384].rearrange("p (j c) -> p j c", j=2)

                    def mmv(pair=pair, pv=pv, hs=hs):
                        for j in range(2):
                            blk = pair * 2 + j
                            for kc in range(8):
                                ins = T.matmul(pv[:, j, :], lhsT=hTt[hs][:, kc, blk * 128:(blk + 1) * 128], rhs=wv_t[:, kc, :],
                                               start=(kc == 0), stop=(kc == 7))
                        return ins
                    P.op("pe", mmv, reads=[hk, "wv"], writes=[f"b{bank}"])
                    b0 = st * 4 + pair * 2
                    P.op("dve", lambda pv=pv, b0=b0: V.tensor_copy(out=Vall[:, b0:b0 + 2, :, 0:64],
                                                                  in_=pv.rearrange("p j (u d) -> p j u d", u=3)),
                         reads=[f"b{bank}"], writes=[("V", st, pair)])
            if do_s5:
                for pr in range(2):
                    def mmu(hs=hs, pr=pr):
                        for kc in range(8):
                            ins = T.matmul(B[4][0:32, :], lhsT=wu_t[:, kc, pr * 32:(pr + 1) * 32], rhs=hTt[hs][:, kc, :], start=(kc == 0), stop=(kc == 7))
                        return ins
                    P.op("pe", mmu, reads=[hk, "wu"], writes=["b4"])
                    P.op("act", lambda pr=pr: A.copy(out=uT[pr][:], in_=B[4][0:32, :]), reads=["b4"], writes=[("uT", pr)])
                def s5_stream(pr):
                    t = s5[pr]
                    k = lambda n, pr=pr: ("s5", "sh" if n in ("bre", "bim", "t1", "t2", "zre", "zim", "xre", "xim", "tab", "roww_scratch") else pr, n)
                    P.op("pe", lambda t=t, pr=pr: T.matmul(B[5][:], lhsT=t["bbr"][:], rhs=uT[pr][:], start=True, stop=True),
                         reads=[("uT", pr), k("roww")], writes=["b5"])
                    P.op("pe", lambda t=t, pr=pr: T.matmul(B[6][:], lhsT=t["bbi"][:], rhs=uT[pr][:], start=True, stop=True),
                         reads=[("uT", pr), k("roww")], writes=["b6"])
                    P.op("act", lambda t=t: A.copy(out=t["bre"][:], in_=B[5][:]), reads=["b5"], writes=[k("bre")])
                    P.op("act", lambda t=t: A.copy(out=t["bim"][:], in_=B[6][:]), reads=["b6"], writes=[k("bim")])
                    P.op("dve", lambda t=t: V.tensor_tensor(out=t["t1"][:], in0=t["bre"][:], in1=t["CS"][:], op=ALU.mult), reads=[k("bre"), k("tab")], writes=[k("t1")])
                    P.op("pool", lambda t=t: G.tensor_tensor(out=t["t2"][:], in0=t["bim"][:], in1=t["SN"][:], op=ALU.mult), reads=[k("bim"), k("tab")], writes=[k("t2")])
                    P.op("dve", lambda t=t: V.tensor_tensor(out=t["t1"][:], in0=t["t1"][:], in1=t["t2"][:], op=ALU.add), reads=[k("t1"), k("t2")], writes=[k("t1")])
                    P.op("pool", lambda t=t: G.tensor_tensor(out=t["t2"][:], in0=t["bim"][:], in1=t["CS"][:], op=ALU.mult), reads=[k("bim"), k("tab"), k("t1")], writes=[k("t2")])
                    P.op("pool", lambda t=t: G.tensor_tensor(out=t["bre"][:], in0=t["bre"][:], in1=t["SN"][:], op=ALU.mult), reads=[k("bre"), k("tab"), k("t1")], writes=[k("bre")])
                    P.op("pool", lambda t=t: G.tensor_tensor(out=t["t2"][:], in0=t["t2"][:], in1=t["bre"][:], op=ALU.subtract), reads=[k("t2"), k("bre")], writes=[k("t2")])
                    P.op("dve", lambda t=t: V.tensor_tensor_scan(out=t["zre"][:], data0=t["RHO"][:], data1=t["t1"][:], initial=t["zi"][:, 0:1],
                                                                  op0=ALU.mult, op1=ALU.add), reads=[k("t1"), k("rho"), k("zi")], writes=[k("zre")])
                    P.op("dve", lambda t=t: V.tensor_tensor_scan(out=t["zim"][:], data0=t["RHO"][:], data1=t["t2"][:], initial=t["zi"][:, 1:2],
                                                                  op0=ALU.mult, op1=ALU.add), reads=[k("t2"), k("rho"), k("zi")], writes=[k("zim")])
                    cdt, cth, crho, ca, ctmp, c512s, c512c, cx = [t["cw"][i][:] for i in range(8)]
                    zl_re, zl_im = t["zre"][:, 511:512], t["zim"][:, 511:512]
                    P.op("dve", lambda t=t, zl_re=zl_re: V.tensor_tensor(out=t["zt"][:, 0:1], in0=zl_re, in1=c512c, op=ALU.mult), reads=[k("zre"), k("colw")], writes=[k("zt")])
                    P.op("dve", lambda t=t, zl_im=zl_im: V.tensor_tensor(out=t["zt"][:, 1:2], in0=zl_im, in1=c512s, op=ALU.mult), reads=[k("zim"), k("colw")], writes=[k("zt")])
                    P.op("dve", lambda t=t: V.tensor_tensor(out=t["zi"][:, 0:1], in0=t["zt"][:, 0:1], in1=t["zt"][:, 1:2], op=ALU.subtract), reads=[k("zt"), k("zi")], writes=[k("zi")])
                    P.op("dve", lambda t=t, zl_re=zl_re: V.tensor_tensor(out=t["zt"][:, 0:1], in0=zl_re, in1=c512s, op=ALU.mult), reads=[k("zre"), k("colw"), k("zi")], writes=[k("zt")])
                    P.op("dve", lambda t=t, zl_im=zl_im: V.tensor_tensor(out=t["zt"][:, 1:2], in0=zl_im, in1=c512c, op=ALU.mult), reads=[k("zim"), k("colw")], writes=[k("zt")])
                    P.op("dve", lambda t=t: V.tensor_tensor(out=t["zi"][:, 1:2], in0=t["zt"][:, 0:1], in1=t["zt"][:, 1:2], op=ALU.add), reads=[k("zt"), k("zi")], writes=[k("zi")])
                    P.op("dve", lambda t=t: V.tensor_tensor(out=t["xre"][:], in0=t["zre"][:], in1=t["CS"][:], op=ALU.mult), reads=[k("zre"), k("tab")], writes=[k("xre")])
                    P.op("pool", lambda t=t: G.tensor_tensor(out=t["t1"][:], in0=t["zim"][:], in1=t["SN"][:], op=ALU.mult), reads=[k("zim"), k("tab"), k("zre")], writes=[k("t1")])
                    P.op("dve", lambda t=t: V.tensor_tensor(out=t["xre"][:], in0=t["xre"][:], in1=t["t1"][:], op=ALU.subtract), reads=[k("xre"), k("t1")], writes=[k("xre")])
                    P.op("pool", lambda t=t: G.tensor_tensor(out=t["xim"][:], in0=t["zre"][:], in1=t["SN"][:], op=ALU.mult), reads=[k("zre"), k("tab")], writes=[k("xim")])
                    P.op("pool", lambda t=t: G.tensor_tensor(out=t["t2"][:], in0=t["zim"][:], in1=t["CS"][:], op=ALU.mult), reads=[k("zim"), k("tab"), k("zim")], writes=[k("t2")])
                    P.op("pool", lambda t=t: G.tensor_tensor(out=t["xim"][:], in0=t["xim"][:], in1=t["t2"][:], op=ALU.add), reads=[k("xim"), k("t2")], writes=[k("xim")])

                    yb_ = 7 if pr == 0 else 3

                    def mmy(t=t, pr=pr, yb_=yb_):
                        T.matmul(B[yb_][0:32, :], lhsT=t["CrBD"][:], rhs=t["xre"][:], start=True, stop=False)
                        return T.matmul(B[yb_][0:32, :], lhsT=t["CiBD"][:], rhs=t["xim"][:], start=False, stop=True)
                    P.op("pe", mmy, reads=[k("xre"), k("xim"), k("CrBD"), k("CiBD")], writes=[f"b{yb_}"])
                    P.op("dve", lambda pr=pr, yb_=yb_: V.scalar_tensor_tensor(out=yT[pr][:], in0=uT[pr][:], scalar=d_col[pr][:, 0:1], in1=B[yb_][0:32, :],
                                                                             op0=ALU.mult, op1=ALU.add),
                         reads=[f"b{yb_}", ("uT", pr), ("dcol", pr)], writes=[("yT", pr)])
                for pr_ in range(2):
                    s5_stream(pr_)
                pvy = B[4][:, 0:256].rearrange("p (j d) -> p j d", j=4)

                def try4(pvy=pvy):
                    for pr in range(2):
                        for j in range(4):
                            ins = T.transpose(out=pvy[:, j, pr * 32:(pr + 1) * 32], in_=yT[pr][:, j * 128:(j + 1) * 128], identity=ident32[0:32, 0:32])
                    return ins
                P.op("pe", try4, reads=[("yT", 0), ("yT", 1), "ident32"], writes=["b4"])
                P.op("act", lambda pvy=pvy, hs=hs: A.copy(out=yc_tm[hs][:], in_=pvy), reads=["b4"], writes=[("yc_tm", hs)])
                dma(P, "poolq", yc_o[cols, :].rearrange("(j p) d -> p j d", p=128), yc_tm[hs][:], reads=[("yc_tm", hs)], writes=[("yc_o", st)])
                outs.append(("yc_o", st))
            if do_gdn:
                gdn_supertile(P, nc, B, st, hs, hk, hTt, wg_t, wt_t, cvw_t, xraw, cvt, qkv, sqn, rn_, Bones, eps_rms, alog_t, dtb_t, ng_t,
                              Tri, SL, Mask2, ones64, ident32, Sst, gd, ybuf, yb_o, outs)

        if do_attn:
            scale = 32 ** -0.5
            allqk = [("QT", s_) for s_ in range(nst)] + [("KT", s_) for s_ in range(nst)] + [("V", s_, p_) for s_ in range(nst) for p_ in range(2)] + ["Vones"]
            cnt = 0
            for u in range(3):
                for qt in range(nst):
                    nkb = 4 * (qt + 1)
                    bo = 3 + (qt % 2)
                    pend = []

                    def issue_s(kb, u=u, qt=qt):
                        nonlocal cnt
                        slot = cnt % 3
                        ps_ = cnt % 4
                        cnt += 1
                        P.op("pe", lambda: T.matmul(B[slot][:], lhsT=KT[32 * u:32 * u + 32, kb * 128:(kb + 1) * 128],
                                                    rhs=QT[32 * u:32 * u + 32, qt * 512:(qt + 1) * 512], start=True, stop=True),
                             reads=allqk, writes=[f"b{slot}"])
                        P.op("act", lambda: A.activation(out=PT[ps_][:], in_=B[slot][:], func=AF.Exp, scale=scale),
                             reads=[f"b{slot}"], writes=[("PT", ps_)])
                        if kb >= 4 * qt:
                            j = kb - 4 * qt
                            P.op("pool", lambda: G.tensor_tensor(out=PT[ps_][:], in0=PT[ps_][:], in1=am_t[:, j, :], op=ALU.mult),
                                 reads=[("PT", ps_), "amask"], writes=[("PT", ps_)])
                        return ps_

                    def issue_av(kb, ps_, u=u, bo=bo, nkb=nkb):
                        P.op("pe", lambda: T.matmul(B[bo][0:65, :], lhsT=Vall[:, kb, u, :], rhs=PT[ps_][:], start=(kb == 0), stop=(kb == nkb - 1)),
                             reads=[("PT", ps_)] + allqk, writes=[f"b{bo}"])
                    for kb in range(nkb):
                        pend.append((kb, issue_s(kb)))
                        if len(pend) > 2:
                            issue_av(*pend.pop(0))
                    while pend:
                        issue_av(*pend.pop(0))
                    P.op("act", lambda bo=bo: A.copy(out=osb[:], in_=B[bo][0:65, :]), reads=[f"b{bo}"], writes=["osb"])
                    pvo = B[5][:, 0:260].rearrange("p (j d) -> p j d", j=4)

                    def tro(pvo=pvo):
                        for j in range(4):
                            ins = T.transpose(out=pvo[:, j, :], in_=osb[:, j * 128:(j + 1) * 128], identity=ident32[0:65, 0:65])
                        return ins
                    P.op("pe", tro, reads=["osb", "ident32"], writes=["b5"])
                    P.op("dve", lambda pvo=pvo: V.reciprocal(out=rec[:], in_=pvo[:, :, 64]), reads=["b5"], writes=["rec"])
                    os_ = qt % 2
                    P.op("dve", lambda pvo=pvo, os_=os_: V.tensor_tensor(out=oT[os_][:], in0=pvo[:, :, 0:64],
                                                                        in1=rec[:].unsqueeze(2).to_broadcast([128, 4, 64]), op=ALU.mult),
                         reads=["b5", "rec"], writes=[("oT", os_)])
                    dma(P, "sp", ya_o[qt * 512:(qt + 1) * 512, u, :].rearrange("(j p) d -> p j d", p=128), oT[os_][:],
                        reads=[("oT", os_)], writes=[("ya_o", u, qt)])
                    outs.append(("ya_o", u, qt))
        P.emit(final_wait_keys=outs)


def gdn_supertile(P, nc, B, st, hs, hk, hTt, wg_t, wt_t, cvw_t, xraw, cvt, qkv, sqn, rn_, Bones, eps_rms, nA_t, dtb_t, ng_t,
                  Tri, SL, Mask2, ones64, ident32, Sst, gd, ybuf, yb_o, outs):
    V, G, A, T = nc.vector, nc.gpsimd, nc.scalar, nc.tensor
    for c in range(3):
        def mm(c=c):
            for kc in range(8):
                ins = T.matmul(B[c][:], lhsT=wg_t[:, kc, c * 128:(c + 1) * 128], rhs=hTt[hs][:, kc, :], start=(kc == 0), stop=(kc == 7))
            return ins
        P.op("pe", mm, reads=[hk, "wg"], writes=[f"b{c}"])
        P.op("pool", lambda c=c: G.tensor_copy(out=xraw[c][:, 0:3], in_=xraw[c][:, 512:515]), reads=[("xraw", c)], writes=[("xraw", c)])
        P.op("act", lambda c=c: A.copy(out=xraw[c][:, 3:515], in_=B[c][:]), reads=[f"b{c}", ("xraw", c)], writes=[("xraw", c)])
        P.op("dve", lambda c=c: V.tensor_scalar(out=cvt[:], in0=xraw[c][:, 0:512], scalar1=cvw_t[:, c, 0:1], scalar2=None, op0=ALU.mult),
             reads=[("xraw", c), "cvw"], writes=["cvt"])
        for kk in range(1, 4):
            P.op("dve", lambda c=c, kk=kk: V.scalar_tensor_tensor(out=cvt[:], in0=xraw[c][:, kk:kk + 512], scalar=cvw_t[:, c, kk:kk + 1],
                                                                  in1=cvt[:], op0=ALU.mult, op1=ALU.add),
                 reads=[("xraw", c), "cvw", "cvt"], writes=["cvt"])
        P.op("act", lambda c=c: A.activation(out=qkv[c][:], in_=cvt[:], func=AF.Silu), reads=["cvt"], writes=[("qkv", c)])
    for c in range(2):
        P.op("pool", lambda c=c: G.tensor_tensor(out=sqn[:], in0=qkv[c][:], in1=qkv[c][:], op=ALU.mult), reads=[("qkv", c)], writes=["sqn"])
        P.op("pe", lambda: T.matmul(B[3][:], lhsT=Bones[:], rhs=sqn[:], start=True, stop=True), reads=["sqn", "Bones"], writes=["b3"])
        P.op("act", lambda: A.activation(out=rn_[:], in_=B[3][:], func=AF.Sqrt, bias=eps_rms[:, 0:1], scale=1.0), reads=["b3", "eps_rms"], writes=["rn"])
        P.op("dve", lambda: V.reciprocal(out=rn_[:], in_=rn_[:]), reads=["rn"], writes=["rn"])
        if c == 0:
            P.op("dve", lambda: V.scalar_tensor_tensor(out=qkv[0][:], in0=qkv[0][:], scalar=0.125, in1=rn_[:], op0=ALU.mult, op1=ALU.mult),
                 reads=[("qkv", 0), "rn"], writes=[("qkv", 0)])
        else:
            P.op("dve", lambda: V.tensor_tensor(out=qkv[1][:], in0=qkv[1][:], in1=rn_[:], op=ALU.mult), reads=[("qkv", 1), "rn"], writes=[("qkv", 1)])
    qk_all = [("qkv", 0), ("qkv", 1), ("qkv", 2)]
    yb_s = st % 2
    for c in range(8):
        cg = st * 8 + c
        cs = slice(c * 64, (c + 1) * 64)
        dch = gd["ch"][cg % 2]
        kch = lambda n, cg=cg: ("gch", cg % 2, n)

        def mmt(cs=cs):
            for kc in range(8):
                ins = T.matmul(B[5][0:64, 0:132], lhsT=hTt[hs][:, kc, cs], rhs=wt_t[:, kc, :], start=(kc == 0), stop=(kc == 7))
            return ins
        P.op("pe", mmt, reads=[hk, "wt"], writes=["b5"])
        P.op("act", lambda dch=dch: A.activation(out=dch["gs"][:], in_=B[5][0:64, 0:128], func=AF.Silu), reads=["b5"], writes=[kch("gs")])
        P.op("act", lambda dch=dch: A.activation(out=dch["bg"][:, 0:2], in_=B[5][0:64, 128:130], func=AF.Sigmoid), reads=["b5"], writes=[kch("bg")])
        P.op("dve", lambda dch=dch: V.tensor_tensor(out=dch["bg"][:, 2:4], in0=B[5][0:64, 130:132], in1=dtb_t[:], op=ALU.add),
             reads=["b5", "dtb", kch("bg")], writes=[kch("bg")])
        P.op("act", lambda dch=dch: A.activation(out=dch["bg"][:, 2:4], in_=dch["bg"][:, 2:4], func=AF.Exp), reads=[kch("bg")], writes=[kch("bg")])
        P.op("act", lambda dch=dch: A.activation(out=dch["bg"][:, 2:4], in_=dch["bg"][:, 2:4], func=AF.Ln, bias=1.0, scale=1.0), reads=[kch("bg")], writes=[kch("bg")])
        P.op("dve", lambda dch=dch: V.tensor_tensor(out=dch["bg"][:, 2:4], in0=dch["bg"][:, 2:4], in1=nA_t[:], op=ALU.mult),
             reads=[kch("bg"), "alog"], writes=[kch("bg")])
        P.op("dve", lambda dch=dch: V.tensor_scalar(out=dch["nbeta"][:], in0=dch["bg"][:, 0:2], scalar1=-1.0, scalar2=None, op0=ALU.mult),
             reads=[kch("bg")], writes=[kch("nbeta")])
        for h in range(2):
            gdn_chunk_head(P, nc, B, h, cs, c, dch, kch, gd["hd"][(cg * 2 + h) % 4], (cg * 2 + h) % 4, qkv, qk_all, ng_t, Tri, SL, Mask2, ones64,
                           ident32, Sst, eps_rms, ybuf[yb_s], yb_s)
    cols = slice(st * 512, (st + 1) * 512)
    dma(P, "poolq", yb_o[cols].rearrange("(c p) h d -> p c h d", p=64), ybuf[yb_s][:], reads=[("ybuf", yb_s, c_, h_) for c_ in range(8) for h_ in range(2)],
        writes=[("yb_o", st)])
    outs.append(("yb_o", st))


def gdn_chunk_head(P, nc, B, h, cs, c, dch, kch, d, sl, qkv, qk_all, ng_t, Tri, SL, Mask2, ones64, ident32, Sst, eps_rms, ybuf, yb_s):
    V, G, A, T = nc.vector, nc.gpsimd, nc.scalar, nc.tensor
    hp = slice(h * 64, (h + 1) * 64)
    idh = ident32[hp, hp]
    id0 = ident32[0:64, 0:64]
    k = lambda n: ("ghd", sl, n)
    g_col = dch["bg"][:, 2 + h:3 + h]
    beta_col = dch["bg"][:, h:h + 1]
    nbeta_col = dch["nbeta"][:, h:h + 1]

    def tr1():
        for c3 in range(3):
            ins = T.transpose(out=B[4][0:64, c3 * 64:(c3 + 1) * 64], in_=qkv[c3][hp, cs], identity=idh)
        return ins
    P.op("pe", tr1, reads=qk_all + ["ident32"], writes=["b4"])
    P.op("act", lambda: A.copy(out=d["qkv_tm"][:].rearrange("p a b -> p (a b)"), in_=B[4][0:64, 0:192]), reads=["b4"], writes=[k("qkv_tm")])

    def mm2():
        T.matmul(B[5][0:64, 256:257], lhsT=Tri[:], rhs=g_col, start=True, stop=True)
        return T.matmul(B[5][0:64, 257:258], lhsT=ones64[:], rhs=g_col, start=True, stop=True)
    P.op("pe", mm2, reads=[kch("bg"), "Tri", "ones64"], writes=["b5"])
    P.op("dve", lambda: V.tensor_copy(out=d["gcl"][:], in_=B[5][0:64, 256:258]), reads=["b5"], writes=[k("gcl")])
    P.op("act", lambda: A.activation(out=d["ex3"][:, 0:1], in_=d["gcl"][:, 0:1], func=AF.Exp), reads=[k("gcl")], writes=[k("ex3")])
    P.op("act", lambda: A.activation(out=d["ex3"][:, 1:2], in_=d["gcl"][:, 0:1], func=AF.Exp, bias=d["gcl"][:, 1:2], scale=-1.0),
         reads=[k("gcl"), k("ex3")], writes=[k("ex3")])
    P.op("act", lambda: A.activation(out=d["ex3"][:, 2:3], in_=d["gcl"][:, 1:2], func=AF.Exp), reads=[k("gcl"), k("ex3")], writes=[k("ex3")])
    P.op("dve", lambda: V.tensor_scalar(out=d["Gm"][:], in0=Tri[:], scalar1=g_col, scalar2=None, op0=ALU.mult), reads=["Tri", kch("bg")], writes=[k("Gm")])

    def mm3():
        T.matmul(B[6][0:64, 0:64], lhsT=d["Gm"][:], rhs=SL[:], start=True, stop=True)
        return T.matmul(B[6][0:64, 64:128], lhsT=SL[:], rhs=d["Gm"][:], start=True, stop=True)
    P.op("pe", mm3, reads=[k("Gm"), "SL"], writes=["b6"])
    P.op("act", lambda: A.activation(out=d["EE"][:].rearrange("p a b -> p (a b)"), in_=B[6][0:64, 0:128], func=AF.Exp), reads=["b6"], writes=[k("EE")])
    P.op("pool", lambda: G.tensor_tensor(out=d["EE"][:], in0=d["EE"][:], in1=Mask2[:], op=ALU.mult), reads=[k("EE"), "Mask2"], writes=[k("EE")])

    def mm4():
        T.matmul(B[7][0:64, 0:64], lhsT=qkv[1][hp, cs], rhs=qkv[1][hp, cs], start=True, stop=True)
        return T.matmul(B[7][0:64, 64:128], lhsT=qkv[1][hp, cs], rhs=qkv[0][hp, cs], start=True, stop=True)
    P.op("pe", mm4, reads=qk_all, writes=["b7"])
    P.op("dve", lambda: V.scalar_tensor_tensor(out=d["Pm"][:], in0=B[7][0:64, 0:64], scalar=nbeta_col, in1=d["EE"][:, 0, :], op0=ALU.mult, op1=ALU.mult),
         reads=["b7", kch("nbeta"), k("EE")], writes=[k("Pm")])
    P.op("dve", lambda: V.tensor_tensor(out=d["AT"][:], in0=B[7][0:64, 64:128], in1=d["EE"][:, 1, :], op=ALU.mult), reads=["b7", k("EE")], writes=[k("AT")])
    P.op("pe", lambda: T.transpose(out=B[4][0:64, 192:256], in_=d["Pm"][:], identity=id0), reads=[k("Pm"), "ident32"], writes=["b4"])
    P.op("act", lambda: A.copy(out=d["PTm"][:], in_=B[4][0:64, 192:256]), reads=["b4"], writes=[k("PTm")])
    P.op("dve", lambda: V.tensor_tensor(out=d["tb"][:], in0=beta_col, in1=d["ex3"][:, 0:1], op=ALU.mult), reads=[kch("bg"), k("ex3")], writes=[k("tb")])
    P.op("dve", lambda: V.tensor_scalar(out=d["X"][:, 0:64], in0=d["qkv_tm"][:, 2, :], scalar1=beta_col, scalar2=None, op0=ALU.mult),
         reads=[k("qkv_tm"), kch("bg")], writes=[k("X")])
    P.op("dve", lambda: V.tensor_scalar(out=d["X"][:, 64:128], in0=d["qkv_tm"][:, 1, :], scalar1=d["tb"][:, 0:1], scalar2=None, op0=ALU.mult),
         reads=[k("qkv_tm"), k("tb"), k("X")], writes=[k("X")])
    cur = (d["Pm"][:], d["PTm"][:], [k("Pm"), k("PTm")])
    for lvl in range(6):
        Pc, PTc, pk = cur
        by = 4 + (lvl % 2)
        P.op("pe", lambda PTc=PTc, by=by: T.matmul(B[by][0:64, 256:384], lhsT=PTc, rhs=d["X"][:], start=True, stop=True),
             reads=pk + [k("X")], writes=[f"b{by}"])
        P.op("dve", lambda by=by: V.tensor_tensor(out=d["X"][:], in0=d["X"][:], in1=B[by][0:64, 256:384], op=ALU.add),
             reads=[f"b{by}", k("X")], writes=[k("X")])
        if lvl < 5:
            bs_ = 6 + (lvl % 2)
            pp = d["PP"][lvl % 2]

            def mmsq(Pc=Pc, PTc=PTc, bs_=bs_):
                T.matmul(B[bs_][0:64, 256:320], lhsT=PTc, rhs=Pc, start=True, stop=True)
                return T.matmul(B[bs_][0:64, 320:384], lhsT=Pc, rhs=PTc, start=True, stop=True)
            P.op("pe", mmsq, reads=pk, writes=[f"b{bs_}"])
            P.op("act", lambda pp=pp, bs_=bs_: A.copy(out=pp[:].rearrange("p a b -> p (a b)"), in_=B[bs_][0:64, 256:384]),
                 reads=[f"b{bs_}"], writes=[k(("PP", lvl % 2))])
            cur = (pp[:, 0, :], pp[:, 1, :], [k(("PP", lvl % 2))])
    P.op("pool", lambda: G.tensor_scalar(out=d["kdec"][:], in0=d["qkv_tm"][:, 1, :], scalar1=d["ex3"][:, 1:2], scalar2=None, op0=ALU.mult),
         reads=[k("qkv_tm"), k("ex3")], writes=[k("kdec")])
    P.op("pool", lambda: G.tensor_scalar(out=d["qdec"][:], in0=d["qkv_tm"][:, 0, :], scalar1=d["ex3"][:, 0:1], scalar2=None, op0=ALU.mult),
         reads=[k("qkv_tm"), k("ex3")], writes=[k("qdec")])

    def tr8():
        T.transpose(out=B[4][0:64, 0:64], in_=d["X"][:, 64:128], identity=id0)
        return T.transpose(out=B[4][0:64, 64:128], in_=d["qdec"][:], identity=id0)
    P.op("pe", tr8, reads=[k("X"), k("qdec"), "ident32"], writes=["b4"])
    P.op("act", lambda: A.copy(out=d["wqT"][:].rearrange("p a b -> p (a b)"), in_=B[4][0:64, 0:128]), reads=["b4"], writes=[k("wqT")])
    S_ = Sst[h]
    P.op("pe", lambda: T.matmul(B[5][0:64, 0:64], lhsT=d["wqT"][:, 0, :], rhs=S_[:], start=True, stop=True), reads=[k("wqT"), ("S", h)], writes=["b5"])
    P.op("dve", lambda: V.tensor_tensor(out=d["vnew"][:], in0=d["X"][:, 0:64], in1=B[5][0:64, 0:64], op=ALU.subtract),
         reads=["b5", k("X")], writes=[k("vnew")])

    def mmo():
        T.matmul(B[6][0:64, 0:64], lhsT=d["wqT"][:, 1, :], rhs=S_[:], start=True, stop=False)
        return T.matmul(B[6][0:64, 0:64], lhsT=d["AT"][:], rhs=d["vnew"][:], start=False, stop=True)
    P.op("pe", mmo, reads=[k("wqT"), ("S", h), k("AT"), k("vnew")], writes=["b6"])
    P.op("pe", lambda: T.matmul(B[7][0:64, 0:64], lhsT=d["kdec"][:], rhs=d["vnew"][:], start=True, stop=True), reads=[k("kdec"), k("vnew")], writes=["b7"])
    P.op("dve", lambda: V.scalar_tensor_tensor(out=S_[:], in0=S_[:], scalar=d["ex3"][:, 2:3], in1=B[7][0:64, 0:64], op0=ALU.mult, op1=ALU.add),
         reads=["b7", ("S", h), k("ex3")], writes=[("S", h)])
    P.op("act", lambda: A.copy(out=d["osb"][:], in_=B[6][0:64, 0:64]), reads=["b6"], writes=[k("osb")])
    P.op("pool", lambda: G.tensor_tensor(out=d["osq"][:], in0=d["osb"][:], in1=d["osb"][:], op=ALU.mult), reads=[k("osb")], writes=[k("osq")])
    P.op("dve", lambda: V.tensor_reduce(out=d["oss"][:], in_=d["osq"][:], axis=AX.X, op=ALU.add), reads=[k("osq")], writes=[k("oss")])
    P.op("act", lambda: A.activation(out=d["oss"][:], in_=d["oss"][:], func=AF.Sqrt, bias=eps_rms[0:64, 0:1], scale=1.0 / 64),
         reads=[k("oss"), "eps_rms"], writes=[k("oss")])
    P.op("dve", lambda: V.reciprocal(out=d["oss"][:], in_=d["oss"][:]), reads=[k("oss")], writes=[k("oss")])
    P.op("pool", lambda: G.tensor_tensor(out=d["ngate"][:], in0=dch["gs"][:, h * 64:(h + 1) * 64], in1=ng_t[:], op=ALU.mult),
         reads=[kch("gs"), "ngt"], writes=[k("ngate")])
    P.op("dve", lambda: V.scalar_tensor_tensor(out=ybuf[:, c, h, :], in0=d["osb"][:], scalar=d["oss"][:, 0:1], in1=d["ngate"][:], op0=ALU.mult, op1=ALU.mult),
         reads=[k("osb"), k("oss"), k("ngate")], writes=[("ybuf", yb_s, c, h)])


OFF_AQ, OFF_AK, OFF_AV, OFF_BQKV, OFF_BGATE, OFF_BBETA, OFF_BA, OFF_CU = 0, 384, 768, 1152, 2304, 2688, 2694, 2700
GDN_HEADS_OF = [(0, 1), (2, 3), (4, 5), (4, 5)]


def _wl(w, cols):
    return w[:, cols].reshape(8, 128, len(cols)).transpose(1, 0, 2)


def mix_consts():
    d = {}
    k = np.arange(128)[:, None, None]; j = np.arange(4)[None, :, None]; q = np.arange(512)[None, None, :]
    d["amask"] = bf((q // 64 >= (j * 128 + k) // 64).astype(np.float32))
    d["idn32"] = f32c(np.eye(128))
    d["srow"] = f32c(np.arange(512).reshape(1, 512))
    m = np.arange(64)[:, None]; i = np.arange(64)[None, :]
    d["cTri"] = f32c(m <= i)
    d["cSL"] = f32c(m > i)
    d["cMask2"] = f32c(np.stack([(m > i), (m <= i)], axis=1))
    bo = np.zeros((128, 128), np.float32); bo[:64, :64] = 1; bo[64:, 64:] = 1
    d["cBones"] = bo
    return d


def mix_inputs(l, p, j):
    w = p["w_in"][l]
    d = {}
    units = [3 * j + i for i in range(3)]
    qc, kc_, vc = [], [], []
    for u in units:
        head, mp = u // 2, u % 2
        qc += list(range(OFF_AQ + head * 64 + mp * 32, OFF_AQ + head * 64 + mp * 32 + 32))
        kc_ += list(range(OFF_AK + head * 64 + mp * 32, OFF_AK + head * 64 + mp * 32 + 32))
        vc += list(range(OFF_AV + head * 64, OFF_AV + head * 64 + 64))
    d["wq"] = bf(_wl(w, qc)); d["wk"] = bf(_wl(w, kc_)); d["wv"] = bf(_wl(w, vc))
    gs = [4 * j + i for i in range(4)]
    d["wu"] = bf(_wl(w, list(range(OFF_CU + gs[0] * 16, OFF_CU + gs[0] * 16 + 64))))
    lre, lim, ldt = p["s5_lambda_re"][l], p["s5_lambda_im"][l], p["s5_log_dt"][l]
    row = np.zeros((2, 3, 128), np.float32)
    bT = np.zeros((2, 2, 2, 16, 64), np.float32); cT = np.zeros((2, 2, 2, 64, 16), np.float32)
    for pr in range(2):
        for g in range(2):
            G_ = gs[pr * 2 + g]
            row[pr, 0, g * 64:(g + 1) * 64] = lre[G_]; row[pr, 1, g * 64:(g + 1) * 64] = lim[G_]; row[pr, 2, g * 64:(g + 1) * 64] = ldt[G_]
            bT[pr, g, 0] = p["s5_b_re"][l][G_].T; bT[pr, g, 1] = p["s5_b_im"][l][G_].T
            cT[pr, g, 0] = p["s5_c_re"][l][G_].T; cT[pr, g, 1] = p["s5_c_im"][l][G_].T
    d["s5row"] = row; d["s5col"] = f32c(row.transpose(0, 2, 1)); d["s5bT"] = bT; d["s5cT"] = cT
    d["s5d"] = f32c(p["s5_d"][l][gs[0] * 16:gs[0] * 16 + 64].reshape(64, 1))
    hA, hB = GDN_HEADS_OF[j]
    gcols = []
    for part in range(3):
        for h in (hA, hB):
            gcols += list(range(OFF_BQKV + part * 384 + h * 64, OFF_BQKV + part * 384 + h * 64 + 64))
    d["wg"] = bf(_wl(w, gcols))
    tcols = list(range(OFF_BGATE + hA * 64, OFF_BGATE + hA * 64 + 64)) + list(range(OFF_BGATE + hB * 64, OFF_BGATE + hB * 64 + 64)) \
        + [OFF_BBETA + hA, OFF_BBETA + hB, OFF_BA + hA, OFF_BA + hB]
    d["wt"] = bf(_wl(w, tcols))
    cw = p["dn_conv_w"][l]
    cv = np.zeros((128, 3, 4), np.float32)
    for part in range(3):
        for hi, h in enumerate((hA, hB)):
            cv[hi * 64:(hi + 1) * 64, part, :] = cw[:, part * 384 + h * 64: part * 384 + h * 64 + 64].T
    d["cvw"] = cv
    d["galog"] = f32c(p["dn_a_log"][l][[hA, hB]].reshape(1, 2)); d["gdtb"] = f32c(p["dn_dt_bias"][l][[hA, hB]].reshape(1, 2))
    d["gng"] = f32c(p["dn_norm_g"][l].reshape(1, 64))
    return d


YCH = 512
NYCH = SEQ // YCH


def build_program(stop=None):
    nc = bass.Bass("TRN2", target_bir_lowering=False)
    G = Glob(nc)
    hbuf = [G.internal(f"hbuf{i}", [TOK_CORE, D]) for i in range(2)]
    hT_loc = G.internal("hT_loc", [D, TOK_CORE], BF16)
    hT_all = G.internal("hT_all", [8, 4 * 128, TOK_CORE], BF16)
    y_o = G.internal("y_o", [SEQ, 384])
    y_all = G.internal("y_all", [NYCH, 4 * YCH, 384])
    ag_h = [(hT_loc[k * 128:(k + 1) * 128, :], hT_all[k]) for k in range(8)]
    ag_y = [(y_o[i * YCH:(i + 1) * YCH, :], y_all[i]) for i in range(NYCH)]
    out = nc.dram_tensor("out", [TOK_CORE, D], F32, kind="ExternalOutput").ap()

    def finish_early(src_ap):
        with nc.semaphore("fin") as fs:
            with nc.Block() as block:
                @block.gpsimd
                def _(g):
                    g.sem_clear(fs)
            with nc.Block() as block:
                @block.sync
                def _(sp):
                    sp.dma_start(out=out, in_=src_ap).then_inc(fs, 16)
                    sp.wait_ge(fs, 16)
        return nc, G
    phase_pre(nc, G, hbuf[0], hT_loc)
    for l in range(DEPTH):
        last = l == DEPTH - 1
        allgather(nc, ag_h)
        if stop == (l, "ag1"):
            return finish_early(hbuf[0])
        phase_mix(nc, G, l, hT_all, y_o, do_attn=True, do_s5=False, do_gdn=False)
        if stop == (l, "mixa"):
            return finish_early(hbuf[0])
        phase_mix(nc, G, l, hT_all, y_o, do_attn=False, do_s5=True, do_gdn=True)
        if stop == (l, "mixb"):
            return finish_early(hbuf[0])
        allgather(nc, ag_y)
        if stop == (l, "ag2"):
            return finish_early(hbuf[0])
        phase_post(nc, G, l, hbuf[l % 2], out if last else hbuf[(l + 1) % 2], None if last else hT_loc, y_all)
        if stop == (l, "post"):
            return finish_early(hbuf[(l + 1) % 2])
    return nc, G


def kernel(_stop=None, **inputs):
    p = {k: np.asarray(v) for k, v in inputs.items()}
    x = f32c(p["x"]).reshape(BATCH * SEQ, D)
    cores = list(range(NCORES))
    nc, G = build_program(_stop)
    shared = dict(mix_consts())
    shared["idn"] = bf(np.eye(128))
    shared["g"] = f32c(p["ln_in_g"].reshape(1, D)); shared["b"] = f32c(p["ln_in_b"].reshape(1, D))
    percore = [dict() for _ in range(4)]
    for l in range(DEPTH):
        lam_init = 0.8 - 0.6 * math.exp(-0.3 * l)
        for k, v in post_inputs(l, p, lam_init).items():
            if k not in ("idn", "idn32"):
                shared[f"L{l}_{k}"] = v
        for j in range(4):
            for k, v in mix_inputs(l, p, j).items():
                percore[j][f"L{l}_{k}"] = v
    ins = []
    for c in cores:
        d = dict(shared); d.update(percore[c % 4])
        d["x"] = x[c * TOK_CORE:(c + 1) * TOK_CORE]
        d["rofs"] = np.array([[(c % 4) * (TOK_CORE // YCH)]], np.int32)
        ins.append({k: v for k, v in d.items() if k in G.t})
    res = run_bass_kernel_spmd(nc, ins, core_ids=cores)
    h = [np.asarray(r["out"]) for r in res.results]
    return np.concatenate(h, axis=0).reshape(BATCH, SEQ, D).astype(np.float32)
```

```python
import math
from contextlib import ExitStack

import numpy as np
import ml_dtypes
import concourse.bass as bass
import concourse.mybir as mybir
from concourse.bass_utils import run_bass_kernel_spmd

F32 = mybir.dt.float32
BF16 = mybir.dt.bfloat16
I32 = mybir.dt.int32
ALU = mybir.AluOpType
AF = mybir.ActivationFunctionType
AX = mybir.AxisListType

NCORES = 8


class Prog:
    COMPUTE = ("pe", "act", "dve", "pool")
    NDMASEM = 6

    _uid = 0
    _phase = 0

    def __init__(self, nc):
        Prog._phase += 1
        self.ph = Prog._phase
        self.nc = nc
        self.ops = []
        self.last_w = {}
        self.readers = {}
        self.dma_count = {"sp": 0, "actq": 0, "poolq": 0}
        self.stack = ExitStack()
        self.nt = 0
        self.excl = set()
        self.quarters = False
        self.bankkeys = {f"b{i}" for i in range(8)}
        self.sp_wrap = None

    def sb(self, shape, dtype, name=None):
        Prog._uid += 1
        return self.stack.enter_context(self.nc.sbuf_tensor(f"{name or 't'}_{Prog._uid}", list(shape), dtype))

    def ps(self, shape, dtype=F32, name=None):
        Prog._uid += 1
        return self.stack.enter_context(self.nc.psum_tensor(f"{name or 'p'}_{Prog._uid}", list(shape), dtype))

    def op(self, eng, fn, reads=(), writes=()):
        idx = len(self.ops)
        isdma = eng in self.dma_count
        issue = {"sp": "sp", "actq": "act", "poolq": "pool"}.get(eng, eng)
        if self.quarters:
            ex = lambda ks: [q for k in ks for q in ([f"{k}q{i}" for i in range(4)] if k in self.bankkeys else [k])]
            reads, writes = ex(reads), ex(writes)
        if self.excl:
            writes = list(writes) + [k for k in reads if k in self.excl]
            reads = [k for k in reads if k not in self.excl]
        deps = set()
        for k in reads:
            w = self.last_w.get(k)
            if w is not None:
                deps.add(w)
        for k in writes:
            w = self.last_w.get(k)
            if w is not None:
                deps.add(w)
            for r in self.readers.get(k, ()):
                deps.add(r)
        o = dict(idx=idx, eng=eng, issue=issue, fn=fn, deps=deps, isdma=isdma, needed=False)
        if isdma:
            n = self.dma_count[eng]
            self.dma_count[eng] = n + 1
            o["dsem"] = n % self.NDMASEM
            o["dtarget"] = 16 * (n // self.NDMASEM + 1)
            o["dprev"] = 16 * (n // self.NDMASEM)
        self.ops.append(o)
        for k in writes:
            self.last_w[k] = idx
            self.readers[k] = []
        for k in reads:
            lst = self.readers.setdefault(k, [])
            if not isdma:
                lst[:] = [r for r in lst if self.ops[r]["isdma"] or self.ops[r]["eng"] != eng]
            lst.append(idx)
        return idx

    def emit(self, final_wait_keys=()):
        nc = self.nc
        ops = self.ops
        for o in ops:
            nd = set()
            for d in o["deps"]:
                p = ops[d]
                if (not p["isdma"]) and (not o["isdma"]) and p["eng"] == o["eng"]:
                    if o["eng"] == "pe":
                        continue
                nd.add(d)
            o["deps"] = nd
            for d in nd:
                ops[d]["needed"] = True
        final = [self.last_w[k] for k in final_wait_keys if k in self.last_w]
        for d in final:
            ops[d]["needed"] = True
        tick = {e: 0 for e in self.COMPUTE}
        for o in ops:
            if not o["isdma"] and o["needed"]:
                tick[o["eng"]] += 1
                o["tick"] = tick[o["eng"]]
        sems = {e: self.stack.enter_context(nc.semaphore(f"s_{e}_{self.ph}")) for e in self.COMPUTE}
        dsems = {q: [self.stack.enter_context(nc.semaphore(f"d_{q}{i}_{self.ph}")) for i in range(self.NDMASEM)]
                 for q in self.dma_count}
        per = {e: [] for e in ("pe", "act", "dve", "pool", "sp")}
        for o in ops:
            per[o["issue"]].append(o)
        engobj = {"pe": nc.tensor, "act": nc.scalar, "dve": nc.vector, "pool": nc.gpsimd, "sp": nc.sync}

        def run(ename, extra_final=False):
            eng = engobj[ename]
            waited = {}

            def wait_for(p):
                if p["isdma"]:
                    key = (p["eng"], p["dsem"])
                    val = p["dtarget"]
                    s = dsems[p["eng"]][p["dsem"]]
                else:
                    key = p["eng"]
                    val = p["tick"]
                    s = sems[p["eng"]]
                if waited.get(key, 0) >= val:
                    return
                waited[key] = val
                eng.wait_ge(s, val)

            for o in per[ename]:
                for d in sorted(o["deps"]):
                    wait_for(ops[d])
                if o["isdma"] and o["dprev"] > 0:
                    key = (o["eng"], o["dsem"])
                    if waited.get(key, 0) < o["dprev"]:
                        waited[key] = o["dprev"]
                        eng.wait_ge(dsems[o["eng"]][o["dsem"]], o["dprev"])
                ins = o["fn"]()
                if o["isdma"]:
                    ins.then_inc(dsems[o["eng"]][o["dsem"]], 16)
                elif o["needed"]:
                    ins.then_inc(sems[o["eng"]], 1)
            if extra_final:
                for d in final:
                    wait_for(ops[d])

        allsems = list(sems.values()) + [s for q in dsems.values() for s in q]
        with nc.Block() as block:
            @block.gpsimd
            def _(e):
                for s in allsems:
                    e.sem_clear(s)

        with nc.Block() as block:
            @block.sync
            def _(e):
                if self.sp_wrap is not None:
                    with self.sp_wrap(e):
                        run("sp", extra_final=True)
                else:
                    run("sp", extra_final=True)

            @block.tensor
            def _(e):
                run("pe")

            @block.scalar
            def _(e):
                run("act")

            @block.vector
            def _(e):
                run("dve")

            @block.gpsimd
            def _(e):
                run("pool")


D = 1024
SEQ = 16384
BATCH = 2
DEPTH = 2
TOK_CORE = 4096
ALPHA = (2 * DEPTH) ** 0.25
LN_EPS = 1e-5
RMS_EPS = 1e-6
NEXP = 16
DEXP = 512


def bf(a):
    return np.ascontiguousarray(np.asarray(a, np.float32).astype(ml_dtypes.bfloat16))


def f32c(a):
    return np.ascontiguousarray(np.asarray(a, np.float32))


class Ctx:
    def __init__(self, nc):
        self.nc = nc
        self.P = Prog(nc)
        self.banks = None

    def alloc_banks(self, quarters=False):
        self.banks = [self.P.ps([128, 512], F32, name=f"bank{i}") for i in range(8)]
        self.P.excl |= {f"b{i}" for i in range(8)}
        if quarters:
            self.P.quarters = True
            self.P.excl |= {f"b{i}q{q}" for i in range(8) for q in range(4)}


class Glob:
    def __init__(self, nc):
        self.nc = nc
        self.t = {}

    def din(self, name, shape, dt=F32):
        if name not in self.t:
            self.t[name] = self.nc.dram_tensor(name, list(shape), dt, kind="ExternalInput").ap()
        return self.t[name]

    def internal(self, name, shape, dt=F32):
        if name not in self.t:
            self.t[name] = self.nc.dram_tensor(name, list(shape), dt).ap()
        return self.t[name]


GROUPS = [[0, 1, 2, 3], [4, 5, 6, 7]]


def allgather(nc, pairs):
    Prog._uid += 1
    with nc.semaphore(f"cc_{Prog._uid}") as cc:
        with nc.Block() as block:
            @block.gpsimd
            def _(g):
                g.sem_clear(cc)
        with nc.Block() as block:
            @block.gpsimd
            def _(g):
                for src, dst in pairs:
                    g.collective_compute("AllGather", ALU.bypass, replica_groups=GROUPS, ins=[src], outs=[dst]).then_inc(cc, 1)
                g.wait_ge(cc, len(pairs))


def dma(P, q, out, in_, reads=(), writes=()):
    eng = {"sp": P.nc.sync, "actq": P.nc.scalar, "poolq": P.nc.gpsimd}[q]
    return P.op(q, lambda: eng.dma_start(out=out, in_=in_), reads=reads, writes=writes)


def layer_norm_tile(P, nc, src, srck, dst, dstk, g_t, b_t, gk, bk, scr, tag):
    st, mv, rstd, xn = scr["st"], scr["mv"], scr["rstd"], scr["xn"]

    def bs():
        nc.vector.bn_stats(out=st[:, 0, :], in_=src[:, 0:512])
        return nc.vector.bn_stats(out=st[:, 1, :], in_=src[:, 512:1024])
    P.op("dve", bs, reads=[srck], writes=[tag + "st"])
    P.op("dve", lambda: nc.vector.bn_aggr(out=mv[:], in_=st[:].rearrange("p a s -> p (a s)")),
         reads=[tag + "st"], writes=[tag + "mv"])
    P.op("act", lambda: nc.scalar.activation(out=rstd[:], in_=mv[:, 1:2], func=AF.Sqrt, bias=scr["eps_ln"][:, 0:1], scale=1.0),
         reads=[tag + "mv"], writes=[tag + "rstd"])
    P.op("dve", lambda: nc.vector.reciprocal(out=rstd[:], in_=rstd[:]), reads=[tag + "rstd"], writes=[tag + "rstd"])
    P.op("dve", lambda: nc.vector.tensor_scalar(out=xn[:], in0=src[:], scalar1=mv[:, 0:1], scalar2=rstd[:, 0:1],
                                                op0=ALU.subtract, op1=ALU.mult),
         reads=[srck, tag + "mv", tag + "rstd"], writes=[tag + "xn"])
    P.op("pool", lambda: nc.gpsimd.tensor_tensor(out=xn[:], in0=xn[:], in1=g_t[:], op=ALU.mult),
         reads=[tag + "xn", gk], writes=[tag + "xn"])
    P.op("pool", lambda: nc.gpsimd.tensor_tensor(out=dst[:], in0=xn[:], in1=b_t[:], op=ALU.add),
         reads=[tag + "xn", bk], writes=[dstk])


def ln_scratch(P, tag):
    return dict(st=P.sb([128, 2, 6], F32), mv=P.sb([128, 2], F32), rstd=P.sb([128, 1], F32),
                xn=P.sb([128, 1024], F32))


def const_col(P, nc, val, key):
    t = P.sb([128, 1], F32)
    P.op("pool", lambda: nc.gpsimd.memset(t[:], val), writes=[key])
    return t


def to_featmajor_bf16(P, nc, src, srck, hb, hbk, bank, bankk, dstT, dstk, ident, cast_eng="act"):
    if cast_eng == "act":
        P.op("act", lambda: nc.scalar.copy(out=hb[:], in_=src[:]), reads=[srck], writes=[hbk])
    else:
        P.op("pool", lambda: nc.gpsimd.tensor_copy(out=hb[:], in_=src[:]), reads=[srck], writes=[hbk])
    pv = bank[:].bitcast(BF16).rearrange("p (k t) -> p k t", k=8)

    def tr():
        for kc in range(8):
            ins = nc.tensor.transpose(out=pv[:, kc, :], in_=hb[:, kc * 128:(kc + 1) * 128], identity=ident[:])
        return ins
    P.op("pe", tr, reads=[hbk, "ident"], writes=[bankk])
    P.op("dve", lambda: nc.vector.tensor_copy(out=dstT, in_=pv), reads=[bankk], writes=[dstk])


def phase_pre(nc, G, h, hT_loc, ntok=TOK_CORE):
    C = Ctx(nc)
    P = C.P
    nt = ntok // 128
    x = G.din("x", [ntok, D])
    g = G.din("g", [1, D])
    b = G.din("b", [1, D])
    idn = G.din("idn", [128, 128], BF16)
    hT = hT_loc.rearrange("(k p) t -> k p t", k=8)
    with P.stack:
        C.alloc_banks()
        gt = P.sb([128, D], F32)
        bt = P.sb([128, D], F32)
        ident = P.sb([128, 128], BF16)
        eps = const_col(P, nc, LN_EPS, "eps_ln")
        xt = [P.sb([128, D], F32) for _ in range(2)]
        ht = [P.sb([128, D], F32) for _ in range(2)]
        hb = P.sb([128, D], BF16)
        hTt = [P.sb([128, 8, 128], BF16) for _ in range(2)]
        scr = ln_scratch(P, "ln")
        scr["eps_ln"] = eps
        dma(P, "sp", gt[:], g.partition_broadcast(128), writes=["g"])
        dma(P, "sp", bt[:], b.partition_broadcast(128), writes=["b"])
        dma(P, "sp", ident[:], idn, writes=["ident"])
        outs = []
        for t in range(nt):
            s = t % 2
            dma(P, "sp", xt[s][:], x[t * 128:(t + 1) * 128, :], writes=[("xt", s)])
            layer_norm_tile(P, nc, xt[s], ("xt", s), ht[s], ("ht", s), gt, bt, "g", "b", scr, "ln")
            dma(P, "poolq", h[t * 128:(t + 1) * 128, :], ht[s][:], reads=[("ht", s)], writes=[("h", t)])
            to_featmajor_bf16(P, nc, ht[s], ("ht", s), hb, "hb", C.banks[s], f"b{s}", hTt[s][:], ("hTt", s), ident)
            dma(P, "sp", hT[:, :, t * 128:(t + 1) * 128].rearrange("k p t -> p k t"), hTt[s][:],
                reads=[("hTt", s)], writes=[("hT", t)])
            outs += [("h", t), ("hT", t)]
        P.emit(final_wait_keys=outs)


BIG = 1.0e4


def phase_post(nc, G, l, h_in, h_out, hT_loc, y_all, ntok=TOK_CORE):
    C = Ctx(nc)
    P = C.P
    nsup = ntok // 512
    pf = f"L{l}_"

    def din(name, shape, dt=F32):
        return G.din(pf + name, shape, dt)
    wout = din("wout", [128, 8, D], BF16)
    wglu = din("wglu", [128, 2, 256], BF16)
    ln1g = din("ln1g", [1, D]); ln1b = din("ln1b", [1, D]); ln2g = din("ln2g", [1, D]); ln2b = din("ln2b", [1, D])
    dng = din("dng", [1, 64]); lamv = din("lamv", [1, 130])
    wr = din("wr", [128, 8, 20]); br = din("br", [1, 20])
    w1 = din("w1", [NEXP, 128, 8, DEXP], BF16); w3 = din("w3", [NEXP, 128, 8, DEXP], BF16)
    w2 = din("w2", [NEXP, 128, 4, D], BF16)
    idn = G.din("idn", [128, 128], BF16); idn32 = G.din("idn32", [128, 128])
    rofs = G.din("rofs", [1, 1], I32)
    hT_out = hT_loc.rearrange("(k p) t -> k p t", k=8) if hT_loc is not None else None
    ymj = G.internal("y_mine", [4, ntok, 384])
    ym = ymj.rearrange("j s c -> s j c")
    offh = {}

    from contextlib import contextmanager

    @contextmanager
    def sp_wrap(sp):
        with sp.register(f"rofs{l}") as reg:
            sp.reg_load(reg, rofs[0:1, 0:1])
            offh["v"] = sp.snap(reg)
            yield
    P.sp_wrap = sp_wrap
    nch = ntok // YCH
    yj = y_all.rearrange("i (j s) c -> j i s c", j=4)
    for j in range(4):
        P.op("sp", lambda j=j: nc.sync.dma_start(out=ymj[j].rearrange("(i a b) c -> i a (b c)", i=nch, a=16),
                                                 in_=yj[j][bass.ds(offh["v"], nch)].rearrange("i (a b) c -> i a (b c)", a=16)),
             writes=[("ymine", j)])

    with P.stack:
        C.alloc_banks()
        B = C.banks
        sb = P.sb
        ident = sb([128, 128], BF16); ident32 = sb([128, 128], F32)
        g1 = sb([128, D], F32); b1 = sb([128, D], F32); g2 = sb([128, D], F32); b2 = sb([128, D], F32)
        wout_t = sb([128, 8, D], BF16); wglu_t = sb([128, 2, 256], BF16)
        wr_t = sb([128, 8, 20], F32); br_t = sb([128, 20], F32)
        gA = sb([128, 64], F32); lam_t = sb([128, 130], F32); lprod = sb([128, 2, 32], F32)
        lsum = sb([128, 2], F32); nlam = sb([128, 1], F32)
        eps_ln = const_col(P, nc, LN_EPS, "eps_ln"); eps_rms = const_col(P, nc, RMS_EPS, "eps_rms")
        scr = ln_scratch(P, "ln"); scr["eps_ln"] = eps_ln
        ht = [sb([128, D], F32) for _ in range(2)]
        ya_t = [sb([128, 768], F32) for _ in range(2)]
        yc_t = [sb([128, 256], F32) for _ in range(2)]
        ymix = [sb([128, D], F32) for _ in range(2)]
        dd = sb([128, 6, 64], F32); sq = sb([128, 6, 64], F32); ss = sb([128, 6], F32)
        c2 = sb([128, 256], F32); c3 = sb([128, 256], F32); ygb = sb([128, 256], BF16)
        ygT = sb([128, 2, 128], BF16); sig = sb([128, 256], F32)
        ymb = sb([128, D], BF16); ymT = sb([128, 8, 128], BF16)
        z = sb([128, D], F32)
        h1 = [sb([128, D], F32) for _ in range(4)]
        h1T32 = sb([128, 8, 128], F32)
        h1T = sb([128, 8, 512], BF16)
        lg = sb([128, 20], F32)
        r = {n: sb([128, s], F32) for n, s in dict(gmax=1, goh=4, ngmax=1, gexp=4, gsum=1, gp=1, em=16, pen=4, m1=1, oh1=16,
                                                   em2=16, m2=1, oh2=16, dl=1, ex=1, den=1, w1=1, w2=1).items()}
        comb = sb([128, 4, 16], F32)
        wA = [sb([128, 8, DEXP], BF16) for _ in range(2)]
        wB = [sb([128, 8, DEXP], BF16) for _ in range(2)]
        wC = [sb([128, 4, D], BF16) for _ in range(2)]
        sil = [sb([128, 512], F32) for _ in range(2)]
        hid = [sb([128, 512], BF16) for _ in range(4)]
        acc = [sb([128, D], F32) for _ in range(4)]
        z2 = sb([128, D], F32)
        ho = [sb([128, D], F32) for _ in range(2)]
        hob = sb([128, D], BF16)
        hoT = [sb([128, 8, 128], BF16) for _ in range(2)]

        dma(P, "sp", ident[:], idn, writes=["ident"]); dma(P, "sp", ident32[:], idn32, writes=["ident32"])
        for t_, s_, k_ in ((g1, ln1g, "g1"), (b1, ln1b, "b1"), (g2, ln2g, "g2"), (b2, ln2b, "b2")):
            dma(P, "sp", t_[:], s_.partition_broadcast(128), writes=[k_])
        dma(P, "sp", wout_t[:], wout, writes=["wout"]); dma(P, "sp", wglu_t[:], wglu, writes=["wglu"])
        dma(P, "sp", wr_t[:], wr, writes=["wr"]); dma(P, "sp", br_t[:], br.partition_broadcast(128), writes=["br"])
        dma(P, "sp", gA[:], dng.partition_broadcast(128), writes=["gA"])
        dma(P, "sp", lam_t[:], lamv.partition_broadcast(128), writes=["lamt"])
        P.op("dve", lambda: nc.vector.tensor_scalar(out=gA[:], in0=gA[:], scalar1=lam_t[:, 128:129], scalar2=None, op0=ALU.mult),
             reads=["gA", "lamt"], writes=["gA"])
        lv = lam_t[:, 0:128].rearrange("p (a b c) -> p a b c", a=2, b=2)
        P.op("dve", lambda: nc.vector.tensor_tensor(out=lprod[:], in0=lv[:, :, 0, :], in1=lv[:, :, 1, :], op=ALU.mult),
             reads=["lamt"], writes=["lprod"])
        P.op("dve", lambda: nc.vector.tensor_reduce(out=lsum[:], in_=lprod[:], axis=AX.X, op=ALU.add), reads=["lprod"], writes=["lsum"])
        P.op("act", lambda: nc.scalar.activation(out=lsum[:], in_=lsum[:], func=AF.Exp), reads=["lsum"], writes=["lsum"])
        P.op("dve", lambda: nc.vector.tensor_tensor(out=nlam[:], in0=lsum[:, 1:2], in1=lsum[:, 0:1], op=ALU.subtract),
             reads=["lsum"], writes=["nlam"])
        P.op("dve", lambda: nc.vector.tensor_scalar(out=nlam[:], in0=nlam[:], scalar1=lam_t[:, 129:130], scalar2=None, op0=ALU.add),
             reads=["nlam", "lamt"], writes=["nlam"])

        outs = []
        for su in range(nsup):
            for tt in range(4):
                t = su * 4 + tt
                s = t % 2
                rows = slice(t * 128, (t + 1) * 128)
                dma(P, "sp", ht[s][:], h_in[rows, :], writes=[("ht", s)])
                ymk_ = [("ymine", j_) for j_ in range(4)]
                dma(P, "sp", ya_t[s][:].rearrange("p (j c) -> p j c", j=4), ym[rows, :, 0:192], reads=ymk_, writes=[("ya", s)])
                dma(P, "sp", ymix[s][:, 384:768].rearrange("p (j c) -> p j c", j=3), ym[rows, 0:3, 192:320], reads=ymk_, writes=[("ymix", s, 1)])
                dma(P, "sp", yc_t[s][:].rearrange("p (j c) -> p j c", j=4), ym[rows, :, 320:384], reads=ymk_, writes=[("yc", s)])
                yav = ya_t[s][:].rearrange("p (h m d) -> p h m d", h=6, m=2)
                P.op("dve", lambda yav=yav: nc.vector.scalar_tensor_tensor(out=dd[:], in0=yav[:, :, 1, :], scalar=nlam[:, 0:1],
                                                                          in1=yav[:, :, 0, :], op0=ALU.mult, op1=ALU.add),
                     reads=[("ya", s), "nlam"], writes=["dd"])
                P.op("pool", lambda: nc.gpsimd.tensor_tensor(out=sq[:], in0=dd[:], in1=dd[:], op=ALU.mult), reads=["dd"], writes=["sq"])
                P.op("dve", lambda: nc.vector.tensor_reduce(out=ss[:], in_=sq[:], axis=AX.X, op=ALU.add), reads=["sq"], writes=["ss"])
                P.op("act", lambda: nc.scalar.activation(out=ss[:], in_=ss[:], func=AF.Sqrt, bias=eps_rms[:, 0:1], scale=1.0 / 64),
                     reads=["ss", "eps_rms"], writes=["ss"])
                P.op("dve", lambda: nc.vector.reciprocal(out=ss[:], in_=ss[:]), reads=["ss"], writes=["ss"])
                P.op("dve", lambda: nc.vector.tensor_tensor(out=dd[:], in0=dd[:], in1=ss[:].unsqueeze(2).to_broadcast([128, 6, 64]), op=ALU.mult),
                     reads=["dd", "ss"], writes=["dd"])
                ym0 = ymix[s][:, 0:384].rearrange("p (h d) -> p h d", h=6)
                P.op("pool", lambda ym0=ym0: nc.gpsimd.tensor_tensor(out=ym0, in0=dd[:], in1=gA[:].unsqueeze(1).to_broadcast([128, 6, 64]), op=ALU.mult),
                     reads=["dd", "gA"], writes=[("ymix", s, 0)])
                yct = yc_t[s]
                P.op("pool", lambda yct=yct: nc.gpsimd.tensor_tensor(out=c2[:], in0=yct[:], in1=yct[:], op=ALU.mult), reads=[("yc", s)], writes=["c2"])
                P.op("dve", lambda: nc.vector.tensor_scalar(out=c2[:], in0=c2[:], scalar1=0.044715, scalar2=1.0, op0=ALU.mult, op1=ALU.add),
                     reads=["c2"], writes=["c2"])
                P.op("dve", lambda yct=yct: nc.vector.tensor_tensor(out=c2[:], in0=c2[:], in1=yct[:], op=ALU.mult), reads=["c2", ("yc", s)], writes=["c2"])
                P.op("act", lambda: nc.scalar.activation(out=c2[:], in_=c2[:], func=AF.Sigmoid, scale=1.5957691216057308),
                     reads=["c2"], writes=["c2"])
                P.op("dve", lambda yct=yct: nc.vector.tensor_tensor(out=c3[:], in0=c2[:], in1=yct[:], op=ALU.mult), reads=["c2", ("yc", s)], writes=["c3"])
                P.op("act", lambda: nc.scalar.copy(out=ygb[:], in_=c3[:]), reads=["c3"], writes=["ygb"])
                pv0 = B[0][:].bitcast(BF16)

                def trg(pv0=pv0):
                    for k in range(2):
                        ins = nc.tensor.transpose(out=pv0[:, k * 128:(k + 1) * 128], in_=ygb[:, k * 128:(k + 1) * 128], identity=ident[:])
                    return ins
                P.op("pe", trg, reads=["ygb", "ident"], writes=["b0"])
                P.op("dve", lambda pv0=pv0: nc.vector.tensor_copy(out=ygT[:].rearrange("p k t -> p (k t)"), in_=pv0[:, 0:256]), reads=["b0"], writes=["ygT"])

                def mmg():
                    for k in range(2):
                        ins = nc.tensor.matmul(B[1][:, 0:256], lhsT=ygT[:, k, :], rhs=wglu_t[:, k, :], start=(k == 0), stop=(k == 1))
                    return ins
                P.op("pe", mmg, reads=["ygT", "wglu"], writes=["b1"])
                P.op("act", lambda: nc.scalar.activation(out=sig[:], in_=B[1][:, 0:256], func=AF.Sigmoid), reads=["b1"], writes=["sig"])
                P.op("dve", lambda s=s: nc.vector.tensor_tensor(out=ymix[s][:, 768:1024], in0=c3[:], in1=sig[:], op=ALU.mult),
                     reads=["c3", "sig"], writes=[("ymix", s, 2)])
                ymk = [("ymix", s, 0), ("ymix", s, 1), ("ymix", s, 2)]
                P.op("act", lambda s=s: nc.scalar.copy(out=ymb[:], in_=ymix[s][:]), reads=ymk, writes=["ymb"])
                pvb = B[0][:].bitcast(BF16).rearrange("p (k t) -> p k t", k=8)

                def try_(pvb=pvb):
                    for kc in range(8):
                        ins = nc.tensor.transpose(out=pvb[:, kc, :], in_=ymb[:, kc * 128:(kc + 1) * 128], identity=ident[:])
                    return ins
                P.op("pe", try_, reads=["ymb", "ident"], writes=["b0"])
                P.op("dve", lambda pvb=pvb: nc.vector.tensor_copy(out=ymT[:], in_=pvb), reads=["b0"], writes=["ymT"])
                for half in range(2):
                    def mmo(half=half):
                        for kc in range(8):
                            ins = nc.tensor.matmul(B[2 + half][:], lhsT=ymT[:, kc, :], rhs=wout_t[:, kc, half * 512:(half + 1) * 512],
                                                   start=(kc == 0), stop=(kc == 7))
                        return ins
                    P.op("pe", mmo, reads=["ymT", "wout"], writes=[f"b{2 + half}"])
                    P.op("dve", lambda half=half, s=s: nc.vector.scalar_tensor_tensor(
                        out=z[:, half * 512:(half + 1) * 512], in0=ht[s][:, half * 512:(half + 1) * 512], scalar=float(ALPHA),
                        in1=B[2 + half][:], op0=ALU.mult, op1=ALU.add), reads=[f"b{2 + half}", ("ht", s)], writes=[("z", half)])
                P.op("pool", lambda: nc.gpsimd.tensor_copy(out=z[:, 0:1], in_=z[:, 0:1]), reads=[("z", 0), ("z", 1)], writes=["zz"])
                layer_norm_tile(P, nc, z, "zz", h1[tt], ("h1", tt), g1, b1, "g1", "b1", scr, "ln")
                for half in range(2):
                    pv32 = B[2 + half][:].rearrange("p (k t) -> p k t", k=4)

                    def trh(half=half, pv32=pv32, tt=tt):
                        for k in range(4):
                            kc = half * 4 + k
                            ins = nc.tensor.transpose(out=pv32[:, k, :], in_=h1[tt][:, kc * 128:(kc + 1) * 128], identity=ident32[:])
                        return ins
                    P.op("pe", trh, reads=[("h1", tt), "ident32"], writes=[f"b{2 + half}"])
                    P.op("dve", lambda half=half, pv32=pv32: nc.vector.tensor_copy(out=h1T32[:, half * 4:(half + 1) * 4, :], in_=pv32),
                         reads=[f"b{2 + half}"], writes=[("h1T32", half)])
                    P.op("act", lambda half=half, pv32=pv32, tt=tt: nc.scalar.copy(
                        out=h1T[:, half * 4:(half + 1) * 4, tt * 128:(tt + 1) * 128], in_=pv32),
                        reads=[f"b{2 + half}"], writes=[("h1T", tt, half)])

                def mmr():
                    for kc in range(8):
                        ins = nc.tensor.matmul(B[1][:, 0:20], lhsT=h1T32[:, kc, :], rhs=wr_t[:, kc, :], start=(kc == 0), stop=(kc == 7))
                    return ins
                P.op("pe", mmr, reads=[("h1T32", 0), ("h1T32", 1), "wr"], writes=["b1"])
                P.op("dve", lambda: nc.vector.tensor_tensor(out=lg[:], in0=B[1][:, 0:20], in1=br_t[:], op=ALU.add), reads=["b1", "br"], writes=["lg"])
                V = nc.vector
                glog = lg[:, 0:4]
                elog = lg[:, 4:20]
                seq = [
                    lambda: V.tensor_reduce(out=r["gmax"][:], in_=glog, axis=AX.X, op=ALU.max),
                    lambda: V.tensor_scalar(out=r["goh"][:], in0=glog, scalar1=r["gmax"][:, 0:1], scalar2=None, op0=ALU.is_ge),
                    lambda: V.tensor_scalar(out=r["ngmax"][:], in0=r["gmax"][:], scalar1=-1.0, scalar2=None, op0=ALU.mult),
                ]
                for f_ in seq:
                    P.op("dve", f_, reads=["lg", "rt"], writes=["rt"])
                P.op("act", lambda: nc.scalar.activation(out=r["gexp"][:], in_=glog, func=AF.Exp, bias=r["ngmax"][:, 0:1], scale=1.0),
                     reads=["lg", "rt"], writes=["rt2"])
                seq = [
                    lambda: V.tensor_reduce(out=r["gsum"][:], in_=r["gexp"][:], axis=AX.X, op=ALU.add),
                    lambda: V.reciprocal(out=r["gp"][:], in_=r["gsum"][:]),
                    lambda: V.tensor_tensor(out=r["em"][:].rearrange("p (g e) -> p g e", g=4), in0=elog.rearrange("p (g e) -> p g e", g=4),
                                            in1=r["goh"][:].unsqueeze(2).to_broadcast([128, 4, 4]), op=ALU.mult),
                    lambda: V.tensor_scalar(out=r["pen"][:], in0=r["goh"][:], scalar1=-1.0, scalar2=BIG, op0=ALU.add, op1=ALU.mult),
                    lambda: V.tensor_tensor(out=r["em"][:].rearrange("p (g e) -> p g e", g=4), in0=r["em"][:].rearrange("p (g e) -> p g e", g=4),
                                            in1=r["pen"][:].unsqueeze(2).to_broadcast([128, 4, 4]), op=ALU.add),
                    lambda: V.tensor_reduce(out=r["m1"][:], in_=r["em"][:], axis=AX.X, op=ALU.max),
                    lambda: V.tensor_scalar(out=r["oh1"][:], in0=r["em"][:], scalar1=r["m1"][:, 0:1], scalar2=None, op0=ALU.is_ge),
                    lambda: V.scalar_tensor_tensor(out=r["em2"][:], in0=r["oh1"][:], scalar=-BIG, in1=r["em"][:], op0=ALU.mult, op1=ALU.add),
                    lambda: V.tensor_reduce(out=r["m2"][:], in_=r["em2"][:], axis=AX.X, op=ALU.max),
                    lambda: V.tensor_scalar(out=r["oh2"][:], in0=r["em2"][:], scalar1=r["m2"][:, 0:1], scalar2=None, op0=ALU.is_ge),
                    lambda: V.tensor_tensor(out=r["dl"][:], in0=r["m2"][:], in1=r["m1"][:], op=ALU.subtract),
                ]
                for f_ in seq:
                    P.op("dve", f_, reads=["lg", "rt", "rt2"], writes=["rt"])
                P.op("act", lambda: nc.scalar.activation(out=r["ex"][:], in_=r["dl"][:], func=AF.Exp), reads=["rt"], writes=["rt2"])
                seq = [
                    lambda: V.tensor_scalar(out=r["den"][:], in0=r["ex"][:], scalar1=1.0, scalar2=None, op0=ALU.add),
                    lambda: V.reciprocal(out=r["w1"][:], in_=r["den"][:]),
                    lambda: V.tensor_tensor(out=r["w1"][:], in0=r["w1"][:], in1=r["gp"][:], op=ALU.mult),
                    lambda: V.tensor_tensor(out=r["w2"][:], in0=r["w1"][:], in1=r["ex"][:], op=ALU.mult),
                    lambda tt=tt: V.tensor_scalar(out=comb[:, tt, :], in0=r["oh1"][:], scalar1=r["w1"][:, 0:1], scalar2=None, op0=ALU.mult),
                    lambda tt=tt: V.scalar_tensor_tensor(out=comb[:, tt, :], in0=r["oh2"][:], scalar=r["w2"][:, 0:1], in1=comb[:, tt, :],
                                                         op0=ALU.mult, op1=ALU.add),
                ]
                for i_, f_ in enumerate(seq):
                    P.op("dve", f_, reads=["rt", "rt2"] + ([("comb", tt)] if i_ == 5 else []), writes=["rt"] if i_ < 4 else [("comb", tt)])
            h1Tk = [("h1T", tt, half) for tt in range(4) for half in range(2)]
            for e in range(NEXP):
                ws = e % 2
                dma(P, "sp", wA[ws][:], w1[e], writes=[("wA", ws)])
                dma(P, "poolq", wB[ws][:], w3[e], writes=[("wB", ws)])
                dma(P, "sp", wC[ws][:], w2[e], writes=[("wC", ws)])
                for hc in range(4):
                    ba, bb = 4 + hc % 2, 6 + hc % 2

                    def mma(hc=hc, ba=ba, ws=ws):
                        for kc in range(8):
                            ins = nc.tensor.matmul(B[ba][:], lhsT=wA[ws][:, kc, hc * 128:(hc + 1) * 128], rhs=h1T[:, kc, :],
                                                   start=(kc == 0), stop=(kc == 7))
                        return ins

                    def mmb(hc=hc, bb=bb, ws=ws):
                        for kc in range(8):
                            ins = nc.tensor.matmul(B[bb][:], lhsT=wB[ws][:, kc, hc * 128:(hc + 1) * 128], rhs=h1T[:, kc, :],
                                                   start=(kc == 0), stop=(kc == 7))
                        return ins
                    P.op("pe", mma, reads=h1Tk + [("wA", ws)], writes=[f"b{ba}"])
                    P.op("pe", mmb, reads=h1Tk + [("wB", ws)], writes=[f"b{bb}"])
                    P.op("act", lambda hc=hc, ba=ba: nc.scalar.activation(out=sil[hc % 2][:], in_=B[ba][:], func=AF.Silu),
                         reads=[f"b{ba}"], writes=[("sil", hc % 2)])
                    P.op("dve", lambda hc=hc, bb=bb: nc.vector.tensor_tensor(out=hid[hc][:], in0=sil[hc % 2][:], in1=B[bb][:], op=ALU.mult),
                         reads=[f"b{bb}", ("sil", hc % 2)], writes=[("hid", hc)])
                for tt in range(4):
                    for half in range(2):
                        bo = 2 + half

                        def mm2(tt=tt, half=half, bo=bo, ws=ws):
                            for hc in range(4):
                                ins = nc.tensor.matmul(B[bo][:], lhsT=hid[hc][:, tt * 128:(tt + 1) * 128],
                                                       rhs=wC[ws][:, hc, half * 512:(half + 1) * 512], start=(hc == 0), stop=(hc == 3))
                            return ins
                        P.op("pe", mm2, reads=[("hid", hc) for hc in range(4)] + [("wC", ws)], writes=[f"b{bo}"])
                        av = acc[tt][:, half * 512:(half + 1) * 512]
                        if e == 0:
                            P.op("dve", lambda av=av, bo=bo, tt=tt, e=e: nc.vector.tensor_scalar(
                                out=av, in0=B[bo][:], scalar1=comb[:, tt, e:e + 1], scalar2=None, op0=ALU.mult),
                                reads=[f"b{bo}", ("comb", tt)], writes=[("acc", tt, half)])
                        else:
                            P.op("dve", lambda av=av, bo=bo, tt=tt, e=e: nc.vector.scalar_tensor_tensor(
                                out=av, in0=B[bo][:], scalar=comb[:, tt, e:e + 1], in1=av, op0=ALU.mult, op1=ALU.add),
                                reads=[f"b{bo}", ("comb", tt), ("acc", tt, half)], writes=[("acc", tt, half)])
            for tt in range(4):
                t = su * 4 + tt
                s = t % 2
                rows = slice(t * 128, (t + 1) * 128)
                P.op("dve", lambda tt=tt: nc.vector.scalar_tensor_tensor(out=z2[:], in0=h1[tt][:], scalar=float(ALPHA), in1=acc[tt][:],
                                                                          op0=ALU.mult, op1=ALU.add),
                     reads=[("h1", tt), ("acc", tt, 0), ("acc", tt, 1)], writes=["z2"])
                layer_norm_tile(P, nc, z2, "z2", ho[s], ("ho", s), g2, b2, "g2", "b2", scr, "ln")
                dma(P, "poolq", h_out[rows, :], ho[s][:], reads=[("ho", s)], writes=[("h_out", t)])
                outs.append(("h_out", t))
                if hT_out is not None:
                    to_featmajor_bf16(P, nc, ho[s], ("ho", s), hob, "hob", B[0], "b0", hoT[s][:], ("hoT", s), ident)
                    dma(P, "poolq", hT_out[:, :, rows].rearrange("k p t -> p k t"), hoT[s][:], reads=[("hoT", s)], writes=[("hT_out", t)])
                    outs.append(("hT_out", t))
        P.emit(final_wait_keys=outs)


def post_inputs(l, p, lam_init):
    d = {}
    d["wout"] = bf(p["w_out"][l].reshape(8, 128, D).transpose(1, 0, 2))
    d["wglu"] = bf(p["s5_w_glu"][l].reshape(2, 128, 256).transpose(1, 0, 2))
    for n in ("ln1_g", "ln1_b", "ln2_g", "ln2_b"):
        d[n.replace("_", "")] = f32c(p[n][l].reshape(1, D))
    d["dng"] = f32c(p["diff_norm_g"][l].reshape(1, 64))
    d["lamv"] = f32c(np.concatenate([p["lam_q1"][l], p["lam_k1"][l], p["lam_q2"][l], p["lam_k2"][l],
                                     np.array([1.0 - lam_init, -lam_init], np.float32)]).reshape(1, 130))
    wr = np.concatenate([p["moe_w_grp"][l], p["moe_w_exp"][l]], axis=1)
    d["wr"] = f32c(wr.reshape(8, 128, 20).transpose(1, 0, 2))
    d["br"] = f32c(np.concatenate([p["moe_b_grp"][l], p["moe_b_exp"][l]]).reshape(1, 20))
    d["w1"] = bf(p["moe_w1"][l].reshape(NEXP, 8, 128, DEXP).transpose(0, 2, 1, 3))
    d["w3"] = bf(p["moe_w3"][l].reshape(NEXP, 8, 128, DEXP).transpose(0, 2, 1, 3))
    d["w2"] = bf(p["moe_w2"][l].reshape(NEXP, 4, 128, D).transpose(0, 2, 1, 3))
    d["idn"] = bf(np.eye(128)); d["idn32"] = f32c(np.eye(128))
    return d


QUARTERS = False
TWO_PI = 2.0 * math.pi
MAGIC = 12582912.0


def phase_mix(nc, G, l, hT_all, y_o, S=SEQ, do_attn=True, do_s5=True, do_gdn=True):
    debug = False
    C = Ctx(nc)
    P = C.P
    nst = S // 512
    nblk = S // 128
    pf = f"L{l}_"

    def din(name, shape, dt=F32):
        return G.din((pf + name) if name not in ("amask", "idn32", "srow", "cTri", "cSL", "cMask2", "cBones") else name, shape, dt)
    hT4 = hT_all.rearrange("k (r p) t -> r p k t", r=4)
    wq = din("wq", [128, 8, 96], BF16); wk = din("wk", [128, 8, 96], BF16); wv = din("wv", [128, 8, 192], BF16)
    amask = din("amask", [128, 4, 512], BF16)
    idn32 = din("idn32", [128, 128])
    wu = din("wu", [128, 8, 64], BF16)
    s5row = din("s5row", [2, 3, 128])
    s5col = din("s5col", [2, 128, 3])
    s5bT = din("s5bT", [2, 2, 2, 16, 64])
    s5cT = din("s5cT", [2, 2, 2, 64, 16])
    s5d = din("s5d", [64, 1])
    srow = din("srow", [1, 512])
    wg = din("wg", [128, 8, 384], BF16); wt = din("wt", [128, 8, 132], BF16)
    cvw = din("cvw", [128, 3, 4])
    galog = din("galog", [1, 2]); gdtb = din("gdtb", [1, 2]); gng = din("gng", [1, 64])
    cTri = din("cTri", [64, 64]); cSL = din("cSL", [64, 64]); cMask2 = din("cMask2", [64, 2, 64]); cBones = din("cBones", [128, 128])
    ya_o = y_o[:, 0:192].rearrange("s (u d) -> s u d", u=3)
    yb_o = y_o[:, 192:320].rearrange("s (h d) -> s h d", h=2)
    yc_o = y_o[:, 320:384]
    outs = []
    dbg = []
    V = nc.vector
    G = nc.gpsimd
    A = nc.scalar
    T = nc.tensor

    with P.stack:
        C.alloc_banks(quarters=do_gdn and QUARTERS)
        B = C.banks
        sb = P.sb
        ident32 = sb([128, 128], F32)
        dma(P, "sp", ident32[:], idn32, writes=["ident32"])
        hTt = [sb([128, 8, 512], BF16)] * 2
        eps_rms = const_col(P, nc, RMS_EPS, "eps_rms")
        if do_attn:
            wq_t = sb([128, 8, 96], BF16); wk_t = sb([128, 8, 96], BF16); wv_t = sb([128, 8, 192], BF16)
            QT = sb([96, S], BF16); KT = sb([96, S], BF16)
            Vall = sb([128, nblk, 3, 65], BF16)
            am_t = sb([128, 4, 512], BF16)
            PT = [sb([128, 512], BF16) for _ in range(4)]
            osb = sb([65, 512], F32); rec = sb([128, 4], F32)
            oT = [sb([128, 4, 64], F32) for _ in range(2)]
            dma(P, "sp", wq_t[:], wq, writes=["wq"]); dma(P, "sp", wk_t[:], wk, writes=["wk"]); dma(P, "sp", wv_t[:], wv, writes=["wv"])
            dma(P, "sp", am_t[:], amask, writes=["amask"])
            P.op("pool", lambda: G.memset(Vall[:, :, :, 64:65], 1.0), writes=["Vones"])
        if do_s5:
            wu_t = sb([128, 8, 64], BF16)
            dma(P, "sp", wu_t[:], wu, writes=["wu"])
            uT = [sb([32, 512], F32) for _ in range(2)]
            srow_t = sb([128, 512], F32)
            dma(P, "sp", srow_t[:], srow.partition_broadcast(128), writes=["srow"])
            d_col = [sb([32, 1], F32) for _ in range(2)]
            for pr_ in range(2):
                dma(P, "sp", d_col[pr_][:], s5d[pr_ * 32:(pr_ + 1) * 32, :], writes=[("dcol", pr_)])
            s5 = []
            for pr in range(2):
                t = dict(row=sb([32, 3, 128], F32), col=sb([128, 3], F32),
                         BrBD=sb([32, 128], F32), BiBD=sb([32, 128], F32), CrBD=sb([128, 32], F32), CiBD=sb([128, 32], F32),
                         bbr=sb([32, 128], F32), bbi=sb([32, 128], F32),
                         w=[sb([32, 128], F32) for _ in range(8)],
                         cw=[sb([128, 1], F32) for _ in range(8)],
                         RHO=sb([128, 512], F32), CS=sb([128, 512], F32), SN=sb([128, 512], F32),
                         zi=sb([128, 2], F32), zt=sb([128, 2], F32))
                if pr == 0:
                    for nm_ in ("bre", "bim", "t1", "t2", "zre", "zim", "xre", "xim", "ang", "tmp"):
                        t[nm_] = sb([128, 512], F32)
                if pr == 1:
                    for nm_ in ("bre", "bim", "t1", "t2", "zre", "zim", "xre", "xim", "ang", "tmp"):
                        t[nm_] = s5[0][nm_]
                s5.append(t)
            yT = [sb([32, 512], F32) for _ in range(2)]
            yc_tm = [sb([128, 4, 64], F32) for _ in range(2)]

            def range_reduce(eng_name, x, tmp, key_x, key_t, shape_all=True):
                P.op("dve", lambda: V.tensor_scalar(out=tmp, in0=x, scalar1=1.0 / TWO_PI, scalar2=MAGIC, op0=ALU.mult, op1=ALU.add),
                     reads=[key_x], writes=[key_t])
                P.op("dve", lambda: V.tensor_scalar(out=tmp, in0=tmp, scalar1=-MAGIC, scalar2=-TWO_PI, op0=ALU.add, op1=ALU.mult),
                     reads=[key_t], writes=[key_t])
                P.op("dve", lambda: V.tensor_tensor(out=x, in0=x, in1=tmp, op=ALU.add), reads=[key_x, key_t], writes=[key_x])

            def s5_setup(pr):
                t = s5[pr]
                k = lambda n, pr=pr: ("s5", "sh" if n in ("bre", "bim", "t1", "t2", "zre", "zim", "xre", "xim", "tab", "roww_scratch") else pr, n)
                dma(P, "sp", t["row"][:], s5row[pr:pr + 1].partition_broadcast(32), writes=[k("row")])
                dma(P, "sp", t["col"][:], s5col[pr], writes=[k("col")])
                for nm in ("BrBD", "BiBD", "CrBD", "CiBD"):
                    P.op("pool", lambda nm=nm, t=t: G.memset(t[nm][:], 0.0), writes=[k(nm)])
                for g in range(2):
                    dma(P, "sp", t["BrBD"][g * 16:(g + 1) * 16, g * 64:(g + 1) * 64], s5bT[pr, g, 0], reads=[k("BrBD")], writes=[k("BrBD")])
                    dma(P, "sp", t["BiBD"][g * 16:(g + 1) * 16, g * 64:(g + 1) * 64], s5bT[pr, g, 1], reads=[k("BiBD")], writes=[k("BiBD")])
                    dma(P, "sp", t["CrBD"][g * 64:(g + 1) * 64, g * 16:(g + 1) * 16], s5cT[pr, g, 0], reads=[k("CrBD")], writes=[k("CrBD")])
                    dma(P, "sp", t["CiBD"][g * 64:(g + 1) * 64, g * 16:(g + 1) * 16], s5cT[pr, g, 1], reads=[k("CiBD")], writes=[k("CiBD")])
                P.op("dve", lambda t=t: V.tensor_scalar(out=t["CiBD"][:], in0=t["CiBD"][:], scalar1=-1.0, scalar2=None, op0=ALU.mult),
                     reads=[k("CiBD")], writes=[k("CiBD")])
                lre, lim, ldt = t["row"][:, 0, :], t["row"][:, 1, :], t["row"][:, 2, :]
                dt_, lr_, mag, ang, tmp_, sn, cs, den = [t["w"][i][:] for i in range(8)]
                rk = k("roww")
                steps = [
                    ("act", lambda: A.activation(out=dt_, in_=ldt, func=AF.Exp)),
                    ("dve", lambda: V.tensor_tensor(out=lr_, in0=lre, in1=dt_, op=ALU.mult)),
                    ("act", lambda: A.activation(out=mag, in_=lr_, func=AF.Exp)),
                    ("dve", lambda: V.tensor_tensor(out=ang, in0=lim, in1=dt_, op=ALU.mult)),
                ]
                for e_, f_ in steps:
                    P.op(e_, f_, reads=[k("row"), rk], writes=[rk])
                range_reduce("dve", ang, tmp_, rk, rk)
                P.op("act", lambda: A.activation(out=sn, in_=ang, func=AF.Sin), reads=[rk], writes=[rk])
                P.op("dve", lambda: V.tensor_scalar(out=ang, in0=ang, scalar1=math.pi / 2, scalar2=None, op0=ALU.add), reads=[rk], writes=[rk])
                range_reduce("dve", ang, tmp_, rk, rk)
                P.op("act", lambda: A.activation(out=cs, in_=ang, func=AF.Sin), reads=[rk], writes=[rk])
                steps = [
                    lambda: V.tensor_tensor(out=cs, in0=cs, in1=mag, op=ALU.mult),
                    lambda: V.tensor_scalar(out=cs, in0=cs, scalar1=-1.0, scalar2=None, op0=ALU.add),
                    lambda: V.tensor_tensor(out=sn, in0=sn, in1=mag, op=ALU.mult),
                    lambda: V.tensor_tensor(out=den, in0=lre, in1=lre, op=ALU.mult),
                    lambda: V.tensor_tensor(out=tmp_, in0=lim, in1=lim, op=ALU.mult),
                    lambda: V.tensor_tensor(out=den, in0=den, in1=tmp_, op=ALU.add),
                    lambda: V.reciprocal(out=den, in_=den),
                    lambda: V.tensor_tensor(out=dt_, in0=cs, in1=lre, op=ALU.mult),
                    lambda: V.tensor_tensor(out=tmp_, in0=sn, in1=lim, op=ALU.mult),
                    lambda: V.tensor_tensor(out=dt_, in0=dt_, in1=tmp_, op=ALU.add),
                    lambda: V.tensor_tensor(out=dt_, in0=dt_, in1=den, op=ALU.mult),
                    lambda: V.tensor_tensor(out=lr_, in0=sn, in1=lre, op=ALU.mult),
                    lambda: V.tensor_tensor(out=tmp_, in0=cs, in1=lim, op=ALU.mult),
                    lambda: V.tensor_tensor(out=lr_, in0=lr_, in1=tmp_, op=ALU.subtract),
                    lambda: V.tensor_tensor(out=lr_, in0=lr_, in1=den, op=ALU.mult),
                    lambda t=t: V.tensor_tensor(out=t["bbr"][:], in0=dt_, in1=t["BrBD"][:], op=ALU.mult),
                    lambda t=t: V.tensor_tensor(out=tmp_, in0=lr_, in1=t["BiBD"][:], op=ALU.mult),
                    lambda t=t: V.tensor_tensor(out=t["bbr"][:], in0=t["bbr"][:], in1=tmp_, op=ALU.subtract),
                    lambda t=t: V.tensor_tensor(out=t["bbi"][:], in0=dt_, in1=t["BiBD"][:], op=ALU.mult),
                    lambda t=t: V.tensor_tensor(out=tmp_, in0=lr_, in1=t["BrBD"][:], op=ALU.mult),
                    lambda t=t: V.tensor_tensor(out=t["bbi"][:], in0=t["bbi"][:], in1=tmp_, op=ALU.add),
                ]
                for f_ in steps:
                    P.op("dve", f_, reads=[k("row"), rk, k("BrBD"), k("BiBD")], writes=[rk])
                cdt, cth, crho, ca, ctmp, c512s, c512c, cx = [t["cw"][i][:] for i in range(8)]
                ck = k("colw")
                steps = [
                    ("act", lambda t=t: A.activation(out=cdt, in_=t["col"][:, 2:3], func=AF.Exp)),
                    ("dve", lambda t=t: V.tensor_tensor(out=cth, in0=t["col"][:, 1:2], in1=cdt, op=ALU.mult)),
                    ("dve", lambda t=t: V.tensor_tensor(out=crho, in0=t["col"][:, 0:1], in1=cdt, op=ALU.mult)),
                    ("act", lambda: A.activation(out=crho, in_=crho, func=AF.Exp)),
                    ("dve", lambda: V.tensor_scalar(out=ca, in0=cth, scalar1=512.0, scalar2=None, op0=ALU.mult)),
                ]
                for e_, f_ in steps:
                    P.op(e_, f_, reads=[k("col"), ck], writes=[ck])
                range_reduce("dve", ca, ctmp, ck, ck)
                P.op("act", lambda: A.activation(out=c512s, in_=ca, func=AF.Sin), reads=[ck], writes=[ck])
                P.op("dve", lambda: V.tensor_scalar(out=ca, in0=ca, scalar1=math.pi / 2, scalar2=None, op0=ALU.add), reads=[ck], writes=[ck])
                range_reduce("dve", ca, ctmp, ck, ck)
                P.op("act", lambda: A.activation(out=c512c, in_=ca, func=AF.Sin), reads=[ck], writes=[ck])
                tk = k("tab")
                P.op("dve", lambda t=t: V.tensor_scalar(out=t["ang"][:], in0=srow_t[:], scalar1=cth, scalar2=None, op0=ALU.mult),
                     reads=["srow", ck], writes=[tk])
                range_reduce("dve", t["ang"][:], t["tmp"][:], tk, tk)
                P.op("act", lambda t=t: A.activation(out=t["SN"][:], in_=t["ang"][:], func=AF.Sin), reads=[tk], writes=[tk])
                P.op("dve", lambda t=t: V.tensor_scalar(out=t["ang"][:], in0=t["ang"][:], scalar1=math.pi / 2, scalar2=None, op0=ALU.add), reads=[tk], writes=[tk])
                range_reduce("dve", t["ang"][:], t["tmp"][:], tk, tk)
                P.op("act", lambda t=t: A.activation(out=t["CS"][:], in_=t["ang"][:], func=AF.Sin), reads=[tk], writes=[tk])
                P.op("pool", lambda t=t: G.memset(t["RHO"][:], 1.0), writes=[k("rho")])
                P.op("dve", lambda t=t: V.tensor_scalar(out=t["RHO"][:], in0=t["RHO"][:], scalar1=crho, scalar2=None, op0=ALU.mult),
                     reads=[k("rho"), ck], writes=[k("rho")])
                P.op("pool", lambda t=t: G.memset(t["zi"][:], 0.0), writes=[k("zi")])
                if pr == 0:
                    dbg.extend([("CS", t["CS"][:], [128, 512], [k("tab")]), ("SN", t["SN"][:], [128, 512], [k("tab")]),
                            ("RHO", t["RHO"][:], [128, 512], [k("rho")]), ("bbr", t["bbr"][:], [32, 128], [k("roww")]),
                            ("bbi", t["bbi"][:], [32, 128], [k("roww")]), ("cr", t["w"][0][:], [32, 128], [k("roww")]),
                            ("ci", t["w"][1][:], [32, 128], [k("roww")]), ("c512", t["cw"][5][:], [128, 1], [k("colw")]),
                            ("row", t["row"][:], [32, 3, 128], [k("row")]), ("col", t["col"][:], [128, 3], [k("col")])])
            for pr_ in range(2):
                s5_setup(pr_)
        if do_gdn:
            wg_t = sb([128, 8, 384], BF16); wt_t = sb([128, 8, 132], BF16)
            dma(P, "sp", wg_t[:], wg, writes=["wg"]); dma(P, "sp", wt_t[:], wt, writes=["wt"])
            cvw_t = sb([128, 3, 4], F32); dma(P, "sp", cvw_t[:], cvw, writes=["cvw"])
            alog_t = sb([64, 2], F32); dtb_t = sb([64, 2], F32); ng_t = sb([64, 64], F32)
            dma(P, "sp", alog_t[:], galog.partition_broadcast(64), writes=["alog"])
            dma(P, "sp", dtb_t[:], gdtb.partition_broadcast(64), writes=["dtb"])
            dma(P, "sp", ng_t[:], gng.partition_broadcast(64), writes=["ngt"])
            Tri = sb([64, 64], F32); SL = sb([64, 64], F32); Mask2 = sb([64, 2, 64], F32); Bones = sb([128, 128], F32); ones64 = sb([64, 64], F32)
            dma(P, "sp", Tri[:], cTri, writes=["Tri"]); dma(P, "sp", SL[:], cSL, writes=["SL"])
            dma(P, "sp", Mask2[:], cMask2, writes=["Mask2"]); dma(P, "sp", Bones[:], cBones, writes=["Bones"])
            P.op("pool", lambda: G.memset(ones64[:], 1.0), writes=["ones64"])
            P.op("act", lambda: A.activation(out=alog_t[:], in_=alog_t[:], func=AF.Exp), reads=["alog"], writes=["alog"])
            P.op("dve", lambda: V.tensor_scalar(out=alog_t[:], in0=alog_t[:], scalar1=-1.0, scalar2=None, op0=ALU.mult), reads=["alog"], writes=["alog"])
            xraw = [sb([128, 515], F32) for _ in range(3)]
            for c_ in range(3):
                P.op("pool", lambda c_=c_: G.memset(xraw[c_][:], 0.0), writes=[("xraw", c_)])
            cvt = sb([128, 512], F32)
            qkv = [sb([128, 512], F32) for _ in range(3)]
            sqn = sb([128, 512], F32); rn_ = sb([128, 512], F32)
            Sst = [sb([64, 64], F32) for _ in range(2)]
            for h_ in range(2):
                P.op("pool", lambda h_=h_: G.memset(Sst[h_][:], 0.0), writes=[("S", h_)])
            gd = dict(ch=[], hd=[])
            for sl in range(2):
                gd["ch"].append(dict(gs=sb([64, 128], F32), bg=sb([64, 4], F32), nbeta=sb([64, 2], F32)))
            for sl in range(4):
                gd["hd"].append(dict(qkv_tm=sb([64, 3, 64], F32), gcl=sb([64, 2], F32), ex3=sb([64, 3], F32), Gm=sb([64, 64], F32),
                                     EE=sb([64, 2, 64], F32), Pm=sb([64, 64], F32), PTm=sb([64, 64], F32), AT=sb([64, 64], F32),
                                     PP=[sb([64, 2, 64], F32) for _ in range(2)], X=sb([64, 128], F32), tb=sb([64, 1], F32),
                                     kdec=sb([64, 64], F32), qdec=sb([64, 64], F32), wqT=sb([64, 2, 64], F32), vnew=sb([64, 64], F32),
                                     osb=sb([64, 64], F32), osq=sb([64, 64], F32), oss=sb([64, 1], F32), ngate=sb([64, 64], F32)))
            ybuf = [sb([64, 8, 2, 64], F32) for _ in range(2)]

        for st in range(nst):
            hs = 0
            cols = slice(st * 512, (st + 1) * 512)
            dma(P, "sp", hTt[hs][:], hT4[st // (nst // 4)][:, :, (st % (nst // 4)) * 512:(st % (nst // 4) + 1) * 512], writes=[("hTt", hs)])
            hk = ("hTt", hs)
            if do_attn:
                for (w_t, wkey, dst, dk_, bank) in ((wq_t, "wq", QT, "QT", 0), (wk_t, "wk", KT, "KT", 1)):
                    def mmqk(w_t=w_t, bank=bank, hs=hs):
                        for kc in range(8):
                            ins = T.matmul(B[bank][0:96, :], lhsT=w_t[:, kc, :], rhs=hTt[hs][:, kc, :], start=(kc == 0), stop=(kc == 7))
                        return ins
                    P.op("pe", mmqk, reads=[hk, wkey], writes=[f"b{bank}"])
                    P.op("act", lambda dst=dst, bank=bank, cols=cols: A.copy(out=dst[:, cols], in_=B[bank][0:96, :]),
                         reads=[f"b{bank}"], writes=[(dk_, st)])
                for pair in range(2):
                    bank = 2 + pair
                    pv = B[bank][:, 0:384].rearrange("p (j c) -> p j c", j=2)

                    def mmv(pair=pair, pv=pv, hs=hs):
                        for j in range(2):
                            blk = pair * 2 + j
                            for kc in range(8):
                                ins = T.matmul(pv[:, j, :], lhsT=hTt[hs][:, kc, blk * 128:(blk + 1) * 128], rhs=wv_t[:, kc, :],
                                               start=(kc == 0), stop=(kc == 7))
                        return ins
                    P.op("pe", mmv, reads=[hk, "wv"], writes=[f"b{bank}"])
                    b0 = st * 4 + pair * 2
                    P.op("dve", lambda pv=pv, b0=b0: V.tensor_copy(out=Vall[:, b0:b0 + 2, :, 0:64],
                                                                  in_=pv.rearrange("p j (u d) -> p j u d", u=3)),
                         reads=[f"b{bank}"], writes=[("V", st, pair)])
            if do_s5:
                for pr in range(2):
                    def mmu(hs=hs, pr=pr):
                        for kc in range(8):
                            ins = T.matmul(B[4][0:32, :], lhsT=wu_t[:, kc, pr * 32:(pr + 1) * 32], rhs=hTt[hs][:, kc, :], start=(kc == 0), stop=(kc == 7))
                        return ins
                    P.op("pe", mmu, reads=[hk, "wu"], writes=["b4"])
                    P.op("act", lambda pr=pr: A.copy(out=uT[pr][:], in_=B[4][0:32, :]), reads=["b4"], writes=[("uT", pr)])
                def s5_stream(pr):
                    t = s5[pr]
                    k = lambda n, pr=pr: ("s5", "sh" if n in ("bre", "bim", "t1", "t2", "zre", "zim", "xre", "xim", "tab", "roww_scratch") else pr, n)
                    P.op("pe", lambda t=t, pr=pr: T.matmul(B[5][:], lhsT=t["bbr"][:], rhs=uT[pr][:], start=True, stop=True),
                         reads=[("uT", pr), k("roww")], writes=["b5"])
                    P.op("pe", lambda t=t, pr=pr: T.matmul(B[6][:], lhsT=t["bbi"][:], rhs=uT[pr][:], start=True, stop=True),
                         reads=[("uT", pr), k("roww")], writes=["b6"])
                    P.op("act", lambda t=t: A.copy(out=t["bre"][:], in_=B[5][:]), reads=["b5"], writes=[k("bre")])
                    P.op("act", lambda t=t: A.copy(out=t["bim"][:], in_=B[6][:]), reads=["b6"], writes=[k("bim")])
                    P.op("dve", lambda t=t: V.tensor_tensor(out=t["t1"][:], in0=t["bre"][:], in1=t["CS"][:], op=ALU.mult), reads=[k("bre"), k("tab")], writes=[k("t1")])
                    P.op("pool", lambda t=t: G.tensor_tensor(out=t["t2"][:], in0=t["bim"][:], in1=t["SN"][:], op=ALU.mult), reads=[k("bim"), k("tab")], writes=[k("t2")])
                    P.op("dve", lambda t=t: V.tensor_tensor(out=t["t1"][:], in0=t["t1"][:], in1=t["t2"][:], op=ALU.add), reads=[k("t1"), k("t2")], writes=[k("t1")])
                    P.op("pool", lambda t=t: G.tensor_tensor(out=t["t2"][:], in0=t["bim"][:], in1=t["CS"][:], op=ALU.mult), reads=[k("bim"), k("tab"), k("t1")], writes=[k("t2")])
                    P.op("pool", lambda t=t: G.tensor_tensor(out=t["bre"][:], in0=t["bre"][:], in1=t["SN"][:], op=ALU.mult), reads=[k("bre"), k("tab"), k("t1")], writes=[k("bre")])
                    P.op("pool", lambda t=t: G.tensor_tensor(out=t["t2"][:], in0=t["t2"][:], in1=t["bre"][:], op=ALU.subtract), reads=[k("t2"), k("bre")], writes=[k("t2")])
                    P.op("dve", lambda t=t: V.tensor_tensor_scan(out=t["zre"][:], data0=t["RHO"][:], data1=t["t1"][:], initial=t["zi"][:, 0:1],
                                                                  op0=ALU.mult, op1=ALU.add), reads=[k("t1"), k("rho"), k("zi")], writes=[k("zre")])
                    P.op("dve", lambda t=t: V.tensor_tensor_scan(out=t["zim"][:], data0=t["RHO"][:], data1=t["t2"][:], initial=t["zi"][:, 1:2],
                                                                  op0=ALU.mult, op1=ALU.add), reads=[k("t2"), k("rho"), k("zi")], writes=[k("zim")])
                    cdt, cth, crho, ca, ctmp, c512s, c512c, cx = [t["cw"][i][:] for i in range(8)]
                    zl_re, zl_im = t["zre"][:, 511:512], t["zim"][:, 511:512]
                    P.op("dve", lambda t=t, zl_re=zl_re: V.tensor_tensor(out=t["zt"][:, 0:1], in0=zl_re, in1=c512c, op=ALU.mult), reads=[k("zre"), k("colw")], writes=[k("zt")])
                    P.op("dve", lambda t=t, zl_im=zl_im: V.tensor_tensor(out=t["zt"][:, 1:2], in0=zl_im, in1=c512s, op=ALU.mult), reads=[k("zim"), k("colw")], writes=[k("zt")])
                    P.op("dve", lambda t=t: V.tensor_tensor(out=t["zi"][:, 0:1], in0=t["zt"][:, 0:1], in1=t["zt"][:, 1:2], op=ALU.subtract), reads=[k("zt"), k("zi")], writes=[k("zi")])
                    P.op("dve", lambda t=t, zl_re=zl_re: V.tensor_tensor(out=t["zt"][:, 0:1], in0=zl_re, in1=c512s, op=ALU.mult), reads=[k("zre"), k("colw"), k("zi")], writes=[k("zt")])
                    P.op("dve", lambda t=t, zl_im=zl_im: V.tensor_tensor(out=t["zt"][:, 1:2], in0=zl_im, in1=c512c, op=ALU.mult), reads=[k("zim"), k("colw")], writes=[k("zt")])
                    P.op("dve", lambda t=t: V.tensor_tensor(out=t["zi"][:, 1:2], in0=t["zt"][:, 0:1], in1=t["zt"][:, 1:2], op=ALU.add), reads=[k("zt"), k("zi")], writes=[k("zi")])
                    P.op("dve", lambda t=t: V.tensor_tensor(out=t["xre"][:], in0=t["zre"][:], in1=t["CS"][:], op=ALU.mult), reads=[k("zre"), k("tab")], writes=[k("xre")])
                    P.op("pool", lambda t=t: G.tensor_tensor(out=t["t1"][:], in0=t["zim"][:], in1=t["SN"][:], op=ALU.mult), reads=[k("zim"), k("tab"), k("zre")], writes=[k("t1")])
                    P.op("dve", lambda t=t: V.tensor_tensor(out=t["xre"][:], in0=t["xre"][:], in1=t["t1"][:], op=ALU.subtract), reads=[k("xre"), k("t1")], writes=[k("xre")])
                    P.op("pool", lambda t=t: G.tensor_tensor(out=t["xim"][:], in0=t["zre"][:], in1=t["SN"][:], op=ALU.mult), reads=[k("zre"), k("tab")], writes=[k("xim")])
                    P.op("pool", lambda t=t: G.tensor_tensor(out=t["t2"][:], in0=t["zim"][:], in1=t["CS"][:], op=ALU.mult), reads=[k("zim"), k("tab"), k("zim")], writes=[k("t2")])
                    P.op("pool", lambda t=t: G.tensor_tensor(out=t["xim"][:], in0=t["xim"][:], in1=t["t2"][:], op=ALU.add), reads=[k("xim"), k("t2")], writes=[k("xim")])

                    yb_ = 7 if pr == 0 else 3

                    def mmy(t=t, pr=pr, yb_=yb_):
                        T.matmul(B[yb_][0:32, :], lhsT=t["CrBD"][:], rhs=t["xre"][:], start=True, stop=False)
                        return T.matmul(B[yb_][0:32, :], lhsT=t["CiBD"][:], rhs=t["xim"][:], start=False, stop=True)
                    P.op("pe", mmy, reads=[k("xre"), k("xim"), k("CrBD"), k("CiBD")], writes=[f"b{yb_}"])
                    P.op("dve", lambda pr=pr, yb_=yb_: V.scalar_tensor_tensor(out=yT[pr][:], in0=uT[pr][:], scalar=d_col[pr][:, 0:1], in1=B[yb_][0:32, :],
                                                                             op0=ALU.mult, op1=ALU.add),
                         reads=[f"b{yb_}", ("uT", pr), ("dcol", pr)], writes=[("yT", pr)])
                for pr_ in range(2):
                    s5_stream(pr_)
                pvy = B[4][:, 0:256].rearrange("p (j d) -> p j d", j=4)

                def try4(pvy=pvy):
                    for pr in range(2):
                        for j in range(4):
                            ins = T.transpose(out=pvy[:, j, pr * 32:(pr + 1) * 32], in_=yT[pr][:, j * 128:(j + 1) * 128], identity=ident32[0:32, 0:32])
                    return ins
                P.op("pe", try4, reads=[("yT", 0), ("yT", 1), "ident32"], writes=["b4"])
                P.op("act", lambda pvy=pvy, hs=hs: A.copy(out=yc_tm[hs][:], in_=pvy), reads=["b4"], writes=[("yc_tm", hs)])
                dma(P, "poolq", yc_o[cols, :].rearrange("(j p) d -> p j d", p=128), yc_tm[hs][:], reads=[("yc_tm", hs)], writes=[("yc_o", st)])
                outs.append(("yc_o", st))
            if do_gdn:
                gdn_supertile(P, nc, B, st, hs, hk, hTt, wg_t, wt_t, cvw_t, xraw, cvt, qkv, sqn, rn_, Bones, eps_rms, alog_t, dtb_t, ng_t,
                              Tri, SL, Mask2, ones64, ident32, Sst, gd, ybuf, yb_o, outs)

        if do_gdn:
            gdn_round(P, gd, [], yb_o, outs)
        if do_attn:
            scale = 32 ** -0.5
            allqk = [("QT", s_) for s_ in range(nst)] + [("KT", s_) for s_ in range(nst)] + [("V", s_, p_) for s_ in range(nst) for p_ in range(2)] + ["Vones"]
            cnt = 0
            for u in range(3):
                for qt in range(nst):
                    nkb = 4 * (qt + 1)
                    bo = 3 + (qt % 2)
                    pend = []

                    def issue_s(kb, u=u, qt=qt):
                        nonlocal cnt
                        slot = cnt % 3
                        ps_ = cnt % 4
                        cnt += 1
                        P.op("pe", lambda: T.matmul(B[slot][:], lhsT=KT[32 * u:32 * u + 32, kb * 128:(kb + 1) * 128],
                                                    rhs=QT[32 * u:32 * u + 32, qt * 512:(qt + 1) * 512], start=True, stop=True),
                             reads=allqk, writes=[f"b{slot}"])
                        P.op("act", lambda: A.activation(out=PT[ps_][:], in_=B[slot][:], func=AF.Exp, scale=scale),
                             reads=[f"b{slot}"], writes=[("PT", ps_)])
                        if kb >= 4 * qt:
                            j = kb - 4 * qt
                            P.op("pool", lambda: G.tensor_tensor(out=PT[ps_][:], in0=PT[ps_][:], in1=am_t[:, j, :], op=ALU.mult),
                                 reads=[("PT", ps_), "amask"], writes=[("PT", ps_)])
                        return ps_

                    def issue_av(kb, ps_, u=u, bo=bo, nkb=nkb):
                        P.op("pe", lambda: T.matmul(B[bo][0:65, :], lhsT=Vall[:, kb, u, :], rhs=PT[ps_][:], start=(kb == 0), stop=(kb == nkb - 1)),
                             reads=[("PT", ps_)] + allqk, writes=[f"b{bo}"])
                    for kb in range(nkb):
                        pend.append((kb, issue_s(kb)))
                        if len(pend) > 2:
                            issue_av(*pend.pop(0))
                    while pend:
                        issue_av(*pend.pop(0))
                    P.op("act", lambda bo=bo: A.copy(out=osb[:], in_=B[bo][0:65, :]), reads=[f"b{bo}"], writes=["osb"])
                    pvo = B[5][:, 0:260].rearrange("p (j d) -> p j d", j=4)

                    def tro(pvo=pvo):
                        for j in range(4):
                            ins = T.transpose(out=pvo[:, j, :], in_=osb[:, j * 128:(j + 1) * 128], identity=ident32[0:65, 0:65])
                        return ins
                    P.op("pe", tro, reads=["osb", "ident32"], writes=["b5"])
                    P.op("dve", lambda pvo=pvo: V.reciprocal(out=rec[:], in_=pvo[:, :, 64]), reads=["b5"], writes=["rec"])
                    os_ = qt % 2
                    P.op("dve", lambda pvo=pvo, os_=os_: V.tensor_tensor(out=oT[os_][:], in0=pvo[:, :, 0:64],
                                                                        in1=rec[:].unsqueeze(2).to_broadcast([128, 4, 64]), op=ALU.mult),
                         reads=["b5", "rec"], writes=[("oT", os_)])
                    dma(P, "sp", ya_o[qt * 512:(qt + 1) * 512, u, :].rearrange("(j p) d -> p j d", p=128), oT[os_][:],
                        reads=[("oT", os_)], writes=[("ya_o", u, qt)])
                    outs.append(("ya_o", u, qt))
        P.emit(final_wait_keys=outs)


def gdn_supertile(P, nc, B, st, hs, hk, hTt, wg_t, wt_t, cvw_t, xraw, cvt, qkv, sqn, rn_, Bones, eps_rms, nA_t, dtb_t, ng_t,
                  Tri, SL, Mask2, ones64, ident32, Sst, gd, ybuf, yb_o, outs):
    V, G, A, T = nc.vector, nc.gpsimd, nc.scalar, nc.tensor
    for c in range(3):
        def mm(c=c):
            for kc in range(8):
                ins = T.matmul(B[c][:], lhsT=wg_t[:, kc, c * 128:(c + 1) * 128], rhs=hTt[hs][:, kc, :], start=(kc == 0), stop=(kc == 7))
            return ins
        P.op("pe", mm, reads=[hk, "wg"], writes=[f"b{c}"])
        P.op("pool", lambda c=c: G.tensor_copy(out=xraw[c][:, 0:3], in_=xraw[c][:, 512:515]), reads=[("xraw", c)], writes=[("xraw", c)])
        P.op("act", lambda c=c: A.copy(out=xraw[c][:, 3:515], in_=B[c][:]), reads=[f"b{c}", ("xraw", c)], writes=[("xraw", c)])
        P.op("dve", lambda c=c: V.tensor_scalar(out=cvt[:], in0=xraw[c][:, 0:512], scalar1=cvw_t[:, c, 0:1], scalar2=None, op0=ALU.mult),
             reads=[("xraw", c), "cvw"], writes=["cvt"])
        for kk in range(1, 4):
            P.op("dve", lambda c=c, kk=kk: V.scalar_tensor_tensor(out=cvt[:], in0=xraw[c][:, kk:kk + 512], scalar=cvw_t[:, c, kk:kk + 1],
                                                                  in1=cvt[:], op0=ALU.mult, op1=ALU.add),
                 reads=[("xraw", c), "cvw", "cvt"], writes=["cvt"])
        P.op("act", lambda c=c: A.activation(out=qkv[c][:], in_=cvt[:], func=AF.Silu), reads=["cvt"], writes=[("qkv", c)])
    for c in range(2):
        P.op("pool", lambda c=c: G.tensor_tensor(out=sqn[:], in0=qkv[c][:], in1=qkv[c][:], op=ALU.mult), reads=[("qkv", c)], writes=["sqn"])
        P.op("pe", lambda: T.matmul(B[3][:], lhsT=Bones[:], rhs=sqn[:], start=True, stop=True), reads=["sqn", "Bones"], writes=["b3"])
        P.op("act", lambda: A.activation(out=rn_[:], in_=B[3][:], func=AF.Sqrt, bias=eps_rms[:, 0:1], scale=1.0), reads=["b3", "eps_rms"], writes=["rn"])
        P.op("dve", lambda: V.reciprocal(out=rn_[:], in_=rn_[:]), reads=["rn"], writes=["rn"])
        if c == 0:
            P.op("dve", lambda: V.scalar_tensor_tensor(out=qkv[0][:], in0=qkv[0][:], scalar=0.125, in1=rn_[:], op0=ALU.mult, op1=ALU.mult),
                 reads=[("qkv", 0), "rn"], writes=[("qkv", 0)])
        else:
            P.op("dve", lambda: V.tensor_tensor(out=qkv[1][:], in0=qkv[1][:], in1=rn_[:], op=ALU.mult), reads=[("qkv", 1), "rn"], writes=[("qkv", 1)])
    qk_all = [("qkv", 0), ("qkv", 1), ("qkv", 2)]
    yb_s = st % 2
    for c in range(8):
        cg = st * 8 + c
        cs = slice(c * 64, (c + 1) * 64)
        dch = gd["ch"][cg % 2]
        kch = lambda n, cg=cg: ("gch", cg % 2, n)

        def mmt(cs=cs):
            for kc in range(8):
                ins = T.matmul(B[0][0:64, 0:132], lhsT=hTt[hs][:, kc, cs], rhs=wt_t[:, kc, :], start=(kc == 0), stop=(kc == 7))
            return ins
        mk = ["b0q0", "b0q1"] if P.quarters else ["b0"]
        P.op("pe", mmt, reads=[hk, "wt"], writes=mk)
        P.op("act", lambda dch=dch: A.activation(out=dch["gs"][:], in_=B[0][0:64, 0:128], func=AF.Silu), reads=mk, writes=[kch("gs")])
        P.op("act", lambda dch=dch: A.activation(out=dch["bg"][:, 0:2], in_=B[0][0:64, 128:130], func=AF.Sigmoid), reads=mk, writes=[kch("bg")])
        P.op("dve", lambda dch=dch: V.tensor_tensor(out=dch["bg"][:, 2:4], in0=B[0][0:64, 130:132], in1=dtb_t[:], op=ALU.add),
             reads=mk + ["dtb", kch("bg")], writes=[kch("bg")])
        P.op("act", lambda dch=dch: A.activation(out=dch["bg"][:, 2:4], in_=dch["bg"][:, 2:4], func=AF.Exp), reads=[kch("bg")], writes=[kch("bg")])
        P.op("act", lambda dch=dch: A.activation(out=dch["bg"][:, 2:4], in_=dch["bg"][:, 2:4], func=AF.Ln, bias=1.0, scale=1.0), reads=[kch("bg")], writes=[kch("bg")])
        P.op("dve", lambda dch=dch: V.tensor_tensor(out=dch["bg"][:, 2:4], in0=dch["bg"][:, 2:4], in1=nA_t[:], op=ALU.mult),
             reads=[kch("bg"), "alog"], writes=[kch("bg")])
        P.op("dve", lambda dch=dch: V.tensor_scalar(out=dch["nbeta"][:], in0=dch["bg"][:, 0:2], scalar1=-1.0, scalar2=None, op0=ALU.mult),
             reads=[kch("bg")], writes=[kch("nbeta")])
        new = [gdn_chunk_head(P, nc, B, h, cs, c, dch, kch, gd["hd"][(cg * 2 + h) % 4], (cg * 2 + h) % 4, qkv, qk_all, ng_t, Tri, SL, Mask2, ones64,
                              ident32, Sst, eps_rms, ybuf[yb_s], yb_s) for h in range(2)]
        gdn_round(P, gd, new, yb_o, outs)
        if c == 7:
            gd["pend_dma"] = (st, yb_s, ybuf[yb_s])


def gdn_round(P, gd, new, yb_o, outs):
    old = gd.get("pendB", [])
    act = [(g, "A") for g in new] + [(g, "B") for g in old]
    nxt = []
    while act:
        for it in list(act):
            g, kind = it
            try:
                r = next(g)
            except StopIteration:
                act.remove(it)
                continue
            if kind == "A" and r == "END_A":
                act.remove(it)
                nxt.append(g)
    gd["pendB"] = nxt
    pd = gd.get("pend_dma")
    if pd is not None and old:
        st, yb_s, ybt = pd
        cols = slice(st * 512, (st + 1) * 512)
        dma(P, "poolq", yb_o[cols].rearrange("(c p) h d -> p c h d", p=64), ybt[:], reads=[("ybuf", yb_s, c_, h_) for c_ in range(8) for h_ in range(2)],
            writes=[("yb_o", st)])
        outs.append(("yb_o", st))
        gd["pend_dma"] = None


def gdn_chunk_head(P, nc, B, h, cs, c, dch, kch, d, sl, qkv, qk_all, ng_t, Tri, SL, Mask2, ones64, ident32, Sst, eps_rms, ybuf, yb_s):
    V, G, A, T = nc.vector, nc.gpsimd, nc.scalar, nc.tensor
    hp = slice(h * 64, (h + 1) * 64)
    idh = ident32[hp, hp]
    id0 = ident32[0:64, 0:64]
    k = lambda n: ("ghd", sl, n)
    bA, bB = 4 + 2 * h, 5 + 2 * h
    PA, PB, P3 = B[bA], B[bB], B[3]
    if P.quarters:
        qa = lambda *qs: [f"b{bA}q{q}" for q in qs]
        qb = lambda *qs: [f"b{bB}q{q}" for q in qs]
        q3 = lambda *qs: [f"b3q{2 * h + q}" for q in qs]
    else:
        qa = lambda *qs: [f"b{bA}"]
        qb = lambda *qs: [f"b{bB}"]
        q3 = lambda *qs: ["b3"]
    o3 = 256 * h
    g_col = dch["bg"][:, 2 + h:3 + h]
    beta_col = dch["bg"][:, h:h + 1]
    nbeta_col = dch["nbeta"][:, h:h + 1]

    def tr1():
        for c3 in range(3):
            ins = T.transpose(out=PA[0:64, c3 * 64:(c3 + 1) * 64], in_=qkv[c3][hp, cs], identity=idh)
        return ins
    P.op("pe", tr1, reads=qk_all + ["ident32"], writes=qa(0, 1)); yield
    P.op("act", lambda: A.copy(out=d["qkv_tm"][:].rearrange("p a b -> p (a b)"), in_=PA[0:64, 0:192]), reads=qa(0, 1), writes=[k("qkv_tm")]); yield

    def mm2():
        T.matmul(PA[0:64, 256:257], lhsT=Tri[:], rhs=g_col, start=True, stop=True)
        return T.matmul(PA[0:64, 257:258], lhsT=ones64[:], rhs=g_col, start=True, stop=True)
    P.op("pe", mm2, reads=[kch("bg"), "Tri", "ones64"], writes=qa(2)); yield
    P.op("dve", lambda: V.tensor_copy(out=d["gcl"][:], in_=PA[0:64, 256:258]), reads=qa(2), writes=[k("gcl")]); yield
    P.op("act", lambda: A.activation(out=d["ex3"][:, 0:1], in_=d["gcl"][:, 0:1], func=AF.Exp), reads=[k("gcl")], writes=[k("ex3")]); yield
    P.op("act", lambda: A.activation(out=d["ex3"][:, 1:2], in_=d["gcl"][:, 0:1], func=AF.Exp, bias=d["gcl"][:, 1:2], scale=-1.0),
         reads=[k("gcl"), k("ex3")], writes=[k("ex3")]); yield
    P.op("act", lambda: A.activation(out=d["ex3"][:, 2:3], in_=d["gcl"][:, 1:2], func=AF.Exp), reads=[k("gcl"), k("ex3")], writes=[k("ex3")]); yield
    P.op("dve", lambda: V.tensor_scalar(out=d["Gm"][:], in0=Tri[:], scalar1=g_col, scalar2=None, op0=ALU.mult), reads=["Tri", kch("bg")], writes=[k("Gm")]); yield

    def mm3():
        T.matmul(PA[0:64, 384:448], lhsT=d["Gm"][:], rhs=SL[:], start=True, stop=True)
        return T.matmul(PA[0:64, 448:512], lhsT=SL[:], rhs=d["Gm"][:], start=True, stop=True)
    P.op("pe", mm3, reads=[k("Gm"), "SL"], writes=qa(3)); yield
    P.op("act", lambda: A.activation(out=d["EE"][:].rearrange("p a b -> p (a b)"), in_=PA[0:64, 384:512], func=AF.Exp), reads=qa(3), writes=[k("EE")]); yield
    P.op("pool", lambda: G.tensor_tensor(out=d["EE"][:], in0=d["EE"][:], in1=Mask2[:], op=ALU.mult), reads=[k("EE"), "Mask2"], writes=[k("EE")]); yield

    def mm4():
        T.matmul(PB[0:64, 0:64], lhsT=qkv[1][hp, cs], rhs=qkv[1][hp, cs], start=True, stop=True)
        return T.matmul(PB[0:64, 64:128], lhsT=qkv[1][hp, cs], rhs=qkv[0][hp, cs], start=True, stop=True)
    P.op("pe", mm4, reads=qk_all, writes=qb(0)); yield
    P.op("dve", lambda: V.scalar_tensor_tensor(out=d["Pm"][:], in0=PB[0:64, 0:64], scalar=nbeta_col, in1=d["EE"][:, 0, :], op0=ALU.mult, op1=ALU.mult),
         reads=qb(0) + [kch("nbeta"), k("EE")], writes=[k("Pm")]); yield
    P.op("dve", lambda: V.tensor_tensor(out=d["AT"][:], in0=PB[0:64, 64:128], in1=d["EE"][:, 1, :], op=ALU.mult), reads=qb(0) + [k("EE")], writes=[k("AT")]); yield
    P.op("pe", lambda: T.transpose(out=PA[0:64, 192:256], in_=d["Pm"][:], identity=id0), reads=[k("Pm"), "ident32"], writes=qa(1)); yield
    P.op("act", lambda: A.copy(out=d["PTm"][:], in_=PA[0:64, 192:256]), reads=qa(1), writes=[k("PTm")]); yield
    P.op("dve", lambda: V.tensor_tensor(out=d["tb"][:], in0=beta_col, in1=d["ex3"][:, 0:1], op=ALU.mult), reads=[kch("bg"), k("ex3")], writes=[k("tb")]); yield
    P.op("dve", lambda: V.tensor_scalar(out=d["X"][:, 0:64], in0=d["qkv_tm"][:, 2, :], scalar1=beta_col, scalar2=None, op0=ALU.mult),
         reads=[k("qkv_tm"), kch("bg")], writes=[k("X")]); yield
    P.op("dve", lambda: V.tensor_scalar(out=d["X"][:, 64:128], in0=d["qkv_tm"][:, 1, :], scalar1=d["tb"][:, 0:1], scalar2=None, op0=ALU.mult),
         reads=[k("qkv_tm"), k("tb"), k("X")], writes=[k("X")]); yield
    cur = (d["Pm"][:], d["PTm"][:], [k("Pm"), k("PTm")])
    for lvl in range(6):
        Pc, PTc, pk = cur
        par = lvl % 2
        xr = PB[0:64, 128 + 128 * par:256 + 128 * par]
        xk = qb(1 + par)
        P.op("pe", lambda PTc=PTc, xr=xr: T.matmul(xr, lhsT=PTc, rhs=d["X"][:], start=True, stop=True),
             reads=pk + [k("X")], writes=xk); yield
        P.op("dve", lambda xr=xr: V.tensor_tensor(out=d["X"][:], in0=d["X"][:], in1=xr, op=ALU.add),
             reads=xk + [k("X")], writes=[k("X")]); yield
        if lvl < 5:
            sr, sk = (PB[0:64, 384:512], qb(3)) if par == 0 else (PA[0:64, 256:384], qa(2))
            pp = d["PP"][par]

            def mmsq(Pc=Pc, PTc=PTc, sr=sr):
                T.matmul(sr[:, 0:64], lhsT=PTc, rhs=Pc, start=True, stop=True)
                return T.matmul(sr[:, 64:128], lhsT=Pc, rhs=PTc, start=True, stop=True)
            P.op("pe", mmsq, reads=pk, writes=sk); yield
            P.op("act", lambda pp=pp, sr=sr: A.copy(out=pp[:].rearrange("p a b -> p (a b)"), in_=sr),
                 reads=sk, writes=[k(("PP", par))]); yield
            cur = (pp[:, 0, :], pp[:, 1, :], [k(("PP", par))])
    P.op("pool", lambda: G.tensor_scalar(out=d["kdec"][:], in0=d["qkv_tm"][:, 1, :], scalar1=d["ex3"][:, 1:2], scalar2=None, op0=ALU.mult),
         reads=[k("qkv_tm"), k("ex3")], writes=[k("kdec")]); yield
    P.op("pool", lambda: G.tensor_scalar(out=d["qdec"][:], in0=d["qkv_tm"][:, 0, :], scalar1=d["ex3"][:, 0:1], scalar2=None, op0=ALU.mult),
         reads=[k("qkv_tm"), k("ex3")], writes=[k("qdec")]); yield

    def tr8():
        T.transpose(out=PA[0:64, 0:64], in_=d["X"][:, 64:128], identity=id0)
        return T.transpose(out=PA[0:64, 64:128], in_=d["qdec"][:], identity=id0)
    P.op("pe", tr8, reads=[k("X"), k("qdec"), "ident32"], writes=qa(0)); yield
    P.op("act", lambda: A.copy(out=d["wqT"][:].rearrange("p a b -> p (a b)"), in_=PA[0:64, 0:128]), reads=qa(0), writes=[k("wqT")]); yield
    P.op("pool", lambda: G.tensor_tensor(out=d["ngate"][:], in0=dch["gs"][:, h * 64:(h + 1) * 64], in1=ng_t[:], op=ALU.mult),
         reads=[kch("gs"), "ngt"], writes=[k("ngate")]); yield
    yield "END_A"
    S_ = Sst[h]
    P.op("pe", lambda: T.matmul(P3[0:64, o3:o3 + 64], lhsT=d["wqT"][:, 0, :], rhs=S_[:], start=True, stop=True), reads=[k("wqT"), ("S", h)], writes=q3(0)); yield
    P.op("dve", lambda: V.tensor_tensor(out=d["vnew"][:], in0=d["X"][:, 0:64], in1=P3[0:64, o3:o3 + 64], op=ALU.subtract),
         reads=q3(0) + [k("X")], writes=[k("vnew")]); yield

    def mmo():
        T.matmul(P3[0:64, o3 + 64:o3 + 128], lhsT=d["wqT"][:, 1, :], rhs=S_[:], start=True, stop=False)
        return T.matmul(P3[0:64, o3 + 64:o3 + 128], lhsT=d["AT"][:], rhs=d["vnew"][:], start=False, stop=True)
    P.op("pe", mmo, reads=[k("wqT"), ("S", h), k("AT"), k("vnew")], writes=q3(0)); yield
    P.op("pe", lambda: T.matmul(P3[0:64, o3 + 128:o3 + 192], lhsT=d["kdec"][:], rhs=d["vnew"][:], start=True, stop=True), reads=[k("kdec"), k("vnew")], writes=q3(1)); yield
    P.op("dve", lambda: V.scalar_tensor_tensor(out=S_[:], in0=S_[:], scalar=d["ex3"][:, 2:3], in1=P3[0:64, o3 + 128:o3 + 192], op0=ALU.mult, op1=ALU.add),
         reads=q3(1) + [("S", h), k("ex3")], writes=[("S", h)]); yield
    P.op("act", lambda: A.copy(out=d["osb"][:], in_=P3[0:64, o3 + 64:o3 + 128]), reads=q3(0), writes=[k("osb")]); yield
    P.op("pool", lambda: G.tensor_tensor(out=d["osq"][:], in0=d["osb"][:], in1=d["osb"][:], op=ALU.mult), reads=[k("osb")], writes=[k("osq")]); yield
    P.op("dve", lambda: V.tensor_reduce(out=d["oss"][:], in_=d["osq"][:], axis=AX.X, op=ALU.add), reads=[k("osq")], writes=[k("oss")]); yield
    P.op("act", lambda: A.activation(out=d["oss"][:], in_=d["oss"][:], func=AF.Sqrt, bias=eps_rms[0:64, 0:1], scale=1.0 / 64),
         reads=[k("oss"), "eps_rms"], writes=[k("oss")]); yield
    P.op("dve", lambda: V.reciprocal(out=d["oss"][:], in_=d["oss"][:]), reads=[k("oss")], writes=[k("oss")]); yield
    P.op("dve", lambda: V.scalar_tensor_tensor(out=ybuf[:, c, h, :], in0=d["osb"][:], scalar=d["oss"][:, 0:1], in1=d["ngate"][:], op0=ALU.mult, op1=ALU.mult),
         reads=[k("osb"), k("oss"), k("ngate")], writes=[("ybuf", yb_s, c, h)]); yield


OFF_AQ, OFF_AK, OFF_AV, OFF_BQKV, OFF_BGATE, OFF_BBETA, OFF_BA, OFF_CU = 0, 384, 768, 1152, 2304, 2688, 2694, 2700
GDN_HEADS_OF = [(0, 1), (2, 3), (4, 5), (4, 5)]


def _wl(w, cols):
    return w[:, cols].reshape(8, 128, len(cols)).transpose(1, 0, 2)


def mix_consts():
    d = {}
    k = np.arange(128)[:, None, None]; j = np.arange(4)[None, :, None]; q = np.arange(512)[None, None, :]
    d["amask"] = bf((q // 64 >= (j * 128 + k) // 64).astype(np.float32))
    d["idn32"] = f32c(np.eye(128))
    d["srow"] = f32c(np.arange(512).reshape(1, 512))
    m = np.arange(64)[:, None]; i = np.arange(64)[None, :]
    d["cTri"] = f32c(m <= i)
    d["cSL"] = f32c(m > i)
    d["cMask2"] = f32c(np.stack([(m > i), (m <= i)], axis=1))
    bo = np.zeros((128, 128), np.float32); bo[:64, :64] = 1; bo[64:, 64:] = 1
    d["cBones"] = bo
    return d


def mix_inputs(l, p, j):
    w = p["w_in"][l]
    d = {}
    units = [3 * j + i for i in range(3)]
    qc, kc_, vc = [], [], []
    for u in units:
        head, mp = u // 2, u % 2
        qc += list(range(OFF_AQ + head * 64 + mp * 32, OFF_AQ + head * 64 + mp * 32 + 32))
        kc_ += list(range(OFF_AK + head * 64 + mp * 32, OFF_AK + head * 64 + mp * 32 + 32))
        vc += list(range(OFF_AV + head * 64, OFF_AV + head * 64 + 64))
    d["wq"] = bf(_wl(w, qc)); d["wk"] = bf(_wl(w, kc_)); d["wv"] = bf(_wl(w, vc))
    gs = [4 * j + i for i in range(4)]
    d["wu"] = bf(_wl(w, list(range(OFF_CU + gs[0] * 16, OFF_CU + gs[0] * 16 + 64))))
    lre, lim, ldt = p["s5_lambda_re"][l], p["s5_lambda_im"][l], p["s5_log_dt"][l]
    row = np.zeros((2, 3, 128), np.float32)
    bT = np.zeros((2, 2, 2, 16, 64), np.float32); cT = np.zeros((2, 2, 2, 64, 16), np.float32)
    for pr in range(2):
        for g in range(2):
            G_ = gs[pr * 2 + g]
            row[pr, 0, g * 64:(g + 1) * 64] = lre[G_]; row[pr, 1, g * 64:(g + 1) * 64] = lim[G_]; row[pr, 2, g * 64:(g + 1) * 64] = ldt[G_]
            bT[pr, g, 0] = p["s5_b_re"][l][G_].T; bT[pr, g, 1] = p["s5_b_im"][l][G_].T
            cT[pr, g, 0] = p["s5_c_re"][l][G_].T; cT[pr, g, 1] = p["s5_c_im"][l][G_].T
    d["s5row"] = row; d["s5col"] = f32c(row.transpose(0, 2, 1)); d["s5bT"] = bT; d["s5cT"] = cT
    d["s5d"] = f32c(p["s5_d"][l][gs[0] * 16:gs[0] * 16 + 64].reshape(64, 1))
    hA, hB = GDN_HEADS_OF[j]
    gcols = []
    for part in range(3):
        for h in (hA, hB):
            gcols += list(range(OFF_BQKV + part * 384 + h * 64, OFF_BQKV + part * 384 + h * 64 + 64))
    d["wg"] = bf(_wl(w, gcols))
    tcols = list(range(OFF_BGATE + hA * 64, OFF_BGATE + hA * 64 + 64)) + list(range(OFF_BGATE + hB * 64, OFF_BGATE + hB * 64 + 64)) \
        + [OFF_BBETA + hA, OFF_BBETA + hB, OFF_BA + hA, OFF_BA + hB]
    d["wt"] = bf(_wl(w, tcols))
    cw = p["dn_conv_w"][l]
    cv = np.zeros((128, 3, 4), np.float32)
    for part in range(3):
        for hi, h in enumerate((hA, hB)):
            cv[hi * 64:(hi + 1) * 64, part, :] = cw[:, part * 384 + h * 64: part * 384 + h * 64 + 64].T
    d["cvw"] = cv
    d["galog"] = f32c(p["dn_a_log"][l][[hA, hB]].reshape(1, 2)); d["gdtb"] = f32c(p["dn_dt_bias"][l][[hA, hB]].reshape(1, 2))
    d["gng"] = f32c(p["dn_norm_g"][l].reshape(1, 64))
    return d


YCH = 512
NYCH = SEQ // YCH


def build_program(stop=None):
    nc = bass.Bass("TRN2", target_bir_lowering=False)
    G = Glob(nc)
    hbuf = [G.internal(f"hbuf{i}", [TOK_CORE, D]) for i in range(2)]
    hT_loc = G.internal("hT_loc", [D, TOK_CORE], BF16)
    hT_all = G.internal("hT_all", [8, 4 * 128, TOK_CORE], BF16)
    y_o = G.internal("y_o", [SEQ, 384])
    y_all = G.internal("y_all", [NYCH, 4 * YCH, 384])
    ag_h = [(hT_loc[k * 128:(k + 1) * 128, :], hT_all[k]) for k in range(8)]
    ag_y = [(y_o[i * YCH:(i + 1) * YCH, :], y_all[i]) for i in range(NYCH)]
    out = nc.dram_tensor("out", [TOK_CORE, D], F32, kind="ExternalOutput").ap()

    def finish_early(src_ap):
        with nc.semaphore("fin") as fs:
            with nc.Block() as block:
                @block.gpsimd
                def _(g):
                    g.sem_clear(fs)
            with nc.Block() as block:
                @block.sync
                def _(sp):
                    sp.dma_start(out=out, in_=src_ap).then_inc(fs, 16)
                    sp.wait_ge(fs, 16)
        return nc, G
    phase_pre(nc, G, hbuf[0], hT_loc)
    for l in range(DEPTH):
        last = l == DEPTH - 1
        allgather(nc, ag_h)
        if stop == (l, "ag1"):
            return finish_early(hbuf[0])
        phase_mix(nc, G, l, hT_all, y_o, do_attn=True, do_s5=False, do_gdn=False)
        if stop == (l, "mixa"):
            return finish_early(hbuf[0])
        phase_mix(nc, G, l, hT_all, y_o, do_attn=False, do_s5=True, do_gdn=True)
        if stop == (l, "mixb"):
            return finish_early(hbuf[0])
        allgather(nc, ag_y)
        if stop == (l, "ag2"):
            return finish_early(hbuf[0])
        phase_post(nc, G, l, hbuf[l % 2], out if last else hbuf[(l + 1) % 2], None if last else hT_loc, y_all)
        if stop == (l, "post"):
            return finish_early(hbuf[(l + 1) % 2])
    return nc, G


def kernel(_stop=None, **inputs):
    p = {k: np.asarray(v) for k, v in inputs.items()}
    x = f32c(p["x"]).reshape(BATCH * SEQ, D)
    cores = list(range(NCORES))
    nc, G = build_program(_stop)
    shared = dict(mix_consts())
    shared["idn"] = bf(np.eye(128))
    shared["g"] = f32c(p["ln_in_g"].reshape(1, D)); shared["b"] = f32c(p["ln_in_b"].reshape(1, D))
    percore = [dict() for _ in range(4)]
    for l in range(DEPTH):
        lam_init = 0.8 - 0.6 * math.exp(-0.3 * l)
        for k, v in post_inputs(l, p, lam_init).items():
            if k not in ("idn", "idn32"):
                shared[f"L{l}_{k}"] = v
        for j in range(4):
            for k, v in mix_inputs(l, p, j).items():
                percore[j][f"L{l}_{k}"] = v
    ins = []
    for c in cores:
        d = dict(shared); d.update(percore[c % 4])
        d["x"] = x[c * TOK_CORE:(c + 1) * TOK_CORE]
        d["rofs"] = np.array([[(c % 4) * (TOK_CORE // YCH)]], np.int32)
        ins.append({k: v for k, v in d.items() if k in G.t})
    res = run_bass_kernel_spmd(nc, ins, core_ids=cores)
    h = [np.asarray(r["out"]) for r in res.results]
    return np.concatenate(h, axis=0).reshape(BATCH, SEQ, D).astype(np.float32)
```

```python
import math
from contextlib import ExitStack

import numpy as np
import ml_dtypes
import concourse.bass as bass
import concourse.mybir as mybir
from concourse.bass_utils import run_bass_kernel_spmd

F32 = mybir.dt.float32
BF16 = mybir.dt.bfloat16
I32 = mybir.dt.int32
ALU = mybir.AluOpType
AF = mybir.ActivationFunctionType
AX = mybir.AxisListType

NCORES = 8


class Prog:
    COMPUTE = ("pe", "act", "dve", "pool")
    NDMASEM = 6

    _uid = 0
    _phase = 0

    def __init__(self, nc):
        Prog._phase += 1
        self.ph = Prog._phase
        self.nc = nc
        self.ops = []
        self.last_w = {}
        self.readers = {}
        self.dma_count = {"sp": 0, "actq": 0, "poolq": 0}
        self.stack = ExitStack()
        self.nt = 0
        self.excl = set()
        self.quarters = False
        self.bankkeys = {f"b{i}" for i in range(8)}
        self.sp_wrap = None

    def sb(self, shape, dtype, name=None):
        Prog._uid += 1
        return self.stack.enter_context(self.nc.sbuf_tensor(f"{name or 't'}_{Prog._uid}", list(shape), dtype))

    def ps(self, shape, dtype=F32, name=None):
        Prog._uid += 1
        return self.stack.enter_context(self.nc.psum_tensor(f"{name or 'p'}_{Prog._uid}", list(shape), dtype))

    def op(self, eng, fn, reads=(), writes=()):
        idx = len(self.ops)
        isdma = eng in self.dma_count
        issue = {"sp": "sp", "actq": "act", "poolq": "pool"}.get(eng, eng)
        if self.quarters:
            ex = lambda ks: [q for k in ks for q in ([f"{k}q{i}" for i in range(4)] if k in self.bankkeys else [k])]
            reads, writes = ex(reads), ex(writes)
        if self.excl:
            writes = list(writes) + [k for k in reads if k in self.excl]
            reads = [k for k in reads if k not in self.excl]
        deps = set()
        for k in reads:
            w = self.last_w.get(k)
            if w is not None:
                deps.add(w)
        for k in writes:
            w = self.last_w.get(k)
            if w is not None:
                deps.add(w)
            for r in self.readers.get(k, ()):
                deps.add(r)
        o = dict(idx=idx, eng=eng, issue=issue, fn=fn, deps=deps, isdma=isdma, needed=False)
        if isdma:
            n = self.dma_count[eng]
            self.dma_count[eng] = n + 1
            o["dsem"] = n % self.NDMASEM
            o["dtarget"] = 16 * (n // self.NDMASEM + 1)
            o["dprev"] = 16 * (n // self.NDMASEM)
        self.ops.append(o)
        for k in writes:
            self.last_w[k] = idx
            self.readers[k] = []
        for k in reads:
            lst = self.readers.setdefault(k, [])
            if not isdma:
                lst[:] = [r for r in lst if self.ops[r]["isdma"] or self.ops[r]["eng"] != eng]
            lst.append(idx)
        return idx

    def emit(self, final_wait_keys=()):
        nc = self.nc
        ops = self.ops
        for o in ops:
            nd = set()
            for d in o["deps"]:
                p = ops[d]
                if (not p["isdma"]) and (not o["isdma"]) and p["eng"] == o["eng"]:
                    if o["eng"] == "pe":
                        continue
                nd.add(d)
            o["deps"] = nd
            for d in nd:
                ops[d]["needed"] = True
        final = [self.last_w[k] for k in final_wait_keys if k in self.last_w]
        for d in final:
            ops[d]["needed"] = True
        tick = {e: 0 for e in self.COMPUTE}
        for o in ops:
            if not o["isdma"] and o["needed"]:
                tick[o["eng"]] += 1
                o["tick"] = tick[o["eng"]]
        sems = {e: self.stack.enter_context(nc.semaphore(f"s_{e}_{self.ph}")) for e in self.COMPUTE}
        dsems = {q: [self.stack.enter_context(nc.semaphore(f"d_{q}{i}_{self.ph}")) for i in range(self.NDMASEM)]
                 for q in self.dma_count}
        per = {e: [] for e in ("pe", "act", "dve", "pool", "sp")}
        for o in ops:
            per[o["issue"]].append(o)
        engobj = {"pe": nc.tensor, "act": nc.scalar, "dve": nc.vector, "pool": nc.gpsimd, "sp": nc.sync}

        def run(ename, extra_final=False):
            eng = engobj[ename]
            waited = {}

            def wait_for(p):
                if p["isdma"]:
                    key = (p["eng"], p["dsem"])
                    val = p["dtarget"]
                    s = dsems[p["eng"]][p["dsem"]]
                else:
                    key = p["eng"]
                    val = p["tick"]
                    s = sems[p["eng"]]
                if waited.get(key, 0) >= val:
                    return
                waited[key] = val
                eng.wait_ge(s, val)

            for o in per[ename]:
                for d in sorted(o["deps"]):
                    wait_for(ops[d])
                if o["isdma"] and o["dprev"] > 0:
                    key = (o["eng"], o["dsem"])
                    if waited.get(key, 0) < o["dprev"]:
                        waited[key] = o["dprev"]
                        eng.wait_ge(dsems[o["eng"]][o["dsem"]], o["dprev"])
                ins = o["fn"]()
                if o["isdma"]:
                    ins.then_inc(dsems[o["eng"]][o["dsem"]], 16)
                elif o["needed"]:
                    ins.then_inc(sems[o["eng"]], 1)
            if extra_final:
                for d in final:
                    wait_for(ops[d])

        allsems = list(sems.values()) + [s for q in dsems.values() for s in q]
        with nc.Block() as block:
            @block.gpsimd
            def _(e):
                for s in allsems:
                    e.sem_clear(s)

        with nc.Block() as block:
            @block.sync
            def _(e):
                if self.sp_wrap is not None:
                    with self.sp_wrap(e):
                        run("sp", extra_final=True)
                else:
                    run("sp", extra_final=True)

            @block.tensor
            def _(e):
                run("pe")

            @block.scalar
            def _(e):
                run("act")

            @block.vector
            def _(e):
                run("dve")

            @block.gpsimd
            def _(e):
                run("pool")


D = 1024
SEQ = 16384
BATCH = 2
DEPTH = 2
TOK_CORE = 4096
ALPHA = (2 * DEPTH) ** 0.25
LN_EPS = 1e-5
RMS_EPS = 1e-6
NEXP = 16
DEXP = 512


def bf(a):
    return np.ascontiguousarray(np.asarray(a, np.float32).astype(ml_dtypes.bfloat16))


def f32c(a):
    return np.ascontiguousarray(np.asarray(a, np.float32))


class Ctx:
    def __init__(self, nc):
        self.nc = nc
        self.P = Prog(nc)
        self.banks = None

    def alloc_banks(self, quarters=False):
        self.banks = [self.P.ps([128, 512], F32, name=f"bank{i}") for i in range(8)]
        self.P.excl |= {f"b{i}" for i in range(8)}
        if quarters:
            self.P.quarters = True
            self.P.excl |= {f"b{i}q{q}" for i in range(8) for q in range(4)}


class Glob:
    def __init__(self, nc):
        self.nc = nc
        self.t = {}

    def din(self, name, shape, dt=F32):
        if name not in self.t:
            self.t[name] = self.nc.dram_tensor(name, list(shape), dt, kind="ExternalInput").ap()
        return self.t[name]

    def internal(self, name, shape, dt=F32):
        if name not in self.t:
            self.t[name] = self.nc.dram_tensor(name, list(shape), dt).ap()
        return self.t[name]


GROUPS = [[0, 1, 2, 3], [4, 5, 6, 7]]


def allgather(nc, pairs):
    Prog._uid += 1
    with nc.semaphore(f"cc_{Prog._uid}") as cc:
        with nc.Block() as block:
            @block.gpsimd
            def _(g):
                g.sem_clear(cc)
        with nc.Block() as block:
            @block.gpsimd
            def _(g):
                for src, dst in pairs:
                    g.collective_compute("AllGather", ALU.bypass, replica_groups=GROUPS, ins=[src], outs=[dst]).then_inc(cc, 1)
                g.wait_ge(cc, len(pairs))


def dma(P, q, out, in_, reads=(), writes=()):
    eng = {"sp": P.nc.sync, "actq": P.nc.scalar, "poolq": P.nc.gpsimd}[q]
    return P.op(q, lambda: eng.dma_start(out=out, in_=in_), reads=reads, writes=writes)


def layer_norm_tile(P, nc, src, srck, dst, dstk, g_t, b_t, gk, bk, scr, tag):
    st, mv, rstd, xn = scr["st"], scr["mv"], scr["rstd"], scr["xn"]

    def bs():
        nc.vector.bn_stats(out=st[:, 0, :], in_=src[:, 0:512])
        return nc.vector.bn_stats(out=st[:, 1, :], in_=src[:, 512:1024])
    P.op("dve", bs, reads=[srck], writes=[tag + "st"])
    P.op("dve", lambda: nc.vector.bn_aggr(out=mv[:], in_=st[:].rearrange("p a s -> p (a s)")),
         reads=[tag + "st"], writes=[tag + "mv"])
    P.op("act", lambda: nc.scalar.activation(out=rstd[:], in_=mv[:, 1:2], func=AF.Sqrt, bias=scr["eps_ln"][:, 0:1], scale=1.0),
         reads=[tag + "mv"], writes=[tag + "rstd"])
    P.op("dve", lambda: nc.vector.reciprocal(out=rstd[:], in_=rstd[:]), reads=[tag + "rstd"], writes=[tag + "rstd"])
    P.op("dve", lambda: nc.vector.tensor_scalar(out=xn[:], in0=src[:], scalar1=mv[:, 0:1], scalar2=rstd[:, 0:1],
                                                op0=ALU.subtract, op1=ALU.mult),
         reads=[srck, tag + "mv", tag + "rstd"], writes=[tag + "xn"])
    P.op("pool", lambda: nc.gpsimd.tensor_tensor(out=xn[:], in0=xn[:], in1=g_t[:], op=ALU.mult),
         reads=[tag + "xn", gk], writes=[tag + "xn"])
    P.op("pool", lambda: nc.gpsimd.tensor_tensor(out=dst[:], in0=xn[:], in1=b_t[:], op=ALU.add),
         reads=[tag + "xn", bk], writes=[dstk])


def ln_scratch(P, tag):
    return dict(st=P.sb([128, 2, 6], F32), mv=P.sb([128, 2], F32), rstd=P.sb([128, 1], F32),
                xn=P.sb([128, 1024], F32))


def const_col(P, nc, val, key):
    t = P.sb([128, 1], F32)
    P.op("pool", lambda: nc.gpsimd.memset(t[:], val), writes=[key])
    return t


def to_featmajor_bf16(P, nc, src, srck, hb, hbk, bank, bankk, dstT, dstk, ident, cast_eng="act"):
    if cast_eng == "act":
        P.op("act", lambda: nc.scalar.copy(out=hb[:], in_=src[:]), reads=[srck], writes=[hbk])
    else:
        P.op("pool", lambda: nc.gpsimd.tensor_copy(out=hb[:], in_=src[:]), reads=[srck], writes=[hbk])
    pv = bank[:].bitcast(BF16).rearrange("p (k t) -> p k t", k=8)

    def tr():
        for kc in range(8):
            ins = nc.tensor.transpose(out=pv[:, kc, :], in_=hb[:, kc * 128:(kc + 1) * 128], identity=ident[:])
        return ins
    P.op("pe", tr, reads=[hbk, "ident"], writes=[bankk])
    P.op("dve", lambda: nc.vector.tensor_copy(out=dstT, in_=pv), reads=[bankk], writes=[dstk])


def phase_pre(nc, G, h, hT_loc, ntok=TOK_CORE):
    C = Ctx(nc)
    P = C.P
    nt = ntok // 128
    x = G.din("x", [ntok, D])
    g = G.din("g", [1, D])
    b = G.din("b", [1, D])
    idn = G.din("idn", [128, 128], BF16)
    hT = hT_loc.rearrange("(k p) t -> k p t", k=8)
    with P.stack:
        C.alloc_banks()
        gt = P.sb([128, D], F32)
        bt = P.sb([128, D], F32)
        ident = P.sb([128, 128], BF16)
        eps = const_col(P, nc, LN_EPS, "eps_ln")
        xt = [P.sb([128, D], F32) for _ in range(2)]
        ht = [P.sb([128, D], F32) for _ in range(2)]
        hb = P.sb([128, D], BF16)
        hTt = [P.sb([128, 8, 128], BF16) for _ in range(2)]
        scr = ln_scratch(P, "ln")
        scr["eps_ln"] = eps
        dma(P, "sp", gt[:], g.partition_broadcast(128), writes=["g"])
        dma(P, "sp", bt[:], b.partition_broadcast(128), writes=["b"])
        dma(P, "sp", ident[:], idn, writes=["ident"])
        outs = []
        for t in range(nt):
            s = t % 2
            dma(P, "sp", xt[s][:], x[t * 128:(t + 1) * 128, :], writes=[("xt", s)])
            layer_norm_tile(P, nc, xt[s], ("xt", s), ht[s], ("ht", s), gt, bt, "g", "b", scr, "ln")
            dma(P, "poolq", h[t * 128:(t + 1) * 128, :], ht[s][:], reads=[("ht", s)], writes=[("h", t)])
            to_featmajor_bf16(P, nc, ht[s], ("ht", s), hb, "hb", C.banks[s], f"b{s}", hTt[s][:], ("hTt", s), ident)
            dma(P, "sp", hT[:, :, t * 128:(t + 1) * 128].rearrange("k p t -> p k t"), hTt[s][:],
                reads=[("hTt", s)], writes=[("hT", t)])
            outs += [("h", t), ("hT", t)]
        P.emit(final_wait_keys=outs)


BIG = 1.0e4


def phase_post(nc, G, l, h_in, h_out, hT_loc, y_all, ntok=TOK_CORE):
    C = Ctx(nc)
    P = C.P
    nsup = ntok // 512
    pf = f"L{l}_"

    def din(name, shape, dt=F32):
        return G.din(pf + name, shape, dt)
    wout = din("wout", [128, 8, D], BF16)
    wglu = din("wglu", [128, 2, 256], BF16)
    ln1g = din("ln1g", [1, D]); ln1b = din("ln1b", [1, D]); ln2g = din("ln2g", [1, D]); ln2b = din("ln2b", [1, D])
    dng = din("dng", [1, 64]); lamv = din("lamv", [1, 130])
    wr = din("wr", [128, 8, 20]); br = din("br", [1, 20])
    w1 = din("w1", [NEXP, 128, 8, DEXP], BF16); w3 = din("w3", [NEXP, 128, 8, DEXP], BF16)
    w2 = din("w2", [NEXP, 128, 4, D], BF16)
    idn = G.din("idn", [128, 128], BF16); idn32 = G.din("idn32", [128, 128])
    rofs = G.din("rofs", [1, 1], I32)
    hT_out = hT_loc.rearrange("(k p) t -> k p t", k=8) if hT_loc is not None else None
    ymj = G.internal("y_mine", [4, ntok, 384])
    ym = ymj.rearrange("j s c -> s j c")
    offh = {}

    from contextlib import contextmanager

    @contextmanager
    def sp_wrap(sp):
        with sp.register(f"rofs{l}") as reg:
            sp.reg_load(reg, rofs[0:1, 0:1])
            offh["v"] = sp.snap(reg)
            yield
    P.sp_wrap = sp_wrap
    nch = ntok // YCH
    yj = y_all.rearrange("i (j s) c -> j i s c", j=4)
    for j in range(4):
        P.op("sp", lambda j=j: nc.sync.dma_start(out=ymj[j].rearrange("(i a b) c -> i a (b c)", i=nch, a=16),
                                                 in_=yj[j][bass.ds(offh["v"], nch)].rearrange("i (a b) c -> i a (b c)", a=16)),
             writes=[("ymine", j)])

    with P.stack:
        C.alloc_banks()
        B = C.banks
        sb = P.sb
        ident = sb([128, 128], BF16); ident32 = sb([128, 128], F32)
        g1 = sb([128, D], F32); b1 = sb([128, D], F32); g2 = sb([128, D], F32); b2 = sb([128, D], F32)
        wout_t = sb([128, 8, D], BF16); wglu_t = sb([128, 2, 256], BF16)
        wr_t = sb([128, 8, 20], F32); br_t = sb([128, 20], F32)
        gA = sb([128, 64], F32); lam_t = sb([128, 130], F32); lprod = sb([128, 2, 32], F32)
        lsum = sb([128, 2], F32); nlam = sb([128, 1], F32)
        eps_ln = const_col(P, nc, LN_EPS, "eps_ln"); eps_rms = const_col(P, nc, RMS_EPS, "eps_rms")
        scr = ln_scratch(P, "ln"); scr["eps_ln"] = eps_ln
        ht = [sb([128, D], F32) for _ in range(2)]
        ya_t = [sb([128, 768], F32) for _ in range(2)]
        yc_t = [sb([128, 256], F32) for _ in range(2)]
        ymix = [sb([128, D], F32) for _ in range(2)]
        dd = sb([128, 6, 64], F32); sq = sb([128, 6, 64], F32); ss = sb([128, 6], F32)
        c2 = sb([128, 256], F32); c3 = sb([128, 256], F32); ygb = sb([128, 256], BF16)
        ygT = sb([128, 2, 128], BF16); sig = sb([128, 256], F32)
        ymb = sb([128, D], BF16); ymT = sb([128, 8, 128], BF16)
        z = sb([128, D], F32)
        h1 = [sb([128, D], F32) for _ in range(4)]
        h1T32 = sb([128, 8, 128], F32)
        h1T = sb([128, 8, 512], BF16)
        lg = sb([128, 20], F32)
        r = {n: sb([128, s], F32) for n, s in dict(gmax=1, goh=4, ngmax=1, gexp=4, gsum=1, gp=1, em=16, pen=4, m1=1, oh1=16,
                                                   em2=16, m2=1, oh2=16, dl=1, ex=1, den=1, w1=1, w2=1).items()}
        comb = sb([128, 4, 16], F32)
        wA = [sb([128, 8, DEXP], BF16) for _ in range(2)]
        wB = [sb([128, 8, DEXP], BF16) for _ in range(2)]
        wC = [sb([128, 4, D], BF16) for _ in range(2)]
        sil = [sb([128, 512], F32) for _ in range(2)]
        hid = [sb([128, 512], BF16) for _ in range(4)]
        acc = [sb([128, D], F32) for _ in range(4)]
        z2 = sb([128, D], F32)
        ho = [sb([128, D], F32) for _ in range(2)]
        hob = sb([128, D], BF16)
        hoT = [sb([128, 8, 128], BF16) for _ in range(2)]

        dma(P, "sp", ident[:], idn, writes=["ident"]); dma(P, "sp", ident32[:], idn32, writes=["ident32"])
        for t_, s_, k_ in ((g1, ln1g, "g1"), (b1, ln1b, "b1"), (g2, ln2g, "g2"), (b2, ln2b, "b2")):
            dma(P, "sp", t_[:], s_.partition_broadcast(128), writes=[k_])
        dma(P, "sp", wout_t[:], wout, writes=["wout"]); dma(P, "sp", wglu_t[:], wglu, writes=["wglu"])
        dma(P, "sp", wr_t[:], wr, writes=["wr"]); dma(P, "sp", br_t[:], br.partition_broadcast(128), writes=["br"])
        dma(P, "sp", gA[:], dng.partition_broadcast(128), writes=["gA"])
        dma(P, "sp", lam_t[:], lamv.partition_broadcast(128), writes=["lamt"])
        P.op("dve", lambda: nc.vector.tensor_scalar(out=gA[:], in0=gA[:], scalar1=lam_t[:, 128:129], scalar2=None, op0=ALU.mult),
             reads=["gA", "lamt"], writes=["gA"])
        lv = lam_t[:, 0:128].rearrange("p (a b c) -> p a b c", a=2, b=2)
        P.op("dve", lambda: nc.vector.tensor_tensor(out=lprod[:], in0=lv[:, :, 0, :], in1=lv[:, :, 1, :], op=ALU.mult),
             reads=["lamt"], writes=["lprod"])
        P.op("dve", lambda: nc.vector.tensor_reduce(out=lsum[:], in_=lprod[:], axis=AX.X, op=ALU.add), reads=["lprod"], writes=["lsum"])
        P.op("act", lambda: nc.scalar.activation(out=lsum[:], in_=lsum[:], func=AF.Exp), reads=["lsum"], writes=["lsum"])
        P.op("dve", lambda: nc.vector.tensor_tensor(out=nlam[:], in0=lsum[:, 1:2], in1=lsum[:, 0:1], op=ALU.subtract),
             reads=["lsum"], writes=["nlam"])
        P.op("dve", lambda: nc.vector.tensor_scalar(out=nlam[:], in0=nlam[:], scalar1=lam_t[:, 129:130], scalar2=None, op0=ALU.add),
             reads=["nlam", "lamt"], writes=["nlam"])

        outs = []
        for su in range(nsup):
            for tt in range(4):
                t = su * 4 + tt
                s = t % 2
                rows = slice(t * 128, (t + 1) * 128)
                dma(P, "sp", ht[s][:], h_in[rows, :], writes=[("ht", s)])
                ymk_ = [("ymine", j_) for j_ in range(4)]
                dma(P, "sp", ya_t[s][:].rearrange("p (j c) -> p j c", j=4), ym[rows, :, 0:192], reads=ymk_, writes=[("ya", s)])
                dma(P, "sp", ymix[s][:, 384:768].rearrange("p (j c) -> p j c", j=3), ym[rows, 0:3, 192:320], reads=ymk_, writes=[("ymix", s, 1)])
                dma(P, "sp", yc_t[s][:].rearrange("p (j c) -> p j c", j=4), ym[rows, :, 320:384], reads=ymk_, writes=[("yc", s)])
                yav = ya_t[s][:].rearrange("p (h m d) -> p h m d", h=6, m=2)
                P.op("dve", lambda yav=yav: nc.vector.scalar_tensor_tensor(out=dd[:], in0=yav[:, :, 1, :], scalar=nlam[:, 0:1],
                                                                          in1=yav[:, :, 0, :], op0=ALU.mult, op1=ALU.add),
                     reads=[("ya", s), "nlam"], writes=["dd"])
                P.op("pool", lambda: nc.gpsimd.tensor_tensor(out=sq[:], in0=dd[:], in1=dd[:], op=ALU.mult), reads=["dd"], writes=["sq"])
                P.op("dve", lambda: nc.vector.tensor_reduce(out=ss[:], in_=sq[:], axis=AX.X, op=ALU.add), reads=["sq"], writes=["ss"])
                P.op("act", lambda: nc.scalar.activation(out=ss[:], in_=ss[:], func=AF.Sqrt, bias=eps_rms[:, 0:1], scale=1.0 / 64),
                     reads=["ss", "eps_rms"], writes=["ss"])
                P.op("dve", lambda: nc.vector.reciprocal(out=ss[:], in_=ss[:]), reads=["ss"], writes=["ss"])
                P.op("dve", lambda: nc.vector.tensor_tensor(out=dd[:], in0=dd[:], in1=ss[:].unsqueeze(2).to_broadcast([128, 6, 64]), op=ALU.mult),
                     reads=["dd", "ss"], writes=["dd"])
                ym0 = ymix[s][:, 0:384].rearrange("p (h d) -> p h d", h=6)
                P.op("pool", lambda ym0=ym0: nc.gpsimd.tensor_tensor(out=ym0, in0=dd[:], in1=gA[:].unsqueeze(1).to_broadcast([128, 6, 64]), op=ALU.mult),
                     reads=["dd", "gA"], writes=[("ymix", s, 0)])
                yct = yc_t[s]
                P.op("pool", lambda yct=yct: nc.gpsimd.tensor_tensor(out=c2[:], in0=yct[:], in1=yct[:], op=ALU.mult), reads=[("yc", s)], writes=["c2"])
                P.op("dve", lambda: nc.vector.tensor_scalar(out=c2[:], in0=c2[:], scalar1=0.044715, scalar2=1.0, op0=ALU.mult, op1=ALU.add),
                     reads=["c2"], writes=["c2"])
                P.op("dve", lambda yct=yct: nc.vector.tensor_tensor(out=c2[:], in0=c2[:], in1=yct[:], op=ALU.mult), reads=["c2", ("yc", s)], writes=["c2"])
                P.op("act", lambda: nc.scalar.activation(out=c2[:], in_=c2[:], func=AF.Sigmoid, scale=1.5957691216057308),
                     reads=["c2"], writes=["c2"])
                P.op("dve", lambda yct=yct: nc.vector.tensor_tensor(out=c3[:], in0=c2[:], in1=yct[:], op=ALU.mult), reads=["c2", ("yc", s)], writes=["c3"])
                P.op("act", lambda: nc.scalar.copy(out=ygb[:], in_=c3[:]), reads=["c3"], writes=["ygb"])
                pv0 = B[0][:].bitcast(BF16)

                def trg(pv0=pv0):
                    for k in range(2):
                        ins = nc.tensor.transpose(out=pv0[:, k * 128:(k + 1) * 128], in_=ygb[:, k * 128:(k + 1) * 128], identity=ident[:])
                    return ins
                P.op("pe", trg, reads=["ygb", "ident"], writes=["b0"])
                P.op("dve", lambda pv0=pv0: nc.vector.tensor_copy(out=ygT[:].rearrange("p k t -> p (k t)"), in_=pv0[:, 0:256]), reads=["b0"], writes=["ygT"])

                def mmg():
                    for k in range(2):
                        ins = nc.tensor.matmul(B[1][:, 0:256], lhsT=ygT[:, k, :], rhs=wglu_t[:, k, :], start=(k == 0), stop=(k == 1))
                    return ins
                P.op("pe", mmg, reads=["ygT", "wglu"], writes=["b1"])
                P.op("act", lambda: nc.scalar.activation(out=sig[:], in_=B[1][:, 0:256], func=AF.Sigmoid), reads=["b1"], writes=["sig"])
                P.op("dve", lambda s=s: nc.vector.tensor_tensor(out=ymix[s][:, 768:1024], in0=c3[:], in1=sig[:], op=ALU.mult),
                     reads=["c3", "sig"], writes=[("ymix", s, 2)])
                ymk = [("ymix", s, 0), ("ymix", s, 1), ("ymix", s, 2)]
                P.op("act", lambda s=s: nc.scalar.copy(out=ymb[:], in_=ymix[s][:]), reads=ymk, writes=["ymb"])
                pvb = B[0][:].bitcast(BF16).rearrange("p (k t) -> p k t", k=8)

                def try_(pvb=pvb):
                    for kc in range(8):
                        ins = nc.tensor.transpose(out=pvb[:, kc, :], in_=ymb[:, kc * 128:(kc + 1) * 128], identity=ident[:])
                    return ins
                P.op("pe", try_, reads=["ymb", "ident"], writes=["b0"])
                P.op("dve", lambda pvb=pvb: nc.vector.tensor_copy(out=ymT[:], in_=pvb), reads=["b0"], writes=["ymT"])
                for half in range(2):
                    def mmo(half=half):
                        for kc in range(8):
                            ins = nc.tensor.matmul(B[2 + half][:], lhsT=ymT[:, kc, :], rhs=wout_t[:, kc, half * 512:(half + 1) * 512],
                                                   start=(kc == 0), stop=(kc == 7))
                        return ins
                    P.op("pe", mmo, reads=["ymT", "wout"], writes=[f"b{2 + half}"])
                    P.op("dve", lambda half=half, s=s: nc.vector.scalar_tensor_tensor(
                        out=z[:, half * 512:(half + 1) * 512], in0=ht[s][:, half * 512:(half + 1) * 512], scalar=float(ALPHA),
                        in1=B[2 + half][:], op0=ALU.mult, op1=ALU.add), reads=[f"b{2 + half}", ("ht", s)], writes=[("z", half)])
                P.op("pool", lambda: nc.gpsimd.tensor_copy(out=z[:, 0:1], in_=z[:, 0:1]), reads=[("z", 0), ("z", 1)], writes=["zz"])
                layer_norm_tile(P, nc, z, "zz", h1[tt], ("h1", tt), g1, b1, "g1", "b1", scr, "ln")
                for half in range(2):
                    pv32 = B[2 + half][:].rearrange("p (k t) -> p k t", k=4)

                    def trh(half=half, pv32=pv32, tt=tt):
                        for k in range(4):
                            kc = half * 4 + k
                            ins = nc.tensor.transpose(out=pv32[:, k, :], in_=h1[tt][:, kc * 128:(kc + 1) * 128], identity=ident32[:])
                        return ins
                    P.op("pe", trh, reads=[("h1", tt), "ident32"], writes=[f"b{2 + half}"])
                    P.op("dve", lambda half=half, pv32=pv32: nc.vector.tensor_copy(out=h1T32[:, half * 4:(half + 1) * 4, :], in_=pv32),
                         reads=[f"b{2 + half}"], writes=[("h1T32", half)])
                    P.op("act", lambda half=half, pv32=pv32, tt=tt: nc.scalar.copy(
                        out=h1T[:, half * 4:(half + 1) * 4, tt * 128:(tt + 1) * 128], in_=pv32),
                        reads=[f"b{2 + half}"], writes=[("h1T", tt, half)])

                def mmr():
                    for kc in range(8):
                        ins = nc.tensor.matmul(B[1][:, 0:20], lhsT=h1T32[:, kc, :], rhs=wr_t[:, kc, :], start=(kc == 0), stop=(kc == 7))
                    return ins
                P.op("pe", mmr, reads=[("h1T32", 0), ("h1T32", 1), "wr"], writes=["b1"])
                P.op("dve", lambda: nc.vector.tensor_tensor(out=lg[:], in0=B[1][:, 0:20], in1=br_t[:], op=ALU.add), reads=["b1", "br"], writes=["lg"])
                V = nc.vector
                glog = lg[:, 0:4]
                elog = lg[:, 4:20]
                seq = [
                    lambda: V.tensor_reduce(out=r["gmax"][:], in_=glog, axis=AX.X, op=ALU.max),
                    lambda: V.tensor_scalar(out=r["goh"][:], in0=glog, scalar1=r["gmax"][:, 0:1], scalar2=None, op0=ALU.is_ge),
                    lambda: V.tensor_scalar(out=r["ngmax"][:], in0=r["gmax"][:], scalar1=-1.0, scalar2=None, op0=ALU.mult),
                ]
                for f_ in seq:
                    P.op("dve", f_, reads=["lg", "rt"], writes=["rt"])
                P.op("act", lambda: nc.scalar.activation(out=r["gexp"][:], in_=glog, func=AF.Exp, bias=r["ngmax"][:, 0:1], scale=1.0),
                     reads=["lg", "rt"], writes=["rt2"])
                seq = [
                    lambda: V.tensor_reduce(out=r["gsum"][:], in_=r["gexp"][:], axis=AX.X, op=ALU.add),
                    lambda: V.reciprocal(out=r["gp"][:], in_=r["gsum"][:]),
                    lambda: V.tensor_tensor(out=r["em"][:].rearrange("p (g e) -> p g e", g=4), in0=elog.rearrange("p (g e) -> p g e", g=4),
                                            in1=r["goh"][:].unsqueeze(2).to_broadcast([128, 4, 4]), op=ALU.mult),
                    lambda: V.tensor_scalar(out=r["pen"][:], in0=r["goh"][:], scalar1=-1.0, scalar2=BIG, op0=ALU.add, op1=ALU.mult),
                    lambda: V.tensor_tensor(out=r["em"][:].rearrange("p (g e) -> p g e", g=4), in0=r["em"][:].rearrange("p (g e) -> p g e", g=4),
                                            in1=r["pen"][:].unsqueeze(2).to_broadcast([128, 4, 4]), op=ALU.add),
                    lambda: V.tensor_reduce(out=r["m1"][:], in_=r["em"][:], axis=AX.X, op=ALU.max),
                    lambda: V.tensor_scalar(out=r["oh1"][:], in0=r["em"][:], scalar1=r["m1"][:, 0:1], scalar2=None, op0=ALU.is_ge),
                    lambda: V.scalar_tensor_tensor(out=r["em2"][:], in0=r["oh1"][:], scalar=-BIG, in1=r["em"][:], op0=ALU.mult, op1=ALU.add),
                    lambda: V.tensor_reduce(out=r["m2"][:], in_=r["em2"][:], axis=AX.X, op=ALU.max),
                    lambda: V.tensor_scalar(out=r["oh2"][:], in0=r["em2"][:], scalar1=r["m2"][:, 0:1], scalar2=None, op0=ALU.is_ge),
                    lambda: V.tensor_tensor(out=r["dl"][:], in0=r["m2"][:], in1=r["m1"][:], op=ALU.subtract),
                ]
                for f_ in seq:
                    P.op("dve", f_, reads=["lg", "rt", "rt2"], writes=["rt"])
                P.op("act", lambda: nc.scalar.activation(out=r["ex"][:], in_=r["dl"][:], func=AF.Exp), reads=["rt"], writes=["rt2"])
                seq = [
                    lambda: V.tensor_scalar(out=r["den"][:], in0=r["ex"][:], scalar1=1.0, scalar2=None, op0=ALU.add),
                    lambda: V.reciprocal(out=r["w1"][:], in_=r["den"][:]),
                    lambda: V.tensor_tensor(out=r["w1"][:], in0=r["w1"][:], in1=r["gp"][:], op=ALU.mult),
                    lambda: V.tensor_tensor(out=r["w2"][:], in0=r["w1"][:], in1=r["ex"][:], op=ALU.mult),
                    lambda tt=tt: V.tensor_scalar(out=comb[:, tt, :], in0=r["oh1"][:], scalar1=r["w1"][:, 0:1], scalar2=None, op0=ALU.mult),
                    lambda tt=tt: V.scalar_tensor_tensor(out=comb[:, tt, :], in0=r["oh2"][:], scalar=r["w2"][:, 0:1], in1=comb[:, tt, :],
                                                         op0=ALU.mult, op1=ALU.add),
                ]
                for i_, f_ in enumerate(seq):
                    P.op("dve", f_, reads=["rt", "rt2"] + ([("comb", tt)] if i_ == 5 else []), writes=["rt"] if i_ < 4 else [("comb", tt)])
            h1Tk = [("h1T", tt, half) for tt in range(4) for half in range(2)]
            for e in range(NEXP):
                ws = e % 2
                dma(P, "sp", wA[ws][:], w1[e], writes=[("wA", ws)])
                dma(P, "poolq", wB[ws][:], w3[e], writes=[("wB", ws)])
                dma(P, "sp", wC[ws][:], w2[e], writes=[("wC", ws)])
                for hc in range(4):
                    ba, bb = 4 + hc % 2, 6 + hc % 2

                    def mma(hc=hc, ba=ba, ws=ws):
                        for kc in range(8):
                            ins = nc.tensor.matmul(B[ba][:], lhsT=wA[ws][:, kc, hc * 128:(hc + 1) * 128], rhs=h1T[:, kc, :],
                                                   start=(kc == 0), stop=(kc == 7))
                        return ins

                    def mmb(hc=hc, bb=bb, ws=ws):
                        for kc in range(8):
                            ins = nc.tensor.matmul(B[bb][:], lhsT=wB[ws][:, kc, hc * 128:(hc + 1) * 128], rhs=h1T[:, kc, :],
                                                   start=(kc == 0), stop=(kc == 7))
                        return ins
                    P.op("pe", mma, reads=h1Tk + [("wA", ws)], writes=[f"b{ba}"])
                    P.op("pe", mmb, reads=h1Tk + [("wB", ws)], writes=[f"b{bb}"])
                    P.op("act", lambda hc=hc, ba=ba: nc.scalar.activation(out=sil[hc % 2][:], in_=B[ba][:], func=AF.Silu),
                         reads=[f"b{ba}"], writes=[("sil", hc % 2)])
                    P.op("dve", lambda hc=hc, bb=bb: nc.vector.tensor_tensor(out=hid[hc][:], in0=sil[hc % 2][:], in1=B[bb][:], op=ALU.mult),
                         reads=[f"b{bb}", ("sil", hc % 2)], writes=[("hid", hc)])
                for tt in range(4):
                    for half in range(2):
                        bo = 2 + half

                        def mm2(tt=tt, half=half, bo=bo, ws=ws):
                            for hc in range(4):
                                ins = nc.tensor.matmul(B[bo][:], lhsT=hid[hc][:, tt * 128:(tt + 1) * 128],
                                                       rhs=wC[ws][:, hc, half * 512:(half + 1) * 512], start=(hc == 0), stop=(hc == 3))
                            return ins
                        P.op("pe", mm2, reads=[("hid", hc) for hc in range(4)] + [("wC", ws)], writes=[f"b{bo}"])
                        av = acc[tt][:, half * 512:(half + 1) * 512]
                        if e == 0:
                            P.op("dve", lambda av=av, bo=bo, tt=tt, e=e: nc.vector.tensor_scalar(
                                out=av, in0=B[bo][:], scalar1=comb[:, tt, e:e + 1], scalar2=None, op0=ALU.mult),
                                reads=[f"b{bo}", ("comb", tt)], writes=[("acc", tt, half)])
                        else:
                            P.op("dve", lambda av=av, bo=bo, tt=tt, e=e: nc.vector.scalar_tensor_tensor(
                                out=av, in0=B[bo][:], scalar=comb[:, tt, e:e + 1], in1=av, op0=ALU.mult, op1=ALU.add),
                                reads=[f"b{bo}", ("comb", tt), ("acc", tt, half)], writes=[("acc", tt, half)])
            for tt in range(4):
                t = su * 4 + tt
                s = t % 2
                rows = slice(t * 128, (t + 1) * 128)
                P.op("dve", lambda tt=tt: nc.vector.scalar_tensor_tensor(out=z2[:], in0=h1[tt][:], scalar=float(ALPHA), in1=acc[tt][:],
                                                                          op0=ALU.mult, op1=ALU.add),
                     reads=[("h1", tt), ("acc", tt, 0), ("acc", tt, 1)], writes=["z2"])
                layer_norm_tile(P, nc, z2, "z2", ho[s], ("ho", s), g2, b2, "g2", "b2", scr, "ln")
                dma(P, "poolq", h_out[rows, :], ho[s][:], reads=[("ho", s)], writes=[("h_out", t)])
                outs.append(("h_out", t))
                if hT_out is not None:
                    to_featmajor_bf16(P, nc, ho[s], ("ho", s), hob, "hob", B[0], "b0", hoT[s][:], ("hoT", s), ident)
                    dma(P, "poolq", hT_out[:, :, rows].rearrange("k p t -> p k t"), hoT[s][:], reads=[("hoT", s)], writes=[("hT_out", t)])
                    outs.append(("hT_out", t))
        P.emit(final_wait_keys=outs)


def post_inputs(l, p, lam_init):
    d = {}
    d["wout"] = bf(p["w_out"][l].reshape(8, 128, D).transpose(1, 0, 2))
    d["wglu"] = bf(p["s5_w_glu"][l].reshape(2, 128, 256).transpose(1, 0, 2))
    for n in ("ln1_g", "ln1_b", "ln2_g", "ln2_b"):
        d[n.replace("_", "")] = f32c(p[n][l].reshape(1, D))
    d["dng"] = f32c(p["diff_norm_g"][l].reshape(1, 64))
    d["lamv"] = f32c(np.concatenate([p["lam_q1"][l], p["lam_k1"][l], p["lam_q2"][l], p["lam_k2"][l],
                                     np.array([1.0 - lam_init, -lam_init], np.float32)]).reshape(1, 130))
    wr = np.concatenate([p["moe_w_grp"][l], p["moe_w_exp"][l]], axis=1)
    d["wr"] = f32c(wr.reshape(8, 128, 20).transpose(1, 0, 2))
    d["br"] = f32c(np.concatenate([p["moe_b_grp"][l], p["moe_b_exp"][l]]).reshape(1, 20))
    d["w1"] = bf(p["moe_w1"][l].reshape(NEXP, 8, 128, DEXP).transpose(0, 2, 1, 3))
    d["w3"] = bf(p["moe_w3"][l].reshape(NEXP, 8, 128, DEXP).transpose(0, 2, 1, 3))
    d["w2"] = bf(p["moe_w2"][l].reshape(NEXP, 4, 128, D).transpose(0, 2, 1, 3))
    d["idn"] = bf(np.eye(128)); d["idn32"] = f32c(np.eye(128))
    return d


SBANKS = [0, 1, 2, 6, 7]
NPT = 7
ADEPTH = 4
QUARTERS = False
TWO_PI = 2.0 * math.pi
MAGIC = 12582912.0


def phase_mix(nc, G, l, hT_all, y_o, S=SEQ, do_attn=True, do_s5=True, do_gdn=True):
    debug = False
    C = Ctx(nc)
    P = C.P
    nst = S // 512
    nblk = S // 128
    pf = f"L{l}_"

    def din(name, shape, dt=F32):
        return G.din((pf + name) if name not in ("amask", "idn32", "srow", "cTri", "cSL", "cMask2", "cBones") else name, shape, dt)
    hT4 = hT_all.rearrange("k (r p) t -> r p k t", r=4)
    wq = din("wq", [128, 8, 96], BF16); wk = din("wk", [128, 8, 96], BF16); wv = din("wv", [128, 8, 192], BF16)
    amask = din("amask", [128, 4, 512], BF16)
    idn32 = din("idn32", [128, 128])
    wu = din("wu", [128, 8, 64], BF16)
    s5row = din("s5row", [2, 3, 128])
    s5col = din("s5col", [2, 128, 3])
    s5bT = din("s5bT", [2, 2, 2, 16, 64])
    s5cT = din("s5cT", [2, 2, 2, 64, 16])
    s5d = din("s5d", [64, 1])
    srow = din("srow", [1, 512])
    wg = din("wg", [128, 8, 384], BF16); wt = din("wt", [128, 8, 132], BF16)
    cvw = din("cvw", [128, 3, 4])
    galog = din("galog", [1, 2]); gdtb = din("gdtb", [1, 2]); gng = din("gng", [1, 64])
    cTri = din("cTri", [64, 64]); cSL = din("cSL", [64, 64]); cMask2 = din("cMask2", [64, 2, 64]); cBones = din("cBones", [128, 128])
    ya_o = y_o[:, 0:192].rearrange("s (u d) -> s u d", u=3)
    yb_o = y_o[:, 192:320].rearrange("s (h d) -> s h d", h=2)
    yc_o = y_o[:, 320:384]
    outs = []
    dbg = []
    V = nc.vector
    G = nc.gpsimd
    A = nc.scalar
    T = nc.tensor

    with P.stack:
        C.alloc_banks(quarters=do_gdn and QUARTERS)
        B = C.banks
        sb = P.sb
        ident32 = sb([128, 128], F32)
        dma(P, "sp", ident32[:], idn32, writes=["ident32"])
        hTt = [sb([128, 8, 512], BF16)] * 2
        eps_rms = const_col(P, nc, RMS_EPS, "eps_rms")
        if do_attn:
            wq_t = sb([128, 8, 96], BF16); wk_t = sb([128, 8, 96], BF16); wv_t = sb([128, 8, 192], BF16)
            QT = sb([96, S], BF16); KT = sb([96, S], BF16)
            Vall = sb([128, nblk, 3, 65], BF16)
            am_t = sb([128, 4, 512], BF16)
            PT = [sb([128, 512], BF16) for _ in range(NPT)]
            osb = sb([65, 512], F32); rec = sb([128, 4], F32)
            oT = [sb([128, 4, 64], F32) for _ in range(2)]
            dma(P, "sp", wq_t[:], wq, writes=["wq"]); dma(P, "sp", wk_t[:], wk, writes=["wk"]); dma(P, "sp", wv_t[:], wv, writes=["wv"])
            dma(P, "sp", am_t[:], amask, writes=["amask"])
            P.op("pool", lambda: G.memset(Vall[:, :, :, 64:65], 1.0), writes=["Vones"])
        if do_s5:
            wu_t = sb([128, 8, 64], BF16)
            dma(P, "sp", wu_t[:], wu, writes=["wu"])
            uT = [sb([32, 512], F32) for _ in range(2)]
            srow_t = sb([128, 512], F32)
            dma(P, "sp", srow_t[:], srow.partition_broadcast(128), writes=["srow"])
            d_col = [sb([32, 1], F32) for _ in range(2)]
            for pr_ in range(2):
                dma(P, "sp", d_col[pr_][:], s5d[pr_ * 32:(pr_ + 1) * 32, :], writes=[("dcol", pr_)])
            s5 = []
            for pr in range(2):
                t = dict(row=sb([32, 3, 128], F32), col=sb([128, 3], F32),
                         BrBD=sb([32, 128], F32), BiBD=sb([32, 128], F32), CrBD=sb([128, 32], F32), CiBD=sb([128, 32], F32),
                         bbr=sb([32, 128], F32), bbi=sb([32, 128], F32),
                         w=[sb([32, 128], F32) for _ in range(8)],
                         cw=[sb([128, 1], F32) for _ in range(8)],
                         RHO=sb([128, 512], F32), CS=sb([128, 512], F32), SN=sb([128, 512], F32),
                         zi=sb([128, 2], F32), zt=sb([128, 2], F32))
                if pr == 0:
                    for nm_ in ("bre", "bim", "t1", "t2", "zre", "zim", "xre", "xim", "ang", "tmp"):
                        t[nm_] = sb([128, 512], F32)
                if pr == 1:
                    for nm_ in ("bre", "bim", "t1", "t2", "zre", "zim", "xre", "xim", "ang", "tmp"):
                        t[nm_] = s5[0][nm_]
                s5.append(t)
            yT = [sb([32, 512], F32) for _ in range(2)]
            yc_tm = [sb([128, 4, 64], F32) for _ in range(2)]

            def range_reduce(eng_name, x, tmp, key_x, key_t, shape_all=True):
                P.op("dve", lambda: V.tensor_scalar(out=tmp, in0=x, scalar1=1.0 / TWO_PI, scalar2=MAGIC, op0=ALU.mult, op1=ALU.add),
                     reads=[key_x], writes=[key_t])
                P.op("dve", lambda: V.tensor_scalar(out=tmp, in0=tmp, scalar1=-MAGIC, scalar2=-TWO_PI, op0=ALU.add, op1=ALU.mult),
                     reads=[key_t], writes=[key_t])
                P.op("dve", lambda: V.tensor_tensor(out=x, in0=x, in1=tmp, op=ALU.add), reads=[key_x, key_t], writes=[key_x])

            def s5_setup(pr):
                t = s5[pr]
                k = lambda n, pr=pr: ("s5", "sh" if n in ("bre", "bim", "t1", "t2", "zre", "zim", "xre", "xim", "tab", "roww_scratch") else pr, n)
                dma(P, "sp", t["row"][:], s5row[pr:pr + 1].partition_broadcast(32), writes=[k("row")])
                dma(P, "sp", t["col"][:], s5col[pr], writes=[k("col")])
                for nm in ("BrBD", "BiBD", "CrBD", "CiBD"):
                    P.op("pool", lambda nm=nm, t=t: G.memset(t[nm][:], 0.0), writes=[k(nm)])
                for g in range(2):
                    dma(P, "sp", t["BrBD"][g * 16:(g + 1) * 16, g * 64:(g + 1) * 64], s5bT[pr, g, 0], reads=[k("BrBD")], writes=[k("BrBD")])
                    dma(P, "sp", t["BiBD"][g * 16:(g + 1) * 16, g * 64:(g + 1) * 64], s5bT[pr, g, 1], reads=[k("BiBD")], writes=[k("BiBD")])
                    dma(P, "sp", t["CrBD"][g * 64:(g + 1) * 64, g * 16:(g + 1) * 16], s5cT[pr, g, 0], reads=[k("CrBD")], writes=[k("CrBD")])
                    dma(P, "sp", t["CiBD"][g * 64:(g + 1) * 64, g * 16:(g + 1) * 16], s5cT[pr, g, 1], reads=[k("CiBD")], writes=[k("CiBD")])
                P.op("dve", lambda t=t: V.tensor_scalar(out=t["CiBD"][:], in0=t["CiBD"][:], scalar1=-1.0, scalar2=None, op0=ALU.mult),
                     reads=[k("CiBD")], writes=[k("CiBD")])
                lre, lim, ldt = t["row"][:, 0, :], t["row"][:, 1, :], t["row"][:, 2, :]
                dt_, lr_, mag, ang, tmp_, sn, cs, den = [t["w"][i][:] for i in range(8)]
                rk = k("roww")
                steps = [
                    ("act", lambda: A.activation(out=dt_, in_=ldt, func=AF.Exp)),
                    ("dve", lambda: V.tensor_tensor(out=lr_, in0=lre, in1=dt_, op=ALU.mult)),
                    ("act", lambda: A.activation(out=mag, in_=lr_, func=AF.Exp)),
                    ("dve", lambda: V.tensor_tensor(out=ang, in0=lim, in1=dt_, op=ALU.mult)),
                ]
                for e_, f_ in steps:
                    P.op(e_, f_, reads=[k("row"), rk], writes=[rk])
                range_reduce("dve", ang, tmp_, rk, rk)
                P.op("act", lambda: A.activation(out=sn, in_=ang, func=AF.Sin), reads=[rk], writes=[rk])
                P.op("dve", lambda: V.tensor_scalar(out=ang, in0=ang, scalar1=math.pi / 2, scalar2=None, op0=ALU.add), reads=[rk], writes=[rk])
                range_reduce("dve", ang, tmp_, rk, rk)
                P.op("act", lambda: A.activation(out=cs, in_=ang, func=AF.Sin), reads=[rk], writes=[rk])
                steps = [
                    lambda: V.tensor_tensor(out=cs, in0=cs, in1=mag, op=ALU.mult),
                    lambda: V.tensor_scalar(out=cs, in0=cs, scalar1=-1.0, scalar2=None, op0=ALU.add),
                    lambda: V.tensor_tensor(out=sn, in0=sn, in1=mag, op=ALU.mult),
                    lambda: V.tensor_tensor(out=den, in0=lre, in1=lre, op=ALU.mult),
                    lambda: V.tensor_tensor(out=tmp_, in0=lim, in1=lim, op=ALU.mult),
                    lambda: V.tensor_tensor(out=den, in0=den, in1=tmp_, op=ALU.add),
                    lambda: V.reciprocal(out=den, in_=den),
                    lambda: V.tensor_tensor(out=dt_, in0=cs, in1=lre, op=ALU.mult),
                    lambda: V.tensor_tensor(out=tmp_, in0=sn, in1=lim, op=ALU.mult),
                    lambda: V.tensor_tensor(out=dt_, in0=dt_, in1=tmp_, op=ALU.add),
                    lambda: V.tensor_tensor(out=dt_, in0=dt_, in1=den, op=ALU.mult),
                    lambda: V.tensor_tensor(out=lr_, in0=sn, in1=lre, op=ALU.mult),
                    lambda: V.tensor_tensor(out=tmp_, in0=cs, in1=lim, op=ALU.mult),
                    lambda: V.tensor_tensor(out=lr_, in0=lr_, in1=tmp_, op=ALU.subtract),
                    lambda: V.tensor_tensor(out=lr_, in0=lr_, in1=den, op=ALU.mult),
                    lambda t=t: V.tensor_tensor(out=t["bbr"][:], in0=dt_, in1=t["BrBD"][:], op=ALU.mult),
                    lambda t=t: V.tensor_tensor(out=tmp_, in0=lr_, in1=t["BiBD"][:], op=ALU.mult),
                    lambda t=t: V.tensor_tensor(out=t["bbr"][:], in0=t["bbr"][:], in1=tmp_, op=ALU.subtract),
                    lambda t=t: V.tensor_tensor(out=t["bbi"][:], in0=dt_, in1=t["BiBD"][:], op=ALU.mult),
                    lambda t=t: V.tensor_tensor(out=tmp_, in0=lr_, in1=t["BrBD"][:], op=ALU.mult),
                    lambda t=t: V.tensor_tensor(out=t["bbi"][:], in0=t["bbi"][:], in1=tmp_, op=ALU.add),
                ]
                for f_ in steps:
                    P.op("dve", f_, reads=[k("row"), rk, k("BrBD"), k("BiBD")], writes=[rk])
                cdt, cth, crho, ca, ctmp, c512s, c512c, cx = [t["cw"][i][:] for i in range(8)]
                ck = k("colw")
                steps = [
                    ("act", lambda t=t: A.activation(out=cdt, in_=t["col"][:, 2:3], func=AF.Exp)),
                    ("dve", lambda t=t: V.tensor_tensor(out=cth, in0=t["col"][:, 1:2], in1=cdt, op=ALU.mult)),
                    ("dve", lambda t=t: V.tensor_tensor(out=crho, in0=t["col"][:, 0:1], in1=cdt, op=ALU.mult)),
                    ("act", lambda: A.activation(out=crho, in_=crho, func=AF.Exp)),
                    ("dve", lambda: V.tensor_scalar(out=ca, in0=cth, scalar1=512.0, scalar2=None, op0=ALU.mult)),
                ]
                for e_, f_ in steps:
                    P.op(e_, f_, reads=[k("col"), ck], writes=[ck])
                range_reduce("dve", ca, ctmp, ck, ck)
                P.op("act", lambda: A.activation(out=c512s, in_=ca, func=AF.Sin), reads=[ck], writes=[ck])
                P.op("dve", lambda: V.tensor_scalar(out=ca, in0=ca, scalar1=math.pi / 2, scalar2=None, op0=ALU.add), reads=[ck], writes=[ck])
                range_reduce("dve", ca, ctmp, ck, ck)
                P.op("act", lambda: A.activation(out=c512c, in_=ca, func=AF.Sin), reads=[ck], writes=[ck])
                tk = k("tab")
                P.op("dve", lambda t=t: V.tensor_scalar(out=t["ang"][:], in0=srow_t[:], scalar1=cth, scalar2=None, op0=ALU.mult),
                     reads=["srow", ck], writes=[tk])
                range_reduce("dve", t["ang"][:], t["tmp"][:], tk, tk)
                P.op("act", lambda t=t: A.activation(out=t["SN"][:], in_=t["ang"][:], func=AF.Sin), reads=[tk], writes=[tk])
                P.op("dve", lambda t=t: V.tensor_scalar(out=t["ang"][:], in0=t["ang"][:], scalar1=math.pi / 2, scalar2=None, op0=ALU.add), reads=[tk], writes=[tk])
                range_reduce("dve", t["ang"][:], t["tmp"][:], tk, tk)
                P.op("act", lambda t=t: A.activation(out=t["CS"][:], in_=t["ang"][:], func=AF.Sin), reads=[tk], writes=[tk])
                P.op("pool", lambda t=t: G.memset(t["RHO"][:], 1.0), writes=[k("rho")])
                P.op("dve", lambda t=t: V.tensor_scalar(out=t["RHO"][:], in0=t["RHO"][:], scalar1=crho, scalar2=None, op0=ALU.mult),
                     reads=[k("rho"), ck], writes=[k("rho")])
                P.op("pool", lambda t=t: G.memset(t["zi"][:], 0.0), writes=[k("zi")])
                if pr == 0:
                    dbg.extend([("CS", t["CS"][:], [128, 512], [k("tab")]), ("SN", t["SN"][:], [128, 512], [k("tab")]),
                            ("RHO", t["RHO"][:], [128, 512], [k("rho")]), ("bbr", t["bbr"][:], [32, 128], [k("roww")]),
                            ("bbi", t["bbi"][:], [32, 128], [k("roww")]), ("cr", t["w"][0][:], [32, 128], [k("roww")]),
                            ("ci", t["w"][1][:], [32, 128], [k("roww")]), ("c512", t["cw"][5][:], [128, 1], [k("colw")]),
                            ("row", t["row"][:], [32, 3, 128], [k("row")]), ("col", t["col"][:], [128, 3], [k("col")])])
            for pr_ in range(2):
                s5_setup(pr_)
        if do_gdn:
            wg_t = sb([128, 8, 384], BF16); wt_t = sb([128, 8, 132], BF16)
            dma(P, "sp", wg_t[:], wg, writes=["wg"]); dma(P, "sp", wt_t[:], wt, writes=["wt"])
            cvw_t = sb([128, 3, 4], F32); dma(P, "sp", cvw_t[:], cvw, writes=["cvw"])
            alog_t = sb([64, 2], F32); dtb_t = sb([64, 2], F32); ng_t = sb([64, 64], F32)
            dma(P, "sp", alog_t[:], galog.partition_broadcast(64), writes=["alog"])
            dma(P, "sp", dtb_t[:], gdtb.partition_broadcast(64), writes=["dtb"])
            dma(P, "sp", ng_t[:], gng.partition_broadcast(64), writes=["ngt"])
            Tri = sb([64, 64], F32); SL = sb([64, 64], F32); Mask2 = sb([64, 2, 64], F32); Bones = sb([128, 128], F32); ones64 = sb([64, 64], F32)
            dma(P, "sp", Tri[:], cTri, writes=["Tri"]); dma(P, "sp", SL[:], cSL, writes=["SL"])
            dma(P, "sp", Mask2[:], cMask2, writes=["Mask2"]); dma(P, "sp", Bones[:], cBones, writes=["Bones"])
            P.op("pool", lambda: G.memset(ones64[:], 1.0), writes=["ones64"])
            P.op("act", lambda: A.activation(out=alog_t[:], in_=alog_t[:], func=AF.Exp), reads=["alog"], writes=["alog"])
            P.op("dve", lambda: V.tensor_scalar(out=alog_t[:], in0=alog_t[:], scalar1=-1.0, scalar2=None, op0=ALU.mult), reads=["alog"], writes=["alog"])
            xraw = [sb([128, 515], F32) for _ in range(3)]
            for c_ in range(3):
                P.op("pool", lambda c_=c_: G.memset(xraw[c_][:], 0.0), writes=[("xraw", c_)])
            cvt = sb([128, 512], F32)
            qkv = [sb([128, 512], F32) for _ in range(3)]
            sqn = sb([128, 512], F32); rn_ = sb([128, 512], F32)
            Sst = [sb([64, 64], F32) for _ in range(2)]
            for h_ in range(2):
                P.op("pool", lambda h_=h_: G.memset(Sst[h_][:], 0.0), writes=[("S", h_)])
            gd = dict(ch=[], hd=[])
            for sl in range(4):
                gd["ch"].append(dict(gs=sb([64, 128], F32), bg=sb([64, 4], F32), nbeta=sb([64, 2], F32)))
            for sl in range(8):
                gd["hd"].append(dict(qkv_tm=sb([64, 3, 64], F32), gcl=sb([64, 2], F32), ex3=sb([64, 3], F32), Gm=sb([64, 64], F32),
                                     EE=sb([64, 2, 64], F32), AT=sb([64, 64], F32),
                                     W=[sb([64, 256], F32) for _ in range(2)], tb=sb([64, 1], F32),
                                     kdec=sb([64, 64], F32), qdec=sb([64, 64], F32), wqT=sb([64, 2, 64], F32), vnew=sb([64, 64], F32),
                                     osb=sb([64, 64], F32), osq=sb([64, 64], F32), oss=sb([64, 1], F32), ngate=sb([64, 64], F32)))
            ybuf = [sb([64, 8, 2, 64], F32) for _ in range(2)]

        for st in range(nst):
            hs = 0
            cols = slice(st * 512, (st + 1) * 512)
            dma(P, "sp", hTt[hs][:], hT4[st // (nst // 4)][:, :, (st % (nst // 4)) * 512:(st % (nst // 4) + 1) * 512], writes=[("hTt", hs)])
            hk = ("hTt", hs)
            if do_attn:
                for (w_t, wkey, dst, dk_, bank) in ((wq_t, "wq", QT, "QT", 0), (wk_t, "wk", KT, "KT", 1)):
                    def mmqk(w_t=w_t, bank=bank, hs=hs):
                        for kc in range(8):
                            ins = T.matmul(B[bank][0:96, :], lhsT=w_t[:, kc, :], rhs=hTt[hs][:, kc, :], start=(kc == 0), stop=(kc == 7))
                        return ins
                    P.op("pe", mmqk, reads=[hk, wkey], writes=[f"b{bank}"])
                    P.op("act", lambda dst=dst, bank=bank, cols=cols: A.copy(out=dst[:, cols], in_=B[bank][0:96, :]),
                         reads=[f"b{bank}"], writes=[(dk_, st)])
                for pair in range(2):
                    bank = 2 + pair
                    pv = B[bank][:, 0:384].rearrange("p (j c) -> p j c", j=2)

                    def mmv(pair=pair, pv=pv, hs=hs):
                        for j in range(2):
                            blk = pair * 2 + j
                            for kc in range(8):
                                ins = T.matmul(pv[:, j, :], lhsT=hTt[hs][:, kc, blk * 128:(blk + 1) * 128], rhs=wv_t[:, kc, :],
                                               start=(kc == 0), stop=(kc == 7))
                        return ins
                    P.op("pe", mmv, reads=[hk, "wv"], writes=[f"b{bank}"])
                    b0 = st * 4 + pair * 2
                    P.op("dve", lambda pv=pv, b0=b0: V.tensor_copy(out=Vall[:, b0:b0 + 2, :, 0:64],
                                                                  in_=pv.rearrange("p j (u d) -> p j u d", u=3)),
                         reads=[f"b{bank}"], writes=[("V", st, pair)])
            if do_s5:
                for pr in range(2):
                    def mmu(hs=hs, pr=pr):
                        for kc in range(8):
                            ins = T.matmul(B[4][0:32, :], lhsT=wu_t[:, kc, pr * 32:(pr + 1) * 32], rhs=hTt[hs][:, kc, :], start=(kc == 0), stop=(kc == 7))
                        return ins
                    P.op("pe", mmu, reads=[hk, "wu"], writes=["b4"])
                    P.op("act", lambda pr=pr: A.copy(out=uT[pr][:], in_=B[4][0:32, :]), reads=["b4"], writes=[("uT", pr)])
                def s5_stream(pr):
                    t = s5[pr]
                    k = lambda n, pr=pr: ("s5", "sh" if n in ("bre", "bim", "t1", "t2", "zre", "zim", "xre", "xim", "tab", "roww_scratch") else pr, n)
                    P.op("pe", lambda t=t, pr=pr: T.matmul(B[5][:], lhsT=t["bbr"][:], rhs=uT[pr][:], start=True, stop=True),
                         reads=[("uT", pr), k("roww")], writes=["b5"])
                    P.op("pe", lambda t=t, pr=pr: T.matmul(B[6][:], lhsT=t["bbi"][:], rhs=uT[pr][:], start=True, stop=True),
                         reads=[("uT", pr), k("roww")], writes=["b6"])
                    P.op("act", lambda t=t: A.copy(out=t["bre"][:], in_=B[5][:]), reads=["b5"], writes=[k("bre")])
                    P.op("act", lambda t=t: A.copy(out=t["bim"][:], in_=B[6][:]), reads=["b6"], writes=[k("bim")])
                    P.op("dve", lambda t=t: V.tensor_tensor(out=t["t1"][:], in0=t["bre"][:], in1=t["CS"][:], op=ALU.mult), reads=[k("bre"), k("tab")], writes=[k("t1")])
                    P.op("pool", lambda t=t: G.tensor_tensor(out=t["t2"][:], in0=t["bim"][:], in1=t["SN"][:], op=ALU.mult), reads=[k("bim"), k("tab")], writes=[k("t2")])
                    P.op("dve", lambda t=t: V.tensor_tensor(out=t["t1"][:], in0=t["t1"][:], in1=t["t2"][:], op=ALU.add), reads=[k("t1"), k("t2")], writes=[k("t1")])
                    P.op("pool", lambda t=t: G.tensor_tensor(out=t["t2"][:], in0=t["bim"][:], in1=t["CS"][:], op=ALU.mult), reads=[k("bim"), k("tab"), k("t1")], writes=[k("t2")])
                    P.op("pool", lambda t=t: G.tensor_tensor(out=t["bre"][:], in0=t["bre"][:], in1=t["SN"][:], op=ALU.mult), reads=[k("bre"), k("tab"), k("t1")], writes=[k("bre")])
                    P.op("pool", lambda t=t: G.tensor_tensor(out=t["t2"][:], in0=t["t2"][:], in1=t["bre"][:], op=ALU.subtract), reads=[k("t2"), k("bre")], writes=[k("t2")])
                    P.op("dve", lambda t=t: V.tensor_tensor_scan(out=t["zre"][:], data0=t["RHO"][:], data1=t["t1"][:], initial=t["zi"][:, 0:1],
                                                                  op0=ALU.mult, op1=ALU.add), reads=[k("t1"), k("rho"), k("zi")], writes=[k("zre")])
                    P.op("dve", lambda t=t: V.tensor_tensor_scan(out=t["zim"][:], data0=t["RHO"][:], data1=t["t2"][:], initial=t["zi"][:, 1:2],
                                                                  op0=ALU.mult, op1=ALU.add), reads=[k("t2"), k("rho"), k("zi")], writes=[k("zim")])
                    cdt, cth, crho, ca, ctmp, c512s, c512c, cx = [t["cw"][i][:] for i in range(8)]
                    zl_re, zl_im = t["zre"][:, 511:512], t["zim"][:, 511:512]
                    P.op("dve", lambda t=t, zl_re=zl_re: V.tensor_tensor(out=t["zt"][:, 0:1], in0=zl_re, in1=c512c, op=ALU.mult), reads=[k("zre"), k("colw")], writes=[k("zt")])
                    P.op("dve", lambda t=t, zl_im=zl_im: V.tensor_tensor(out=t["zt"][:, 1:2], in0=zl_im, in1=c512s, op=ALU.mult), reads=[k("zim"), k("colw")], writes=[k("zt")])
                    P.op("dve", lambda t=t: V.tensor_tensor(out=t["zi"][:, 0:1], in0=t["zt"][:, 0:1], in1=t["zt"][:, 1:2], op=ALU.subtract), reads=[k("zt"), k("zi")], writes=[k("zi")])
                    P.op("dve", lambda t=t, zl_re=zl_re: V.tensor_tensor(out=t["zt"][:, 0:1], in0=zl_re, in1=c512s, op=ALU.mult), reads=[k("zre"), k("colw"), k("zi")], writes=[k("zt")])
                    P.op("dve", lambda t=t, zl_im=zl_im: V.tensor_tensor(out=t["zt"][:, 1:2], in0=zl_im, in1=c512c, op=ALU.mult), reads=[k("zim"), k("colw")], writes=[k("zt")])
                    P.op("dve", lambda t=t: V.tensor_tensor(out=t["zi"][:, 1:2], in0=t["zt"][:, 0:1], in1=t["zt"][:, 1:2], op=ALU.add), reads=[k("zt"), k("zi")], writes=[k("zi")])
                    P.op("dve", lambda t=t: V.tensor_tensor(out=t["xre"][:], in0=t["zre"][:], in1=t["CS"][:], op=ALU.mult), reads=[k("zre"), k("tab")], writes=[k("xre")])
                    P.op("pool", lambda t=t: G.tensor_tensor(out=t["t1"][:], in0=t["zim"][:], in1=t["SN"][:], op=ALU.mult), reads=[k("zim"), k("tab"), k("zre")], writes=[k("t1")])
                    P.op("dve", lambda t=t: V.tensor_tensor(out=t["xre"][:], in0=t["xre"][:], in1=t["t1"][:], op=ALU.subtract), reads=[k("xre"), k("t1")], writes=[k("xre")])
                    P.op("pool", lambda t=t: G.tensor_tensor(out=t["xim"][:], in0=t["zre"][:], in1=t["SN"][:], op=ALU.mult), reads=[k("zre"), k("tab")], writes=[k("xim")])
                    P.op("pool", lambda t=t: G.tensor_tensor(out=t["t2"][:], in0=t["zim"][:], in1=t["CS"][:], op=ALU.mult), reads=[k("zim"), k("tab"), k("zim")], writes=[k("t2")])
                    P.op("pool", lambda t=t: G.tensor_tensor(out=t["xim"][:], in0=t["xim"][:], in1=t["t2"][:], op=ALU.add), reads=[k("xim"), k("t2")], writes=[k("xim")])

                    yb_ = 7 if pr == 0 else 3

                    def mmy(t=t, pr=pr, yb_=yb_):
                        T.matmul(B[yb_][0:32, :], lhsT=t["CrBD"][:], rhs=t["xre"][:], start=True, stop=False)
                        return T.matmul(B[yb_][0:32, :], lhsT=t["CiBD"][:], rhs=t["xim"][:], start=False, stop=True)
                    P.op("pe", mmy, reads=[k("xre"), k("xim"), k("CrBD"), k("CiBD")], writes=[f"b{yb_}"])
                    P.op("dve", lambda pr=pr, yb_=yb_: V.scalar_tensor_tensor(out=yT[pr][:], in0=uT[pr][:], scalar=d_col[pr][:, 0:1], in1=B[yb_][0:32, :],
                                                                             op0=ALU.mult, op1=ALU.add),
                         reads=[f"b{yb_}", ("uT", pr), ("dcol", pr)], writes=[("yT", pr)])
                for pr_ in range(2):
                    s5_stream(pr_)
                pvy = B[4][:, 0:256].rearrange("p (j d) -> p j d", j=4)

                def try4(pvy=pvy):
                    for pr in range(2):
                        for j in range(4):
                            ins = T.transpose(out=pvy[:, j, pr * 32:(pr + 1) * 32], in_=yT[pr][:, j * 128:(j + 1) * 128], identity=ident32[0:32, 0:32])
                    return ins
                P.op("pe", try4, reads=[("yT", 0), ("yT", 1), "ident32"], writes=["b4"])
                P.op("act", lambda pvy=pvy, hs=hs: A.copy(out=yc_tm[hs][:], in_=pvy), reads=["b4"], writes=[("yc_tm", hs)])
                dma(P, "poolq", yc_o[cols, :].rearrange("(j p) d -> p j d", p=128), yc_tm[hs][:], reads=[("yc_tm", hs)], writes=[("yc_o", st)])
                outs.append(("yc_o", st))
            if do_gdn:
                gdn_supertile(P, nc, B, st, hs, hk, hTt, wg_t, wt_t, cvw_t, xraw, cvt, qkv, sqn, rn_, Bones, eps_rms, alog_t, dtb_t, ng_t,
                              Tri, SL, Mask2, ones64, ident32, Sst, gd, ybuf, yb_o, outs)

        if do_gdn:
            gdn_round(P, gd, [], yb_o, outs)
        if do_attn:
            scale = 32 ** -0.5
            allqk = [("QT", s_) for s_ in range(nst)] + [("KT", s_) for s_ in range(nst)] + [("V", s_, p_) for s_ in range(nst) for p_ in range(2)] + ["Vones"]
            cnt = 0
            for u in range(3):
                for qt in range(nst):
                    nkb = 4 * (qt + 1)
                    bo = 3 + (qt % 2)
                    pend = []

                    def issue_s(kb, u=u, qt=qt):
                        nonlocal cnt
                        slot = SBANKS[cnt % len(SBANKS)]
                        ps_ = cnt % NPT
                        cnt += 1
                        P.op("pe", lambda: T.matmul(B[slot][:], lhsT=KT[32 * u:32 * u + 32, kb * 128:(kb + 1) * 128],
                                                    rhs=QT[32 * u:32 * u + 32, qt * 512:(qt + 1) * 512], start=True, stop=True),
                             reads=allqk, writes=[f"b{slot}"])
                        P.op("act", lambda: A.activation(out=PT[ps_][:], in_=B[slot][:], func=AF.Exp, scale=scale),
                             reads=[f"b{slot}"], writes=[("PT", ps_)])
                        if kb >= 4 * qt:
                            j = kb - 4 * qt
                            P.op("pool", lambda: G.tensor_tensor(out=PT[ps_][:], in0=PT[ps_][:], in1=am_t[:, j, :], op=ALU.mult),
                                 reads=[("PT", ps_), "amask"], writes=[("PT", ps_)])
                        return ps_

                    def issue_av(kb, ps_, u=u, bo=bo, nkb=nkb):
                        P.op("pe", lambda: T.matmul(B[bo][0:65, :], lhsT=Vall[:, kb, u, :], rhs=PT[ps_][:], start=(kb == 0), stop=(kb == nkb - 1)),
                             reads=[("PT", ps_)] + allqk, writes=[f"b{bo}"])
                    for kb in range(nkb):
                        pend.append((kb, issue_s(kb)))
                        if len(pend) > ADEPTH:
                            issue_av(*pend.pop(0))
                    while pend:
                        issue_av(*pend.pop(0))
                    P.op("act", lambda bo=bo: A.copy(out=osb[:], in_=B[bo][0:65, :]), reads=[f"b{bo}"], writes=["osb"])
                    pvo = B[5][:, 0:260].rearrange("p (j d) -> p j d", j=4)

                    def tro(pvo=pvo):
                        for j in range(4):
                            ins = T.transpose(out=pvo[:, j, :], in_=osb[:, j * 128:(j + 1) * 128], identity=ident32[0:65, 0:65])
                        return ins
                    P.op("pe", tro, reads=["osb", "ident32"], writes=["b5"])
                    P.op("dve", lambda pvo=pvo: V.reciprocal(out=rec[:], in_=pvo[:, :, 64]), reads=["b5"], writes=["rec"])
                    os_ = qt % 2
                    P.op("dve", lambda pvo=pvo, os_=os_: V.tensor_tensor(out=oT[os_][:], in0=pvo[:, :, 0:64],
                                                                        in1=rec[:].unsqueeze(2).to_broadcast([128, 4, 64]), op=ALU.mult),
                         reads=["b5", "rec"], writes=[("oT", os_)])
                    dma(P, "sp", ya_o[qt * 512:(qt + 1) * 512, u, :].rearrange("(j p) d -> p j d", p=128), oT[os_][:],
                        reads=[("oT", os_)], writes=[("ya_o", u, qt)])
                    outs.append(("ya_o", u, qt))
        P.emit(final_wait_keys=outs)


def gdn_supertile(P, nc, B, st, hs, hk, hTt, wg_t, wt_t, cvw_t, xraw, cvt, qkv, sqn, rn_, Bones, eps_rms, nA_t, dtb_t, ng_t,
                  Tri, SL, Mask2, ones64, ident32, Sst, gd, ybuf, yb_o, outs):
    V, G, A, T = nc.vector, nc.gpsimd, nc.scalar, nc.tensor
    for c in range(3):
        def mm(c=c):
            for kc in range(8):
                ins = T.matmul(B[c][:], lhsT=wg_t[:, kc, c * 128:(c + 1) * 128], rhs=hTt[hs][:, kc, :], start=(kc == 0), stop=(kc == 7))
            return ins
        P.op("pe", mm, reads=[hk, "wg"], writes=[f"b{c}"])
        P.op("pool", lambda c=c: G.tensor_copy(out=xraw[c][:, 0:3], in_=xraw[c][:, 512:515]), reads=[("xraw", c)], writes=[("xraw", c)])
        P.op("act", lambda c=c: A.copy(out=xraw[c][:, 3:515], in_=B[c][:]), reads=[f"b{c}", ("xraw", c)], writes=[("xraw", c)])
        P.op("dve", lambda c=c: V.tensor_scalar(out=cvt[:], in0=xraw[c][:, 0:512], scalar1=cvw_t[:, c, 0:1], scalar2=None, op0=ALU.mult),
             reads=[("xraw", c), "cvw"], writes=["cvt"])
        for kk in range(1, 4):
            P.op("dve", lambda c=c, kk=kk: V.scalar_tensor_tensor(out=cvt[:], in0=xraw[c][:, kk:kk + 512], scalar=cvw_t[:, c, kk:kk + 1],
                                                                  in1=cvt[:], op0=ALU.mult, op1=ALU.add),
                 reads=[("xraw", c), "cvw", "cvt"], writes=["cvt"])
        P.op("act", lambda c=c: A.activation(out=qkv[c][:], in_=cvt[:], func=AF.Silu), reads=["cvt"], writes=[("qkv", c)])
    for c in range(2):
        P.op("pool", lambda c=c: G.tensor_tensor(out=sqn[:], in0=qkv[c][:], in1=qkv[c][:], op=ALU.mult), reads=[("qkv", c)], writes=["sqn"])
        P.op("pe", lambda: T.matmul(B[3][:], lhsT=Bones[:], rhs=sqn[:], start=True, stop=True), reads=["sqn", "Bones"], writes=["b3"])
        P.op("act", lambda: A.activation(out=rn_[:], in_=B[3][:], func=AF.Sqrt, bias=eps_rms[:, 0:1], scale=1.0), reads=["b3", "eps_rms"], writes=["rn"])
        P.op("dve", lambda: V.reciprocal(out=rn_[:], in_=rn_[:]), reads=["rn"], writes=["rn"])
        if c == 0:
            P.op("dve", lambda: V.scalar_tensor_tensor(out=qkv[0][:], in0=qkv[0][:], scalar=0.125, in1=rn_[:], op0=ALU.mult, op1=ALU.mult),
                 reads=[("qkv", 0), "rn"], writes=[("qkv", 0)])
        else:
            P.op("dve", lambda: V.tensor_tensor(out=qkv[1][:], in0=qkv[1][:], in1=rn_[:], op=ALU.mult), reads=[("qkv", 1), "rn"], writes=[("qkv", 1)])
    qk_all = [("qkv", 0), ("qkv", 1), ("qkv", 2)]
    yb_s = st % 2
    for cp in range(4):
        new = []
        for c in (2 * cp, 2 * cp + 1):
            cg = st * 8 + c
            cs = slice(c * 64, (c + 1) * 64)
            dch = gd["ch"][cg % 4]
            kch = lambda n, cg=cg: ("gch", cg % 4, n)

            def mmt(cs=cs):
                for kc in range(8):
                    ins = T.matmul(B[0][0:64, 0:132], lhsT=hTt[hs][:, kc, cs], rhs=wt_t[:, kc, :], start=(kc == 0), stop=(kc == 7))
                return ins
            mk = ["b0"]
            P.op("pe", mmt, reads=[hk, "wt"], writes=mk)
            P.op("act", lambda dch=dch: A.activation(out=dch["gs"][:], in_=B[0][0:64, 0:128], func=AF.Silu), reads=mk, writes=[kch("gs")])
            P.op("act", lambda dch=dch: A.activation(out=dch["bg"][:, 0:2], in_=B[0][0:64, 128:130], func=AF.Sigmoid), reads=mk, writes=[kch("bg")])
            P.op("dve", lambda dch=dch: V.tensor_tensor(out=dch["bg"][:, 2:4], in0=B[0][0:64, 130:132], in1=dtb_t[:], op=ALU.add),
                 reads=mk + ["dtb", kch("bg")], writes=[kch("bg")])
            P.op("act", lambda dch=dch: A.activation(out=dch["bg"][:, 2:4], in_=dch["bg"][:, 2:4], func=AF.Exp), reads=[kch("bg")], writes=[kch("bg")])
            P.op("act", lambda dch=dch: A.activation(out=dch["bg"][:, 2:4], in_=dch["bg"][:, 2:4], func=AF.Ln, bias=1.0, scale=1.0), reads=[kch("bg")], writes=[kch("bg")])
            P.op("dve", lambda dch=dch: V.tensor_tensor(out=dch["bg"][:, 2:4], in0=dch["bg"][:, 2:4], in1=nA_t[:], op=ALU.mult),
                 reads=[kch("bg"), "alog"], writes=[kch("bg")])
            P.op("dve", lambda dch=dch: V.tensor_scalar(out=dch["nbeta"][:], in0=dch["bg"][:, 0:2], scalar1=-1.0, scalar2=None, op0=ALU.mult),
                 reads=[kch("bg")], writes=[kch("nbeta")])
            sl0 = (cg % 4) * 2
            new.append([gdn_chunk_head(P, nc, B, h, cs, c, dch, kch, gd["hd"][sl0 + h], sl0 + h, 4 + (cg % 2) * 2 + h, qkv, qk_all, ng_t, Tri, SL, Mask2,
                                       ones64, ident32, Sst, eps_rms, ybuf[yb_s], yb_s) for h in range(2)])
        gdn_round(P, gd, new, yb_o, outs)
        if cp == 3:
            gd["pend_dma"] = (st, yb_s, ybuf[yb_s])


def gdn_round(P, gd, new, yb_o, outs):
    oldg = list(gd.get("pendB", []))
    had_old = bool(oldg)
    actA = [g for grp in new for g in grp]
    curB = oldg.pop(0) if oldg else []
    while actA or curB:
        for g in list(actA):
            try:
                r = next(g)
            except StopIteration:
                raise RuntimeError("chain ended inside stage A")
            if r == "END_A":
                actA.remove(g)
        for g in list(curB):
            try:
                next(g)
            except StopIteration:
                curB.remove(g)
        if not curB and oldg:
            curB = oldg.pop(0)
    gd["pendB"] = [list(grp) for grp in new]
    pd = gd.get("pend_dma")
    if pd is not None and had_old:
        st, yb_s, ybt = pd
        cols = slice(st * 512, (st + 1) * 512)
        dma(P, "poolq", yb_o[cols].rearrange("(c p) h d -> p c h d", p=64), ybt[:], reads=[("ybuf", yb_s, c_, h_) for c_ in range(8) for h_ in range(2)],
            writes=[("yb_o", st)])
        outs.append(("yb_o", st))
        gd["pend_dma"] = None


def gdn_chunk_head(P, nc, B, h, cs, c, dch, kch, d, sl, bank, qkv, qk_all, ng_t, Tri, SL, Mask2, ones64, ident32, Sst, eps_rms, ybuf, yb_s):
    V, G, A, T = nc.vector, nc.gpsimd, nc.scalar, nc.tensor
    hp = slice(h * 64, (h + 1) * 64)
    idh = ident32[hp, hp]
    id0 = ident32[0:64, 0:64]
    k = lambda n: ("ghd", sl, n)
    PA, P3 = B[bank], B[3]
    bk, b3 = [f"b{bank}"], ["b3"]
    o3 = 256 * h
    W = d["W"]
    g_col = dch["bg"][:, 2 + h:3 + h]
    beta_col = dch["bg"][:, h:h + 1]
    nbeta_col = dch["nbeta"][:, h:h + 1]

    def tr1():
        for c3 in range(3):
            ins = T.transpose(out=PA[0:64, c3 * 64:(c3 + 1) * 64], in_=qkv[c3][hp, cs], identity=idh)
        return ins
    P.op("pe", tr1, reads=qk_all + ["ident32"], writes=bk); yield
    P.op("act", lambda: A.copy(out=d["qkv_tm"][:].rearrange("p a b -> p (a b)"), in_=PA[0:64, 0:192]), reads=bk, writes=[k("qkv_tm")]); yield

    def mm2():
        T.matmul(PA[0:64, 256:257], lhsT=Tri[:], rhs=g_col, start=True, stop=True)
        return T.matmul(PA[0:64, 257:258], lhsT=ones64[:], rhs=g_col, start=True, stop=True)
    P.op("pe", mm2, reads=[kch("bg"), "Tri", "ones64"], writes=bk); yield
    P.op("dve", lambda: V.tensor_copy(out=d["gcl"][:], in_=PA[0:64, 256:258]), reads=bk, writes=[k("gcl")]); yield
    P.op("act", lambda: A.activation(out=d["ex3"][:, 0:1], in_=d["gcl"][:, 0:1], func=AF.Exp), reads=[k("gcl")], writes=[k("ex3")]); yield
    P.op("act", lambda: A.activation(out=d["ex3"][:, 1:2], in_=d["gcl"][:, 0:1], func=AF.Exp, bias=d["gcl"][:, 1:2], scale=-1.0),
         reads=[k("gcl"), k("ex3")], writes=[k("ex3")]); yield
    P.op("act", lambda: A.activation(out=d["ex3"][:, 2:3], in_=d["gcl"][:, 1:2], func=AF.Exp), reads=[k("gcl"), k("ex3")], writes=[k("ex3")]); yield
    P.op("dve", lambda: V.tensor_scalar(out=d["Gm"][:], in0=Tri[:], scalar1=g_col, scalar2=None, op0=ALU.mult), reads=["Tri", kch("bg")], writes=[k("Gm")]); yield

    def mm3():
        T.matmul(PA[0:64, 384:448], lhsT=d["Gm"][:], rhs=SL[:], start=True, stop=True)
        return T.matmul(PA[0:64, 448:512], lhsT=SL[:], rhs=d["Gm"][:], start=True, stop=True)
    P.op("pe", mm3, reads=[k("Gm"), "SL"], writes=bk); yield
    P.op("act", lambda: A.activation(out=d["EE"][:].rearrange("p a b -> p (a b)"), in_=PA[0:64, 384:512], func=AF.Exp), reads=bk, writes=[k("EE")]); yield
    P.op("pool", lambda: G.tensor_tensor(out=d["EE"][:], in0=d["EE"][:], in1=Mask2[:], op=ALU.mult), reads=[k("EE"), "Mask2"], writes=[k("EE")]); yield

    def mm4():
        T.matmul(PA[0:64, 0:64], lhsT=qkv[1][hp, cs], rhs=qkv[1][hp, cs], start=True, stop=True)
        return T.matmul(PA[0:64, 64:128], lhsT=qkv[1][hp, cs], rhs=qkv[0][hp, cs], start=True, stop=True)
    P.op("pe", mm4, reads=qk_all, writes=bk); yield
    P.op("dve", lambda: V.scalar_tensor_tensor(out=W[0][:, 128:192], in0=PA[0:64, 0:64], scalar=nbeta_col, in1=d["EE"][:, 0, :], op0=ALU.mult, op1=ALU.mult),
         reads=bk + [kch("nbeta"), k("EE")], writes=[k("W0p")]); yield
    P.op("dve", lambda: V.tensor_tensor(out=d["AT"][:], in0=PA[0:64, 64:128], in1=d["EE"][:, 1, :], op=ALU.mult), reads=bk + [k("EE")], writes=[k("AT")]); yield
    P.op("pe", lambda: T.transpose(out=PA[0:64, 192:256], in_=W[0][:, 128:192], identity=id0), reads=[k("W0p"), "ident32"], writes=bk); yield
    P.op("act", lambda: A.copy(out=W[0][:, 192:256], in_=PA[0:64, 192:256]), reads=bk, writes=[k("W0t")]); yield
    P.op("dve", lambda: V.tensor_tensor(out=d["tb"][:], in0=beta_col, in1=d["ex3"][:, 0:1], op=ALU.mult), reads=[kch("bg"), k("ex3")], writes=[k("tb")]); yield
    P.op("dve", lambda: V.tensor_scalar(out=W[0][:, 0:64], in0=d["qkv_tm"][:, 2, :], scalar1=beta_col, scalar2=None, op0=ALU.mult),
         reads=[k("qkv_tm"), kch("bg")], writes=[k("W0x")]); yield
    P.op("dve", lambda: V.tensor_scalar(out=W[0][:, 64:128], in0=d["qkv_tm"][:, 1, :], scalar1=d["tb"][:, 0:1], scalar2=None, op0=ALU.mult),
         reads=[k("qkv_tm"), k("tb"), k("W0x")], writes=[k("W0x")]); yield
    wk = [[k("W0x"), k("W0p"), k("W0t")], [k("W1")]]
    for lvl in range(6):
        s_, d_ = W[lvl % 2], W[(lvl + 1) % 2]
        last = lvl == 5

        def mml(s_=s_, last=last):
            T.matmul(PA[0:64, 256:384], lhsT=s_[:, 192:256], rhs=s_[:, 0:128], start=True, stop=False)
            ins = T.matmul(PA[0:64, 256:384], lhsT=id0, rhs=s_[:, 0:128], start=False, stop=True)
            if not last:
                T.matmul(PA[0:64, 384:448], lhsT=s_[:, 192:256], rhs=s_[:, 128:192], start=True, stop=True)
                ins = T.matmul(PA[0:64, 448:512], lhsT=s_[:, 128:192], rhs=s_[:, 192:256], start=True, stop=True)
            return ins
        P.op("pe", mml, reads=wk[lvl % 2] + ["ident32"], writes=bk); yield
        n_ = 128 if last else 256
        wkeys = [k("W1")] if (lvl + 1) % 2 == 1 else [k("W0x"), k("W0p"), k("W0t")]
        if lvl % 2 == 0:
            P.op("act", lambda d_=d_, n_=n_: A.copy(out=d_[:, 0:n_], in_=PA[0:64, 256:256 + n_]), reads=bk, writes=wkeys); yield
        else:
            P.op("dve", lambda d_=d_, n_=n_: V.tensor_copy(out=d_[:, 0:n_], in_=PA[0:64, 256:256 + n_]), reads=bk, writes=wkeys); yield
    X = W[0]
    xk = [k("W0x"), k("W0p"), k("W0t")]
    P.op("pool", lambda: G.tensor_scalar(out=d["kdec"][:], in0=d["qkv_tm"][:, 1, :], scalar1=d["ex3"][:, 1:2], scalar2=None, op0=ALU.mult),
         reads=[k("qkv_tm"), k("ex3")], writes=[k("kdec")]); yield
    P.op("pool", lambda: G.tensor_scalar(out=d["qdec"][:], in0=d["qkv_tm"][:, 0, :], scalar1=d["ex3"][:, 0:1], scalar2=None, op0=ALU.mult),
         reads=[k("qkv_tm"), k("ex3")], writes=[k("qdec")]); yield

    def tr8():
        T.transpose(out=PA[0:64, 0:64], in_=X[:, 64:128], identity=id0)
        return T.transpose(out=PA[0:64, 64:128], in_=d["qdec"][:], identity=id0)
    P.op("pe", tr8, reads=xk + [k("qdec"), "ident32"], writes=bk); yield
    P.op("act", lambda: A.copy(out=d["wqT"][:].rearrange("p a b -> p (a b)"), in_=PA[0:64, 0:128]), reads=bk, writes=[k("wqT")]); yield
    P.op("pool", lambda: G.tensor_tensor(out=d["ngate"][:], in0=dch["gs"][:, h * 64:(h + 1) * 64], in1=ng_t[:], op=ALU.mult),
         reads=[kch("gs"), "ngt"], writes=[k("ngate")]); yield
    yield "END_A"
    S_ = Sst[h]
    P.op("pe", lambda: T.matmul(P3[0:64, o3:o3 + 64], lhsT=d["wqT"][:, 0, :], rhs=S_[:], start=True, stop=True), reads=[k("wqT"), ("S", h)], writes=b3); yield
    P.op("dve", lambda: V.tensor_tensor(out=d["vnew"][:], in0=X[:, 0:64], in1=P3[0:64, o3:o3 + 64], op=ALU.subtract),
         reads=b3 + xk, writes=[k("vnew")]); yield

    def mmo():
        T.matmul(P3[0:64, o3 + 64:o3 + 128], lhsT=d["wqT"][:, 1, :], rhs=S_[:], start=True, stop=False)
        T.matmul(P3[0:64, o3 + 64:o3 + 128], lhsT=d["AT"][:], rhs=d["vnew"][:], start=False, stop=True)
        return T.matmul(P3[0:64, o3 + 128:o3 + 192], lhsT=d["kdec"][:], rhs=d["vnew"][:], start=True, stop=True)
    P.op("pe", mmo, reads=[k("wqT"), ("S", h), k("AT"), k("vnew"), k("kdec")], writes=b3); yield
    P.op("dve", lambda: V.scalar_tensor_tensor(out=S_[:], in0=S_[:], scalar=d["ex3"][:, 2:3], in1=P3[0:64, o3 + 128:o3 + 192], op0=ALU.mult, op1=ALU.add),
         reads=b3 + [("S", h), k("ex3")], writes=[("S", h)]); yield
    P.op("act", lambda: A.copy(out=d["osb"][:], in_=P3[0:64, o3 + 64:o3 + 128]), reads=b3, writes=[k("osb")]); yield
    P.op("pool", lambda: G.tensor_tensor(out=d["osq"][:], in0=d["osb"][:], in1=d["osb"][:], op=ALU.mult), reads=[k("osb")], writes=[k("osq")]); yield
    P.op("dve", lambda: V.tensor_reduce(out=d["oss"][:], in_=d["osq"][:], axis=AX.X, op=ALU.add), reads=[k("osq")], writes=[k("oss")]); yield
    P.op("act", lambda: A.activation(out=d["oss"][:], in_=d["oss"][:], func=AF.Sqrt, bias=eps_rms[0:64, 0:1], scale=1.0 / 64),
         reads=[k("oss"), "eps_rms"], writes=[k("oss")]); yield
    P.op("dve", lambda: V.reciprocal(out=d["oss"][:], in_=d["oss"][:]), reads=[k("oss")], writes=[k("oss")]); yield
    P.op("dve", lambda: V.scalar_tensor_tensor(out=ybuf[:, c, h, :], in0=d["osb"][:], scalar=d["oss"][:, 0:1], in1=d["ngate"][:], op0=ALU.mult, op1=ALU.mult),
         reads=[k("osb"), k("oss"), k("ngate")], writes=[("ybuf", yb_s, c, h)]); yield


OFF_AQ, OFF_AK, OFF_AV, OFF_BQKV, OFF_BGATE, OFF_BBETA, OFF_BA, OFF_CU = 0, 384, 768, 1152, 2304, 2688, 2694, 2700
GDN_HEADS_OF = [(0, 1), (2, 3), (4, 5), (4, 5)]


def _wl(w, cols):
    return w[:, cols].reshape(8, 128, len(cols)).transpose(1, 0, 2)


def mix_consts():
    d = {}
    k = np.arange(128)[:, None, None]; j = np.arange(4)[None, :, None]; q = np.arange(512)[None, None, :]
    d["amask"] = bf((q // 64 >= (j * 128 + k) // 64).astype(np.float32))
    d["idn32"] = f32c(np.eye(128))
    d["srow"] = f32c(np.arange(512).reshape(1, 512))
    m = np.arange(64)[:, None]; i = np.arange(64)[None, :]
    d["cTri"] = f32c(m <= i)
    d["cSL"] = f32c(m > i)
    d["cMask2"] = f32c(np.stack([(m > i), (m <= i)], axis=1))
    bo = np.zeros((128, 128), np.float32); bo[:64, :64] = 1; bo[64:, 64:] = 1
    d["cBones"] = bo
    return d


def mix_inputs(l, p, j):
    w = p["w_in"][l]
    d = {}
    units = [3 * j + i for i in range(3)]
    qc, kc_, vc = [], [], []
    for u in units:
        head, mp = u // 2, u % 2
        qc += list(range(OFF_AQ + head * 64 + mp * 32, OFF_AQ + head * 64 + mp * 32 + 32))
        kc_ += list(range(OFF_AK + head * 64 + mp * 32, OFF_AK + head * 64 + mp * 32 + 32))
        vc += list(range(OFF_AV + head * 64, OFF_AV + head * 64 + 64))
    d["wq"] = bf(_wl(w, qc)); d["wk"] = bf(_wl(w, kc_)); d["wv"] = bf(_wl(w, vc))
    gs = [4 * j + i for i in range(4)]
    d["wu"] = bf(_wl(w, list(range(OFF_CU + gs[0] * 16, OFF_CU + gs[0] * 16 + 64))))
    lre, lim, ldt = p["s5_lambda_re"][l], p["s5_lambda_im"][l], p["s5_log_dt"][l]
    row = np.zeros((2, 3, 128), np.float32)
    bT = np.zeros((2, 2, 2, 16, 64), np.float32); cT = np.zeros((2, 2, 2, 64, 16), np.float32)
    for pr in range(2):
        for g in range(2):
            G_ = gs[pr * 2 + g]
            row[pr, 0, g * 64:(g + 1) * 64] = lre[G_]; row[pr, 1, g * 64:(g + 1) * 64] = lim[G_]; row[pr, 2, g * 64:(g + 1) * 64] = ldt[G_]
            bT[pr, g, 0] = p["s5_b_re"][l][G_].T; bT[pr, g, 1] = p["s5_b_im"][l][G_].T
            cT[pr, g, 0] = p["s5_c_re"][l][G_].T; cT[pr, g, 1] = p["s5_c_im"][l][G_].T
    d["s5row"] = row; d["s5col"] = f32c(row.transpose(0, 2, 1)); d["s5bT"] = bT; d["s5cT"] = cT
    d["s5d"] = f32c(p["s5_d"][l][gs[0] * 16:gs[0] * 16 + 64].reshape(64, 1))
    hA, hB = GDN_HEADS_OF[j]
    gcols = []
    for part in range(3):
        for h in (hA, hB):
            gcols += list(range(OFF_BQKV + part * 384 + h * 64, OFF_BQKV + part * 384 + h * 64 + 64))
    d["wg"] = bf(_wl(w, gcols))
    tcols = list(range(OFF_BGATE + hA * 64, OFF_BGATE + hA * 64 + 64)) + list(range(OFF_BGATE + hB * 64, OFF_BGATE + hB * 64 + 64)) \
        + [OFF_BBETA + hA, OFF_BBETA + hB, OFF_BA + hA, OFF_BA + hB]
    d["wt"] = bf(_wl(w, tcols))
    cw = p["dn_conv_w"][l]
    cv = np.zeros((128, 3, 4), np.float32)
    for part in range(3):
        for hi, h in enumerate((hA, hB)):
            cv[hi * 64:(hi + 1) * 64, part, :] = cw[:, part * 384 + h * 64: part * 384 + h * 64 + 64].T
    d["cvw"] = cv
    d["galog"] = f32c(p["dn_a_log"][l][[hA, hB]].reshape(1, 2)); d["gdtb"] = f32c(p["dn_dt_bias"][l][[hA, hB]].reshape(1, 2))
    d["gng"] = f32c(p["dn_norm_g"][l].reshape(1, 64))
    return d


YCH = 512
NYCH = SEQ // YCH


def build_program(stop=None):
    nc = bass.Bass("TRN2", target_bir_lowering=False)
    G = Glob(nc)
    hbuf = [G.internal(f"hbuf{i}", [TOK_CORE, D]) for i in range(2)]
    hT_loc = G.internal("hT_loc", [D, TOK_CORE], BF16)
    hT_all = G.internal("hT_all", [8, 4 * 128, TOK_CORE], BF16)
    y_o = G.internal("y_o", [SEQ, 384])
    y_all = G.internal("y_all", [NYCH, 4 * YCH, 384])
    ag_h = [(hT_loc[k * 128:(k + 1) * 128, :], hT_all[k]) for k in range(8)]
    ag_y = [(y_o[i * YCH:(i + 1) * YCH, :], y_all[i]) for i in range(NYCH)]
    out = nc.dram_tensor("out", [TOK_CORE, D], F32, kind="ExternalOutput").ap()

    def finish_early(src_ap):
        with nc.semaphore("fin") as fs:
            with nc.Block() as block:
                @block.gpsimd
                def _(g):
                    g.sem_clear(fs)
            with nc.Block() as block:
                @block.sync
                def _(sp):
                    sp.dma_start(out=out, in_=src_ap).then_inc(fs, 16)
                    sp.wait_ge(fs, 16)
        return nc, G
    phase_pre(nc, G, hbuf[0], hT_loc)
    for l in range(DEPTH):
        last = l == DEPTH - 1
        allgather(nc, ag_h)
        if stop == (l, "ag1"):
            return finish_early(hbuf[0])
        phase_mix(nc, G, l, hT_all, y_o, do_attn=True, do_s5=False, do_gdn=False)
        if stop == (l, "mixa"):
            return finish_early(hbuf[0])
        phase_mix(nc, G, l, hT_all, y_o, do_attn=False, do_s5=True, do_gdn=True)
        if stop == (l, "mixb"):
            return finish_early(hbuf[0])
        allgather(nc, ag_y)
        if stop == (l, "ag2"):
            return finish_early(hbuf[0])
        phase_post(nc, G, l, hbuf[l % 2], out if last else hbuf[(l + 1) % 2], None if last else hT_loc, y_all)
        if stop == (l, "post"):
            return finish_early(hbuf[(l + 1) % 2])
    return nc, G


def kernel(_stop=None, **inputs):
    p = {k: np.asarray(v) for k, v in inputs.items()}
    x = f32c(p["x"]).reshape(BATCH * SEQ, D)
    cores = list(range(NCORES))
    nc, G = build_program(_stop)
    shared = dict(mix_consts())
    shared["idn"] = bf(np.eye(128))
    shared["g"] = f32c(p["ln_in_g"].reshape(1, D)); shared["b"] = f32c(p["ln_in_b"].reshape(1, D))
    percore = [dict() for _ in range(4)]
    for l in range(DEPTH):
        lam_init = 0.8 - 0.6 * math.exp(-0.3 * l)
        for k, v in post_inputs(l, p, lam_init).items():
            if k not in ("idn", "idn32"):
                shared[f"L{l}_{k}"] = v
        for j in range(4):
            for k, v in mix_inputs(l, p, j).items():
                percore[j][f"L{l}_{k}"] = v
    ins = []
    for c in cores:
        d = dict(shared); d.update(percore[c % 4])
        d["x"] = x[c * TOK_CORE:(c + 1) * TOK_CORE]
        d["rofs"] = np.array([[(c % 4) * (TOK_CORE // YCH)]], np.int32)
        ins.append({k: v for k, v in d.items() if k in G.t})
    res = run_bass_kernel_spmd(nc, ins, core_ids=cores)
    h = [np.asarray(r["out"]) for r in res.results]
    return np.concatenate(h, axis=0).reshape(BATCH, SEQ, D).astype(np.float32)
```

```python
import math
from contextlib import ExitStack

import numpy as np
import ml_dtypes
import concourse.bass as bass
import concourse.mybir as mybir
from concourse.bass_utils import run_bass_kernel_spmd

F32 = mybir.dt.float32
BF16 = mybir.dt.bfloat16
I32 = mybir.dt.int32
ALU = mybir.AluOpType
AF = mybir.ActivationFunctionType
AX = mybir.AxisListType

NCORES = 8


class Prog:
    COMPUTE = ("pe", "act", "dve", "pool")
    NDMASEM = 6

    _uid = 0
    _phase = 0

    def __init__(self, nc):
        Prog._phase += 1
        self.ph = Prog._phase
        self.nc = nc
        self.ops = []
        self.last_w = {}
        self.readers = {}
        self.dma_count = {"sp": 0, "actq": 0, "poolq": 0}
        self.stack = ExitStack()
        self.nt = 0
        self.excl = set()
        self.quarters = False
        self.bankkeys = {f"b{i}" for i in range(8)}
        self.pool_pre = None
        self.pool_post = None
        self.sp_wrap = None

    def sb(self, shape, dtype, name=None):
        Prog._uid += 1
        return self.stack.enter_context(self.nc.sbuf_tensor(f"{name or 't'}_{Prog._uid}", list(shape), dtype))

    def ps(self, shape, dtype=F32, name=None):
        Prog._uid += 1
        return self.stack.enter_context(self.nc.psum_tensor(f"{name or 'p'}_{Prog._uid}", list(shape), dtype))

    def op(self, eng, fn, reads=(), writes=()):
        idx = len(self.ops)
        isdma = eng in self.dma_count
        issue = {"sp": "sp", "actq": "act", "poolq": "pool"}.get(eng, eng)
        if self.quarters:
            ex = lambda ks: [q for k in ks for q in ([f"{k}q{i}" for i in range(4)] if k in self.bankkeys else [k])]
            reads, writes = ex(reads), ex(writes)
        if self.excl:
            writes = list(writes) + [k for k in reads if k in self.excl]
            reads = [k for k in reads if k not in self.excl]
        deps = set()
        for k in reads:
            w = self.last_w.get(k)
            if w is not None:
                deps.add(w)
        for k in writes:
            w = self.last_w.get(k)
            if w is not None:
                deps.add(w)
            for r in self.readers.get(k, ()):
                deps.add(r)
        o = dict(idx=idx, eng=eng, issue=issue, fn=fn, deps=deps, isdma=isdma, needed=False)
        if isdma:
            n = self.dma_count[eng]
            self.dma_count[eng] = n + 1
            o["dsem"] = n % self.NDMASEM
            o["dtarget"] = 16 * (n // self.NDMASEM + 1)
            o["dprev"] = 16 * (n // self.NDMASEM)
        self.ops.append(o)
        for k in writes:
            self.last_w[k] = idx
            self.readers[k] = []
        for k in reads:
            lst = self.readers.setdefault(k, [])
            if not isdma:
                lst[:] = [r for r in lst if self.ops[r]["isdma"] or self.ops[r]["eng"] != eng]
            lst.append(idx)
        return idx

    def emit(self, final_wait_keys=()):
        nc = self.nc
        ops = self.ops
        for o in ops:
            nd = set()
            for d in o["deps"]:
                p = ops[d]
                if (not p["isdma"]) and (not o["isdma"]) and p["eng"] == o["eng"]:
                    if o["eng"] == "pe":
                        continue
                nd.add(d)
            o["deps"] = nd
            for d in nd:
                ops[d]["needed"] = True
        final = [self.last_w[k] for k in final_wait_keys if k in self.last_w]
        for d in final:
            ops[d]["needed"] = True
        tick = {e: 0 for e in self.COMPUTE}
        for o in ops:
            if not o["isdma"] and o["needed"]:
                tick[o["eng"]] += 1
                o["tick"] = tick[o["eng"]]
        sems = {e: self.stack.enter_context(nc.semaphore(f"s_{e}_{self.ph}")) for e in self.COMPUTE}
        dsems = {q: [self.stack.enter_context(nc.semaphore(f"d_{q}{i}_{self.ph}")) for i in range(self.NDMASEM)]
                 for q in self.dma_count}
        per = {e: [] for e in ("pe", "act", "dve", "pool", "sp")}
        for o in ops:
            per[o["issue"]].append(o)
        engobj = {"pe": nc.tensor, "act": nc.scalar, "dve": nc.vector, "pool": nc.gpsimd, "sp": nc.sync}

        def run(ename, extra_final=False):
            eng = engobj[ename]
            waited = {}

            def wait_for(p):
                if p["isdma"]:
                    key = (p["eng"], p["dsem"])
                    val = p["dtarget"]
                    s = dsems[p["eng"]][p["dsem"]]
                else:
                    key = p["eng"]
                    val = p["tick"]
                    s = sems[p["eng"]]
                if waited.get(key, 0) >= val:
                    return
                waited[key] = val
                eng.wait_ge(s, val)

            for o in per[ename]:
                for d in sorted(o["deps"]):
                    wait_for(ops[d])
                if o["isdma"] and o["dprev"] > 0:
                    key = (o["eng"], o["dsem"])
                    if waited.get(key, 0) < o["dprev"]:
                        waited[key] = o["dprev"]
                        eng.wait_ge(dsems[o["eng"]][o["dsem"]], o["dprev"])
                ins = o["fn"]()
                if o["isdma"]:
                    ins.then_inc(dsems[o["eng"]][o["dsem"]], 16)
                elif o["needed"]:
                    ins.then_inc(sems[o["eng"]], 1)
            if extra_final:
                for d in final:
                    wait_for(ops[d])

        allsems = list(sems.values()) + [s for q in dsems.values() for s in q]
        with nc.Block() as block:
            @block.gpsimd
            def _(e):
                for s in allsems:
                    e.sem_clear(s)

        with nc.Block() as block:
            @block.sync
            def _(e):
                if self.sp_wrap is not None:
                    with self.sp_wrap(e):
                        run("sp", extra_final=True)
                else:
                    run("sp", extra_final=True)

            @block.tensor
            def _(e):
                run("pe")

            @block.scalar
            def _(e):
                run("act")

            @block.vector
            def _(e):
                run("dve")

            @block.gpsimd
            def _(e):
                if self.pool_pre is not None:
                    self.pool_pre(e)
                run("pool")
                if self.pool_post is not None:
                    self.pool_post(e)


D = 1024
SEQ = 16384
BATCH = 2
DEPTH = 2
TOK_CORE = 4096
ALPHA = (2 * DEPTH) ** 0.25
LN_EPS = 1e-5
RMS_EPS = 1e-6
NEXP = 16
DEXP = 512


def bf(a):
    return np.ascontiguousarray(np.asarray(a, np.float32).astype(ml_dtypes.bfloat16))


def f32c(a):
    return np.ascontiguousarray(np.asarray(a, np.float32))


class Ctx:
    def __init__(self, nc):
        self.nc = nc
        self.P = Prog(nc)
        self.banks = None

    def alloc_banks(self, quarters=False):
        self.banks = [self.P.ps([128, 512], F32, name=f"bank{i}") for i in range(8)]
        self.P.excl |= {f"b{i}" for i in range(8)}
        if quarters:
            self.P.quarters = True
            self.P.excl |= {f"b{i}q{q}" for i in range(8) for q in range(4)}


class Glob:
    def __init__(self, nc):
        self.nc = nc
        self.t = {}

    def din(self, name, shape, dt=F32):
        if name not in self.t:
            self.t[name] = self.nc.dram_tensor(name, list(shape), dt, kind="ExternalInput").ap()
        return self.t[name]

    def internal(self, name, shape, dt=F32):
        if name not in self.t:
            self.t[name] = self.nc.dram_tensor(name, list(shape), dt).ap()
        return self.t[name]


GROUPS = [[0, 1, 2, 3], [4, 5, 6, 7]]


def allgather(nc, pairs):
    Prog._uid += 1
    with nc.semaphore(f"cc_{Prog._uid}") as cc:
        with nc.Block() as block:
            @block.gpsimd
            def _(g):
                g.sem_clear(cc)
        with nc.Block() as block:
            @block.gpsimd
            def _(g):
                for src, dst in pairs:
                    g.collective_compute("AllGather", ALU.bypass, replica_groups=GROUPS, ins=[src], outs=[dst]).then_inc(cc, 1)
                g.wait_ge(cc, len(pairs))


def dma(P, q, out, in_, reads=(), writes=()):
    eng = {"sp": P.nc.sync, "actq": P.nc.scalar, "poolq": P.nc.gpsimd}[q]
    return P.op(q, lambda: eng.dma_start(out=out, in_=in_), reads=reads, writes=writes)


def layer_norm_tile(P, nc, src, srck, dst, dstk, g_t, b_t, gk, bk, scr, tag):
    st, mv, rstd, xn = scr["st"], scr["mv"], scr["rstd"], scr["xn"]

    def bs():
        nc.vector.bn_stats(out=st[:, 0, :], in_=src[:, 0:512])
        return nc.vector.bn_stats(out=st[:, 1, :], in_=src[:, 512:1024])
    P.op("dve", bs, reads=[srck], writes=[tag + "st"])
    P.op("dve", lambda: nc.vector.bn_aggr(out=mv[:], in_=st[:].rearrange("p a s -> p (a s)")),
         reads=[tag + "st"], writes=[tag + "mv"])
    P.op("act", lambda: nc.scalar.activation(out=rstd[:], in_=mv[:, 1:2], func=AF.Sqrt, bias=scr["eps_ln"][:, 0:1], scale=1.0),
         reads=[tag + "mv"], writes=[tag + "rstd"])
    P.op("dve", lambda: nc.vector.reciprocal(out=rstd[:], in_=rstd[:]), reads=[tag + "rstd"], writes=[tag + "rstd"])
    P.op("dve", lambda: nc.vector.tensor_scalar(out=xn[:], in0=src[:], scalar1=mv[:, 0:1], scalar2=rstd[:, 0:1],
                                                op0=ALU.subtract, op1=ALU.mult),
         reads=[srck, tag + "mv", tag + "rstd"], writes=[tag + "xn"])
    P.op("pool", lambda: nc.gpsimd.tensor_tensor(out=xn[:], in0=xn[:], in1=g_t[:], op=ALU.mult),
         reads=[tag + "xn", gk], writes=[tag + "xn"])
    P.op("pool", lambda: nc.gpsimd.tensor_tensor(out=dst[:], in0=xn[:], in1=b_t[:], op=ALU.add),
         reads=[tag + "xn", bk], writes=[dstk])


def ln_scratch(P, tag):
    return dict(st=P.sb([128, 2, 6], F32), mv=P.sb([128, 2], F32), rstd=P.sb([128, 1], F32),
                xn=P.sb([128, 1024], F32))


def const_col(P, nc, val, key):
    t = P.sb([128, 1], F32)
    P.op("pool", lambda: nc.gpsimd.memset(t[:], val), writes=[key])
    return t


def to_featmajor_bf16(P, nc, src, srck, hb, hbk, bank, bankk, dstT, dstk, ident, cast_eng="act"):
    if cast_eng == "act":
        P.op("act", lambda: nc.scalar.copy(out=hb[:], in_=src[:]), reads=[srck], writes=[hbk])
    else:
        P.op("pool", lambda: nc.gpsimd.tensor_copy(out=hb[:], in_=src[:]), reads=[srck], writes=[hbk])
    pv = bank[:].bitcast(BF16).rearrange("p (k t) -> p k t", k=8)

    def tr():
        for kc in range(8):
            ins = nc.tensor.transpose(out=pv[:, kc, :], in_=hb[:, kc * 128:(kc + 1) * 128], identity=ident[:])
        return ins
    P.op("pe", tr, reads=[hbk, "ident"], writes=[bankk])
    P.op("dve", lambda: nc.vector.tensor_copy(out=dstT, in_=pv), reads=[bankk], writes=[dstk])


def phase_pre(nc, G, h, hT_loc, ntok=TOK_CORE):
    C = Ctx(nc)
    P = C.P
    nt = ntok // 128
    x = G.din("x", [ntok, D])
    g = G.din("g", [1, D])
    b = G.din("b", [1, D])
    idn = G.din("idn", [128, 128], BF16)
    hT = hT_loc.rearrange("(k p) t -> k p t", k=8)
    with P.stack:
        C.alloc_banks()
        gt = P.sb([128, D], F32)
        bt = P.sb([128, D], F32)
        ident = P.sb([128, 128], BF16)
        eps = const_col(P, nc, LN_EPS, "eps_ln")
        xt = [P.sb([128, D], F32) for _ in range(2)]
        ht = [P.sb([128, D], F32) for _ in range(2)]
        hb = P.sb([128, D], BF16)
        hTt = [P.sb([128, 8, 128], BF16) for _ in range(2)]
        scr = ln_scratch(P, "ln")
        scr["eps_ln"] = eps
        dma(P, "sp", gt[:], g.partition_broadcast(128), writes=["g"])
        dma(P, "sp", bt[:], b.partition_broadcast(128), writes=["b"])
        dma(P, "sp", ident[:], idn, writes=["ident"])
        outs = []
        for t in range(nt):
            s = t % 2
            dma(P, "sp", xt[s][:], x[t * 128:(t + 1) * 128, :], writes=[("xt", s)])
            layer_norm_tile(P, nc, xt[s], ("xt", s), ht[s], ("ht", s), gt, bt, "g", "b", scr, "ln")
            dma(P, "poolq", h[t * 128:(t + 1) * 128, :], ht[s][:], reads=[("ht", s)], writes=[("h", t)])
            to_featmajor_bf16(P, nc, ht[s], ("ht", s), hb, "hb", C.banks[s], f"b{s}", hTt[s][:], ("hTt", s), ident)
            dma(P, "sp", hT[:, :, t * 128:(t + 1) * 128].rearrange("k p t -> p k t"), hTt[s][:],
                reads=[("hTt", s)], writes=[("hT", t)])
            outs += [("h", t), ("hT", t)]
        P.emit(final_wait_keys=outs)


BIG = 1.0e4


def phase_post(nc, G, l, h_in, h_out, hT_loc, y_all, ntok=TOK_CORE):
    C = Ctx(nc)
    P = C.P
    nsup = ntok // 512
    pf = f"L{l}_"

    def din(name, shape, dt=F32):
        return G.din(pf + name, shape, dt)
    wout = din("wout", [128, 8, D], BF16)
    wglu = din("wglu", [128, 2, 256], BF16)
    ln1g = din("ln1g", [1, D]); ln1b = din("ln1b", [1, D]); ln2g = din("ln2g", [1, D]); ln2b = din("ln2b", [1, D])
    dng = din("dng", [1, 64]); lamv = din("lamv", [1, 130])
    wr = din("wr", [128, 8, 20]); br = din("br", [1, 20])
    w1 = din("w1", [NEXP, 128, 8, DEXP], BF16); w3 = din("w3", [NEXP, 128, 8, DEXP], BF16)
    w2 = din("w2", [NEXP, 128, 4, D], BF16)
    idn = G.din("idn", [128, 128], BF16); idn32 = G.din("idn32", [128, 128])
    rofs = G.din("rofs", [1, 1], I32)
    hT_out = hT_loc.rearrange("(k p) t -> k p t", k=8) if hT_loc is not None else None
    ya_all, ybc_all = y_all
    nch = ntok // YCH
    yam = G.internal("ya_mine", [nch, 4 * YCH, 192])
    ybm = G.internal("ybc_mine", [nch, 4 * YCH, 192])
    offh = {}

    from contextlib import contextmanager

    @contextmanager
    def sp_wrap(sp):
        with sp.register(f"rofs{l}") as reg:
            sp.reg_load(reg, rofs[0:1, 0:1])
            offh["v"] = sp.snap(reg)
            yield
    P.sp_wrap = sp_wrap
    for nm_, src_, dst_ in (("a", ya_all, yam), ("b", ybc_all, ybm)):
        P.op("sp", lambda src_=src_, dst_=dst_: nc.sync.dma_start(
            out=dst_.rearrange("i (a b) c -> (i a) (b c)", a=64),
            in_=src_[bass.ds(offh["v"], nch)].rearrange("i (a b) c -> (i a) (b c)", a=64)),
            writes=[("ymine", nm_)])
    yas = yam.rearrange("i (j s) c -> i s j c", j=4)
    ybs = ybm.rearrange("i (j s) c -> i s j c", j=4)

    with P.stack:
        C.alloc_banks()
        B = C.banks
        sb = P.sb
        ident = sb([128, 128], BF16); ident32 = sb([128, 128], F32)
        g1 = sb([128, D], F32); b1 = sb([128, D], F32); g2 = sb([128, D], F32); b2 = sb([128, D], F32)
        wout_t = sb([128, 8, D], BF16); wglu_t = sb([128, 2, 256], BF16)
        wr_t = sb([128, 8, 20], F32); br_t = sb([128, 20], F32)
        gA = sb([128, 64], F32); lam_t = sb([128, 130], F32); lprod = sb([128, 2, 32], F32)
        lsum = sb([128, 2], F32); nlam = sb([128, 1], F32)
        eps_ln = const_col(P, nc, LN_EPS, "eps_ln"); eps_rms = const_col(P, nc, RMS_EPS, "eps_rms")
        scr = ln_scratch(P, "ln"); scr["eps_ln"] = eps_ln
        ht = [sb([128, D], F32) for _ in range(2)]
        ya_t = [sb([128, 768], F32) for _ in range(2)]
        yc_t = [sb([128, 256], F32) for _ in range(2)]
        ymix = [sb([128, D], F32) for _ in range(2)]
        dd = sb([128, 6, 64], F32); sq = sb([128, 6, 64], F32); ss = sb([128, 6], F32)
        c2 = sb([128, 256], F32); c3 = sb([128, 256], F32); ygb = sb([128, 256], BF16)
        ygT = sb([128, 2, 128], BF16); sig = sb([128, 256], F32)
        ymb = sb([128, D], BF16); ymT = sb([128, 8, 128], BF16)
        z = sb([128, D], F32)
        h1 = [sb([128, D], F32) for _ in range(4)]
        h1T32 = sb([128, 8, 128], F32)
        h1T = sb([128, 8, 512], BF16)
        lg = sb([128, 20], F32)
        r = {n: sb([128, s], F32) for n, s in dict(gmax=1, goh=4, ngmax=1, gexp=4, gsum=1, gp=1, em=16, pen=4, m1=1, oh1=16,
                                                   em2=16, m2=1, oh2=16, dl=1, ex=1, den=1, w1=1, w2=1).items()}
        comb = sb([128, 4, 16], F32)
        wA = [sb([128, 8, DEXP], BF16) for _ in range(2)]
        wB = [sb([128, 8, DEXP], BF16) for _ in range(2)]
        wC = [sb([128, 4, D], BF16) for _ in range(2)]
        sil = [sb([128, 512], F32) for _ in range(2)]
        hid = [sb([128, 512], BF16) for _ in range(4)]
        acc = [sb([128, D], F32) for _ in range(4)]
        z2 = sb([128, D], F32)
        ho = [sb([128, D], F32) for _ in range(2)]
        hob = sb([128, D], BF16)
        hoT = [sb([128, 8, 128], BF16) for _ in range(2)]

        dma(P, "sp", ident[:], idn, writes=["ident"]); dma(P, "sp", ident32[:], idn32, writes=["ident32"])
        for t_, s_, k_ in ((g1, ln1g, "g1"), (b1, ln1b, "b1"), (g2, ln2g, "g2"), (b2, ln2b, "b2")):
            dma(P, "sp", t_[:], s_.partition_broadcast(128), writes=[k_])
        dma(P, "sp", wout_t[:], wout, writes=["wout"]); dma(P, "sp", wglu_t[:], wglu, writes=["wglu"])
        dma(P, "sp", wr_t[:], wr, writes=["wr"]); dma(P, "sp", br_t[:], br.partition_broadcast(128), writes=["br"])
        dma(P, "sp", gA[:], dng.partition_broadcast(128), writes=["gA"])
        dma(P, "sp", lam_t[:], lamv.partition_broadcast(128), writes=["lamt"])
        P.op("dve", lambda: nc.vector.tensor_scalar(out=gA[:], in0=gA[:], scalar1=lam_t[:, 128:129], scalar2=None, op0=ALU.mult),
             reads=["gA", "lamt"], writes=["gA"])
        lv = lam_t[:, 0:128].rearrange("p (a b c) -> p a b c", a=2, b=2)
        P.op("dve", lambda: nc.vector.tensor_tensor(out=lprod[:], in0=lv[:, :, 0, :], in1=lv[:, :, 1, :], op=ALU.mult),
             reads=["lamt"], writes=["lprod"])
        P.op("dve", lambda: nc.vector.tensor_reduce(out=lsum[:], in_=lprod[:], axis=AX.X, op=ALU.add), reads=["lprod"], writes=["lsum"])
        P.op("act", lambda: nc.scalar.activation(out=lsum[:], in_=lsum[:], func=AF.Exp), reads=["lsum"], writes=["lsum"])
        P.op("dve", lambda: nc.vector.tensor_tensor(out=nlam[:], in0=lsum[:, 1:2], in1=lsum[:, 0:1], op=ALU.subtract),
             reads=["lsum"], writes=["nlam"])
        P.op("dve", lambda: nc.vector.tensor_scalar(out=nlam[:], in0=nlam[:], scalar1=lam_t[:, 129:130], scalar2=None, op0=ALU.add),
             reads=["nlam", "lamt"], writes=["nlam"])

        outs = []
        for su in range(nsup):
            for tt in range(4):
                t = su * 4 + tt
                s = t % 2
                rows = slice(t * 128, (t + 1) * 128)
                dma(P, "sp", ht[s][:], h_in[rows, :], writes=[("ht", s)])
                ci_, r0_ = (t * 128) // YCH, (t * 128) % YCH
                rw_ = slice(r0_, r0_ + 128)
                dma(P, "sp", ya_t[s][:].rearrange("p (j c) -> p j c", j=4), yas[ci_][rw_, :, :], reads=[("ymine", "a")], writes=[("ya", s)])
                dma(P, "sp", ymix[s][:, 384:768].rearrange("p (j c) -> p j c", j=3), ybs[ci_][rw_, 0:3, 0:128], reads=[("ymine", "b")], writes=[("ymix", s, 1)])
                dma(P, "sp", yc_t[s][:].rearrange("p (j c) -> p j c", j=4), ybs[ci_][rw_, :, 128:192], reads=[("ymine", "b")], writes=[("yc", s)])
                yav = ya_t[s][:].rearrange("p (h m d) -> p h m d", h=6, m=2)
                P.op("dve", lambda yav=yav: nc.vector.scalar_tensor_tensor(out=dd[:], in0=yav[:, :, 1, :], scalar=nlam[:, 0:1],
                                                                          in1=yav[:, :, 0, :], op0=ALU.mult, op1=ALU.add),
                     reads=[("ya", s), "nlam"], writes=["dd"])
                P.op("pool", lambda: nc.gpsimd.tensor_tensor(out=sq[:], in0=dd[:], in1=dd[:], op=ALU.mult), reads=["dd"], writes=["sq"])
                P.op("dve", lambda: nc.vector.tensor_reduce(out=ss[:], in_=sq[:], axis=AX.X, op=ALU.add), reads=["sq"], writes=["ss"])
                P.op("act", lambda: nc.scalar.activation(out=ss[:], in_=ss[:], func=AF.Sqrt, bias=eps_rms[:, 0:1], scale=1.0 / 64),
                     reads=["ss", "eps_rms"], writes=["ss"])
                P.op("dve", lambda: nc.vector.reciprocal(out=ss[:], in_=ss[:]), reads=["ss"], writes=["ss"])
                P.op("dve", lambda: nc.vector.tensor_tensor(out=dd[:], in0=dd[:], in1=ss[:].unsqueeze(2).to_broadcast([128, 6, 64]), op=ALU.mult),
                     reads=["dd", "ss"], writes=["dd"])
                ym0 = ymix[s][:, 0:384].rearrange("p (h d) -> p h d", h=6)
                P.op("pool", lambda ym0=ym0: nc.gpsimd.tensor_tensor(out=ym0, in0=dd[:], in1=gA[:].unsqueeze(1).to_broadcast([128, 6, 64]), op=ALU.mult),
                     reads=["dd", "gA"], writes=[("ymix", s, 0)])
                yct = yc_t[s]
                P.op("pool", lambda yct=yct: nc.gpsimd.tensor_tensor(out=c2[:], in0=yct[:], in1=yct[:], op=ALU.mult), reads=[("yc", s)], writes=["c2"])
                P.op("dve", lambda: nc.vector.tensor_scalar(out=c2[:], in0=c2[:], scalar1=0.044715, scalar2=1.0, op0=ALU.mult, op1=ALU.add),
                     reads=["c2"], writes=["c2"])
                P.op("dve", lambda yct=yct: nc.vector.tensor_tensor(out=c2[:], in0=c2[:], in1=yct[:], op=ALU.mult), reads=["c2", ("yc", s)], writes=["c2"])
                P.op("act", lambda: nc.scalar.activation(out=c2[:], in_=c2[:], func=AF.Sigmoid, scale=1.5957691216057308),
                     reads=["c2"], writes=["c2"])
                P.op("dve", lambda yct=yct: nc.vector.tensor_tensor(out=c3[:], in0=c2[:], in1=yct[:], op=ALU.mult), reads=["c2", ("yc", s)], writes=["c3"])
                P.op("act", lambda: nc.scalar.copy(out=ygb[:], in_=c3[:]), reads=["c3"], writes=["ygb"])
                pv0 = B[0][:].bitcast(BF16)

                def trg(pv0=pv0):
                    for k in range(2):
                        ins = nc.tensor.transpose(out=pv0[:, k * 128:(k + 1) * 128], in_=ygb[:, k * 128:(k + 1) * 128], identity=ident[:])
                    return ins
                P.op("pe", trg, reads=["ygb", "ident"], writes=["b0"])
                P.op("dve", lambda pv0=pv0: nc.vector.tensor_copy(out=ygT[:].rearrange("p k t -> p (k t)"), in_=pv0[:, 0:256]), reads=["b0"], writes=["ygT"])

                def mmg():
                    for k in range(2):
                        ins = nc.tensor.matmul(B[1][:, 0:256], lhsT=ygT[:, k, :], rhs=wglu_t[:, k, :], start=(k == 0), stop=(k == 1))
                    return ins
                P.op("pe", mmg, reads=["ygT", "wglu"], writes=["b1"])
                P.op("act", lambda: nc.scalar.activation(out=sig[:], in_=B[1][:, 0:256], func=AF.Sigmoid), reads=["b1"], writes=["sig"])
                P.op("dve", lambda s=s: nc.vector.tensor_tensor(out=ymix[s][:, 768:1024], in0=c3[:], in1=sig[:], op=ALU.mult),
                     reads=["c3", "sig"], writes=[("ymix", s, 2)])
                ymk = [("ymix", s, 0), ("ymix", s, 1), ("ymix", s, 2)]
                P.op("act", lambda s=s: nc.scalar.copy(out=ymb[:], in_=ymix[s][:]), reads=ymk, writes=["ymb"])
                pvb = B[0][:].bitcast(BF16).rearrange("p (k t) -> p k t", k=8)

                def try_(pvb=pvb):
                    for kc in range(8):
                        ins = nc.tensor.transpose(out=pvb[:, kc, :], in_=ymb[:, kc * 128:(kc + 1) * 128], identity=ident[:])
                    return ins
                P.op("pe", try_, reads=["ymb", "ident"], writes=["b0"])
                P.op("dve", lambda pvb=pvb: nc.vector.tensor_copy(out=ymT[:], in_=pvb), reads=["b0"], writes=["ymT"])
                for half in range(2):
                    def mmo(half=half):
                        for kc in range(8):
                            ins = nc.tensor.matmul(B[2 + half][:], lhsT=ymT[:, kc, :], rhs=wout_t[:, kc, half * 512:(half + 1) * 512],
                                                   start=(kc == 0), stop=(kc == 7))
                        return ins
                    P.op("pe", mmo, reads=["ymT", "wout"], writes=[f"b{2 + half}"])
                    P.op("dve", lambda half=half, s=s: nc.vector.scalar_tensor_tensor(
                        out=z[:, half * 512:(half + 1) * 512], in0=ht[s][:, half * 512:(half + 1) * 512], scalar=float(ALPHA),
                        in1=B[2 + half][:], op0=ALU.mult, op1=ALU.add), reads=[f"b{2 + half}", ("ht", s)], writes=[("z", half)])
                P.op("pool", lambda: nc.gpsimd.tensor_copy(out=z[:, 0:1], in_=z[:, 0:1]), reads=[("z", 0), ("z", 1)], writes=["zz"])
                layer_norm_tile(P, nc, z, "zz", h1[tt], ("h1", tt), g1, b1, "g1", "b1", scr, "ln")
                for half in range(2):
                    pv32 = B[2 + half][:].rearrange("p (k t) -> p k t", k=4)

                    def trh(half=half, pv32=pv32, tt=tt):
                        for k in range(4):
                            kc = half * 4 + k
                            ins = nc.tensor.transpose(out=pv32[:, k, :], in_=h1[tt][:, kc * 128:(kc + 1) * 128], identity=ident32[:])
                        return ins
                    P.op("pe", trh, reads=[("h1", tt), "ident32"], writes=[f"b{2 + half}"])
                    P.op("dve", lambda half=half, pv32=pv32: nc.vector.tensor_copy(out=h1T32[:, half * 4:(half + 1) * 4, :], in_=pv32),
                         reads=[f"b{2 + half}"], writes=[("h1T32", half)])
                    P.op("act", lambda half=half, pv32=pv32, tt=tt: nc.scalar.copy(
                        out=h1T[:, half * 4:(half + 1) * 4, tt * 128:(tt + 1) * 128], in_=pv32),
                        reads=[f"b{2 + half}"], writes=[("h1T", tt, half)])

                def mmr():
                    for kc in range(8):
                        ins = nc.tensor.matmul(B[1][:, 0:20], lhsT=h1T32[:, kc, :], rhs=wr_t[:, kc, :], start=(kc == 0), stop=(kc == 7))
                    return ins
                P.op("pe", mmr, reads=[("h1T32", 0), ("h1T32", 1), "wr"], writes=["b1"])
                P.op("dve", lambda: nc.vector.tensor_tensor(out=lg[:], in0=B[1][:, 0:20], in1=br_t[:], op=ALU.add), reads=["b1", "br"], writes=["lg"])
                V = nc.vector
                glog = lg[:, 0:4]
                elog = lg[:, 4:20]
                seq = [
                    lambda: V.tensor_reduce(out=r["gmax"][:], in_=glog, axis=AX.X, op=ALU.max),
                    lambda: V.tensor_scalar(out=r["goh"][:], in0=glog, scalar1=r["gmax"][:, 0:1], scalar2=None, op0=ALU.is_ge),
                    lambda: V.tensor_scalar(out=r["ngmax"][:], in0=r["gmax"][:], scalar1=-1.0, scalar2=None, op0=ALU.mult),
                ]
                for f_ in seq:
                    P.op("dve", f_, reads=["lg", "rt"], writes=["rt"])
                P.op("act", lambda: nc.scalar.activation(out=r["gexp"][:], in_=glog, func=AF.Exp, bias=r["ngmax"][:, 0:1], scale=1.0),
                     reads=["lg", "rt"], writes=["rt2"])
                seq = [
                    lambda: V.tensor_reduce(out=r["gsum"][:], in_=r["gexp"][:], axis=AX.X, op=ALU.add),
                    lambda: V.reciprocal(out=r["gp"][:], in_=r["gsum"][:]),
                    lambda: V.tensor_tensor(out=r["em"][:].rearrange("p (g e) -> p g e", g=4), in0=elog.rearrange("p (g e) -> p g e", g=4),
                                            in1=r["goh"][:].unsqueeze(2).to_broadcast([128, 4, 4]), op=ALU.mult),
                    lambda: V.tensor_scalar(out=r["pen"][:], in0=r["goh"][:], scalar1=-1.0, scalar2=BIG, op0=ALU.add, op1=ALU.mult),
                    lambda: V.tensor_tensor(out=r["em"][:].rearrange("p (g e) -> p g e", g=4), in0=r["em"][:].rearrange("p (g e) -> p g e", g=4),
                                            in1=r["pen"][:].unsqueeze(2).to_broadcast([128, 4, 4]), op=ALU.add),
                    lambda: V.tensor_reduce(out=r["m1"][:], in_=r["em"][:], axis=AX.X, op=ALU.max),
                    lambda: V.tensor_scalar(out=r["oh1"][:], in0=r["em"][:], scalar1=r["m1"][:, 0:1], scalar2=None, op0=ALU.is_ge),
                    lambda: V.scalar_tensor_tensor(out=r["em2"][:], in0=r["oh1"][:], scalar=-BIG, in1=r["em"][:], op0=ALU.mult, op1=ALU.add),
                    lambda: V.tensor_reduce(out=r["m2"][:], in_=r["em2"][:], axis=AX.X, op=ALU.max),
                    lambda: V.tensor_scalar(out=r["oh2"][:], in0=r["em2"][:], scalar1=r["m2"][:, 0:1], scalar2=None, op0=ALU.is_ge),
                    lambda: V.tensor_tensor(out=r["dl"][:], in0=r["m2"][:], in1=r["m1"][:], op=ALU.subtract),
                ]
                for f_ in seq:
                    P.op("dve", f_, reads=["lg", "rt", "rt2"], writes=["rt"])
                P.op("act", lambda: nc.scalar.activation(out=r["ex"][:], in_=r["dl"][:], func=AF.Exp), reads=["rt"], writes=["rt2"])
                seq = [
                    lambda: V.tensor_scalar(out=r["den"][:], in0=r["ex"][:], scalar1=1.0, scalar2=None, op0=ALU.add),
                    lambda: V.reciprocal(out=r["w1"][:], in_=r["den"][:]),
                    lambda: V.tensor_tensor(out=r["w1"][:], in0=r["w1"][:], in1=r["gp"][:], op=ALU.mult),
                    lambda: V.tensor_tensor(out=r["w2"][:], in0=r["w1"][:], in1=r["ex"][:], op=ALU.mult),
                    lambda tt=tt: V.tensor_scalar(out=comb[:, tt, :], in0=r["oh1"][:], scalar1=r["w1"][:, 0:1], scalar2=None, op0=ALU.mult),
                    lambda tt=tt: V.scalar_tensor_tensor(out=comb[:, tt, :], in0=r["oh2"][:], scalar=r["w2"][:, 0:1], in1=comb[:, tt, :],
                                                         op0=ALU.mult, op1=ALU.add),
                ]
                for i_, f_ in enumerate(seq):
                    P.op("dve", f_, reads=["rt", "rt2"] + ([("comb", tt)] if i_ == 5 else []), writes=["rt"] if i_ < 4 else [("comb", tt)])
            h1Tk = [("h1T", tt, half) for tt in range(4) for half in range(2)]
            for e in range(NEXP):
                ws = e % 2
                dma(P, "sp", wA[ws][:], w1[e], writes=[("wA", ws)])
                dma(P, "poolq", wB[ws][:], w3[e], writes=[("wB", ws)])
                dma(P, "sp", wC[ws][:], w2[e], writes=[("wC", ws)])
                for hc in range(4):
                    ba, bb = 4 + hc % 2, 6 + hc % 2

                    def mma(hc=hc, ba=ba, ws=ws):
                        for kc in range(8):
                            ins = nc.tensor.matmul(B[ba][:], lhsT=wA[ws][:, kc, hc * 128:(hc + 1) * 128], rhs=h1T[:, kc, :],
                                                   start=(kc == 0), stop=(kc == 7))
                        return ins

                    def mmb(hc=hc, bb=bb, ws=ws):
                        for kc in range(8):
                            ins = nc.tensor.matmul(B[bb][:], lhsT=wB[ws][:, kc, hc * 128:(hc + 1) * 128], rhs=h1T[:, kc, :],
                                                   start=(kc == 0), stop=(kc == 7))
                        return ins
                    P.op("pe", mma, reads=h1Tk + [("wA", ws)], writes=[f"b{ba}"])
                    P.op("pe", mmb, reads=h1Tk + [("wB", ws)], writes=[f"b{bb}"])
                    P.op("act", lambda hc=hc, ba=ba: nc.scalar.activation(out=sil[hc % 2][:], in_=B[ba][:], func=AF.Silu),
                         reads=[f"b{ba}"], writes=[("sil", hc % 2)])
                    P.op("dve", lambda hc=hc, bb=bb: nc.vector.tensor_tensor(out=hid[hc][:], in0=sil[hc % 2][:], in1=B[bb][:], op=ALU.mult),
                         reads=[f"b{bb}", ("sil", hc % 2)], writes=[("hid", hc)])
                for tt in range(4):
                    for half in range(2):
                        bo = 2 + half

                        def mm2(tt=tt, half=half, bo=bo, ws=ws):
                            for hc in range(4):
                                ins = nc.tensor.matmul(B[bo][:], lhsT=hid[hc][:, tt * 128:(tt + 1) * 128],
                                                       rhs=wC[ws][:, hc, half * 512:(half + 1) * 512], start=(hc == 0), stop=(hc == 3))
                            return ins
                        P.op("pe", mm2, reads=[("hid", hc) for hc in range(4)] + [("wC", ws)], writes=[f"b{bo}"])
                        av = acc[tt][:, half * 512:(half + 1) * 512]
                        if e == 0:
                            P.op("dve", lambda av=av, bo=bo, tt=tt, e=e: nc.vector.tensor_scalar(
                                out=av, in0=B[bo][:], scalar1=comb[:, tt, e:e + 1], scalar2=None, op0=ALU.mult),
                                reads=[f"b{bo}", ("comb", tt)], writes=[("acc", tt, half)])
                        else:
                            P.op("dve", lambda av=av, bo=bo, tt=tt, e=e: nc.vector.scalar_tensor_tensor(
                                out=av, in0=B[bo][:], scalar=comb[:, tt, e:e + 1], in1=av, op0=ALU.mult, op1=ALU.add),
                                reads=[f"b{bo}", ("comb", tt), ("acc", tt, half)], writes=[("acc", tt, half)])
            for tt in range(4):
                t = su * 4 + tt
                s = t % 2
                rows = slice(t * 128, (t + 1) * 128)
                P.op("dve", lambda tt=tt: nc.vector.scalar_tensor_tensor(out=z2[:], in0=h1[tt][:], scalar=float(ALPHA), in1=acc[tt][:],
                                                                          op0=ALU.mult, op1=ALU.add),
                     reads=[("h1", tt), ("acc", tt, 0), ("acc", tt, 1)], writes=["z2"])
                layer_norm_tile(P, nc, z2, "z2", ho[s], ("ho", s), g2, b2, "g2", "b2", scr, "ln")
                dma(P, "poolq", h_out[rows, :], ho[s][:], reads=[("ho", s)], writes=[("h_out", t)])
                outs.append(("h_out", t))
                if hT_out is not None:
                    to_featmajor_bf16(P, nc, ho[s], ("ho", s), hob, "hob", B[0], "b0", hoT[s][:], ("hoT", s), ident)
                    dma(P, "poolq", hT_out[:, :, rows].rearrange("k p t -> p k t"), hoT[s][:], reads=[("hoT", s)], writes=[("hT_out", t)])
                    outs.append(("hT_out", t))
        P.emit(final_wait_keys=outs)


def post_inputs(l, p, lam_init):
    d = {}
    d["wout"] = bf(p["w_out"][l].reshape(8, 128, D).transpose(1, 0, 2))
    d["wglu"] = bf(p["s5_w_glu"][l].reshape(2, 128, 256).transpose(1, 0, 2))
    for n in ("ln1_g", "ln1_b", "ln2_g", "ln2_b"):
        d[n.replace("_", "")] = f32c(p[n][l].reshape(1, D))
    d["dng"] = f32c(p["diff_norm_g"][l].reshape(1, 64))
    d["lamv"] = f32c(np.concatenate([p["lam_q1"][l], p["lam_k1"][l], p["lam_q2"][l], p["lam_k2"][l],
                                     np.array([1.0 - lam_init, -lam_init], np.float32)]).reshape(1, 130))
    wr = np.concatenate([p["moe_w_grp"][l], p["moe_w_exp"][l]], axis=1)
    d["wr"] = f32c(wr.reshape(8, 128, 20).transpose(1, 0, 2))
    d["br"] = f32c(np.concatenate([p["moe_b_grp"][l], p["moe_b_exp"][l]]).reshape(1, 20))
    d["w1"] = bf(p["moe_w1"][l].reshape(NEXP, 8, 128, DEXP).transpose(0, 2, 1, 3))
    d["w3"] = bf(p["moe_w3"][l].reshape(NEXP, 8, 128, DEXP).transpose(0, 2, 1, 3))
    d["w2"] = bf(p["moe_w2"][l].reshape(NEXP, 4, 128, D).transpose(0, 2, 1, 3))
    d["idn"] = bf(np.eye(128)); d["idn32"] = f32c(np.eye(128))
    return d


SBANKS = [0, 1, 2, 6, 7]
NPT = 7
ADEPTH = 4
QUARTERS = False
TWO_PI = 2.0 * math.pi
MAGIC = 12582912.0


def phase_mix(nc, G, l, hT_all, ya_d, ybc_d, S=SEQ, do_attn=True, do_s5=True, do_gdn=True, pool_pre=None, pool_post=None):
    debug = False
    C = Ctx(nc)
    P = C.P
    nst = S // 512
    nblk = S // 128
    pf = f"L{l}_"

    def din(name, shape, dt=F32):
        return G.din((pf + name) if name not in ("amask", "idn32", "srow", "cTri", "cSL", "cMask2", "cBones") else name, shape, dt)
    hT4 = hT_all.rearrange("k (r p) t -> r p k t", r=4)
    wq = din("wq", [128, 8, 96], BF16); wk = din("wk", [128, 8, 96], BF16); wv = din("wv", [128, 8, 192], BF16)
    amask = din("amask", [128, 4, 512], BF16)
    idn32 = din("idn32", [128, 128])
    wu = din("wu", [128, 8, 64], BF16)
    s5row = din("s5row", [2, 3, 128])
    s5col = din("s5col", [2, 128, 3])
    s5bT = din("s5bT", [2, 2, 2, 16, 64])
    s5cT = din("s5cT", [2, 2, 2, 64, 16])
    s5d = din("s5d", [64, 1])
    srow = din("srow", [1, 512])
    wg = din("wg", [128, 8, 384], BF16); wt = din("wt", [128, 8, 132], BF16)
    cvw = din("cvw", [128, 3, 4])
    galog = din("galog", [1, 2]); gdtb = din("gdtb", [1, 2]); gng = din("gng", [1, 64])
    cTri = din("cTri", [64, 64]); cSL = din("cSL", [64, 64]); cMask2 = din("cMask2", [64, 2, 64]); cBones = din("cBones", [128, 128])
    ya_o = ya_d.rearrange("s (u d) -> s u d", u=3)
    yb_o = ybc_d[:, 0:128].rearrange("s (h d) -> s h d", h=2)
    yc_o = ybc_d[:, 128:192]
    P.pool_pre, P.pool_post = pool_pre, pool_post
    outs = []
    dbg = []
    V = nc.vector
    G = nc.gpsimd
    A = nc.scalar
    T = nc.tensor

    with P.stack:
        C.alloc_banks(quarters=do_gdn and QUARTERS)
        B = C.banks
        sb = P.sb
        ident32 = sb([128, 128], F32)
        dma(P, "sp", ident32[:], idn32, writes=["ident32"])
        hTt = [sb([128, 8, 512], BF16) for _ in range(2)]
        eps_rms = const_col(P, nc, RMS_EPS, "eps_rms")
        if do_attn:
            wq_t = sb([128, 8, 96], BF16); wk_t = sb([128, 8, 96], BF16); wv_t = sb([128, 8, 192], BF16)
            QT = sb([96, S], BF16); KT = sb([96, S], BF16)
            Vall = sb([128, nblk, 3, 65], BF16)
            am_t = sb([128, 4, 512], BF16)
            PT = [sb([128, 512], BF16) for _ in range(NPT)]
            osb = sb([65, 512], F32); rec = sb([128, 4], F32)
            oT = [sb([128, 4, 64], F32) for _ in range(2)]
            dma(P, "sp", wq_t[:], wq, writes=["wq"]); dma(P, "sp", wk_t[:], wk, writes=["wk"]); dma(P, "sp", wv_t[:], wv, writes=["wv"])
            dma(P, "sp", am_t[:], amask, writes=["amask"])
            P.op("pool", lambda: G.memset(Vall[:, :, :, 64:65], 1.0), writes=["Vones"])
        if do_s5:
            wu_t = sb([128, 8, 64], BF16)
            dma(P, "sp", wu_t[:], wu, writes=["wu"])
            uT = [sb([32, 512], F32) for _ in range(2)]
            srow_t = sb([128, 512], F32)
            dma(P, "sp", srow_t[:], srow.partition_broadcast(128), writes=["srow"])
            d_col = [sb([32, 1], F32) for _ in range(2)]
            for pr_ in range(2):
                dma(P, "sp", d_col[pr_][:], s5d[pr_ * 32:(pr_ + 1) * 32, :], writes=[("dcol", pr_)])
            s5 = []
            for pr in range(2):
                t = dict(row=sb([32, 3, 128], F32), col=sb([128, 3], F32),
                         BrBD=sb([32, 128], F32), BiBD=sb([32, 128], F32), CrBD=sb([128, 32], F32), CiBD=sb([128, 32], F32),
                         bbr=sb([32, 128], F32), bbi=sb([32, 128], F32),
                         w=[sb([32, 128], F32) for _ in range(8)],
                         cw=[sb([128, 1], F32) for _ in range(8)],
                         RHO=sb([128, 512], F32), CS=sb([128, 512], F32), SN=sb([128, 512], F32),
                         zi=sb([128, 2], F32), zt=sb([128, 2], F32))
                if pr == 0:
                    for nm_ in ("bre", "bim", "t1", "t2", "zre", "zim", "xre", "xim", "ang", "tmp"):
                        t[nm_] = sb([128, 512], F32)
                if pr == 1:
                    for nm_ in ("bre", "bim", "t1", "t2", "zre", "zim", "xre", "xim", "ang", "tmp"):
                        t[nm_] = s5[0][nm_]
                s5.append(t)
            yT = [sb([32, 512], F32) for _ in range(2)]
            yc_tm = [sb([128, 4, 64], F32) for _ in range(2)]

            def range_reduce(eng_name, x, tmp, key_x, key_t, shape_all=True):
                P.op("dve", lambda: V.tensor_scalar(out=tmp, in0=x, scalar1=1.0 / TWO_PI, scalar2=MAGIC, op0=ALU.mult, op1=ALU.add),
                     reads=[key_x], writes=[key_t])
                P.op("dve", lambda: V.tensor_scalar(out=tmp, in0=tmp, scalar1=-MAGIC, scalar2=-TWO_PI, op0=ALU.add, op1=ALU.mult),
                     reads=[key_t], writes=[key_t])
                P.op("dve", lambda: V.tensor_tensor(out=x, in0=x, in1=tmp, op=ALU.add), reads=[key_x, key_t], writes=[key_x])

            def s5_setup(pr):
                t = s5[pr]
                k = lambda n, pr=pr: ("s5", "sh" if n in ("bre", "bim", "t1", "t2", "zre", "zim", "xre", "xim", "tab", "roww_scratch") else pr, n)
                dma(P, "sp", t["row"][:], s5row[pr:pr + 1].partition_broadcast(32), writes=[k("row")])
                dma(P, "sp", t["col"][:], s5col[pr], writes=[k("col")])
                for nm in ("BrBD", "BiBD", "CrBD", "CiBD"):
                    P.op("pool", lambda nm=nm, t=t: G.memset(t[nm][:], 0.0), writes=[k(nm)])
                for g in range(2):
                    dma(P, "sp", t["BrBD"][g * 16:(g + 1) * 16, g * 64:(g + 1) * 64], s5bT[pr, g, 0], reads=[k("BrBD")], writes=[k("BrBD")])
                    dma(P, "sp", t["BiBD"][g * 16:(g + 1) * 16, g * 64:(g + 1) * 64], s5bT[pr, g, 1], reads=[k("BiBD")], writes=[k("BiBD")])
                    dma(P, "sp", t["CrBD"][g * 64:(g + 1) * 64, g * 16:(g + 1) * 16], s5cT[pr, g, 0], reads=[k("CrBD")], writes=[k("CrBD")])
                    dma(P, "sp", t["CiBD"][g * 64:(g + 1) * 64, g * 16:(g + 1) * 16], s5cT[pr, g, 1], reads=[k("CiBD")], writes=[k("CiBD")])
                P.op("dve", lambda t=t: V.tensor_scalar(out=t["CiBD"][:], in0=t["CiBD"][:], scalar1=-1.0, scalar2=None, op0=ALU.mult),
                     reads=[k("CiBD")], writes=[k("CiBD")])
                lre, lim, ldt = t["row"][:, 0, :], t["row"][:, 1, :], t["row"][:, 2, :]
                dt_, lr_, mag, ang, tmp_, sn, cs, den = [t["w"][i][:] for i in range(8)]
                rk = k("roww")
                steps = [
                    ("act", lambda: A.activation(out=dt_, in_=ldt, func=AF.Exp)),
                    ("dve", lambda: V.tensor_tensor(out=lr_, in0=lre, in1=dt_, op=ALU.mult)),
                    ("act", lambda: A.activation(out=mag, in_=lr_, func=AF.Exp)),
                    ("dve", lambda: V.tensor_tensor(out=ang, in0=lim, in1=dt_, op=ALU.mult)),
                ]
                for e_, f_ in steps:
                    P.op(e_, f_, reads=[k("row"), rk], writes=[rk])
                range_reduce("dve", ang, tmp_, rk, rk)
                P.op("act", lambda: A.activation(out=sn, in_=ang, func=AF.Sin), reads=[rk], writes=[rk])
                P.op("dve", lambda: V.tensor_scalar(out=ang, in0=ang, scalar1=math.pi / 2, scalar2=None, op0=ALU.add), reads=[rk], writes=[rk])
                range_reduce("dve", ang, tmp_, rk, rk)
                P.op("act", lambda: A.activation(out=cs, in_=ang, func=AF.Sin), reads=[rk], writes=[rk])
                steps = [
                    lambda: V.tensor_tensor(out=cs, in0=cs, in1=mag, op=ALU.mult),
                    lambda: V.tensor_scalar(out=cs, in0=cs, scalar1=-1.0, scalar2=None, op0=ALU.add),
                    lambda: V.tensor_tensor(out=sn, in0=sn, in1=mag, op=ALU.mult),
                    lambda: V.tensor_tensor(out=den, in0=lre, in1=lre, op=ALU.mult),
                    lambda: V.tensor_tensor(out=tmp_, in0=lim, in1=lim, op=ALU.mult),
                    lambda: V.tensor_tensor(out=den, in0=den, in1=tmp_, op=ALU.add),
                    lambda: V.reciprocal(out=den, in_=den),
                    lambda: V.tensor_tensor(out=dt_, in0=cs, in1=lre, op=ALU.mult),
                    lambda: V.tensor_tensor(out=tmp_, in0=sn, in1=lim, op=ALU.mult),
                    lambda: V.tensor_tensor(out=dt_, in0=dt_, in1=tmp_, op=ALU.add),
                    lambda: V.tensor_tensor(out=dt_, in0=dt_, in1=den, op=ALU.mult),
                    lambda: V.tensor_tensor(out=lr_, in0=sn, in1=lre, op=ALU.mult),
                    lambda: V.tensor_tensor(out=tmp_, in0=cs, in1=lim, op=ALU.mult),
                    lambda: V.tensor_tensor(out=lr_, in0=lr_, in1=tmp_, op=ALU.subtract),
                    lambda: V.tensor_tensor(out=lr_, in0=lr_, in1=den, op=ALU.mult),
                    lambda t=t: V.tensor_tensor(out=t["bbr"][:], in0=dt_, in1=t["BrBD"][:], op=ALU.mult),
                    lambda t=t: V.tensor_tensor(out=tmp_, in0=lr_, in1=t["BiBD"][:], op=ALU.mult),
                    lambda t=t: V.tensor_tensor(out=t["bbr"][:], in0=t["bbr"][:], in1=tmp_, op=ALU.subtract),
                    lambda t=t: V.tensor_tensor(out=t["bbi"][:], in0=dt_, in1=t["BiBD"][:], op=ALU.mult),
                    lambda t=t: V.tensor_tensor(out=tmp_, in0=lr_, in1=t["BrBD"][:], op=ALU.mult),
                    lambda t=t: V.tensor_tensor(out=t["bbi"][:], in0=t["bbi"][:], in1=tmp_, op=ALU.add),
                ]
                for f_ in steps:
                    P.op("dve", f_, reads=[k("row"), rk, k("BrBD"), k("BiBD")], writes=[rk])
                cdt, cth, crho, ca, ctmp, c512s, c512c, cx = [t["cw"][i][:] for i in range(8)]
                ck = k("colw")
                steps = [
                    ("act", lambda t=t: A.activation(out=cdt, in_=t["col"][:, 2:3], func=AF.Exp)),
                    ("dve", lambda t=t: V.tensor_tensor(out=cth, in0=t["col"][:, 1:2], in1=cdt, op=ALU.mult)),
                    ("dve", lambda t=t: V.tensor_tensor(out=crho, in0=t["col"][:, 0:1], in1=cdt, op=ALU.mult)),
                    ("act", lambda: A.activation(out=crho, in_=crho, func=AF.Exp)),
                    ("dve", lambda: V.tensor_scalar(out=ca, in0=cth, scalar1=512.0, scalar2=None, op0=ALU.mult)),
                ]
                for e_, f_ in steps:
                    P.op(e_, f_, reads=[k("col"), ck], writes=[ck])
                range_reduce("dve", ca, ctmp, ck, ck)
                P.op("act", lambda: A.activation(out=c512s, in_=ca, func=AF.Sin), reads=[ck], writes=[ck])
                P.op("dve", lambda: V.tensor_scalar(out=ca, in0=ca, scalar1=math.pi / 2, scalar2=None, op0=ALU.add), reads=[ck], writes=[ck])
                range_reduce("dve", ca, ctmp, ck, ck)
                P.op("act", lambda: A.activation(out=c512c, in_=ca, func=AF.Sin), reads=[ck], writes=[ck])
                tk = k("tab")
                P.op("dve", lambda t=t: V.tensor_scalar(out=t["ang"][:], in0=srow_t[:], scalar1=cth, scalar2=None, op0=ALU.mult),
                     reads=["srow", ck], writes=[tk])
                range_reduce("dve", t["ang"][:], t["tmp"][:], tk, tk)
                P.op("act", lambda t=t: A.activation(out=t["SN"][:], in_=t["ang"][:], func=AF.Sin), reads=[tk], writes=[tk])
                P.op("dve", lambda t=t: V.tensor_scalar(out=t["ang"][:], in0=t["ang"][:], scalar1=math.pi / 2, scalar2=None, op0=ALU.add), reads=[tk], writes=[tk])
                range_reduce("dve", t["ang"][:], t["tmp"][:], tk, tk)
                P.op("act", lambda t=t: A.activation(out=t["CS"][:], in_=t["ang"][:], func=AF.Sin), reads=[tk], writes=[tk])
                P.op("pool", lambda t=t: G.memset(t["RHO"][:], 1.0), writes=[k("rho")])
                P.op("dve", lambda t=t: V.tensor_scalar(out=t["RHO"][:], in0=t["RHO"][:], scalar1=crho, scalar2=None, op0=ALU.mult),
                     reads=[k("rho"), ck], writes=[k("rho")])
                P.op("pool", lambda t=t: G.memset(t["zi"][:], 0.0), writes=[k("zi")])
                if pr == 0:
                    dbg.extend([("CS", t["CS"][:], [128, 512], [k("tab")]), ("SN", t["SN"][:], [128, 512], [k("tab")]),
                            ("RHO", t["RHO"][:], [128, 512], [k("rho")]), ("bbr", t["bbr"][:], [32, 128], [k("roww")]),
                            ("bbi", t["bbi"][:], [32, 128], [k("roww")]), ("cr", t["w"][0][:], [32, 128], [k("roww")]),
                            ("ci", t["w"][1][:], [32, 128], [k("roww")]), ("c512", t["cw"][5][:], [128, 1], [k("colw")]),
                            ("row", t["row"][:], [32, 3, 128], [k("row")]), ("col", t["col"][:], [128, 3], [k("col")])])
            for pr_ in range(2):
                s5_setup(pr_)
        if do_gdn:
            wg_t = sb([128, 8, 384], BF16); wt_t = sb([128, 8, 132], BF16)
            dma(P, "sp", wg_t[:], wg, writes=["wg"]); dma(P, "sp", wt_t[:], wt, writes=["wt"])
            cvw_t = sb([128, 3, 4], F32); dma(P, "sp", cvw_t[:], cvw, writes=["cvw"])
            alog_t = sb([64, 2], F32); dtb_t = sb([64, 2], F32); ng_t = sb([64, 64], F32)
            dma(P, "sp", alog_t[:], galog.partition_broadcast(64), writes=["alog"])
            dma(P, "sp", dtb_t[:], gdtb.partition_broadcast(64), writes=["dtb"])
            dma(P, "sp", ng_t[:], gng.partition_broadcast(64), writes=["ngt"])
            Tri = sb([64, 64], F32); SL = sb([64, 64], F32); Mask2 = sb([64, 2, 64], F32); Bones = sb([128, 128], F32); ones64 = sb([64, 64], F32)
            dma(P, "sp", Tri[:], cTri, writes=["Tri"]); dma(P, "sp", SL[:], cSL, writes=["SL"])
            dma(P, "sp", Mask2[:], cMask2, writes=["Mask2"]); dma(P, "sp", Bones[:], cBones, writes=["Bones"])
            P.op("pool", lambda: G.memset(ones64[:], 1.0), writes=["ones64"])
            P.op("act", lambda: A.activation(out=alog_t[:], in_=alog_t[:], func=AF.Exp), reads=["alog"], writes=["alog"])
            P.op("dve", lambda: V.tensor_scalar(out=alog_t[:], in0=alog_t[:], scalar1=-1.0, scalar2=None, op0=ALU.mult), reads=["alog"], writes=["alog"])
            xraw = [sb([128, 515], F32) for _ in range(3)]
            for c_ in range(3):
                P.op("pool", lambda c_=c_: G.memset(xraw[c_][:], 0.0), writes=[("xraw", c_)])
            cvt = sb([128, 512], F32)
            qkv = [sb([128, 512], F32) for _ in range(3)]
            sqn = sb([128, 512], F32); rn_ = sb([128, 512], F32)
            Sst = [sb([64, 64], F32) for _ in range(2)]
            for h_ in range(2):
                P.op("pool", lambda h_=h_: G.memset(Sst[h_][:], 0.0), writes=[("S", h_)])
            gd = dict(ch=[], hd=[])
            for sl in range(4):
                gd["ch"].append(dict(gs=sb([64, 128], F32), bg=sb([64, 4], F32), nbeta=sb([64, 2], F32)))
            for sl in range(8):
                gd["hd"].append(dict(qkv_tm=sb([64, 3, 64], F32), gcl=sb([64, 2], F32), ex3=sb([64, 3], F32), Gm=sb([64, 64], F32),
                                     EE=sb([64, 2, 64], F32), AT=sb([64, 64], F32),
                                     W=[sb([64, 256], F32) for _ in range(2)], tb=sb([64, 1], F32),
                                     kdec=sb([64, 64], F32), qdec=sb([64, 64], F32), wqT=sb([64, 2, 64], F32), vnew=sb([64, 64], F32),
                                     osb=sb([64, 64], F32), osq=sb([64, 64], F32), oss=sb([64, 1], F32), ngate=sb([64, 64], F32)))
            ybuf = [sb([64, 8, 2, 64], F32) for _ in range(2)]

        for st in range(nst):
            hs = st % 2
            cols = slice(st * 512, (st + 1) * 512)
            dma(P, "sp", hTt[hs][:], hT4[st // (nst // 4)][:, :, (st % (nst // 4)) * 512:(st % (nst // 4) + 1) * 512], writes=[("hTt", hs)])
            hk = ("hTt", hs)
            if do_attn:
                for (w_t, wkey, dst, dk_, bank) in ((wq_t, "wq", QT, "QT", 0), (wk_t, "wk", KT, "KT", 1)):
                    def mmqk(w_t=w_t, bank=bank, hs=hs):
                        for kc in range(8):
                            ins = T.matmul(B[bank][0:96, :], lhsT=w_t[:, kc, :], rhs=hTt[hs][:, kc, :], start=(kc == 0), stop=(kc == 7))
                        return ins
                    P.op("pe", mmqk, reads=[hk, wkey], writes=[f"b{bank}"])
                    P.op("act", lambda dst=dst, bank=bank, cols=cols: A.copy(out=dst[:, cols], in_=B[bank][0:96, :]),
                         reads=[f"b{bank}"], writes=[(dk_, st)])
                for pair in range(2):
                    bank = 2 + pair
                    pv = B[bank][:, 0:384].rearrange("p (j c) -> p j c", j=2)

                    def mmv(pair=pair, pv=pv, hs=hs):
                        for j in range(2):
                            blk = pair * 2 + j
                            for kc in range(8):
                                ins = T.matmul(pv[:, j, :], lhsT=hTt[hs][:, kc, blk * 128:(blk + 1) * 128], rhs=wv_t[:, kc, :],
                                               start=(kc == 0), stop=(kc == 7))
                        return ins
                    P.op("pe", mmv, reads=[hk, "wv"], writes=[f"b{bank}"])
                    b0 = st * 4 + pair * 2
                    P.op("dve", lambda pv=pv, b0=b0: V.tensor_copy(out=Vall[:, b0:b0 + 2, :, 0:64],
                                                                  in_=pv.rearrange("p j (u d) -> p j u d", u=3)),
                         reads=[f"b{bank}"], writes=[("V", st, pair)])
            if do_s5:
                for pr in range(2):
                    def mmu(hs=hs, pr=pr):
                        for kc in range(8):
                            ins = T.matmul(B[4][0:32, :], lhsT=wu_t[:, kc, pr * 32:(pr + 1) * 32], rhs=hTt[hs][:, kc, :], start=(kc == 0), stop=(kc == 7))
                        return ins
                    P.op("pe", mmu, reads=[hk, "wu"], writes=["b4"])
                    P.op("act", lambda pr=pr: A.copy(out=uT[pr][:], in_=B[4][0:32, :]), reads=["b4"], writes=[("uT", pr)])
                def s5_stream(pr):
                    t = s5[pr]
                    k = lambda n, pr=pr: ("s5", "sh" if n in ("bre", "bim", "t1", "t2", "zre", "zim", "xre", "xim", "tab", "roww_scratch") else pr, n)
                    P.op("pe", lambda t=t, pr=pr: T.matmul(B[5][:], lhsT=t["bbr"][:], rhs=uT[pr][:], start=True, stop=True),
                         reads=[("uT", pr), k("roww")], writes=["b5"])
                    P.op("pe", lambda t=t, pr=pr: T.matmul(B[6][:], lhsT=t["bbi"][:], rhs=uT[pr][:], start=True, stop=True),
                         reads=[("uT", pr), k("roww")], writes=["b6"])
                    P.op("act", lambda t=t: A.copy(out=t["bre"][:], in_=B[5][:]), reads=["b5"], writes=[k("bre")])
                    P.op("act", lambda t=t: A.copy(out=t["bim"][:], in_=B[6][:]), reads=["b6"], writes=[k("bim")])
                    P.op("dve", lambda t=t: V.tensor_tensor(out=t["t1"][:], in0=t["bre"][:], in1=t["CS"][:], op=ALU.mult), reads=[k("bre"), k("tab")], writes=[k("t1")])
                    P.op("pool", lambda t=t: G.tensor_tensor(out=t["t2"][:], in0=t["bim"][:], in1=t["SN"][:], op=ALU.mult), reads=[k("bim"), k("tab")], writes=[k("t2")])
                    P.op("dve", lambda t=t: V.tensor_tensor(out=t["t1"][:], in0=t["t1"][:], in1=t["t2"][:], op=ALU.add), reads=[k("t1"), k("t2")], writes=[k("t1")])
                    P.op("pool", lambda t=t: G.tensor_tensor(out=t["t2"][:], in0=t["bim"][:], in1=t["CS"][:], op=ALU.mult), reads=[k("bim"), k("tab"), k("t1")], writes=[k("t2")])
                    P.op("pool", lambda t=t: G.tensor_tensor(out=t["bre"][:], in0=t["bre"][:], in1=t["SN"][:], op=ALU.mult), reads=[k("bre"), k("tab"), k("t1")], writes=[k("bre")])
                    P.op("pool", lambda t=t: G.tensor_tensor(out=t["t2"][:], in0=t["t2"][:], in1=t["bre"][:], op=ALU.subtract), reads=[k("t2"), k("bre")], writes=[k("t2")])
                    P.op("dve", lambda t=t: V.tensor_tensor_scan(out=t["zre"][:], data0=t["RHO"][:], data1=t["t1"][:], initial=t["zi"][:, 0:1],
                                                                  op0=ALU.mult, op1=ALU.add), reads=[k("t1"), k("rho"), k("zi")], writes=[k("zre")])
                    P.op("dve", lambda t=t: V.tensor_tensor_scan(out=t["zim"][:], data0=t["RHO"][:], data1=t["t2"][:], initial=t["zi"][:, 1:2],
                                                                  op0=ALU.mult, op1=ALU.add), reads=[k("t2"), k("rho"), k("zi")], writes=[k("zim")])
                    cdt, cth, crho, ca, ctmp, c512s, c512c, cx = [t["cw"][i][:] for i in range(8)]
                    zl_re, zl_im = t["zre"][:, 511:512], t["zim"][:, 511:512]
                    P.op("dve", lambda t=t, zl_re=zl_re: V.tensor_tensor(out=t["zt"][:, 0:1], in0=zl_re, in1=c512c, op=ALU.mult), reads=[k("zre"), k("colw")], writes=[k("zt")])
                    P.op("dve", lambda t=t, zl_im=zl_im: V.tensor_tensor(out=t["zt"][:, 1:2], in0=zl_im, in1=c512s, op=ALU.mult), reads=[k("zim"), k("colw")], writes=[k("zt")])
                    P.op("dve", lambda t=t: V.tensor_tensor(out=t["zi"][:, 0:1], in0=t["zt"][:, 0:1], in1=t["zt"][:, 1:2], op=ALU.subtract), reads=[k("zt"), k("zi")], writes=[k("zi")])
                    P.op("dve", lambda t=t, zl_re=zl_re: V.tensor_tensor(out=t["zt"][:, 0:1], in0=zl_re, in1=c512s, op=ALU.mult), reads=[k("zre"), k("colw"), k("zi")], writes=[k("zt")])
                    P.op("dve", lambda t=t, zl_im=zl_im: V.tensor_tensor(out=t["zt"][:, 1:2], in0=zl_im, in1=c512c, op=ALU.mult), reads=[k("zim"), k("colw")], writes=[k("zt")])
                    P.op("dve", lambda t=t: V.tensor_tensor(out=t["zi"][:, 1:2], in0=t["zt"][:, 0:1], in1=t["zt"][:, 1:2], op=ALU.add), reads=[k("zt"), k("zi")], writes=[k("zi")])
                    P.op("dve", lambda t=t: V.tensor_tensor(out=t["xre"][:], in0=t["zre"][:], in1=t["CS"][:], op=ALU.mult), reads=[k("zre"), k("tab")], writes=[k("xre")])
                    P.op("pool", lambda t=t: G.tensor_tensor(out=t["t1"][:], in0=t["zim"][:], in1=t["SN"][:], op=ALU.mult), reads=[k("zim"), k("tab"), k("zre")], writes=[k("t1")])
                    P.op("dve", lambda t=t: V.tensor_tensor(out=t["xre"][:], in0=t["xre"][:], in1=t["t1"][:], op=ALU.subtract), reads=[k("xre"), k("t1")], writes=[k("xre")])
                    P.op("pool", lambda t=t: G.tensor_tensor(out=t["xim"][:], in0=t["zre"][:], in1=t["SN"][:], op=ALU.mult), reads=[k("zre"), k("tab")], writes=[k("xim")])
                    P.op("pool", lambda t=t: G.tensor_tensor(out=t["t2"][:], in0=t["zim"][:], in1=t["CS"][:], op=ALU.mult), reads=[k("zim"), k("tab"), k("zim")], writes=[k("t2")])
                    P.op("pool", lambda t=t: G.tensor_tensor(out=t["xim"][:], in0=t["xim"][:], in1=t["t2"][:], op=ALU.add), reads=[k("xim"), k("t2")], writes=[k("xim")])

                    yb_ = 7 if pr == 0 else 3

                    def mmy(t=t, pr=pr, yb_=yb_):
                        T.matmul(B[yb_][0:32, :], lhsT=t["CrBD"][:], rhs=t["xre"][:], start=True, stop=False)
                        return T.matmul(B[yb_][0:32, :], lhsT=t["CiBD"][:], rhs=t["xim"][:], start=False, stop=True)
                    P.op("pe", mmy, reads=[k("xre"), k("xim"), k("CrBD"), k("CiBD")], writes=[f"b{yb_}"])
                    P.op("dve", lambda pr=pr, yb_=yb_: V.scalar_tensor_tensor(out=yT[pr][:], in0=uT[pr][:], scalar=d_col[pr][:, 0:1], in1=B[yb_][0:32, :],
                                                                             op0=ALU.mult, op1=ALU.add),
                         reads=[f"b{yb_}", ("uT", pr), ("dcol", pr)], writes=[("yT", pr)])
                for pr_ in range(2):
                    s5_stream(pr_)
                pvy = B[4][:, 0:256].rearrange("p (j d) -> p j d", j=4)

                def try4(pvy=pvy):
                    for pr in range(2):
                        for j in range(4):
                            ins = T.transpose(out=pvy[:, j, pr * 32:(pr + 1) * 32], in_=yT[pr][:, j * 128:(j + 1) * 128], identity=ident32[0:32, 0:32])
                    return ins
                P.op("pe", try4, reads=[("yT", 0), ("yT", 1), "ident32"], writes=["b4"])
                P.op("act", lambda pvy=pvy, hs=hs: A.copy(out=yc_tm[hs][:], in_=pvy), reads=["b4"], writes=[("yc_tm", hs)])
                dma(P, "poolq", yc_o[cols, :].rearrange("(j p) d -> p j d", p=128), yc_tm[hs][:], reads=[("yc_tm", hs)], writes=[("yc_o", st)])
                outs.append(("yc_o", st))
            if do_gdn:
                gdn_supertile(P, nc, B, st, hs, hk, hTt, wg_t, wt_t, cvw_t, xraw, cvt, qkv, sqn, rn_, Bones, eps_rms, alog_t, dtb_t, ng_t,
                              Tri, SL, Mask2, ones64, ident32, Sst, gd, ybuf, yb_o, outs)

        if do_gdn:
            gdn_round(P, gd, [], yb_o, outs)
        if do_attn:
            scale = 32 ** -0.5
            allqk = [("QT", s_) for s_ in range(nst)] + [("KT", s_) for s_ in range(nst)] + [("V", s_, p_) for s_ in range(nst) for p_ in range(2)] + ["Vones"]
            cnt = 0
            for u in range(3):
                for qt in range(nst):
                    nkb = 4 * (qt + 1)
                    bo = 3 + (qt % 2)
                    pend = []

                    def issue_s(kb, u=u, qt=qt):
                        nonlocal cnt
                        slot = SBANKS[cnt % len(SBANKS)]
                        ps_ = cnt % NPT
                        cnt += 1
                        P.op("pe", lambda: T.matmul(B[slot][:], lhsT=KT[32 * u:32 * u + 32, kb * 128:(kb + 1) * 128],
                                                    rhs=QT[32 * u:32 * u + 32, qt * 512:(qt + 1) * 512], start=True, stop=True),
                             reads=allqk, writes=[f"b{slot}"])
                        P.op("act", lambda: A.activation(out=PT[ps_][:], in_=B[slot][:], func=AF.Exp, scale=scale),
                             reads=[f"b{slot}"], writes=[("PT", ps_)])
                        if kb >= 4 * qt:
                            j = kb - 4 * qt
                            P.op("pool", lambda: G.tensor_tensor(out=PT[ps_][:], in0=PT[ps_][:], in1=am_t[:, j, :], op=ALU.mult),
                                 reads=[("PT", ps_), "amask"], writes=[("PT", ps_)])
                        return ps_

                    def issue_av(kb, ps_, u=u, bo=bo, nkb=nkb):
                        P.op("pe", lambda: T.matmul(B[bo][0:65, :], lhsT=Vall[:, kb, u, :], rhs=PT[ps_][:], start=(kb == 0), stop=(kb == nkb - 1)),
                             reads=[("PT", ps_)] + allqk, writes=[f"b{bo}"])
                    for kb in range(nkb):
                        pend.append((kb, issue_s(kb)))
                        if len(pend) > ADEPTH:
                            issue_av(*pend.pop(0))
                    while pend:
                        issue_av(*pend.pop(0))
                    P.op("act", lambda bo=bo: A.copy(out=osb[:], in_=B[bo][0:65, :]), reads=[f"b{bo}"], writes=["osb"])
                    pvo = B[5][:, 0:260].rearrange("p (j d) -> p j d", j=4)

                    def tro(pvo=pvo):
                        for j in range(4):
                            ins = T.transpose(out=pvo[:, j, :], in_=osb[:, j * 128:(j + 1) * 128], identity=ident32[0:65, 0:65])
                        return ins
                    P.op("pe", tro, reads=["osb", "ident32"], writes=["b5"])
                    P.op("dve", lambda pvo=pvo: V.reciprocal(out=rec[:], in_=pvo[:, :, 64]), reads=["b5"], writes=["rec"])
                    os_ = qt % 2
                    P.op("dve", lambda pvo=pvo, os_=os_: V.tensor_tensor(out=oT[os_][:], in0=pvo[:, :, 0:64],
                                                                        in1=rec[:].unsqueeze(2).to_broadcast([128, 4, 64]), op=ALU.mult),
                         reads=["b5", "rec"], writes=[("oT", os_)])
                    dma(P, "sp", ya_o[qt * 512:(qt + 1) * 512, u, :].rearrange("(j p) d -> p j d", p=128), oT[os_][:],
                        reads=[("oT", os_)], writes=[("ya_o", u, qt)])
                    outs.append(("ya_o", u, qt))
        P.emit(final_wait_keys=outs)


def gdn_supertile(P, nc, B, st, hs, hk, hTt, wg_t, wt_t, cvw_t, xraw, cvt, qkv, sqn, rn_, Bones, eps_rms, nA_t, dtb_t, ng_t,
                  Tri, SL, Mask2, ones64, ident32, Sst, gd, ybuf, yb_o, outs):
    V, G, A, T = nc.vector, nc.gpsimd, nc.scalar, nc.tensor
    for c in range(3):
        def mm(c=c):
            for kc in range(8):
                ins = T.matmul(B[c][:], lhsT=wg_t[:, kc, c * 128:(c + 1) * 128], rhs=hTt[hs][:, kc, :], start=(kc == 0), stop=(kc == 7))
            return ins
        P.op("pe", mm, reads=[hk, "wg"], writes=[f"b{c}"])
        P.op("pool", lambda c=c: G.tensor_copy(out=xraw[c][:, 0:3], in_=xraw[c][:, 512:515]), reads=[("xraw", c)], writes=[("xraw", c)])
        P.op("act", lambda c=c: A.copy(out=xraw[c][:, 3:515], in_=B[c][:]), reads=[f"b{c}", ("xraw", c)], writes=[("xraw", c)])
        P.op("dve", lambda c=c: V.tensor_scalar(out=cvt[:], in0=xraw[c][:, 0:512], scalar1=cvw_t[:, c, 0:1], scalar2=None, op0=ALU.mult),
             reads=[("xraw", c), "cvw"], writes=["cvt"])
        for kk in range(1, 4):
            P.op("dve", lambda c=c, kk=kk: V.scalar_tensor_tensor(out=cvt[:], in0=xraw[c][:, kk:kk + 512], scalar=cvw_t[:, c, kk:kk + 1],
                                                                  in1=cvt[:], op0=ALU.mult, op1=ALU.add),
                 reads=[("xraw", c), "cvw", "cvt"], writes=["cvt"])
        P.op("act", lambda c=c: A.activation(out=qkv[c][:], in_=cvt[:], func=AF.Silu), reads=["cvt"], writes=[("qkv", c)])
    for c in range(2):
        P.op("pool", lambda c=c: G.tensor_tensor(out=sqn[:], in0=qkv[c][:], in1=qkv[c][:], op=ALU.mult), reads=[("qkv", c)], writes=["sqn"])
        P.op("pe", lambda: T.matmul(B[3][:], lhsT=Bones[:], rhs=sqn[:], start=True, stop=True), reads=["sqn", "Bones"], writes=["b3"])
        P.op("act", lambda: A.activation(out=rn_[:], in_=B[3][:], func=AF.Sqrt, bias=eps_rms[:, 0:1], scale=1.0), reads=["b3", "eps_rms"], writes=["rn"])
        P.op("dve", lambda: V.reciprocal(out=rn_[:], in_=rn_[:]), reads=["rn"], writes=["rn"])
        if c == 0:
            P.op("dve", lambda: V.scalar_tensor_tensor(out=qkv[0][:], in0=qkv[0][:], scalar=0.125, in1=rn_[:], op0=ALU.mult, op1=ALU.mult),
                 reads=[("qkv", 0), "rn"], writes=[("qkv", 0)])
        else:
            P.op("dve", lambda: V.tensor_tensor(out=qkv[1][:], in0=qkv[1][:], in1=rn_[:], op=ALU.mult), reads=[("qkv", 1), "rn"], writes=[("qkv", 1)])
    qk_all = [("qkv", 0), ("qkv", 1), ("qkv", 2)]
    yb_s = st % 2
    for cp in range(4):
        new = []
        for c in (2 * cp, 2 * cp + 1):
            cg = st * 8 + c
            cs = slice(c * 64, (c + 1) * 64)
            dch = gd["ch"][cg % 4]
            kch = lambda n, cg=cg: ("gch", cg % 4, n)

            def mmt(cs=cs):
                for kc in range(8):
                    ins = T.matmul(B[0][0:64, 0:132], lhsT=hTt[hs][:, kc, cs], rhs=wt_t[:, kc, :], start=(kc == 0), stop=(kc == 7))
                return ins
            mk = ["b0"]
            P.op("pe", mmt, reads=[hk, "wt"], writes=mk)
            P.op("act", lambda dch=dch: A.activation(out=dch["gs"][:], in_=B[0][0:64, 0:128], func=AF.Silu), reads=mk, writes=[kch("gs")])
            P.op("act", lambda dch=dch: A.activation(out=dch["bg"][:, 0:2], in_=B[0][0:64, 128:130], func=AF.Sigmoid), reads=mk, writes=[kch("bg")])
            P.op("dve", lambda dch=dch: V.tensor_tensor(out=dch["bg"][:, 2:4], in0=B[0][0:64, 130:132], in1=dtb_t[:], op=ALU.add),
                 reads=mk + ["dtb", kch("bg")], writes=[kch("bg")])
            P.op("act", lambda dch=dch: A.activation(out=dch["bg"][:, 2:4], in_=dch["bg"][:, 2:4], func=AF.Exp), reads=[kch("bg")], writes=[kch("bg")])
            P.op("act", lambda dch=dch: A.activation(out=dch["bg"][:, 2:4], in_=dch["bg"][:, 2:4], func=AF.Ln, bias=1.0, scale=1.0), reads=[kch("bg")], writes=[kch("bg")])
            P.op("dve", lambda dch=dch: V.tensor_tensor(out=dch["bg"][:, 2:4], in0=dch["bg"][:, 2:4], in1=nA_t[:], op=ALU.mult),
                 reads=[kch("bg"), "alog"], writes=[kch("bg")])
            P.op("dve", lambda dch=dch: V.tensor_scalar(out=dch["nbeta"][:], in0=dch["bg"][:, 0:2], scalar1=-1.0, scalar2=None, op0=ALU.mult),
                 reads=[kch("bg")], writes=[kch("nbeta")])
            sl0 = (cg % 4) * 2
            new.append([gdn_chunk_head(P, nc, B, h, cs, c, dch, kch, gd["hd"][sl0 + h], sl0 + h, 4 + (cg % 2) * 2 + h, qkv, qk_all, ng_t, Tri, SL, Mask2,
                                       ones64, ident32, Sst, eps_rms, ybuf[yb_s], yb_s) for h in range(2)])
        gdn_round(P, gd, new, yb_o, outs)
        if cp == 3:
            gd["pend_dma"] = (st, yb_s, ybuf[yb_s])


def gdn_round(P, gd, new, yb_o, outs):
    oldg = list(gd.get("pendB", []))
    had_old = bool(oldg)
    actA = [g for grp in new for g in grp]
    curB = oldg.pop(0) if oldg else []
    while actA or curB:
        for g in list(actA):
            try:
                r = next(g)
            except StopIteration:
                raise RuntimeError("chain ended inside stage A")
            if r == "END_A":
                actA.remove(g)
        for g in list(curB):
            try:
                next(g)
            except StopIteration:
                curB.remove(g)
        if not curB and oldg:
            curB = oldg.pop(0)
    gd["pendB"] = [list(grp) for grp in new]
    pd = gd.get("pend_dma")
    if pd is not None and had_old:
        st, yb_s, ybt = pd
        cols = slice(st * 512, (st + 1) * 512)
        dma(P, "poolq", yb_o[cols].rearrange("(c p) h d -> p c h d", p=64), ybt[:], reads=[("ybuf", yb_s, c_, h_) for c_ in range(8) for h_ in range(2)],
            writes=[("yb_o", st)])
        outs.append(("yb_o", st))
        gd["pend_dma"] = None


def gdn_chunk_head(P, nc, B, h, cs, c, dch, kch, d, sl, bank, qkv, qk_all, ng_t, Tri, SL, Mask2, ones64, ident32, Sst, eps_rms, ybuf, yb_s):
    V, G, A, T = nc.vector, nc.gpsimd, nc.scalar, nc.tensor
    hp = slice(h * 64, (h + 1) * 64)
    idh = ident32[hp, hp]
    id0 = ident32[0:64, 0:64]
    k = lambda n: ("ghd", sl, n)
    PA, P3 = B[bank], B[3]
    bk, b3 = [f"b{bank}"], ["b3"]
    o3 = 256 * h
    W = d["W"]
    g_col = dch["bg"][:, 2 + h:3 + h]
    beta_col = dch["bg"][:, h:h + 1]
    nbeta_col = dch["nbeta"][:, h:h + 1]

    def tr1():
        for c3 in range(3):
            ins = T.transpose(out=PA[0:64, c3 * 64:(c3 + 1) * 64], in_=qkv[c3][hp, cs], identity=idh)
        return ins
    P.op("pe", tr1, reads=qk_all + ["ident32"], writes=bk); yield
    P.op("act", lambda: A.copy(out=d["qkv_tm"][:].rearrange("p a b -> p (a b)"), in_=PA[0:64, 0:192]), reads=bk, writes=[k("qkv_tm")]); yield

    def mm2():
        T.matmul(PA[0:64, 256:257], lhsT=Tri[:], rhs=g_col, start=True, stop=True)
        return T.matmul(PA[0:64, 257:258], lhsT=ones64[:], rhs=g_col, start=True, stop=True)
    P.op("pe", mm2, reads=[kch("bg"), "Tri", "ones64"], writes=bk); yield
    P.op("dve", lambda: V.tensor_copy(out=d["gcl"][:], in_=PA[0:64, 256:258]), reads=bk, writes=[k("gcl")]); yield
    P.op("act", lambda: A.activation(out=d["ex3"][:, 0:1], in_=d["gcl"][:, 0:1], func=AF.Exp), reads=[k("gcl")], writes=[k("ex3")]); yield
    P.op("act", lambda: A.activation(out=d["ex3"][:, 1:2], in_=d["gcl"][:, 0:1], func=AF.Exp, bias=d["gcl"][:, 1:2], scale=-1.0),
         reads=[k("gcl"), k("ex3")], writes=[k("ex3")]); yield
    P.op("act", lambda: A.activation(out=d["ex3"][:, 2:3], in_=d["gcl"][:, 1:2], func=AF.Exp), reads=[k("gcl"), k("ex3")], writes=[k("ex3")]); yield
    P.op("dve", lambda: V.tensor_scalar(out=d["Gm"][:], in0=Tri[:], scalar1=g_col, scalar2=None, op0=ALU.mult), reads=["Tri", kch("bg")], writes=[k("Gm")]); yield

    def mm3():
        T.matmul(PA[0:64, 384:448], lhsT=d["Gm"][:], rhs=SL[:], start=True, stop=True)
        return T.matmul(PA[0:64, 448:512], lhsT=SL[:], rhs=d["Gm"][:], start=True, stop=True)
    P.op("pe", mm3, reads=[k("Gm"), "SL"], writes=bk); yield
    P.op("act", lambda: A.activation(out=d["EE"][:].rearrange("p a b -> p (a b)"), in_=PA[0:64, 384:512], func=AF.Exp), reads=bk, writes=[k("EE")]); yield
    P.op("pool", lambda: G.tensor_tensor(out=d["EE"][:], in0=d["EE"][:], in1=Mask2[:], op=ALU.mult), reads=[k("EE"), "Mask2"], writes=[k("EE")]); yield

    def mm4():
        T.matmul(PA[0:64, 0:64], lhsT=qkv[1][hp, cs], rhs=qkv[1][hp, cs], start=True, stop=True)
        return T.matmul(PA[0:64, 64:128], lhsT=qkv[1][hp, cs], rhs=qkv[0][hp, cs], start=True, stop=True)
    P.op("pe", mm4, reads=qk_all, writes=bk); yield
    P.op("dve", lambda: V.scalar_tensor_tensor(out=W[0][:, 128:192], in0=PA[0:64, 0:64], scalar=nbeta_col, in1=d["EE"][:, 0, :], op0=ALU.mult, op1=ALU.mult),
         reads=bk + [kch("nbeta"), k("EE")], writes=[k("W0p")]); yield
    P.op("dve", lambda: V.tensor_tensor(out=d["AT"][:], in0=PA[0:64, 64:128], in1=d["EE"][:, 1, :], op=ALU.mult), reads=bk + [k("EE")], writes=[k("AT")]); yield
    P.op("pe", lambda: T.transpose(out=PA[0:64, 192:256], in_=W[0][:, 128:192], identity=id0), reads=[k("W0p"), "ident32"], writes=bk); yield
    P.op("act", lambda: A.copy(out=W[0][:, 192:256], in_=PA[0:64, 192:256]), reads=bk, writes=[k("W0t")]); yield
    P.op("dve", lambda: V.tensor_tensor(out=d["tb"][:], in0=beta_col, in1=d["ex3"][:, 0:1], op=ALU.mult), reads=[kch("bg"), k("ex3")], writes=[k("tb")]); yield
    P.op("dve", lambda: V.tensor_scalar(out=W[0][:, 0:64], in0=d["qkv_tm"][:, 2, :], scalar1=beta_col, scalar2=None, op0=ALU.mult),
         reads=[k("qkv_tm"), kch("bg")], writes=[k("W0x")]); yield
    P.op("dve", lambda: V.tensor_scalar(out=W[0][:, 64:128], in0=d["qkv_tm"][:, 1, :], scalar1=d["tb"][:, 0:1], scalar2=None, op0=ALU.mult),
         reads=[k("qkv_tm"), k("tb"), k("W0x")], writes=[k("W0x")]); yield
    wk = [[k("W0x"), k("W0p"), k("W0t")], [k("W1")]]
    for lvl in range(6):
        s_, d_ = W[lvl % 2], W[(lvl + 1) % 2]
        last = lvl == 5

        def mml(s_=s_, last=last):
            T.matmul(PA[0:64, 256:384], lhsT=s_[:, 192:256], rhs=s_[:, 0:128], start=True, stop=False)
            ins = T.matmul(PA[0:64, 256:384], lhsT=id0, rhs=s_[:, 0:128], start=False, stop=True)
            if not last:
                T.matmul(PA[0:64, 384:448], lhsT=s_[:, 192:256], rhs=s_[:, 128:192], start=True, stop=True)
                ins = T.matmul(PA[0:64, 448:512], lhsT=s_[:, 128:192], rhs=s_[:, 192:256], start=True, stop=True)
            return ins
        P.op("pe", mml, reads=wk[lvl % 2] + ["ident32"], writes=bk); yield
        n_ = 128 if last else 256
        wkeys = [k("W1")] if (lvl + 1) % 2 == 1 else [k("W0x"), k("W0p"), k("W0t")]
        if lvl % 2 == 0:
            P.op("act", lambda d_=d_, n_=n_: A.copy(out=d_[:, 0:n_], in_=PA[0:64, 256:256 + n_]), reads=bk, writes=wkeys); yield
        else:
            P.op("dve", lambda d_=d_, n_=n_: V.tensor_copy(out=d_[:, 0:n_], in_=PA[0:64, 256:256 + n_]), reads=bk, writes=wkeys); yield
    X = W[0]
    xk = [k("W0x"), k("W0p"), k("W0t")]
    P.op("pool", lambda: G.tensor_scalar(out=d["kdec"][:], in0=d["qkv_tm"][:, 1, :], scalar1=d["ex3"][:, 1:2], scalar2=None, op0=ALU.mult),
         reads=[k("qkv_tm"), k("ex3")], writes=[k("kdec")]); yield
    P.op("pool", lambda: G.tensor_scalar(out=d["qdec"][:], in0=d["qkv_tm"][:, 0, :], scalar1=d["ex3"][:, 0:1], scalar2=None, op0=ALU.mult),
         reads=[k("qkv_tm"), k("ex3")], writes=[k("qdec")]); yield

    def tr8():
        T.transpose(out=PA[0:64, 0:64], in_=X[:, 64:128], identity=id0)
        return T.transpose(out=PA[0:64, 64:128], in_=d["qdec"][:], identity=id0)
    P.op("pe", tr8, reads=xk + [k("qdec"), "ident32"], writes=bk); yield
    P.op("act", lambda: A.copy(out=d["wqT"][:].rearrange("p a b -> p (a b)"), in_=PA[0:64, 0:128]), reads=bk, writes=[k("wqT")]); yield
    P.op("pool", lambda: G.tensor_tensor(out=d["ngate"][:], in0=dch["gs"][:, h * 64:(h + 1) * 64], in1=ng_t[:], op=ALU.mult),
         reads=[kch("gs"), "ngt"], writes=[k("ngate")]); yield
    yield "END_A"
    S_ = Sst[h]
    P.op("pe", lambda: T.matmul(P3[0:64, o3:o3 + 64], lhsT=d["wqT"][:, 0, :], rhs=S_[:], start=True, stop=True), reads=[k("wqT"), ("S", h)], writes=b3); yield
    P.op("dve", lambda: V.tensor_tensor(out=d["vnew"][:], in0=X[:, 0:64], in1=P3[0:64, o3:o3 + 64], op=ALU.subtract),
         reads=b3 + xk, writes=[k("vnew")]); yield

    def mmo():
        T.matmul(P3[0:64, o3 + 64:o3 + 128], lhsT=d["wqT"][:, 1, :], rhs=S_[:], start=True, stop=False)
        T.matmul(P3[0:64, o3 + 64:o3 + 128], lhsT=d["AT"][:], rhs=d["vnew"][:], start=False, stop=True)
        return T.matmul(P3[0:64, o3 + 128:o3 + 192], lhsT=d["kdec"][:], rhs=d["vnew"][:], start=True, stop=True)
    P.op("pe", mmo, reads=[k("wqT"), ("S", h), k("AT"), k("vnew"), k("kdec")], writes=b3); yield
    P.op("dve", lambda: V.scalar_tensor_tensor(out=S_[:], in0=S_[:], scalar=d["ex3"][:, 2:3], in1=P3[0:64, o3 + 128:o3 + 192], op0=ALU.mult, op1=ALU.add),
         reads=b3 + [("S", h), k("ex3")], writes=[("S", h)]); yield
    P.op("act", lambda: A.copy(out=d["osb"][:], in_=P3[0:64, o3 + 64:o3 + 128]), reads=b3, writes=[k("osb")]); yield
    P.op("pool", lambda: G.tensor_tensor(out=d["osq"][:], in0=d["osb"][:], in1=d["osb"][:], op=ALU.mult), reads=[k("osb")], writes=[k("osq")]); yield
    P.op("dve", lambda: V.tensor_reduce(out=d["oss"][:], in_=d["osq"][:], axis=AX.X, op=ALU.add), reads=[k("osq")], writes=[k("oss")]); yield
    P.op("act", lambda: A.activation(out=d["oss"][:], in_=d["oss"][:], func=AF.Sqrt, bias=eps_rms[0:64, 0:1], scale=1.0 / 64),
         reads=[k("oss"), "eps_rms"], writes=[k("oss")]); yield
    P.op("dve", lambda: V.reciprocal(out=d["oss"][:], in_=d["oss"][:]), reads=[k("oss")], writes=[k("oss")]); yield
    P.op("dve", lambda: V.scalar_tensor_tensor(out=ybuf[:, c, h, :], in0=d["osb"][:], scalar=d["oss"][:, 0:1], in1=d["ngate"][:], op0=ALU.mult, op1=ALU.mult),
         reads=[k("osb"), k("oss"), k("ngate")], writes=[("ybuf", yb_s, c, h)]); yield


OFF_AQ, OFF_AK, OFF_AV, OFF_BQKV, OFF_BGATE, OFF_BBETA, OFF_BA, OFF_CU = 0, 384, 768, 1152, 2304, 2688, 2694, 2700
GDN_HEADS_OF = [(0, 1), (2, 3), (4, 5), (4, 5)]


def _wl(w, cols):
    return w[:, cols].reshape(8, 128, len(cols)).transpose(1, 0, 2)


def mix_consts():
    d = {}
    k = np.arange(128)[:, None, None]; j = np.arange(4)[None, :, None]; q = np.arange(512)[None, None, :]
    d["amask"] = bf((q // 64 >= (j * 128 + k) // 64).astype(np.float32))
    d["idn32"] = f32c(np.eye(128))
    d["srow"] = f32c(np.arange(512).reshape(1, 512))
    m = np.arange(64)[:, None]; i = np.arange(64)[None, :]
    d["cTri"] = f32c(m <= i)
    d["cSL"] = f32c(m > i)
    d["cMask2"] = f32c(np.stack([(m > i), (m <= i)], axis=1))
    bo = np.zeros((128, 128), np.float32); bo[:64, :64] = 1; bo[64:, 64:] = 1
    d["cBones"] = bo
    return d


def mix_inputs(l, p, j):
    w = p["w_in"][l]
    d = {}
    units = [3 * j + i for i in range(3)]
    qc, kc_, vc = [], [], []
    for u in units:
        head, mp = u // 2, u % 2
        qc += list(range(OFF_AQ + head * 64 + mp * 32, OFF_AQ + head * 64 + mp * 32 + 32))
        kc_ += list(range(OFF_AK + head * 64 + mp * 32, OFF_AK + head * 64 + mp * 32 + 32))
        vc += list(range(OFF_AV + head * 64, OFF_AV + head * 64 + 64))
    d["wq"] = bf(_wl(w, qc)); d["wk"] = bf(_wl(w, kc_)); d["wv"] = bf(_wl(w, vc))
    gs = [4 * j + i for i in range(4)]
    d["wu"] = bf(_wl(w, list(range(OFF_CU + gs[0] * 16, OFF_CU + gs[0] * 16 + 64))))
    lre, lim, ldt = p["s5_lambda_re"][l], p["s5_lambda_im"][l], p["s5_log_dt"][l]
    row = np.zeros((2, 3, 128), np.float32)
    bT = np.zeros((2, 2, 2, 16, 64), np.float32); cT = np.zeros((2, 2, 2, 64, 16), np.float32)
    for pr in range(2):
        for g in range(2):
            G_ = gs[pr * 2 + g]
            row[pr, 0, g * 64:(g + 1) * 64] = lre[G_]; row[pr, 1, g * 64:(g + 1) * 64] = lim[G_]; row[pr, 2, g * 64:(g + 1) * 64] = ldt[G_]
            bT[pr, g, 0] = p["s5_b_re"][l][G_].T; bT[pr, g, 1] = p["s5_b_im"][l][G_].T
            cT[pr, g, 0] = p["s5_c_re"][l][G_].T; cT[pr, g, 1] = p["s5_c_im"][l][G_].T
    d["s5row"] = row; d["s5col"] = f32c(row.transpose(0, 2, 1)); d["s5bT"] = bT; d["s5cT"] = cT
    d["s5d"] = f32c(p["s5_d"][l][gs[0] * 16:gs[0] * 16 + 64].reshape(64, 1))
    hA, hB = GDN_HEADS_OF[j]
    gcols = []
    for part in range(3):
        for h in (hA, hB):
            gcols += list(range(OFF_BQKV + part * 384 + h * 64, OFF_BQKV + part * 384 + h * 64 + 64))
    d["wg"] = bf(_wl(w, gcols))
    tcols = list(range(OFF_BGATE + hA * 64, OFF_BGATE + hA * 64 + 64)) + list(range(OFF_BGATE + hB * 64, OFF_BGATE + hB * 64 + 64)) \
        + [OFF_BBETA + hA, OFF_BBETA + hB, OFF_BA + hA, OFF_BA + hB]
    d["wt"] = bf(_wl(w, tcols))
    cw = p["dn_conv_w"][l]
    cv = np.zeros((128, 3, 4), np.float32)
    for part in range(3):
        for hi, h in enumerate((hA, hB)):
            cv[hi * 64:(hi + 1) * 64, part, :] = cw[:, part * 384 + h * 64: part * 384 + h * 64 + 64].T
    d["cvw"] = cv
    d["galog"] = f32c(p["dn_a_log"][l][[hA, hB]].reshape(1, 2)); d["gdtb"] = f32c(p["dn_dt_bias"][l][[hA, hB]].reshape(1, 2))
    d["gng"] = f32c(p["dn_norm_g"][l].reshape(1, 64))
    return d


YCH = 1024
NYCH = SEQ // YCH


def build_program(stop=None):
    nc = bass.Bass("TRN2", target_bir_lowering=False)
    G = Glob(nc)
    hbuf = [G.internal(f"hbuf{i}", [TOK_CORE, D]) for i in range(2)]
    hT_loc = G.internal("hT_loc", [D, TOK_CORE], BF16)
    hT_all = G.internal("hT_all", [8, 4 * 128, TOK_CORE], BF16)
    ya_o = G.internal("ya_o", [SEQ, 192])
    ybc_o = G.internal("ybc_o", [SEQ, 192])
    ya_all = G.internal("ya_all", [NYCH, 4 * YCH, 192])
    ybc_all = G.internal("ybc_all", [NYCH, 4 * YCH, 192])
    ag_h = [(hT_loc[k * 128:(k + 1) * 128, :], hT_all[k]) for k in range(8)]
    ag_ya = [(ya_o[i * YCH:(i + 1) * YCH, :], ya_all[i]) for i in range(NYCH)]
    ag_yb = [(ybc_o[i * YCH:(i + 1) * YCH, :], ybc_all[i]) for i in range(NYCH)]
    out = nc.dram_tensor("out", [TOK_CORE, D], F32, kind="ExternalOutput").ap()
    phase_pre(nc, G, hbuf[0], hT_loc)
    for l in range(DEPTH):
        last = l == DEPTH - 1
        allgather(nc, ag_h)
        phase_mix(nc, G, l, hT_all, ya_o, ybc_o, do_attn=True, do_s5=False, do_gdn=False)
        Prog._uid += 1
        with nc.semaphore(f"ccy_{Prog._uid}") as cc:
            with nc.Block() as block:
                @block.gpsimd
                def _(g):
                    g.sem_clear(cc)

            def pre(g, cc=cc):
                for s_, d_ in ag_ya:
                    g.collective_compute("AllGather", ALU.bypass, replica_groups=GROUPS, ins=[s_], outs=[d_]).then_inc(cc, 1)

            def post(g, cc=cc):
                g.wait_ge(cc, len(ag_ya))
            phase_mix(nc, G, l, hT_all, ya_o, ybc_o, do_attn=False, do_s5=True, do_gdn=True, pool_pre=pre, pool_post=post)
        allgather(nc, ag_yb)
        phase_post(nc, G, l, hbuf[l % 2], out if last else hbuf[(l + 1) % 2], None if last else hT_loc, (ya_all, ybc_all))
    return nc, G


def kernel(_stop=None, **inputs):
    p = {k: np.asarray(v) for k, v in inputs.items()}
    x = f32c(p["x"]).reshape(BATCH * SEQ, D)
    cores = list(range(NCORES))
    nc, G = build_program()
    shared = dict(mix_consts())
    shared["idn"] = bf(np.eye(128))
    shared["g"] = f32c(p["ln_in_g"].reshape(1, D)); shared["b"] = f32c(p["ln_in_b"].reshape(1, D))
    percore = [dict() for _ in range(4)]
    for l in range(DEPTH):
        lam_init = 0.8 - 0.6 * math.exp(-0.3 * l)
        for k, v in post_inputs(l, p, lam_init).items():
            if k not in ("idn", "idn32"):
                shared[f"L{l}_{k}"] = v
        for j in range(4):
            for k, v in mix_inputs(l, p, j).items():
                percore[j][f"L{l}_{k}"] = v
    ins = []
    for c in cores:
        d = dict(shared); d.update(percore[c % 4])
        d["x"] = x[c * TOK_CORE:(c + 1) * TOK_CORE]
        d["rofs"] = np.array([[(c % 4) * (TOK_CORE // YCH)]], np.int32)
        ins.append({k: v for k, v in d.items() if k in G.t})
    res = run_bass_kernel_spmd(nc, ins, core_ids=cores)
    h = [np.asarray(r["out"]) for r in res.results]
    return np.concatenate(h, axis=0).reshape(BATCH, SEQ, D).astype(np.float32)
```

```python
import math
from contextlib import ExitStack

import numpy as np
import ml_dtypes
import concourse.bass as bass
import concourse.mybir as mybir
from concourse.bass_utils import run_bass_kernel_spmd

F32 = mybir.dt.float32
BF16 = mybir.dt.bfloat16
I32 = mybir.dt.int32
ALU = mybir.AluOpType
AF = mybir.ActivationFunctionType
AX = mybir.AxisListType

NCORES = 8


class Prog:
    COMPUTE = ("pe", "act", "dve", "pool")
    NDMASEM = 6

    _uid = 0
    _phase = 0

    def __init__(self, nc):
        Prog._phase += 1
        self.ph = Prog._phase
        self.nc = nc
        self.ops = []
        self.last_w = {}
        self.readers = {}
        self.dma_count = {"sp": 0, "actq": 0, "poolq": 0}
        self.stack = ExitStack()
        self.nt = 0
        self.excl = set()
        self.quarters = False
        self.bankkeys = {f"b{i}" for i in range(8)}
        self.pool_pre = None
        self.pool_post = None
        self.sp_wrap = None

    def sb(self, shape, dtype, name=None):
        Prog._uid += 1
        return self.stack.enter_context(self.nc.sbuf_tensor(f"{name or 't'}_{Prog._uid}", list(shape), dtype))

    def ps(self, shape, dtype=F32, name=None):
        Prog._uid += 1
        return self.stack.enter_context(self.nc.psum_tensor(f"{name or 'p'}_{Prog._uid}", list(shape), dtype))

    def op(self, eng, fn, reads=(), writes=()):
        idx = len(self.ops)
        isdma = eng in self.dma_count
        issue = {"sp": "sp", "actq": "act", "poolq": "pool"}.get(eng, eng)
        if self.quarters:
            ex = lambda ks: [q for k in ks for q in ([f"{k}q{i}" for i in range(4)] if k in self.bankkeys else [k])]
            reads, writes = ex(reads), ex(writes)
        if self.excl:
            writes = list(writes) + [k for k in reads if k in self.excl]
            reads = [k for k in reads if k not in self.excl]
        deps = set()
        for k in reads:
            w = self.last_w.get(k)
            if w is not None:
                deps.add(w)
        for k in writes:
            w = self.last_w.get(k)
            if w is not None:
                deps.add(w)
            for r in self.readers.get(k, ()):
                deps.add(r)
        o = dict(idx=idx, eng=eng, issue=issue, fn=fn, deps=deps, isdma=isdma, needed=False)
        if isdma:
            n = self.dma_count[eng]
            self.dma_count[eng] = n + 1
            o["dsem"] = n % self.NDMASEM
            o["dtarget"] = 16 * (n // self.NDMASEM + 1)
            o["dprev"] = 16 * (n // self.NDMASEM)
        self.ops.append(o)
        for k in writes:
            self.last_w[k] = idx
            self.readers[k] = []
        for k in reads:
            lst = self.readers.setdefault(k, [])
            if not isdma:
                lst[:] = [r for r in lst if self.ops[r]["isdma"] or self.ops[r]["eng"] != eng]
            lst.append(idx)
        return idx

    def emit(self, final_wait_keys=()):
        nc = self.nc
        ops = self.ops
        for o in ops:
            nd = set()
            for d in o["deps"]:
                p = ops[d]
                if (not p["isdma"]) and (not o["isdma"]) and p["eng"] == o["eng"]:
                    if o["eng"] == "pe":
                        continue
                nd.add(d)
            o["deps"] = nd
            for d in nd:
                ops[d]["needed"] = True
        final = [self.last_w[k] for k in final_wait_keys if k in self.last_w]
        for d in final:
            ops[d]["needed"] = True
        tick = {e: 0 for e in self.COMPUTE}
        for o in ops:
            if not o["isdma"] and o["needed"]:
                tick[o["eng"]] += 1
                o["tick"] = tick[o["eng"]]
        sems = {e: self.stack.enter_context(nc.semaphore(f"s_{e}_{self.ph}")) for e in self.COMPUTE}
        dsems = {q: [self.stack.enter_context(nc.semaphore(f"d_{q}{i}_{self.ph}")) for i in range(self.NDMASEM)]
                 for q in self.dma_count}
        per = {e: [] for e in ("pe", "act", "dve", "pool", "sp")}
        for o in ops:
            per[o["issue"]].append(o)
        engobj = {"pe": nc.tensor, "act": nc.scalar, "dve": nc.vector, "pool": nc.gpsimd, "sp": nc.sync}

        def run(ename, extra_final=False):
            eng = engobj[ename]
            waited = {}

            def wait_for(p):
                if p["isdma"]:
                    key = (p["eng"], p["dsem"])
                    val = p["dtarget"]
                    s = dsems[p["eng"]][p["dsem"]]
                else:
                    key = p["eng"]
                    val = p["tick"]
                    s = sems[p["eng"]]
                if waited.get(key, 0) >= val:
                    return
                waited[key] = val
                eng.wait_ge(s, val)

            for o in per[ename]:
                for d in sorted(o["deps"]):
                    wait_for(ops[d])
                if o["isdma"] and o["dprev"] > 0:
                    key = (o["eng"], o["dsem"])
                    if waited.get(key, 0) < o["dprev"]:
                        waited[key] = o["dprev"]
                        eng.wait_ge(dsems[o["eng"]][o["dsem"]], o["dprev"])
                ins = o["fn"]()
                if o["isdma"]:
                    ins.then_inc(dsems[o["eng"]][o["dsem"]], 16)
                elif o["needed"]:
                    ins.then_inc(sems[o["eng"]], 1)
            if extra_final:
                for d in final:
                    wait_for(ops[d])

        allsems = list(sems.values()) + [s for q in dsems.values() for s in q]
        with nc.Block() as block:
            @block.gpsimd
            def _(e):
                for s in allsems:
                    e.sem_clear(s)

        with nc.Block() as block:
            @block.sync
            def _(e):
                if self.sp_wrap is not None:
                    with self.sp_wrap(e):
                        run("sp", extra_final=True)
                else:
                    run("sp", extra_final=True)

            @block.tensor
            def _(e):
                run("pe")

            @block.scalar
            def _(e):
                run("act")

            @block.vector
            def _(e):
                run("dve")

            @block.gpsimd
            def _(e):
                if self.pool_pre is not None:
                    self.pool_pre(e)
                run("pool")
                if self.pool_post is not None:
                    self.pool_post(e)


D = 1024
SEQ = 16384
BATCH = 2
DEPTH = 2
TOK_CORE = 4096
ALPHA = (2 * DEPTH) ** 0.25
LN_EPS = 1e-5
RMS_EPS = 1e-6
NEXP = 16
DEXP = 512


def bf(a):
    return np.ascontiguousarray(np.asarray(a, np.float32).astype(ml_dtypes.bfloat16))


def f32c(a):
    return np.ascontiguousarray(np.asarray(a, np.float32))


class Ctx:
    def __init__(self, nc):
        self.nc = nc
        self.P = Prog(nc)
        self.banks = None

    def alloc_banks(self, quarters=False):
        self.banks = [self.P.ps([128, 512], F32, name=f"bank{i}") for i in range(8)]
        self.P.excl |= {f"b{i}" for i in range(8)}
        if quarters:
            self.P.quarters = True
            self.P.excl |= {f"b{i}q{q}" for i in range(8) for q in range(4)}


class Glob:
    def __init__(self, nc):
        self.nc = nc
        self.t = {}

    def din(self, name, shape, dt=F32):
        if name not in self.t:
            self.t[name] = self.nc.dram_tensor(name, list(shape), dt, kind="ExternalInput").ap()
        return self.t[name]

    def internal(self, name, shape, dt=F32):
        if name not in self.t:
            self.t[name] = self.nc.dram_tensor(name, list(shape), dt).ap()
        return self.t[name]


GROUPS = [[0, 1, 2, 3], [4, 5, 6, 7]]


def allgather(nc, pairs):
    Prog._uid += 1
    with nc.semaphore(f"cc_{Prog._uid}") as cc:
        with nc.Block() as block:
            @block.gpsimd
            def _(g):
                g.sem_clear(cc)
        with nc.Block() as block:
            @block.gpsimd
            def _(g):
                for src, dst in pairs:
                    g.collective_compute("AllGather", ALU.bypass, replica_groups=GROUPS, ins=[src], outs=[dst]).then_inc(cc, 1)
                g.wait_ge(cc, len(pairs))


def dma(P, q, out, in_, reads=(), writes=()):
    eng = {"sp": P.nc.sync, "actq": P.nc.scalar, "poolq": P.nc.gpsimd}[q]
    return P.op(q, lambda: eng.dma_start(out=out, in_=in_), reads=reads, writes=writes)


def layer_norm_tile(P, nc, src, srck, dst, dstk, g_t, b_t, gk, bk, scr, tag):
    st, mv, rstd, xn = scr["st"], scr["mv"], scr["rstd"], scr["xn"]

    def bs():
        nc.vector.bn_stats(out=st[:, 0, :], in_=src[:, 0:512])
        return nc.vector.bn_stats(out=st[:, 1, :], in_=src[:, 512:1024])
    P.op("dve", bs, reads=[srck], writes=[tag + "st"])
    P.op("dve", lambda: nc.vector.bn_aggr(out=mv[:], in_=st[:].rearrange("p a s -> p (a s)")),
         reads=[tag + "st"], writes=[tag + "mv"])
    P.op("act", lambda: nc.scalar.activation(out=rstd[:], in_=mv[:, 1:2], func=AF.Sqrt, bias=scr["eps_ln"][:, 0:1], scale=1.0),
         reads=[tag + "mv", "eps_ln"], writes=[tag + "rstd"])
    P.op("dve", lambda: nc.vector.reciprocal(out=rstd[:], in_=rstd[:]), reads=[tag + "rstd"], writes=[tag + "rstd"])
    P.op("dve", lambda: nc.vector.tensor_scalar(out=xn[:], in0=src[:], scalar1=mv[:, 0:1], scalar2=rstd[:, 0:1],
                                                op0=ALU.subtract, op1=ALU.mult),
         reads=[srck, tag + "mv", tag + "rstd"], writes=[tag + "xn"])
    P.op("pool", lambda: nc.gpsimd.tensor_tensor(out=xn[:], in0=xn[:], in1=g_t[:], op=ALU.mult),
         reads=[tag + "xn", gk], writes=[tag + "xn"])
    P.op("pool", lambda: nc.gpsimd.tensor_tensor(out=dst[:], in0=xn[:], in1=b_t[:], op=ALU.add),
         reads=[tag + "xn", bk], writes=[dstk])


def ln_scratch(P, tag):
    return dict(st=P.sb([128, 2, 6], F32), mv=P.sb([128, 2], F32), rstd=P.sb([128, 1], F32),
                xn=P.sb([128, 1024], F32))


def const_col(P, nc, val, key):
    t = P.sb([128, 1], F32)
    P.op("pool", lambda: nc.gpsimd.memset(t[:], val), writes=[key])
    return t


def to_featmajor_bf16(P, nc, src, srck, hb, hbk, bank, bankk, dstT, dstk, ident, cast_eng="act"):
    if cast_eng == "act":
        P.op("act", lambda: nc.scalar.copy(out=hb[:], in_=src[:]), reads=[srck], writes=[hbk])
    else:
        P.op("pool", lambda: nc.gpsimd.tensor_copy(out=hb[:], in_=src[:]), reads=[srck], writes=[hbk])
    pv = bank[:].bitcast(BF16).rearrange("p (k t) -> p k t", k=8)

    def tr():
        for kc in range(8):
            ins = nc.tensor.transpose(out=pv[:, kc, :], in_=hb[:, kc * 128:(kc + 1) * 128], identity=ident[:])
        return ins
    P.op("pe", tr, reads=[hbk, "ident"], writes=[bankk])
    P.op("dve", lambda: nc.vector.tensor_copy(out=dstT, in_=pv), reads=[bankk], writes=[dstk])


def phase_pre(nc, G, h, hT_loc, ntok=TOK_CORE):
    C = Ctx(nc)
    P = C.P
    nt = ntok // 128
    x = G.din("x", [ntok, D])
    g = G.din("g", [1, D])
    b = G.din("b", [1, D])
    idn = G.din("idn", [128, 128], BF16)
    hT = hT_loc.rearrange("(k p) t -> k p t", k=8)
    with P.stack:
        C.alloc_banks()
        gt = P.sb([128, D], F32)
        bt = P.sb([128, D], F32)
        ident = P.sb([128, 128], BF16)
        eps = const_col(P, nc, LN_EPS, "eps_ln")
        xt = [P.sb([128, D], F32) for _ in range(2)]
        ht = [P.sb([128, D], F32) for _ in range(2)]
        hb = P.sb([128, D], BF16)
        hTt = [P.sb([128, 8, 128], BF16) for _ in range(2)]
        scr = ln_scratch(P, "ln")
        scr["eps_ln"] = eps
        dma(P, "sp", gt[:], g.partition_broadcast(128), writes=["g"])
        dma(P, "sp", bt[:], b.partition_broadcast(128), writes=["b"])
        dma(P, "sp", ident[:], idn, writes=["ident"])
        outs = []
        for t in range(nt):
            s = t % 2
            dma(P, "sp", xt[s][:], x[t * 128:(t + 1) * 128, :], writes=[("xt", s)])
            layer_norm_tile(P, nc, xt[s], ("xt", s), ht[s], ("ht", s), gt, bt, "g", "b", scr, "ln")
            dma(P, "poolq", h[t * 128:(t + 1) * 128, :], ht[s][:], reads=[("ht", s)], writes=[("h", t)])
            to_featmajor_bf16(P, nc, ht[s], ("ht", s), hb, "hb", C.banks[s], f"b{s}", hTt[s][:], ("hTt", s), ident)
            dma(P, "sp", hT[:, :, t * 128:(t + 1) * 128].rearrange("k p t -> p k t"), hTt[s][:],
                reads=[("hTt", s)], writes=[("hT", t)])
            outs += [("h", t), ("hT", t)]
        P.emit(final_wait_keys=outs)


BIG = 1.0e4


def phase_post(nc, G, l, h_in, h_out, hT_loc, y_all, ntok=TOK_CORE):
    C = Ctx(nc)
    P = C.P
    nsup = ntok // 512
    pf = f"L{l}_"

    def din(name, shape, dt=F32):
        return G.din(pf + name, shape, dt)
    wout = din("wout", [128, 8, D], BF16)
    wglu = din("wglu", [128, 2, 256], BF16)
    ln1g = din("ln1g", [1, D]); ln1b = din("ln1b", [1, D]); ln2g = din("ln2g", [1, D]); ln2b = din("ln2b", [1, D])
    dng = din("dng", [1, 64]); lamv = din("lamv", [1, 130])
    wr = din("wr", [128, 8, 20]); br = din("br", [1, 20])
    w1 = din("w1", [NEXP, 128, 8, DEXP], BF16); w3 = din("w3", [NEXP, 128, 8, DEXP], BF16)
    w2 = din("w2", [NEXP, 128, 4, D], BF16)
    idn = G.din("idn", [128, 128], BF16); idn32 = G.din("idn32", [128, 128])
    rofs = G.din("rofs", [1, 1], I32)
    hT_out = hT_loc.rearrange("(k p) t -> k p t", k=8) if hT_loc is not None else None
    ya_all, ybc_all = y_all
    nch = ntok // YCH
    yam = G.internal("ya_mine", [nch, 4 * YCH, 192])
    ybm = G.internal("ybc_mine", [nch, 4 * YCH, 192])
    offh = {}

    from contextlib import contextmanager

    @contextmanager
    def sp_wrap(sp):
        with sp.register(f"rofs{l}") as reg:
            sp.reg_load(reg, rofs[0:1, 0:1])
            offh["v"] = sp.snap(reg)
            yield
    P.sp_wrap = sp_wrap
    for nm_, src_, dst_ in (("a", ya_all, yam), ("b", ybc_all, ybm)):
        P.op("sp", lambda src_=src_, dst_=dst_: nc.sync.dma_start(
            out=dst_.rearrange("i (a b) c -> (i a) (b c)", a=64),
            in_=src_[bass.ds(offh["v"], nch)].rearrange("i (a b) c -> (i a) (b c)", a=64)),
            writes=[("ymine", nm_)])
    yas = yam.rearrange("i (j s) c -> i s j c", j=4)
    ybs = ybm.rearrange("i (j s) c -> i s j c", j=4)

    with P.stack:
        C.alloc_banks()
        B = C.banks
        sb = P.sb
        ident = sb([128, 128], BF16); ident32 = sb([128, 128], F32)
        g1 = sb([128, D], F32); b1 = sb([128, D], F32); g2 = sb([128, D], F32); b2 = sb([128, D], F32)
        wout_t = sb([128, 8, D], BF16); wglu_t = sb([128, 2, 256], BF16)
        wr_t = sb([128, 8, 20], F32); br_t = sb([128, 20], F32)
        gA = sb([128, 64], F32); lam_t = sb([128, 130], F32); lprod = sb([128, 2, 32], F32)
        lsum = sb([128, 2], F32); nlam = sb([128, 1], F32)
        eps_ln = const_col(P, nc, LN_EPS, "eps_ln"); eps_rms = const_col(P, nc, RMS_EPS, "eps_rms")
        scr = ln_scratch(P, "ln"); scr["eps_ln"] = eps_ln
        ht = [sb([128, D], F32) for _ in range(2)]
        ya_t = [sb([128, 768], F32) for _ in range(2)]
        yc_t = [sb([128, 256], F32) for _ in range(2)]
        ymix = [sb([128, D], F32) for _ in range(2)]
        dd = sb([128, 6, 64], F32); sq = sb([128, 6, 64], F32); ss = sb([128, 6], F32)
        c2 = sb([128, 256], F32); c3 = sb([128, 256], F32); ygb = sb([128, 256], BF16)
        ygT = sb([128, 2, 128], BF16); sig = sb([128, 256], F32)
        ymb = sb([128, D], BF16); ymT = sb([128, 8, 128], BF16)
        z = sb([128, D], F32)
        h1 = [sb([128, D], F32) for _ in range(4)]
        h1T32 = sb([128, 8, 128], F32)
        h1T = sb([128, 8, 512], BF16)
        lg = sb([128, 20], F32)
        r = {n: sb([128, s], F32) for n, s in dict(gmax=1, goh=4, ngmax=1, gexp=4, gsum=1, gp=1, em=16, pen=4, m1=1, oh1=16,
                                                   em2=16, m2=1, oh2=16, dl=1, ex=1, den=1, w1=1, w2=1).items()}
        comb = sb([128, 4, 16], F32)
        wA = [sb([128, 8, DEXP], BF16) for _ in range(2)]
        wB = [sb([128, 8, DEXP], BF16) for _ in range(2)]
        wC = [sb([128, 4, D], BF16) for _ in range(2)]
        sil = [sb([128, 512], F32) for _ in range(2)]
        hid = [sb([128, 512], BF16) for _ in range(4)]
        acc = [sb([128, D], F32) for _ in range(4)]
        z2 = sb([128, D], F32)
        ho = [sb([128, D], F32) for _ in range(2)]
        hob = sb([128, D], BF16)
        hoT = [sb([128, 8, 128], BF16) for _ in range(2)]

        dma(P, "sp", ident[:], idn, writes=["ident"]); dma(P, "sp", ident32[:], idn32, writes=["ident32"])
        for t_, s_, k_ in ((g1, ln1g, "g1"), (b1, ln1b, "b1"), (g2, ln2g, "g2"), (b2, ln2b, "b2")):
            dma(P, "sp", t_[:], s_.partition_broadcast(128), writes=[k_])
        dma(P, "sp", wout_t[:], wout, writes=["wout"]); dma(P, "sp", wglu_t[:], wglu, writes=["wglu"])
        dma(P, "sp", wr_t[:], wr, writes=["wr"]); dma(P, "sp", br_t[:], br.partition_broadcast(128), writes=["br"])
        dma(P, "sp", gA[:], dng.partition_broadcast(128), writes=["gA"])
        dma(P, "sp", lam_t[:], lamv.partition_broadcast(128), writes=["lamt"])
        P.op("dve", lambda: nc.vector.tensor_scalar(out=gA[:], in0=gA[:], scalar1=lam_t[:, 128:129], scalar2=None, op0=ALU.mult),
             reads=["gA", "lamt"], writes=["gA"])
        lv = lam_t[:, 0:128].rearrange("p (a b c) -> p a b c", a=2, b=2)
        P.op("dve", lambda: nc.vector.tensor_tensor(out=lprod[:], in0=lv[:, :, 0, :], in1=lv[:, :, 1, :], op=ALU.mult),
             reads=["lamt"], writes=["lprod"])
        P.op("dve", lambda: nc.vector.tensor_reduce(out=lsum[:], in_=lprod[:], axis=AX.X, op=ALU.add), reads=["lprod"], writes=["lsum"])
        P.op("act", lambda: nc.scalar.activation(out=lsum[:], in_=lsum[:], func=AF.Exp), reads=["lsum"], writes=["lsum"])
        P.op("dve", lambda: nc.vector.tensor_tensor(out=nlam[:], in0=lsum[:, 1:2], in1=lsum[:, 0:1], op=ALU.subtract),
             reads=["lsum"], writes=["nlam"])
        P.op("dve", lambda: nc.vector.tensor_scalar(out=nlam[:], in0=nlam[:], scalar1=lam_t[:, 129:130], scalar2=None, op0=ALU.add),
             reads=["nlam", "lamt"], writes=["nlam"])

        outs = []
        for su in range(nsup):
            for tt in range(4):
                t = su * 4 + tt
                s = t % 2
                rows = slice(t * 128, (t + 1) * 128)
                dma(P, "sp", ht[s][:], h_in[rows, :], writes=[("ht", s)])
                ci_, r0_ = (t * 128) // YCH, (t * 128) % YCH
                rw_ = slice(r0_, r0_ + 128)
                dma(P, "sp", ya_t[s][:].rearrange("p (j c) -> p j c", j=4), yas[ci_][rw_, :, :], reads=[("ymine", "a")], writes=[("ya", s)])
                dma(P, "sp", ymix[s][:, 384:768].rearrange("p (j c) -> p j c", j=3), ybs[ci_][rw_, 0:3, 0:128], reads=[("ymine", "b")], writes=[("ymix", s, 1)])
                dma(P, "sp", yc_t[s][:].rearrange("p (j c) -> p j c", j=4), ybs[ci_][rw_, :, 128:192], reads=[("ymine", "b")], writes=[("yc", s)])
                yav = ya_t[s][:].rearrange("p (h m d) -> p h m d", h=6, m=2)
                P.op("dve", lambda yav=yav: nc.vector.scalar_tensor_tensor(out=dd[:], in0=yav[:, :, 1, :], scalar=nlam[:, 0:1],
                                                                          in1=yav[:, :, 0, :], op0=ALU.mult, op1=ALU.add),
                     reads=[("ya", s), "nlam"], writes=["dd"])
                P.op("pool", lambda: nc.gpsimd.tensor_tensor(out=sq[:], in0=dd[:], in1=dd[:], op=ALU.mult), reads=["dd"], writes=["sq"])
                P.op("dve", lambda: nc.vector.tensor_reduce(out=ss[:], in_=sq[:], axis=AX.X, op=ALU.add), reads=["sq"], writes=["ss"])
                P.op("act", lambda: nc.scalar.activation(out=ss[:], in_=ss[:], func=AF.Sqrt, bias=eps_rms[:, 0:1], scale=1.0 / 64),
                     reads=["ss", "eps_rms"], writes=["ss"])
                P.op("dve", lambda: nc.vector.reciprocal(out=ss[:], in_=ss[:]), reads=["ss"], writes=["ss"])
                P.op("dve", lambda: nc.vector.tensor_tensor(out=dd[:], in0=dd[:], in1=ss[:].unsqueeze(2).to_broadcast([128, 6, 64]), op=ALU.mult),
                     reads=["dd", "ss"], writes=["dd"])
                ym0 = ymix[s][:, 0:384].rearrange("p (h d) -> p h d", h=6)
                P.op("pool", lambda ym0=ym0: nc.gpsimd.tensor_tensor(out=ym0, in0=dd[:], in1=gA[:].unsqueeze(1).to_broadcast([128, 6, 64]), op=ALU.mult),
                     reads=["dd", "gA"], writes=[("ymix", s, 0)])
                yct = yc_t[s]
                P.op("pool", lambda yct=yct: nc.gpsimd.tensor_tensor(out=c2[:], in0=yct[:], in1=yct[:], op=ALU.mult), reads=[("yc", s)], writes=["c2"])
                P.op("dve", lambda: nc.vector.tensor_scalar(out=c2[:], in0=c2[:], scalar1=0.044715, scalar2=1.0, op0=ALU.mult, op1=ALU.add),
                     reads=["c2"], writes=["c2"])
                P.op("dve", lambda yct=yct: nc.vector.tensor_tensor(out=c2[:], in0=c2[:], in1=yct[:], op=ALU.mult), reads=["c2", ("yc", s)], writes=["c2"])
                P.op("act", lambda: nc.scalar.activation(out=c2[:], in_=c2[:], func=AF.Sigmoid, scale=1.5957691216057308),
                     reads=["c2"], writes=["c2"])
                P.op("dve", lambda yct=yct: nc.vector.tensor_tensor(out=c3[:], in0=c2[:], in1=yct[:], op=ALU.mult), reads=["c2", ("yc", s)], writes=["c3"])
                P.op("act", lambda: nc.scalar.copy(out=ygb[:], in_=c3[:]), reads=["c3"], writes=["ygb"])
                pv0 = B[0][:].bitcast(BF16)

                def trg(pv0=pv0):
                    for k in range(2):
                        ins = nc.tensor.transpose(out=pv0[:, k * 128:(k + 1) * 128], in_=ygb[:, k * 128:(k + 1) * 128], identity=ident[:])
                    return ins
                P.op("pe", trg, reads=["ygb", "ident"], writes=["b0"])
                P.op("dve", lambda pv0=pv0: nc.vector.tensor_copy(out=ygT[:].rearrange("p k t -> p (k t)"), in_=pv0[:, 0:256]), reads=["b0"], writes=["ygT"])

                def mmg():
                    for k in range(2):
                        ins = nc.tensor.matmul(B[1][:, 0:256], lhsT=ygT[:, k, :], rhs=wglu_t[:, k, :], start=(k == 0), stop=(k == 1))
                    return ins
                P.op("pe", mmg, reads=["ygT", "wglu"], writes=["b1"])
                P.op("act", lambda: nc.scalar.activation(out=sig[:], in_=B[1][:, 0:256], func=AF.Sigmoid), reads=["b1"], writes=["sig"])
                P.op("dve", lambda s=s: nc.vector.tensor_tensor(out=ymix[s][:, 768:1024], in0=c3[:], in1=sig[:], op=ALU.mult),
                     reads=["c3", "sig"], writes=[("ymix", s, 2)])
                ymk = [("ymix", s, 0), ("ymix", s, 1), ("ymix", s, 2)]
                P.op("act", lambda s=s: nc.scalar.copy(out=ymb[:], in_=ymix[s][:]), reads=ymk, writes=["ymb"])
                pvb = B[0][:].bitcast(BF16).rearrange("p (k t) -> p k t", k=8)

                def try_(pvb=pvb):
                    for kc in range(8):
                        ins = nc.tensor.transpose(out=pvb[:, kc, :], in_=ymb[:, kc * 128:(kc + 1) * 128], identity=ident[:])
                    return ins
                P.op("pe", try_, reads=["ymb", "ident"], writes=["b0"])
                P.op("dve", lambda pvb=pvb: nc.vector.tensor_copy(out=ymT[:], in_=pvb), reads=["b0"], writes=["ymT"])
                for half in range(2):
                    def mmo(half=half):
                        for kc in range(8):
                            ins = nc.tensor.matmul(B[2 + half][:], lhsT=ymT[:, kc, :], rhs=wout_t[:, kc, half * 512:(half + 1) * 512],
                                                   start=(kc == 0), stop=(kc == 7))
                        return ins
                    P.op("pe", mmo, reads=["ymT", "wout"], writes=[f"b{2 + half}"])
                    P.op("dve", lambda half=half, s=s: nc.vector.scalar_tensor_tensor(
                        out=z[:, half * 512:(half + 1) * 512], in0=ht[s][:, half * 512:(half + 1) * 512], scalar=float(ALPHA),
                        in1=B[2 + half][:], op0=ALU.mult, op1=ALU.add), reads=[f"b{2 + half}", ("ht", s)], writes=[("z", half)])
                P.op("pool", lambda: nc.gpsimd.tensor_copy(out=z[:, 0:1], in_=z[:, 0:1]), reads=[("z", 0), ("z", 1)], writes=["zz"])
                layer_norm_tile(P, nc, z, "zz", h1[tt], ("h1", tt), g1, b1, "g1", "b1", scr, "ln")
                for half in range(2):
                    pv32 = B[2 + half][:].rearrange("p (k t) -> p k t", k=4)

                    def trh(half=half, pv32=pv32, tt=tt):
                        for k in range(4):
                            kc = half * 4 + k
                            ins = nc.tensor.transpose(out=pv32[:, k, :], in_=h1[tt][:, kc * 128:(kc + 1) * 128], identity=ident32[:])
                        return ins
                    P.op("pe", trh, reads=[("h1", tt), "ident32"], writes=[f"b{2 + half}"])
                    P.op("dve", lambda half=half, pv32=pv32: nc.vector.tensor_copy(out=h1T32[:, half * 4:(half + 1) * 4, :], in_=pv32),
                         reads=[f"b{2 + half}"], writes=[("h1T32", half)])
                    P.op("act", lambda half=half, pv32=pv32, tt=tt: nc.scalar.copy(
                        out=h1T[:, half * 4:(half + 1) * 4, tt * 128:(tt + 1) * 128], in_=pv32),
                        reads=[f"b{2 + half}"], writes=[("h1T", tt, half)])

                def mmr():
                    for kc in range(8):
                        ins = nc.tensor.matmul(B[1][:, 0:20], lhsT=h1T32[:, kc, :], rhs=wr_t[:, kc, :], start=(kc == 0), stop=(kc == 7))
                    return ins
                P.op("pe", mmr, reads=[("h1T32", 0), ("h1T32", 1), "wr"], writes=["b1"])
                P.op("dve", lambda: nc.vector.tensor_tensor(out=lg[:], in0=B[1][:, 0:20], in1=br_t[:], op=ALU.add), reads=["b1", "br"], writes=["lg"])
                V = nc.vector
                glog = lg[:, 0:4]
                elog = lg[:, 4:20]
                seq = [
                    lambda: V.tensor_reduce(out=r["gmax"][:], in_=glog, axis=AX.X, op=ALU.max),
                    lambda: V.tensor_scalar(out=r["goh"][:], in0=glog, scalar1=r["gmax"][:, 0:1], scalar2=None, op0=ALU.is_ge),
                    lambda: V.tensor_scalar(out=r["ngmax"][:], in0=r["gmax"][:], scalar1=-1.0, scalar2=None, op0=ALU.mult),
                ]
                for f_ in seq:
                    P.op("dve", f_, reads=["lg", "rt"], writes=["rt"])
                P.op("act", lambda: nc.scalar.activation(out=r["gexp"][:], in_=glog, func=AF.Exp, bias=r["ngmax"][:, 0:1], scale=1.0),
                     reads=["lg", "rt"], writes=["rt2"])
                seq = [
                    lambda: V.tensor_reduce(out=r["gsum"][:], in_=r["gexp"][:], axis=AX.X, op=ALU.add),
                    lambda: V.reciprocal(out=r["gp"][:], in_=r["gsum"][:]),
                    lambda: V.tensor_tensor(out=r["em"][:].rearrange("p (g e) -> p g e", g=4), in0=elog.rearrange("p (g e) -> p g e", g=4),
                                            in1=r["goh"][:].unsqueeze(2).to_broadcast([128, 4, 4]), op=ALU.mult),
                    lambda: V.tensor_scalar(out=r["pen"][:], in0=r["goh"][:], scalar1=-1.0, scalar2=BIG, op0=ALU.add, op1=ALU.mult),
                    lambda: V.tensor_tensor(out=r["em"][:].rearrange("p (g e) -> p g e", g=4), in0=r["em"][:].rearrange("p (g e) -> p g e", g=4),
                                            in1=r["pen"][:].unsqueeze(2).to_broadcast([128, 4, 4]), op=ALU.add),
                    lambda: V.tensor_reduce(out=r["m1"][:], in_=r["em"][:], axis=AX.X, op=ALU.max),
                    lambda: V.tensor_scalar(out=r["oh1"][:], in0=r["em"][:], scalar1=r["m1"][:, 0:1], scalar2=None, op0=ALU.is_ge),
                    lambda: V.scalar_tensor_tensor(out=r["em2"][:], in0=r["oh1"][:], scalar=-BIG, in1=r["em"][:], op0=ALU.mult, op1=ALU.add),
                    lambda: V.tensor_reduce(out=r["m2"][:], in_=r["em2"][:], axis=AX.X, op=ALU.max),
                    lambda: V.tensor_scalar(out=r["oh2"][:], in0=r["em2"][:], scalar1=r["m2"][:, 0:1], scalar2=None, op0=ALU.is_ge),
                    lambda: V.tensor_tensor(out=r["dl"][:], in0=r["m2"][:], in1=r["m1"][:], op=ALU.subtract),
                ]
                for f_ in seq:
                    P.op("dve", f_, reads=["lg", "rt", "rt2"], writes=["rt"])
                P.op("act", lambda: nc.scalar.activation(out=r["ex"][:], in_=r["dl"][:], func=AF.Exp), reads=["rt"], writes=["rt2"])
                seq = [
                    lambda: V.tensor_scalar(out=r["den"][:], in0=r["ex"][:], scalar1=1.0, scalar2=None, op0=ALU.add),
                    lambda: V.reciprocal(out=r["w1"][:], in_=r["den"][:]),
                    lambda: V.tensor_tensor(out=r["w1"][:], in0=r["w1"][:], in1=r["gp"][:], op=ALU.mult),
                    lambda: V.tensor_tensor(out=r["w2"][:], in0=r["w1"][:], in1=r["ex"][:], op=ALU.mult),
                    lambda tt=tt: V.tensor_scalar(out=comb[:, tt, :], in0=r["oh1"][:], scalar1=r["w1"][:, 0:1], scalar2=None, op0=ALU.mult),
                    lambda tt=tt: V.scalar_tensor_tensor(out=comb[:, tt, :], in0=r["oh2"][:], scalar=r["w2"][:, 0:1], in1=comb[:, tt, :],
                                                         op0=ALU.mult, op1=ALU.add),
                ]
                for i_, f_ in enumerate(seq):
                    P.op("dve", f_, reads=["rt", "rt2"] + ([("comb", tt)] if i_ == 5 else []), writes=["rt"] if i_ < 4 else [("comb", tt)])
            h1Tk = [("h1T", tt, half) for tt in range(4) for half in range(2)]
            for e in range(NEXP):
                ws = e % 2
                dma(P, "sp", wA[ws][:], w1[e], writes=[("wA", ws)])
                dma(P, "poolq", wB[ws][:], w3[e], writes=[("wB", ws)])
                dma(P, "sp", wC[ws][:], w2[e], writes=[("wC", ws)])
                for hc in range(4):
                    ba, bb = 4 + hc % 2, 6 + hc % 2

                    def mma(hc=hc, ba=ba, ws=ws):
                        for kc in range(8):
                            ins = nc.tensor.matmul(B[ba][:], lhsT=wA[ws][:, kc, hc * 128:(hc + 1) * 128], rhs=h1T[:, kc, :],
                                                   start=(kc == 0), stop=(kc == 7))
                        return ins

                    def mmb(hc=hc, bb=bb, ws=ws):
                        for kc in range(8):
                            ins = nc.tensor.matmul(B[bb][:], lhsT=wB[ws][:, kc, hc * 128:(hc + 1) * 128], rhs=h1T[:, kc, :],
                                                   start=(kc == 0), stop=(kc == 7))
                        return ins
                    P.op("pe", mma, reads=h1Tk + [("wA", ws)], writes=[f"b{ba}"])
                    P.op("pe", mmb, reads=h1Tk + [("wB", ws)], writes=[f"b{bb}"])
                    P.op("act", lambda hc=hc, ba=ba: nc.scalar.activation(out=sil[hc % 2][:], in_=B[ba][:], func=AF.Silu),
                         reads=[f"b{ba}"], writes=[("sil", hc % 2)])
                    P.op("dve", lambda hc=hc, bb=bb: nc.vector.tensor_tensor(out=hid[hc][:], in0=sil[hc % 2][:], in1=B[bb][:], op=ALU.mult),
                         reads=[f"b{bb}", ("sil", hc % 2)], writes=[("hid", hc)])
                for tt in range(4):
                    for half in range(2):
                        bo = 2 + half

                        def mm2(tt=tt, half=half, bo=bo, ws=ws):
                            for hc in range(4):
                                ins = nc.tensor.matmul(B[bo][:], lhsT=hid[hc][:, tt * 128:(tt + 1) * 128],
                                                       rhs=wC[ws][:, hc, half * 512:(half + 1) * 512], start=(hc == 0), stop=(hc == 3))
                            return ins
                        P.op("pe", mm2, reads=[("hid", hc) for hc in range(4)] + [("wC", ws)], writes=[f"b{bo}"])
                        av = acc[tt][:, half * 512:(half + 1) * 512]
                        if e == 0:
                            P.op("dve", lambda av=av, bo=bo, tt=tt, e=e: nc.vector.tensor_scalar(
                                out=av, in0=B[bo][:], scalar1=comb[:, tt, e:e + 1], scalar2=None, op0=ALU.mult),
                                reads=[f"b{bo}", ("comb", tt)], writes=[("acc", tt, half)])
                        else:
                            P.op("dve", lambda av=av, bo=bo, tt=tt, e=e: nc.vector.scalar_tensor_tensor(
                                out=av, in0=B[bo][:], scalar=comb[:, tt, e:e + 1], in1=av, op0=ALU.mult, op1=ALU.add),
                                reads=[f"b{bo}", ("comb", tt), ("acc", tt, half)], writes=[("acc", tt, half)])
            for tt in range(4):
                t = su * 4 + tt
                s = t % 2
                rows = slice(t * 128, (t + 1) * 128)
                P.op("dve", lambda tt=tt: nc.vector.scalar_tensor_tensor(out=z2[:], in0=h1[tt][:], scalar=float(ALPHA), in1=acc[tt][:],
                                                                          op0=ALU.mult, op1=ALU.add),
                     reads=[("h1", tt), ("acc", tt, 0), ("acc", tt, 1)], writes=["z2"])
                layer_norm_tile(P, nc, z2, "z2", ho[s], ("ho", s), g2, b2, "g2", "b2", scr, "ln")
                dma(P, "poolq", h_out[rows, :], ho[s][:], reads=[("ho", s)], writes=[("h_out", t)])
                outs.append(("h_out", t))
                if hT_out is not None:
                    to_featmajor_bf16(P, nc, ho[s], ("ho", s), hob, "hob", B[0], "b0", hoT[s][:], ("hoT", s), ident)
                    dma(P, "poolq", hT_out[:, :, rows].rearrange("k p t -> p k t"), hoT[s][:], reads=[("hoT", s)], writes=[("hT_out", t)])
                    outs.append(("hT_out", t))
        P.emit(final_wait_keys=outs)


def post_inputs(l, p, lam_init):
    d = {}
    d["wout"] = bf(p["w_out"][l].reshape(8, 128, D).transpose(1, 0, 2))
    d["wglu"] = bf(p["s5_w_glu"][l].reshape(2, 128, 256).transpose(1, 0, 2))
    for n in ("ln1_g", "ln1_b", "ln2_g", "ln2_b"):
        d[n.replace("_", "")] = f32c(p[n][l].reshape(1, D))
    d["dng"] = f32c(p["diff_norm_g"][l].reshape(1, 64))
    d["lamv"] = f32c(np.concatenate([p["lam_q1"][l], p["lam_k1"][l], p["lam_q2"][l], p["lam_k2"][l],
                                     np.array([1.0 - lam_init, -lam_init], np.float32)]).reshape(1, 130))
    wr = np.concatenate([p["moe_w_grp"][l], p["moe_w_exp"][l]], axis=1)
    d["wr"] = f32c(wr.reshape(8, 128, 20).transpose(1, 0, 2))
    d["br"] = f32c(np.concatenate([p["moe_b_grp"][l], p["moe_b_exp"][l]]).reshape(1, 20))
    d["w1"] = bf(p["moe_w1"][l].reshape(NEXP, 8, 128, DEXP).transpose(0, 2, 1, 3))
    d["w3"] = bf(p["moe_w3"][l].reshape(NEXP, 8, 128, DEXP).transpose(0, 2, 1, 3))
    d["w2"] = bf(p["moe_w2"][l].reshape(NEXP, 4, 128, D).transpose(0, 2, 1, 3))
    d["idn"] = bf(np.eye(128)); d["idn32"] = f32c(np.eye(128))
    return d


SBANKS = [0, 1, 2, 6, 7]
NPT = 7
ADEPTH = 4
QUARTERS = False
TWO_PI = 2.0 * math.pi
MAGIC = 12582912.0


def phase_mix(nc, G, l, hT_all, ya_d, ybc_d, S=SEQ, do_attn=True, do_s5=True, do_gdn=True, pool_pre=None, pool_post=None, ag_stream=None):
    debug = False
    C = Ctx(nc)
    P = C.P
    nst = S // 512
    nblk = S // 128
    pf = f"L{l}_"

    def din(name, shape, dt=F32):
        return G.din((pf + name) if name not in ("amask", "idn32", "srow", "cTri", "cSL", "cMask2", "cBones") else name, shape, dt)
    hT4 = hT_all.rearrange("k (r p) t -> r p k t", r=4)
    wq = din("wq", [128, 8, 96], BF16); wk = din("wk", [128, 8, 96], BF16); wv = din("wv", [128, 8, 192], BF16)
    amask = din("amask", [128, 4, 512], BF16)
    idn32 = din("idn32", [128, 128])
    wu = din("wu", [128, 8, 64], BF16)
    s5row = din("s5row", [2, 3, 128])
    s5col = din("s5col", [2, 128, 3])
    s5bT = din("s5bT", [2, 2, 2, 16, 64])
    s5cT = din("s5cT", [2, 2, 2, 64, 16])
    s5d = din("s5d", [64, 1])
    srow = din("srow", [1, 512])
    wg = din("wg", [128, 8, 384], BF16); wt = din("wt", [128, 8, 132], BF16)
    cvw = din("cvw", [128, 3, 4])
    galog = din("galog", [1, 2]); gdtb = din("gdtb", [1, 2]); gng = din("gng", [1, 64])
    cTri = din("cTri", [64, 64]); cSL = din("cSL", [64, 64]); cMask2 = din("cMask2", [64, 2, 64]); cBones = din("cBones", [128, 128])
    ya_o = ya_d.rearrange("s (u d) -> s u d", u=3)
    yb_o = ybc_d[:, 0:128].rearrange("s (h d) -> s h d", h=2)
    yc_o = ybc_d[:, 128:192]
    P.pool_pre, P.pool_post = pool_pre, pool_post
    ag_n = [0]

    def try_ag():
        if ag_stream is None:
            return
        pairs, cc = ag_stream
        per = YCH // 512
        while ag_n[0] < len(pairs):
            sts = range(ag_n[0] * per, (ag_n[0] + 1) * per)
            keys = [("yb_o", s_) for s_ in sts] + [("yc_o", s_) for s_ in sts]
            if not all(k_ in P.last_w for k_ in keys):
                break
            s_ap, d_ap = pairs[ag_n[0]]
            P.op("pool", lambda s_ap=s_ap, d_ap=d_ap: nc.gpsimd.collective_compute(
                "AllGather", ALU.bypass, replica_groups=GROUPS, ins=[s_ap], outs=[d_ap]).then_inc(cc, 1), reads=keys)
            ag_n[0] += 1
    outs = []
    dbg = []
    V = nc.vector
    G = nc.gpsimd
    A = nc.scalar
    T = nc.tensor

    with P.stack:
        C.alloc_banks(quarters=do_gdn and QUARTERS)
        B = C.banks
        sb = P.sb
        ident32 = sb([128, 128], F32)
        dma(P, "sp", ident32[:], idn32, writes=["ident32"])
        hTt = [sb([128, 8, 512], BF16) for _ in range(2)]
        eps_rms = const_col(P, nc, RMS_EPS, "eps_rms")
        if do_attn:
            wq_t = sb([128, 8, 96], BF16); wk_t = sb([128, 8, 96], BF16); wv_t = sb([128, 8, 192], BF16)
            QT = sb([96, S], BF16); KT = sb([96, S], BF16)
            Vall = sb([128, nblk, 3, 65], BF16)
            am_t = sb([128, 4, 512], BF16)
            PT = [sb([128, 512], BF16) for _ in range(NPT)]
            osb = sb([65, 512], F32); rec = sb([128, 4], F32)
            oT = [sb([128, 4, 64], F32) for _ in range(2)]
            dma(P, "sp", wq_t[:], wq, writes=["wq"]); dma(P, "sp", wk_t[:], wk, writes=["wk"]); dma(P, "sp", wv_t[:], wv, writes=["wv"])
            dma(P, "sp", am_t[:], amask, writes=["amask"])
            P.op("pool", lambda: G.memset(Vall[:, :, :, 64:65], 1.0), writes=["Vones"])
        if do_s5:
            wu_t = sb([128, 8, 64], BF16)
            dma(P, "sp", wu_t[:], wu, writes=["wu"])
            uT = [sb([32, 512], F32) for _ in range(2)]
            srow_t = sb([128, 512], F32)
            dma(P, "sp", srow_t[:], srow.partition_broadcast(128), writes=["srow"])
            d_col = [sb([32, 1], F32) for _ in range(2)]
            for pr_ in range(2):
                dma(P, "sp", d_col[pr_][:], s5d[pr_ * 32:(pr_ + 1) * 32, :], writes=[("dcol", pr_)])
            s5 = []
            for pr in range(2):
                t = dict(row=sb([32, 3, 128], F32), col=sb([128, 3], F32),
                         BrBD=sb([32, 128], F32), BiBD=sb([32, 128], F32), CrBD=sb([128, 32], F32), CiBD=sb([128, 32], F32),
                         bbr=sb([32, 128], F32), bbi=sb([32, 128], F32),
                         w=[sb([32, 128], F32) for _ in range(8)],
                         cw=[sb([128, 1], F32) for _ in range(8)],
                         RHO=sb([128, 512], F32), CS=sb([128, 512], F32), SN=sb([128, 512], F32),
                         zi=sb([128, 2], F32), zt=sb([128, 2], F32))
                if pr == 0:
                    for nm_ in ("bre", "bim", "t1", "t2", "zre", "zim", "xre", "xim", "ang", "tmp"):
                        t[nm_] = sb([128, 512], F32)
                if pr == 1:
                    for nm_ in ("bre", "bim", "t1", "t2", "zre", "zim", "xre", "xim", "ang", "tmp"):
                        t[nm_] = s5[0][nm_]
                s5.append(t)
            yT = [sb([32, 512], F32) for _ in range(2)]
            yc_tm = [sb([128, 4, 64], F32) for _ in range(2)]

            def range_reduce(eng_name, x, tmp, key_x, key_t, shape_all=True):
                P.op("dve", lambda: V.tensor_scalar(out=tmp, in0=x, scalar1=1.0 / TWO_PI, scalar2=MAGIC, op0=ALU.mult, op1=ALU.add),
                     reads=[key_x], writes=[key_t])
                P.op("dve", lambda: V.tensor_scalar(out=tmp, in0=tmp, scalar1=-MAGIC, scalar2=-TWO_PI, op0=ALU.add, op1=ALU.mult),
                     reads=[key_t], writes=[key_t])
                P.op("dve", lambda: V.tensor_tensor(out=x, in0=x, in1=tmp, op=ALU.add), reads=[key_x, key_t], writes=[key_x])

            def s5_setup(pr):
                t = s5[pr]
                k = lambda n, pr=pr: ("s5", "sh" if n in ("bre", "bim", "t1", "t2", "zre", "zim", "xre", "xim", "tab", "roww_scratch") else pr, n)
                dma(P, "sp", t["row"][:], s5row[pr:pr + 1].partition_broadcast(32), writes=[k("row")])
                dma(P, "sp", t["col"][:], s5col[pr], writes=[k("col")])
                for nm in ("BrBD", "BiBD", "CrBD", "CiBD"):
                    P.op("pool", lambda nm=nm, t=t: G.memset(t[nm][:], 0.0), writes=[k(nm)])
                for g in range(2):
                    dma(P, "sp", t["BrBD"][g * 16:(g + 1) * 16, g * 64:(g + 1) * 64], s5bT[pr, g, 0], reads=[k("BrBD")], writes=[k("BrBD")])
                    dma(P, "sp", t["BiBD"][g * 16:(g + 1) * 16, g * 64:(g + 1) * 64], s5bT[pr, g, 1], reads=[k("BiBD")], writes=[k("BiBD")])
                    dma(P, "sp", t["CrBD"][g * 64:(g + 1) * 64, g * 16:(g + 1) * 16], s5cT[pr, g, 0], reads=[k("CrBD")], writes=[k("CrBD")])
                    dma(P, "sp", t["CiBD"][g * 64:(g + 1) * 64, g * 16:(g + 1) * 16], s5cT[pr, g, 1], reads=[k("CiBD")], writes=[k("CiBD")])
                P.op("dve", lambda t=t: V.tensor_scalar(out=t["CiBD"][:], in0=t["CiBD"][:], scalar1=-1.0, scalar2=None, op0=ALU.mult),
                     reads=[k("CiBD")], writes=[k("CiBD")])
                lre, lim, ldt = t["row"][:, 0, :], t["row"][:, 1, :], t["row"][:, 2, :]
                dt_, lr_, mag, ang, tmp_, sn, cs, den = [t["w"][i][:] for i in range(8)]
                rk = k("roww")
                steps = [
                    ("act", lambda: A.activation(out=dt_, in_=ldt, func=AF.Exp)),
                    ("dve", lambda: V.tensor_tensor(out=lr_, in0=lre, in1=dt_, op=ALU.mult)),
                    ("act", lambda: A.activation(out=mag, in_=lr_, func=AF.Exp)),
                    ("dve", lambda: V.tensor_tensor(out=ang, in0=lim, in1=dt_, op=ALU.mult)),
                ]
                for e_, f_ in steps:
                    P.op(e_, f_, reads=[k("row"), rk], writes=[rk])
                range_reduce("dve", ang, tmp_, rk, rk)
                P.op("act", lambda: A.activation(out=sn, in_=ang, func=AF.Sin), reads=[rk], writes=[rk])
                P.op("dve", lambda: V.tensor_scalar(out=ang, in0=ang, scalar1=math.pi / 2, scalar2=None, op0=ALU.add), reads=[rk], writes=[rk])
                range_reduce("dve", ang, tmp_, rk, rk)
                P.op("act", lambda: A.activation(out=cs, in_=ang, func=AF.Sin), reads=[rk], writes=[rk])
                steps = [
                    lambda: V.tensor_tensor(out=cs, in0=cs, in1=mag, op=ALU.mult),
                    lambda: V.tensor_scalar(out=cs, in0=cs, scalar1=-1.0, scalar2=None, op0=ALU.add),
                    lambda: V.tensor_tensor(out=sn, in0=sn, in1=mag, op=ALU.mult),
                    lambda: V.tensor_tensor(out=den, in0=lre, in1=lre, op=ALU.mult),
                    lambda: V.tensor_tensor(out=tmp_, in0=lim, in1=lim, op=ALU.mult),
                    lambda: V.tensor_tensor(out=den, in0=den, in1=tmp_, op=ALU.add),
                    lambda: V.reciprocal(out=den, in_=den),
                    lambda: V.tensor_tensor(out=dt_, in0=cs, in1=lre, op=ALU.mult),
                    lambda: V.tensor_tensor(out=tmp_, in0=sn, in1=lim, op=ALU.mult),
                    lambda: V.tensor_tensor(out=dt_, in0=dt_, in1=tmp_, op=ALU.add),
                    lambda: V.tensor_tensor(out=dt_, in0=dt_, in1=den, op=ALU.mult),
                    lambda: V.tensor_tensor(out=lr_, in0=sn, in1=lre, op=ALU.mult),
                    lambda: V.tensor_tensor(out=tmp_, in0=cs, in1=lim, op=ALU.mult),
                    lambda: V.tensor_tensor(out=lr_, in0=lr_, in1=tmp_, op=ALU.subtract),
                    lambda: V.tensor_tensor(out=lr_, in0=lr_, in1=den, op=ALU.mult),
                    lambda t=t: V.tensor_tensor(out=t["bbr"][:], in0=dt_, in1=t["BrBD"][:], op=ALU.mult),
                    lambda t=t: V.tensor_tensor(out=tmp_, in0=lr_, in1=t["BiBD"][:], op=ALU.mult),
                    lambda t=t: V.tensor_tensor(out=t["bbr"][:], in0=t["bbr"][:], in1=tmp_, op=ALU.subtract),
                    lambda t=t: V.tensor_tensor(out=t["bbi"][:], in0=dt_, in1=t["BiBD"][:], op=ALU.mult),
                    lambda t=t: V.tensor_tensor(out=tmp_, in0=lr_, in1=t["BrBD"][:], op=ALU.mult),
                    lambda t=t: V.tensor_tensor(out=t["bbi"][:], in0=t["bbi"][:], in1=tmp_, op=ALU.add),
                ]
                for f_ in steps:
                    P.op("dve", f_, reads=[k("row"), rk, k("BrBD"), k("BiBD")], writes=[rk])
                cdt, cth, crho, ca, ctmp, c512s, c512c, cx = [t["cw"][i][:] for i in range(8)]
                ck = k("colw")
                steps = [
                    ("act", lambda t=t: A.activation(out=cdt, in_=t["col"][:, 2:3], func=AF.Exp)),
                    ("dve", lambda t=t: V.tensor_tensor(out=cth, in0=t["col"][:, 1:2], in1=cdt, op=ALU.mult)),
                    ("dve", lambda t=t: V.tensor_tensor(out=crho, in0=t["col"][:, 0:1], in1=cdt, op=ALU.mult)),
                    ("act", lambda: A.activation(out=crho, in_=crho, func=AF.Exp)),
                    ("dve", lambda: V.tensor_scalar(out=ca, in0=cth, scalar1=512.0, scalar2=None, op0=ALU.mult)),
                ]
                for e_, f_ in steps:
                    P.op(e_, f_, reads=[k("col"), ck], writes=[ck])
                range_reduce("dve", ca, ctmp, ck, ck)
                P.op("act", lambda: A.activation(out=c512s, in_=ca, func=AF.Sin), reads=[ck], writes=[ck])
                P.op("dve", lambda: V.tensor_scalar(out=ca, in0=ca, scalar1=math.pi / 2, scalar2=None, op0=ALU.add), reads=[ck], writes=[ck])
                range_reduce("dve", ca, ctmp, ck, ck)
                P.op("act", lambda: A.activation(out=c512c, in_=ca, func=AF.Sin), reads=[ck], writes=[ck])
                tk = k("tab")
                P.op("dve", lambda t=t: V.tensor_scalar(out=t["ang"][:], in0=srow_t[:], scalar1=cth, scalar2=None, op0=ALU.mult),
                     reads=["srow", ck], writes=[tk])
                range_reduce("dve", t["ang"][:], t["tmp"][:], tk, tk)
                P.op("act", lambda t=t: A.activation(out=t["SN"][:], in_=t["ang"][:], func=AF.Sin), reads=[tk], writes=[tk])
                P.op("dve", lambda t=t: V.tensor_scalar(out=t["ang"][:], in0=t["ang"][:], scalar1=math.pi / 2, scalar2=None, op0=ALU.add), reads=[tk], writes=[tk])
                range_reduce("dve", t["ang"][:], t["tmp"][:], tk, tk)
                P.op("act", lambda t=t: A.activation(out=t["CS"][:], in_=t["ang"][:], func=AF.Sin), reads=[tk], writes=[tk])
                P.op("pool", lambda t=t: G.memset(t["RHO"][:], 1.0), writes=[k("rho")])
                P.op("dve", lambda t=t: V.tensor_scalar(out=t["RHO"][:], in0=t["RHO"][:], scalar1=crho, scalar2=None, op0=ALU.mult),
                     reads=[k("rho"), ck], writes=[k("rho")])
                P.op("pool", lambda t=t: G.memset(t["zi"][:], 0.0), writes=[k("zi")])
                if pr == 0:
                    dbg.extend([("CS", t["CS"][:], [128, 512], [k("tab")]), ("SN", t["SN"][:], [128, 512], [k("tab")]),
                            ("RHO", t["RHO"][:], [128, 512], [k("rho")]), ("bbr", t["bbr"][:], [32, 128], [k("roww")]),
                            ("bbi", t["bbi"][:], [32, 128], [k("roww")]), ("cr", t["w"][0][:], [32, 128], [k("roww")]),
                            ("ci", t["w"][1][:], [32, 128], [k("roww")]), ("c512", t["cw"][5][:], [128, 1], [k("colw")]),
                            ("row", t["row"][:], [32, 3, 128], [k("row")]), ("col", t["col"][:], [128, 3], [k("col")])])
            for pr_ in range(2):
                s5_setup(pr_)
        if do_gdn:
            wg_t = sb([128, 8, 384], BF16); wt_t = sb([128, 8, 132], BF16)
            dma(P, "sp", wg_t[:], wg, writes=["wg"]); dma(P, "sp", wt_t[:], wt, writes=["wt"])
            cvw_t = sb([128, 3, 4], F32); dma(P, "sp", cvw_t[:], cvw, writes=["cvw"])
            alog_t = sb([64, 2], F32); dtb_t = sb([64, 2], F32); ng_t = sb([64, 64], F32)
            dma(P, "sp", alog_t[:], galog.partition_broadcast(64), writes=["alog"])
            dma(P, "sp", dtb_t[:], gdtb.partition_broadcast(64), writes=["dtb"])
            dma(P, "sp", ng_t[:], gng.partition_broadcast(64), writes=["ngt"])
            Tri = sb([64, 64], F32); SL = sb([64, 64], F32); Mask2 = sb([64, 2, 64], F32); Bones = sb([128, 128], F32); ones64 = sb([64, 64], F32)
            dma(P, "sp", Tri[:], cTri, writes=["Tri"]); dma(P, "sp", SL[:], cSL, writes=["SL"])
            dma(P, "sp", Mask2[:], cMask2, writes=["Mask2"]); dma(P, "sp", Bones[:], cBones, writes=["Bones"])
            P.op("pool", lambda: G.memset(ones64[:], 1.0), writes=["ones64"])
            P.op("act", lambda: A.activation(out=alog_t[:], in_=alog_t[:], func=AF.Exp), reads=["alog"], writes=["alog"])
            P.op("dve", lambda: V.tensor_scalar(out=alog_t[:], in0=alog_t[:], scalar1=-1.0, scalar2=None, op0=ALU.mult), reads=["alog"], writes=["alog"])
            xraw = [sb([128, 515], F32) for _ in range(3)]
            for c_ in range(3):
                P.op("pool", lambda c_=c_: G.memset(xraw[c_][:], 0.0), writes=[("xraw", c_)])
            cvt = sb([128, 512], F32)
            qkv = [sb([128, 512], F32) for _ in range(3)]
            sqn = sb([128, 512], F32); rn_ = sb([128, 512], F32)
            Sst = [sb([64, 64], F32) for _ in range(2)]
            for h_ in range(2):
                P.op("pool", lambda h_=h_: G.memset(Sst[h_][:], 0.0), writes=[("S", h_)])
            gd = dict(ch=[], hd=[])
            for sl in range(4):
                gd["ch"].append(dict(gs=sb([64, 128], F32), bg=sb([64, 4], F32), nbeta=sb([64, 2], F32)))
            for sl in range(8):
                gd["hd"].append(dict(qkv_tm=sb([64, 3, 64], F32), gcl=sb([64, 2], F32), ex3=sb([64, 3], F32), Gm=sb([64, 64], F32),
                                     EE=sb([64, 2, 64], F32), AT=sb([64, 64], F32),
                                     W=[sb([64, 256], F32) for _ in range(2)], tb=sb([64, 1], F32),
                                     kdec=sb([64, 64], F32), qdec=sb([64, 64], F32), wqT=sb([64, 2, 64], F32), vnew=sb([64, 64], F32),
                                     osb=sb([64, 64], F32), osq=sb([64, 64], F32), oss=sb([64, 1], F32), ngate=sb([64, 64], F32)))
            ybuf = [sb([64, 8, 2, 64], F32) for _ in range(2)]

        for st in range(nst):
            hs = st % 2
            cols = slice(st * 512, (st + 1) * 512)
            dma(P, "sp", hTt[hs][:], hT4[st // (nst // 4)][:, :, (st % (nst // 4)) * 512:(st % (nst // 4) + 1) * 512], writes=[("hTt", hs)])
            hk = ("hTt", hs)
            if do_attn:
                for (w_t, wkey, dst, dk_, bank) in ((wq_t, "wq", QT, "QT", 0), (wk_t, "wk", KT, "KT", 1)):
                    def mmqk(w_t=w_t, bank=bank, hs=hs):
                        for kc in range(8):
                            ins = T.matmul(B[bank][0:96, :], lhsT=w_t[:, kc, :], rhs=hTt[hs][:, kc, :], start=(kc == 0), stop=(kc == 7))
                        return ins
                    P.op("pe", mmqk, reads=[hk, wkey], writes=[f"b{bank}"])
                    P.op("act", lambda dst=dst, bank=bank, cols=cols: A.copy(out=dst[:, cols], in_=B[bank][0:96, :]),
                         reads=[f"b{bank}"], writes=[(dk_, st)])
                for pair in range(2):
                    bank = 2 + pair
                    pv = B[bank][:, 0:384].rearrange("p (j c) -> p j c", j=2)

                    def mmv(pair=pair, pv=pv, hs=hs):
                        for j in range(2):
                            blk = pair * 2 + j
                            for kc in range(8):
                                ins = T.matmul(pv[:, j, :], lhsT=hTt[hs][:, kc, blk * 128:(blk + 1) * 128], rhs=wv_t[:, kc, :],
                                               start=(kc == 0), stop=(kc == 7))
                        return ins
                    P.op("pe", mmv, reads=[hk, "wv"], writes=[f"b{bank}"])
                    b0 = st * 4 + pair * 2
                    P.op("dve", lambda pv=pv, b0=b0: V.tensor_copy(out=Vall[:, b0:b0 + 2, :, 0:64],
                                                                  in_=pv.rearrange("p j (u d) -> p j u d", u=3)),
                         reads=[f"b{bank}"], writes=[("V", st, pair)])
            if do_s5:
                for pr in range(2):
                    def mmu(hs=hs, pr=pr):
                        for kc in range(8):
                            ins = T.matmul(B[4][0:32, :], lhsT=wu_t[:, kc, pr * 32:(pr + 1) * 32], rhs=hTt[hs][:, kc, :], start=(kc == 0), stop=(kc == 7))
                        return ins
                    P.op("pe", mmu, reads=[hk, "wu"], writes=["b4"])
                    P.op("act", lambda pr=pr: A.copy(out=uT[pr][:], in_=B[4][0:32, :]), reads=["b4"], writes=[("uT", pr)])
                def s5_stream(pr):
                    t = s5[pr]
                    k = lambda n, pr=pr: ("s5", "sh" if n in ("bre", "bim", "t1", "t2", "zre", "zim", "xre", "xim", "tab", "roww_scratch") else pr, n)
                    P.op("pe", lambda t=t, pr=pr: T.matmul(B[5][:], lhsT=t["bbr"][:], rhs=uT[pr][:], start=True, stop=True),
                         reads=[("uT", pr), k("roww")], writes=["b5"])
                    P.op("pe", lambda t=t, pr=pr: T.matmul(B[6][:], lhsT=t["bbi"][:], rhs=uT[pr][:], start=True, stop=True),
                         reads=[("uT", pr), k("roww")], writes=["b6"])
                    P.op("act", lambda t=t: A.copy(out=t["bre"][:], in_=B[5][:]), reads=["b5"], writes=[k("bre")])
                    P.op("act", lambda t=t: A.copy(out=t["bim"][:], in_=B[6][:]), reads=["b6"], writes=[k("bim")])
                    P.op("dve", lambda t=t: V.tensor_tensor(out=t["t1"][:], in0=t["bre"][:], in1=t["CS"][:], op=ALU.mult), reads=[k("bre"), k("tab")], writes=[k("t1")])
                    P.op("pool", lambda t=t: G.tensor_tensor(out=t["t2"][:], in0=t["bim"][:], in1=t["SN"][:], op=ALU.mult), reads=[k("bim"), k("tab")], writes=[k("t2")])
                    P.op("dve", lambda t=t: V.tensor_tensor(out=t["t1"][:], in0=t["t1"][:], in1=t["t2"][:], op=ALU.add), reads=[k("t1"), k("t2")], writes=[k("t1")])
                    P.op("pool", lambda t=t: G.tensor_tensor(out=t["t2"][:], in0=t["bim"][:], in1=t["CS"][:], op=ALU.mult), reads=[k("bim"), k("tab"), k("t1")], writes=[k("t2")])
                    P.op("pool", lambda t=t: G.tensor_tensor(out=t["bre"][:], in0=t["bre"][:], in1=t["SN"][:], op=ALU.mult), reads=[k("bre"), k("tab"), k("t1")], writes=[k("bre")])
                    P.op("pool", lambda t=t: G.tensor_tensor(out=t["t2"][:], in0=t["t2"][:], in1=t["bre"][:], op=ALU.subtract), reads=[k("t2"), k("bre")], writes=[k("t2")])
                    P.op("dve", lambda t=t: V.tensor_tensor_scan(out=t["zre"][:], data0=t["RHO"][:], data1=t["t1"][:], initial=t["zi"][:, 0:1],
                                                                  op0=ALU.mult, op1=ALU.add), reads=[k("t1"), k("rho"), k("zi")], writes=[k("zre")])
                    P.op("dve", lambda t=t: V.tensor_tensor_scan(out=t["zim"][:], data0=t["RHO"][:], data1=t["t2"][:], initial=t["zi"][:, 1:2],
                                                                  op0=ALU.mult, op1=ALU.add), reads=[k("t2"), k("rho"), k("zi")], writes=[k("zim")])
                    cdt, cth, crho, ca, ctmp, c512s, c512c, cx = [t["cw"][i][:] for i in range(8)]
                    zl_re, zl_im = t["zre"][:, 511:512], t["zim"][:, 511:512]
                    P.op("dve", lambda t=t, zl_re=zl_re: V.tensor_tensor(out=t["zt"][:, 0:1], in0=zl_re, in1=c512c, op=ALU.mult), reads=[k("zre"), k("colw")], writes=[k("zt")])
                    P.op("dve", lambda t=t, zl_im=zl_im: V.tensor_tensor(out=t["zt"][:, 1:2], in0=zl_im, in1=c512s, op=ALU.mult), reads=[k("zim"), k("colw")], writes=[k("zt")])
                    P.op("dve", lambda t=t: V.tensor_tensor(out=t["zi"][:, 0:1], in0=t["zt"][:, 0:1], in1=t["zt"][:, 1:2], op=ALU.subtract), reads=[k("zt"), k("zi")], writes=[k("zi")])
                    P.op("dve", lambda t=t, zl_re=zl_re: V.tensor_tensor(out=t["zt"][:, 0:1], in0=zl_re, in1=c512s, op=ALU.mult), reads=[k("zre"), k("colw"), k("zi")], writes=[k("zt")])
                    P.op("dve", lambda t=t, zl_im=zl_im: V.tensor_tensor(out=t["zt"][:, 1:2], in0=zl_im, in1=c512c, op=ALU.mult), reads=[k("zim"), k("colw")], writes=[k("zt")])
                    P.op("dve", lambda t=t: V.tensor_tensor(out=t["zi"][:, 1:2], in0=t["zt"][:, 0:1], in1=t["zt"][:, 1:2], op=ALU.add), reads=[k("zt"), k("zi")], writes=[k("zi")])
                    P.op("dve", lambda t=t: V.tensor_tensor(out=t["xre"][:], in0=t["zre"][:], in1=t["CS"][:], op=ALU.mult), reads=[k("zre"), k("tab")], writes=[k("xre")])
                    P.op("pool", lambda t=t: G.tensor_tensor(out=t["t1"][:], in0=t["zim"][:], in1=t["SN"][:], op=ALU.mult), reads=[k("zim"), k("tab"), k("zre")], writes=[k("t1")])
                    P.op("dve", lambda t=t: V.tensor_tensor(out=t["xre"][:], in0=t["xre"][:], in1=t["t1"][:], op=ALU.subtract), reads=[k("xre"), k("t1")], writes=[k("xre")])
                    P.op("pool", lambda t=t: G.tensor_tensor(out=t["xim"][:], in0=t["zre"][:], in1=t["SN"][:], op=ALU.mult), reads=[k("zre"), k("tab")], writes=[k("xim")])
                    P.op("pool", lambda t=t: G.tensor_tensor(out=t["t2"][:], in0=t["zim"][:], in1=t["CS"][:], op=ALU.mult), reads=[k("zim"), k("tab"), k("zim")], writes=[k("t2")])
                    P.op("pool", lambda t=t: G.tensor_tensor(out=t["xim"][:], in0=t["xim"][:], in1=t["t2"][:], op=ALU.add), reads=[k("xim"), k("t2")], writes=[k("xim")])

                    yb_ = 7 if pr == 0 else 3

                    def mmy(t=t, pr=pr, yb_=yb_):
                        T.matmul(B[yb_][0:32, :], lhsT=t["CrBD"][:], rhs=t["xre"][:], start=True, stop=False)
                        return T.matmul(B[yb_][0:32, :], lhsT=t["CiBD"][:], rhs=t["xim"][:], start=False, stop=True)
                    P.op("pe", mmy, reads=[k("xre"), k("xim"), k("CrBD"), k("CiBD")], writes=[f"b{yb_}"])
                    P.op("dve", lambda pr=pr, yb_=yb_: V.scalar_tensor_tensor(out=yT[pr][:], in0=uT[pr][:], scalar=d_col[pr][:, 0:1], in1=B[yb_][0:32, :],
                                                                             op0=ALU.mult, op1=ALU.add),
                         reads=[f"b{yb_}", ("uT", pr), ("dcol", pr)], writes=[("yT", pr)])
                for pr_ in range(2):
                    s5_stream(pr_)
                pvy = B[4][:, 0:256].rearrange("p (j d) -> p j d", j=4)

                def try4(pvy=pvy):
                    for pr in range(2):
                        for j in range(4):
                            ins = T.transpose(out=pvy[:, j, pr * 32:(pr + 1) * 32], in_=yT[pr][:, j * 128:(j + 1) * 128], identity=ident32[0:32, 0:32])
                    return ins
                P.op("pe", try4, reads=[("yT", 0), ("yT", 1), "ident32"], writes=["b4"])
                P.op("act", lambda pvy=pvy, hs=hs: A.copy(out=yc_tm[hs][:], in_=pvy), reads=["b4"], writes=[("yc_tm", hs)])
                dma(P, "poolq", yc_o[cols, :].rearrange("(j p) d -> p j d", p=128), yc_tm[hs][:], reads=[("yc_tm", hs)], writes=[("yc_o", st)])
                outs.append(("yc_o", st))
            if do_gdn:
                gdn_supertile(P, nc, B, st, hs, hk, hTt, wg_t, wt_t, cvw_t, xraw, cvt, qkv, sqn, rn_, Bones, eps_rms, alog_t, dtb_t, ng_t,
                              Tri, SL, Mask2, ones64, ident32, Sst, gd, ybuf, yb_o, outs)
                try_ag()

        if do_gdn:
            gdn_round(P, gd, [], yb_o, outs)
            try_ag()
            assert ag_stream is None or ag_n[0] == len(ag_stream[0])
        if do_attn:
            scale = 32 ** -0.5
            allqk = [("QT", s_) for s_ in range(nst)] + [("KT", s_) for s_ in range(nst)] + [("V", s_, p_) for s_ in range(nst) for p_ in range(2)] + ["Vones"]
            cnt = 0
            for u in range(3):
                for qt in range(nst):
                    nkb = 4 * (qt + 1)
                    bo = 3 + (qt % 2)
                    pend = []

                    def issue_s(kb, u=u, qt=qt):
                        nonlocal cnt
                        slot = SBANKS[cnt % len(SBANKS)]
                        ps_ = cnt % NPT
                        cnt += 1
                        P.op("pe", lambda: T.matmul(B[slot][:], lhsT=KT[32 * u:32 * u + 32, kb * 128:(kb + 1) * 128],
                                                    rhs=QT[32 * u:32 * u + 32, qt * 512:(qt + 1) * 512], start=True, stop=True),
                             reads=allqk, writes=[f"b{slot}"])
                        P.op("act", lambda: A.activation(out=PT[ps_][:], in_=B[slot][:], func=AF.Exp, scale=scale),
                             reads=[f"b{slot}"], writes=[("PT", ps_)])
                        if kb >= 4 * qt:
                            j = kb - 4 * qt
                            P.op("pool", lambda: G.tensor_tensor(out=PT[ps_][:], in0=PT[ps_][:], in1=am_t[:, j, :], op=ALU.mult),
                                 reads=[("PT", ps_), "amask"], writes=[("PT", ps_)])
                        return ps_

                    def issue_av(kb, ps_, u=u, bo=bo, nkb=nkb):
                        P.op("pe", lambda: T.matmul(B[bo][0:65, :], lhsT=Vall[:, kb, u, :], rhs=PT[ps_][:], start=(kb == 0), stop=(kb == nkb - 1)),
                             reads=[("PT", ps_)] + allqk, writes=[f"b{bo}"])
                    for kb in range(nkb):
                        pend.append((kb, issue_s(kb)))
                        if len(pend) > ADEPTH:
                            issue_av(*pend.pop(0))
                    while pend:
                        issue_av(*pend.pop(0))
                    P.op("act", lambda bo=bo: A.copy(out=osb[:], in_=B[bo][0:65, :]), reads=[f"b{bo}"], writes=["osb"])
                    pvo = B[5][:, 0:260].rearrange("p (j d) -> p j d", j=4)

                    def tro(pvo=pvo):
                        for j in range(4):
                            ins = T.transpose(out=pvo[:, j, :], in_=osb[:, j * 128:(j + 1) * 128], identity=ident32[0:65, 0:65])
                        return ins
                    P.op("pe", tro, reads=["osb", "ident32"], writes=["b5"])
                    P.op("dve", lambda pvo=pvo: V.reciprocal(out=rec[:], in_=pvo[:, :, 64]), reads=["b5"], writes=["rec"])
                    os_ = qt % 2
                    P.op("dve", lambda pvo=pvo, os_=os_: V.tensor_tensor(out=oT[os_][:], in0=pvo[:, :, 0:64],
                                                                        in1=rec[:].unsqueeze(2).to_broadcast([128, 4, 64]), op=ALU.mult),
                         reads=["b5", "rec"], writes=[("oT", os_)])
                    dma(P, "sp", ya_o[qt * 512:(qt + 1) * 512, u, :].rearrange("(j p) d -> p j d", p=128), oT[os_][:],
                        reads=[("oT", os_)], writes=[("ya_o", u, qt)])
                    outs.append(("ya_o", u, qt))
        P.emit(final_wait_keys=outs)


def gdn_supertile(P, nc, B, st, hs, hk, hTt, wg_t, wt_t, cvw_t, xraw, cvt, qkv, sqn, rn_, Bones, eps_rms, nA_t, dtb_t, ng_t,
                  Tri, SL, Mask2, ones64, ident32, Sst, gd, ybuf, yb_o, outs):
    V, G, A, T = nc.vector, nc.gpsimd, nc.scalar, nc.tensor
    for c in range(3):
        def mm(c=c):
            for kc in range(8):
                ins = T.matmul(B[c][:], lhsT=wg_t[:, kc, c * 128:(c + 1) * 128], rhs=hTt[hs][:, kc, :], start=(kc == 0), stop=(kc == 7))
            return ins
        P.op("pe", mm, reads=[hk, "wg"], writes=[f"b{c}"])
        P.op("pool", lambda c=c: G.tensor_copy(out=xraw[c][:, 0:3], in_=xraw[c][:, 512:515]), reads=[("xraw", c)], writes=[("xraw", c)])
        P.op("act", lambda c=c: A.copy(out=xraw[c][:, 3:515], in_=B[c][:]), reads=[f"b{c}", ("xraw", c)], writes=[("xraw", c)])
        P.op("dve", lambda c=c: V.tensor_scalar(out=cvt[:], in0=xraw[c][:, 0:512], scalar1=cvw_t[:, c, 0:1], scalar2=None, op0=ALU.mult),
             reads=[("xraw", c), "cvw"], writes=["cvt"])
        for kk in range(1, 4):
            P.op("dve", lambda c=c, kk=kk: V.scalar_tensor_tensor(out=cvt[:], in0=xraw[c][:, kk:kk + 512], scalar=cvw_t[:, c, kk:kk + 1],
                                                                  in1=cvt[:], op0=ALU.mult, op1=ALU.add),
                 reads=[("xraw", c), "cvw", "cvt"], writes=["cvt"])
        P.op("act", lambda c=c: A.activation(out=qkv[c][:], in_=cvt[:], func=AF.Silu), reads=["cvt"], writes=[("qkv", c)])
    for c in range(2):
        P.op("pool", lambda c=c: G.tensor_tensor(out=sqn[:], in0=qkv[c][:], in1=qkv[c][:], op=ALU.mult), reads=[("qkv", c)], writes=["sqn"])
        P.op("pe", lambda: T.matmul(B[3][:], lhsT=Bones[:], rhs=sqn[:], start=True, stop=True), reads=["sqn", "Bones"], writes=["b3"])
        P.op("act", lambda: A.activation(out=rn_[:], in_=B[3][:], func=AF.Sqrt, bias=eps_rms[:, 0:1], scale=1.0), reads=["b3", "eps_rms"], writes=["rn"])
        P.op("dve", lambda: V.reciprocal(out=rn_[:], in_=rn_[:]), reads=["rn"], writes=["rn"])
        if c == 0:
            P.op("dve", lambda: V.scalar_tensor_tensor(out=qkv[0][:], in0=qkv[0][:], scalar=0.125, in1=rn_[:], op0=ALU.mult, op1=ALU.mult),
                 reads=[("qkv", 0), "rn"], writes=[("qkv", 0)])
        else:
            P.op("dve", lambda: V.tensor_tensor(out=qkv[1][:], in0=qkv[1][:], in1=rn_[:], op=ALU.mult), reads=[("qkv", 1), "rn"], writes=[("qkv", 1)])
    qk_all = [("qkv", 0), ("qkv", 1), ("qkv", 2)]
    yb_s = st % 2
    for cp in range(4):
        new = []
        for c in (2 * cp, 2 * cp + 1):
            cg = st * 8 + c
            cs = slice(c * 64, (c + 1) * 64)
            dch = gd["ch"][cg % 4]
            kch = lambda n, cg=cg: ("gch", cg % 4, n)

            def mmt(cs=cs):
                for kc in range(8):
                    ins = T.matmul(B[0][0:64, 0:132], lhsT=hTt[hs][:, kc, cs], rhs=wt_t[:, kc, :], start=(kc == 0), stop=(kc == 7))
                return ins
            mk = ["b0"]
            P.op("pe", mmt, reads=[hk, "wt"], writes=mk)
            P.op("act", lambda dch=dch: A.activation(out=dch["gs"][:], in_=B[0][0:64, 0:128], func=AF.Silu), reads=mk, writes=[kch("gs")])
            P.op("act", lambda dch=dch: A.activation(out=dch["bg"][:, 0:2], in_=B[0][0:64, 128:130], func=AF.Sigmoid), reads=mk, writes=[kch("bg")])
            P.op("dve", lambda dch=dch: V.tensor_tensor(out=dch["bg"][:, 2:4], in0=B[0][0:64, 130:132], in1=dtb_t[:], op=ALU.add),
                 reads=mk + ["dtb", kch("bg")], writes=[kch("bg")])
            P.op("act", lambda dch=dch: A.activation(out=dch["bg"][:, 2:4], in_=dch["bg"][:, 2:4], func=AF.Exp), reads=[kch("bg")], writes=[kch("bg")])
            P.op("act", lambda dch=dch: A.activation(out=dch["bg"][:, 2:4], in_=dch["bg"][:, 2:4], func=AF.Ln, bias=1.0, scale=1.0), reads=[kch("bg")], writes=[kch("bg")])
            P.op("dve", lambda dch=dch: V.tensor_tensor(out=dch["bg"][:, 2:4], in0=dch["bg"][:, 2:4], in1=nA_t[:], op=ALU.mult),
                 reads=[kch("bg"), "alog"], writes=[kch("bg")])
            P.op("dve", lambda dch=dch: V.tensor_scalar(out=dch["nbeta"][:], in0=dch["bg"][:, 0:2], scalar1=-1.0, scalar2=None, op0=ALU.mult),
                 reads=[kch("bg")], writes=[kch("nbeta")])
            sl0 = (cg % 4) * 2
            new.append([gdn_chunk_head(P, nc, B, h, cs, c, dch, kch, gd["hd"][sl0 + h], sl0 + h, 4 + (cg % 2) * 2 + h, qkv, qk_all, ng_t, Tri, SL, Mask2,
                                       ones64, ident32, Sst, eps_rms, ybuf[yb_s], yb_s) for h in range(2)])
        gdn_round(P, gd, new, yb_o, outs)
        if cp == 3:
            gd["pend_dma"] = (st, yb_s, ybuf[yb_s])


def gdn_round(P, gd, new, yb_o, outs):
    oldg = list(gd.get("pendB", []))
    had_old = bool(oldg)
    actA = [g for grp in new for g in grp]
    curB = oldg.pop(0) if oldg else []
    while actA or curB:
        for g in list(actA):
            try:
                r = next(g)
            except StopIteration:
                raise RuntimeError("chain ended inside stage A")
            if r == "END_A":
                actA.remove(g)
        for g in list(curB):
            try:
                next(g)
            except StopIteration:
                curB.remove(g)
        if not curB and oldg:
            curB = oldg.pop(0)
    gd["pendB"] = [list(grp) for grp in new]
    pd = gd.get("pend_dma")
    if pd is not None and had_old:
        st, yb_s, ybt = pd
        cols = slice(st * 512, (st + 1) * 512)
        dma(P, "poolq", yb_o[cols].rearrange("(c p) h d -> p c h d", p=64), ybt[:], reads=[("ybuf", yb_s, c_, h_) for c_ in range(8) for h_ in range(2)],
            writes=[("yb_o", st)])
        outs.append(("yb_o", st))
        gd["pend_dma"] = None


def gdn_chunk_head(P, nc, B, h, cs, c, dch, kch, d, sl, bank, qkv, qk_all, ng_t, Tri, SL, Mask2, ones64, ident32, Sst, eps_rms, ybuf, yb_s):
    V, G, A, T = nc.vector, nc.gpsimd, nc.scalar, nc.tensor
    hp = slice(h * 64, (h + 1) * 64)
    idh = ident32[hp, hp]
    id0 = ident32[0:64, 0:64]
    k = lambda n: ("ghd", sl, n)
    PA, P3 = B[bank], B[3]
    bk, b3 = [f"b{bank}"], ["b3"]
    o3 = 256 * h
    W = d["W"]
    g_col = dch["bg"][:, 2 + h:3 + h]
    beta_col = dch["bg"][:, h:h + 1]
    nbeta_col = dch["nbeta"][:, h:h + 1]

    def tr1():
        for c3 in range(3):
            ins = T.transpose(out=PA[0:64, c3 * 64:(c3 + 1) * 64], in_=qkv[c3][hp, cs], identity=idh)
        return ins
    P.op("pe", tr1, reads=qk_all + ["ident32"], writes=bk); yield
    P.op("act", lambda: A.copy(out=d["qkv_tm"][:].rearrange("p a b -> p (a b)"), in_=PA[0:64, 0:192]), reads=bk, writes=[k("qkv_tm")]); yield

    def mm2():
        T.matmul(PA[0:64, 256:257], lhsT=Tri[:], rhs=g_col, start=True, stop=True)
        return T.matmul(PA[0:64, 257:258], lhsT=ones64[:], rhs=g_col, start=True, stop=True)
    P.op("pe", mm2, reads=[kch("bg"), "Tri", "ones64"], writes=bk); yield
    P.op("dve", lambda: V.tensor_copy(out=d["gcl"][:], in_=PA[0:64, 256:258]), reads=bk, writes=[k("gcl")]); yield
    P.op("act", lambda: A.activation(out=d["ex3"][:, 0:1], in_=d["gcl"][:, 0:1], func=AF.Exp), reads=[k("gcl")], writes=[k("ex3")]); yield
    P.op("act", lambda: A.activation(out=d["ex3"][:, 1:2], in_=d["gcl"][:, 0:1], func=AF.Exp, bias=d["gcl"][:, 1:2], scale=-1.0),
         reads=[k("gcl"), k("ex3")], writes=[k("ex3")]); yield
    P.op("act", lambda: A.activation(out=d["ex3"][:, 2:3], in_=d["gcl"][:, 1:2], func=AF.Exp), reads=[k("gcl"), k("ex3")], writes=[k("ex3")]); yield
    P.op("dve", lambda: V.tensor_scalar(out=d["Gm"][:], in0=Tri[:], scalar1=g_col, scalar2=None, op0=ALU.mult), reads=["Tri", kch("bg")], writes=[k("Gm")]); yield

    def mm3():
        T.matmul(PA[0:64, 384:448], lhsT=d["Gm"][:], rhs=SL[:], start=True, stop=True)
        return T.matmul(PA[0:64, 448:512], lhsT=SL[:], rhs=d["Gm"][:], start=True, stop=True)
    P.op("pe", mm3, reads=[k("Gm"), "SL"], writes=bk); yield
    P.op("act", lambda: A.activation(out=d["EE"][:].rearrange("p a b -> p (a b)"), in_=PA[0:64, 384:512], func=AF.Exp), reads=bk, writes=[k("EE")]); yield
    P.op("pool", lambda: G.tensor_tensor(out=d["EE"][:], in0=d["EE"][:], in1=Mask2[:], op=ALU.mult), reads=[k("EE"), "Mask2"], writes=[k("EE")]); yield

    def mm4():
        T.matmul(PA[0:64, 0:64], lhsT=qkv[1][hp, cs], rhs=qkv[1][hp, cs], start=True, stop=True)
        return T.matmul(PA[0:64, 64:128], lhsT=qkv[1][hp, cs], rhs=qkv[0][hp, cs], start=True, stop=True)
    P.op("pe", mm4, reads=qk_all, writes=bk); yield
    P.op("dve", lambda: V.scalar_tensor_tensor(out=W[0][:, 128:192], in0=PA[0:64, 0:64], scalar=nbeta_col, in1=d["EE"][:, 0, :], op0=ALU.mult, op1=ALU.mult),
         reads=bk + [kch("nbeta"), k("EE")], writes=[k("W0p")]); yield
    P.op("dve", lambda: V.tensor_tensor(out=d["AT"][:], in0=PA[0:64, 64:128], in1=d["EE"][:, 1, :], op=ALU.mult), reads=bk + [k("EE")], writes=[k("AT")]); yield
    P.op("pe", lambda: T.transpose(out=PA[0:64, 192:256], in_=W[0][:, 128:192], identity=id0), reads=[k("W0p"), "ident32"], writes=bk); yield
    P.op("act", lambda: A.copy(out=W[0][:, 192:256], in_=PA[0:64, 192:256]), reads=bk, writes=[k("W0t")]); yield
    P.op("dve", lambda: V.tensor_tensor(out=d["tb"][:], in0=beta_col, in1=d["ex3"][:, 0:1], op=ALU.mult), reads=[kch("bg"), k("ex3")], writes=[k("tb")]); yield
    P.op("dve", lambda: V.tensor_scalar(out=W[0][:, 0:64], in0=d["qkv_tm"][:, 2, :], scalar1=beta_col, scalar2=None, op0=ALU.mult),
         reads=[k("qkv_tm"), kch("bg")], writes=[k("W0x")]); yield
    P.op("dve", lambda: V.tensor_scalar(out=W[0][:, 64:128], in0=d["qkv_tm"][:, 1, :], scalar1=d["tb"][:, 0:1], scalar2=None, op0=ALU.mult),
         reads=[k("qkv_tm"), k("tb"), k("W0x")], writes=[k("W0x")]); yield
    wk = [[k("W0x"), k("W0p"), k("W0t")], [k("W1")]]
    for lvl in range(6):
        s_, d_ = W[lvl % 2], W[(lvl + 1) % 2]
        last = lvl == 5

        def mml(s_=s_, last=last):
            T.matmul(PA[0:64, 256:384], lhsT=s_[:, 192:256], rhs=s_[:, 0:128], start=True, stop=False)
            ins = T.matmul(PA[0:64, 256:384], lhsT=id0, rhs=s_[:, 0:128], start=False, stop=True)
            if not last:
                T.matmul(PA[0:64, 384:448], lhsT=s_[:, 192:256], rhs=s_[:, 128:192], start=True, stop=True)
                ins = T.matmul(PA[0:64, 448:512], lhsT=s_[:, 128:192], rhs=s_[:, 192:256], start=True, stop=True)
            return ins
        P.op("pe", mml, reads=wk[lvl % 2] + ["ident32"], writes=bk); yield
        n_ = 128 if last else 256
        wkeys = [k("W1")] if (lvl + 1) % 2 == 1 else [k("W0x"), k("W0p"), k("W0t")]
        if lvl % 2 == 0:
            P.op("act", lambda d_=d_, n_=n_: A.copy(out=d_[:, 0:n_], in_=PA[0:64, 256:256 + n_]), reads=bk, writes=wkeys); yield
        else:
            P.op("dve", lambda d_=d_, n_=n_: V.tensor_copy(out=d_[:, 0:n_], in_=PA[0:64, 256:256 + n_]), reads=bk, writes=wkeys); yield
    X = W[0]
    xk = [k("W0x"), k("W0p"), k("W0t")]
    P.op("pool", lambda: G.tensor_scalar(out=d["kdec"][:], in0=d["qkv_tm"][:, 1, :], scalar1=d["ex3"][:, 1:2], scalar2=None, op0=ALU.mult),
         reads=[k("qkv_tm"), k("ex3")], writes=[k("kdec")]); yield
    P.op("pool", lambda: G.tensor_scalar(out=d["qdec"][:], in0=d["qkv_tm"][:, 0, :], scalar1=d["ex3"][:, 0:1], scalar2=None, op0=ALU.mult),
         reads=[k("qkv_tm"), k("ex3")], writes=[k("qdec")]); yield

    def tr8():
        T.transpose(out=PA[0:64, 0:64], in_=X[:, 64:128], identity=id0)
        return T.transpose(out=PA[0:64, 64:128], in_=d["qdec"][:], identity=id0)
    P.op("pe", tr8, reads=xk + [k("qdec"), "ident32"], writes=bk); yield
    P.op("act", lambda: A.copy(out=d["wqT"][:].rearrange("p a b -> p (a b)"), in_=PA[0:64, 0:128]), reads=bk, writes=[k("wqT")]); yield
    P.op("pool", lambda: G.tensor_tensor(out=d["ngate"][:], in0=dch["gs"][:, h * 64:(h + 1) * 64], in1=ng_t[:], op=ALU.mult),
         reads=[kch("gs"), "ngt"], writes=[k("ngate")]); yield
    yield "END_A"
    S_ = Sst[h]
    P.op("pe", lambda: T.matmul(P3[0:64, o3:o3 + 64], lhsT=d["wqT"][:, 0, :], rhs=S_[:], start=True, stop=True), reads=[k("wqT"), ("S", h)], writes=b3); yield
    P.op("dve", lambda: V.tensor_tensor(out=d["vnew"][:], in0=X[:, 0:64], in1=P3[0:64, o3:o3 + 64], op=ALU.subtract),
         reads=b3 + xk, writes=[k("vnew")]); yield

    def mmo():
        T.matmul(P3[0:64, o3 + 64:o3 + 128], lhsT=d["wqT"][:, 1, :], rhs=S_[:], start=True, stop=False)
        T.matmul(P3[0:64, o3 + 64:o3 + 128], lhsT=d["AT"][:], rhs=d["vnew"][:], start=False, stop=True)
        return T.matmul(P3[0:64, o3 + 128:o3 + 192], lhsT=d["kdec"][:], rhs=d["vnew"][:], start=True, stop=True)
    P.op("pe", mmo, reads=[k("wqT"), ("S", h), k("AT"), k("vnew"), k("kdec")], writes=b3); yield
    P.op("dve", lambda: V.scalar_tensor_tensor(out=S_[:], in0=S_[:], scalar=d["ex3"][:, 2:3], in1=P3[0:64, o3 + 128:o3 + 192], op0=ALU.mult, op1=ALU.add),
         reads=b3 + [("S", h), k("ex3")], writes=[("S", h)]); yield
    P.op("act", lambda: A.copy(out=d["osb"][:], in_=P3[0:64, o3 + 64:o3 + 128]), reads=b3, writes=[k("osb")]); yield
    P.op("pool", lambda: G.tensor_tensor(out=d["osq"][:], in0=d["osb"][:], in1=d["osb"][:], op=ALU.mult), reads=[k("osb")], writes=[k("osq")]); yield
    P.op("dve", lambda: V.tensor_reduce(out=d["oss"][:], in_=d["osq"][:], axis=AX.X, op=ALU.add), reads=[k("osq")], writes=[k("oss")]); yield
    P.op("act", lambda: A.activation(out=d["oss"][:], in_=d["oss"][:], func=AF.Sqrt, bias=eps_rms[0:64, 0:1], scale=1.0 / 64),
         reads=[k("oss"), "eps_rms"], writes=[k("oss")]); yield
    P.op("dve", lambda: V.reciprocal(out=d["oss"][:], in_=d["oss"][:]), reads=[k("oss")], writes=[k("oss")]); yield
    P.op("dve", lambda: V.scalar_tensor_tensor(out=ybuf[:, c, h, :], in0=d["osb"][:], scalar=d["oss"][:, 0:1], in1=d["ngate"][:], op0=ALU.mult, op1=ALU.mult),
         reads=[k("osb"), k("oss"), k("ngate")], writes=[("ybuf", yb_s, c, h)]); yield


OFF_AQ, OFF_AK, OFF_AV, OFF_BQKV, OFF_BGATE, OFF_BBETA, OFF_BA, OFF_CU = 0, 384, 768, 1152, 2304, 2688, 2694, 2700
GDN_HEADS_OF = [(0, 1), (2, 3), (4, 5), (4, 5)]


def _wl(w, cols):
    return w[:, cols].reshape(8, 128, len(cols)).transpose(1, 0, 2)


def mix_consts():
    d = {}
    k = np.arange(128)[:, None, None]; j = np.arange(4)[None, :, None]; q = np.arange(512)[None, None, :]
    d["amask"] = bf((q // 64 >= (j * 128 + k) // 64).astype(np.float32))
    d["idn32"] = f32c(np.eye(128))
    d["srow"] = f32c(np.arange(512).reshape(1, 512))
    m = np.arange(64)[:, None]; i = np.arange(64)[None, :]
    d["cTri"] = f32c(m <= i)
    d["cSL"] = f32c(m > i)
    d["cMask2"] = f32c(np.stack([(m > i), (m <= i)], axis=1))
    bo = np.zeros((128, 128), np.float32); bo[:64, :64] = 1; bo[64:, 64:] = 1
    d["cBones"] = bo
    return d


def mix_inputs(l, p, j):
    w = p["w_in"][l]
    d = {}
    units = [3 * j + i for i in range(3)]
    qc, kc_, vc = [], [], []
    for u in units:
        head, mp = u // 2, u % 2
        qc += list(range(OFF_AQ + head * 64 + mp * 32, OFF_AQ + head * 64 + mp * 32 + 32))
        kc_ += list(range(OFF_AK + head * 64 + mp * 32, OFF_AK + head * 64 + mp * 32 + 32))
        vc += list(range(OFF_AV + head * 64, OFF_AV + head * 64 + 64))
    d["wq"] = bf(_wl(w, qc)); d["wk"] = bf(_wl(w, kc_)); d["wv"] = bf(_wl(w, vc))
    gs = [4 * j + i for i in range(4)]
    d["wu"] = bf(_wl(w, list(range(OFF_CU + gs[0] * 16, OFF_CU + gs[0] * 16 + 64))))
    lre, lim, ldt = p["s5_lambda_re"][l], p["s5_lambda_im"][l], p["s5_log_dt"][l]
    row = np.zeros((2, 3, 128), np.float32)
    bT = np.zeros((2, 2, 2, 16, 64), np.float32); cT = np.zeros((2, 2, 2, 64, 16), np.float32)
    for pr in range(2):
        for g in range(2):
            G_ = gs[pr * 2 + g]
            row[pr, 0, g * 64:(g + 1) * 64] = lre[G_]; row[pr, 1, g * 64:(g + 1) * 64] = lim[G_]; row[pr, 2, g * 64:(g + 1) * 64] = ldt[G_]
            bT[pr, g, 0] = p["s5_b_re"][l][G_].T; bT[pr, g, 1] = p["s5_b_im"][l][G_].T
            cT[pr, g, 0] = p["s5_c_re"][l][G_].T; cT[pr, g, 1] = p["s5_c_im"][l][G_].T
    d["s5row"] = row; d["s5col"] = f32c(row.transpose(0, 2, 1)); d["s5bT"] = bT; d["s5cT"] = cT
    d["s5d"] = f32c(p["s5_d"][l][gs[0] * 16:gs[0] * 16 + 64].reshape(64, 1))
    hA, hB = GDN_HEADS_OF[j]
    gcols = []
    for part in range(3):
        for h in (hA, hB):
            gcols += list(range(OFF_BQKV + part * 384 + h * 64, OFF_BQKV + part * 384 + h * 64 + 64))
    d["wg"] = bf(_wl(w, gcols))
    tcols = list(range(OFF_BGATE + hA * 64, OFF_BGATE + hA * 64 + 64)) + list(range(OFF_BGATE + hB * 64, OFF_BGATE + hB * 64 + 64)) \
        + [OFF_BBETA + hA, OFF_BBETA + hB, OFF_BA + hA, OFF_BA + hB]
    d["wt"] = bf(_wl(w, tcols))
    cw = p["dn_conv_w"][l]
    cv = np.zeros((128, 3, 4), np.float32)
    for part in range(3):
        for hi, h in enumerate((hA, hB)):
            cv[hi * 64:(hi + 1) * 64, part, :] = cw[:, part * 384 + h * 64: part * 384 + h * 64 + 64].T
    d["cvw"] = cv
    d["galog"] = f32c(p["dn_a_log"][l][[hA, hB]].reshape(1, 2)); d["gdtb"] = f32c(p["dn_dt_bias"][l][[hA, hB]].reshape(1, 2))
    d["gng"] = f32c(p["dn_norm_g"][l].reshape(1, 64))
    return d


YCH = 1024
NYCH = SEQ // YCH


def build_program(stop=None):
    nc = bass.Bass("TRN2", target_bir_lowering=False)
    G = Glob(nc)
    hbuf = [G.internal(f"hbuf{i}", [TOK_CORE, D]) for i in range(2)]
    hT_loc = G.internal("hT_loc", [D, TOK_CORE], BF16)
    hT_all = G.internal("hT_all", [8, 4 * 128, TOK_CORE], BF16)
    ya_o = G.internal("ya_o", [SEQ, 192])
    ybc_o = G.internal("ybc_o", [SEQ, 192])
    ya_all = G.internal("ya_all", [NYCH, 4 * YCH, 192])
    ybc_all = G.internal("ybc_all", [NYCH, 4 * YCH, 192])
    ag_h = [(hT_loc[k * 128:(k + 1) * 128, :], hT_all[k]) for k in range(8)]
    ag_ya = [(ya_o[i * YCH:(i + 1) * YCH, :], ya_all[i]) for i in range(NYCH)]
    ag_yb = [(ybc_o[i * YCH:(i + 1) * YCH, :], ybc_all[i]) for i in range(NYCH)]
    out = nc.dram_tensor("out", [TOK_CORE, D], F32, kind="ExternalOutput").ap()
    phase_pre(nc, G, hbuf[0], hT_loc)
    for l in range(DEPTH):
        last = l == DEPTH - 1
        allgather(nc, ag_h)
        phase_mix(nc, G, l, hT_all, ya_o, ybc_o, do_attn=True, do_s5=False, do_gdn=False)
        Prog._uid += 1
        with nc.semaphore(f"ccy_{Prog._uid}") as cc:
            with nc.Block() as block:
                @block.gpsimd
                def _(g):
                    g.sem_clear(cc)

            def pre(g, cc=cc):
                for s_, d_ in ag_ya:
                    g.collective_compute("AllGather", ALU.bypass, replica_groups=GROUPS, ins=[s_], outs=[d_]).then_inc(cc, 1)

            def post(g, cc=cc):
                g.wait_ge(cc, len(ag_ya) + len(ag_yb))
            phase_mix(nc, G, l, hT_all, ya_o, ybc_o, do_attn=False, do_s5=True, do_gdn=True, pool_pre=pre, pool_post=post, ag_stream=(ag_yb, cc))
        phase_post(nc, G, l, hbuf[l % 2], out if last else hbuf[(l + 1) % 2], None if last else hT_loc, (ya_all, ybc_all))
    return nc, G


def kernel(_stop=None, **inputs):
    p = {k: np.asarray(v) for k, v in inputs.items()}
    x = f32c(p["x"]).reshape(BATCH * SEQ, D)
    cores = list(range(NCORES))
    nc, G = build_program()
    shared = dict(mix_consts())
    shared["idn"] = bf(np.eye(128))
    shared["g"] = f32c(p["ln_in_g"].reshape(1, D)); shared["b"] = f32c(p["ln_in_b"].reshape(1, D))
    percore = [dict() for _ in range(4)]
    for l in range(DEPTH):
        lam_init = 0.8 - 0.6 * math.exp(-0.3 * l)
        for k, v in post_inputs(l, p, lam_init).items():
            if k not in ("idn", "idn32"):
                shared[f"L{l}_{k}"] = v
        for j in range(4):
            for k, v in mix_inputs(l, p, j).items():
                percore[j][f"L{l}_{k}"] = v
    ins = []
    for c in cores:
        d = dict(shared); d.update(percore[c % 4])
        d["x"] = x[c * TOK_CORE:(c + 1) * TOK_CORE]
        d["rofs"] = np.array([[(c % 4) * (TOK_CORE // YCH)]], np.int32)
        ins.append({k: v for k, v in d.items() if k in G.t})
    res = run_bass_kernel_spmd(nc, ins, core_ids=cores)
    h = [np.asarray(r["out"]) for r in res.results]
    return np.concatenate(h, axis=0).reshape(BATCH, SEQ, D).astype(np.float32)
```

```python
import math
from contextlib import ExitStack

import numpy as np
import ml_dtypes
import concourse.bass as bass
import concourse.mybir as mybir
from concourse.bass_utils import run_bass_kernel_spmd

F32 = mybir.dt.float32
BF16 = mybir.dt.bfloat16
I32 = mybir.dt.int32
ALU = mybir.AluOpType
AF = mybir.ActivationFunctionType
AX = mybir.AxisListType

NCORES = 8


class Prog:
    COMPUTE = ("pe", "act", "dve", "pool")
    NDMASEM = 6

    _uid = 0
    _phase = 0

    def __init__(self, nc):
        Prog._phase += 1
        self.ph = Prog._phase
        self.nc = nc
        self.ops = []
        self.last_w = {}
        self.readers = {}
        self.dma_count = {"sp": 0, "actq": 0, "poolq": 0}
        self.stack = ExitStack()
        self.nt = 0
        self.excl = set()
        self.quarters = False
        self.bankkeys = {f"b{i}" for i in range(8)}
        self.pool_pre = None
        self.pool_post = None
        self.sp_wrap = None

    def sb(self, shape, dtype, name=None):
        Prog._uid += 1
        return self.stack.enter_context(self.nc.sbuf_tensor(f"{name or 't'}_{Prog._uid}", list(shape), dtype))

    def ps(self, shape, dtype=F32, name=None):
        Prog._uid += 1
        return self.stack.enter_context(self.nc.psum_tensor(f"{name or 'p'}_{Prog._uid}", list(shape), dtype))

    def op(self, eng, fn, reads=(), writes=()):
        idx = len(self.ops)
        isdma = eng in self.dma_count
        issue = {"sp": "sp", "actq": "act", "poolq": "pool"}.get(eng, eng)
        if self.quarters:
            ex = lambda ks: [q for k in ks for q in ([f"{k}q{i}" for i in range(4)] if k in self.bankkeys else [k])]
            reads, writes = ex(reads), ex(writes)
        if self.excl:
            writes = list(writes) + [k for k in reads if k in self.excl]
            reads = [k for k in reads if k not in self.excl]
        deps = set()
        for k in reads:
            w = self.last_w.get(k)
            if w is not None:
                deps.add(w)
        for k in writes:
            w = self.last_w.get(k)
            if w is not None:
                deps.add(w)
            for r in self.readers.get(k, ()):
                deps.add(r)
        o = dict(idx=idx, eng=eng, issue=issue, fn=fn, deps=deps, isdma=isdma, needed=False)
        if isdma:
            n = self.dma_count[eng]
            self.dma_count[eng] = n + 1
            o["dsem"] = n % self.NDMASEM
            o["dtarget"] = 16 * (n // self.NDMASEM + 1)
            o["dprev"] = 16 * (n // self.NDMASEM)
        self.ops.append(o)
        for k in writes:
            self.last_w[k] = idx
            self.readers[k] = []
        for k in reads:
            lst = self.readers.setdefault(k, [])
            if not isdma:
                lst[:] = [r for r in lst if self.ops[r]["isdma"] or self.ops[r]["eng"] != eng]
            lst.append(idx)
        return idx

    def emit(self, final_wait_keys=()):
        nc = self.nc
        ops = self.ops
        for o in ops:
            nd = set()
            for d in o["deps"]:
                p = ops[d]
                if (not p["isdma"]) and (not o["isdma"]) and p["eng"] == o["eng"]:
                    if o["eng"] == "pe":
                        continue
                nd.add(d)
            o["deps"] = nd
            for d in nd:
                ops[d]["needed"] = True
        final = [self.last_w[k] for k in final_wait_keys if k in self.last_w]
        for d in final:
            ops[d]["needed"] = True
        tick = {e: 0 for e in self.COMPUTE}
        for o in ops:
            if not o["isdma"] and o["needed"]:
                tick[o["eng"]] += 1
                o["tick"] = tick[o["eng"]]
        sems = {e: self.stack.enter_context(nc.semaphore(f"s_{e}_{self.ph}")) for e in self.COMPUTE}
        dsems = {q: [self.stack.enter_context(nc.semaphore(f"d_{q}{i}_{self.ph}")) for i in range(self.NDMASEM)]
                 for q in self.dma_count}
        per = {e: [] for e in ("pe", "act", "dve", "pool", "sp")}
        for o in ops:
            per[o["issue"]].append(o)
        engobj = {"pe": nc.tensor, "act": nc.scalar, "dve": nc.vector, "pool": nc.gpsimd, "sp": nc.sync}

        def run(ename, extra_final=False):
            eng = engobj[ename]
            waited = {}

            def wait_for(p):
                if p["isdma"]:
                    key = (p["eng"], p["dsem"])
                    val = p["dtarget"]
                    s = dsems[p["eng"]][p["dsem"]]
                else:
                    key = p["eng"]
                    val = p["tick"]
                    s = sems[p["eng"]]
                if waited.get(key, 0) >= val:
                    return
                waited[key] = val
                eng.wait_ge(s, val)

            for o in per[ename]:
                for d in sorted(o["deps"]):
                    wait_for(ops[d])
                if o["isdma"] and o["dprev"] > 0:
                    key = (o["eng"], o["dsem"])
                    if waited.get(key, 0) < o["dprev"]:
                        waited[key] = o["dprev"]
                        eng.wait_ge(dsems[o["eng"]][o["dsem"]], o["dprev"])
                ins = o["fn"]()
                if o["isdma"]:
                    ins.then_inc(dsems[o["eng"]][o["dsem"]], 16)
                elif o["needed"]:
                    ins.then_inc(sems[o["eng"]], 1)
            if extra_final:
                for d in final:
                    wait_for(ops[d])

        allsems = list(sems.values()) + [s for q in dsems.values() for s in q]
        with nc.Block() as block:
            @block.gpsimd
            def _(e):
                for s in allsems:
                    e.sem_clear(s)

        with nc.Block() as block:
            @block.sync
            def _(e):
                if self.sp_wrap is not None:
                    with self.sp_wrap(e):
                        run("sp", extra_final=True)
                else:
                    run("sp", extra_final=True)

            @block.tensor
            def _(e):
                run("pe")

            @block.scalar
            def _(e):
                run("act")

            @block.vector
            def _(e):
                run("dve")

            @block.gpsimd
            def _(e):
                if self.pool_pre is not None:
                    self.pool_pre(e)
                run("pool")
                if self.pool_post is not None:
                    self.pool_post(e)


D = 1024
SEQ = 16384
BATCH = 2
DEPTH = 2
TOK_CORE = 4096
ALPHA = (2 * DEPTH) ** 0.25
LN_EPS = 1e-5
RMS_EPS = 1e-6
NEXP = 16
DEXP = 512


def bf(a):
    return np.ascontiguousarray(np.asarray(a, np.float32).astype(ml_dtypes.bfloat16))


def f32c(a):
    return np.ascontiguousarray(np.asarray(a, np.float32))


class Ctx:
    def __init__(self, nc):
        self.nc = nc
        self.P = Prog(nc)
        self.banks = None

    def alloc_banks(self, quarters=False):
        self.banks = [self.P.ps([128, 512], F32, name=f"bank{i}") for i in range(8)]
        self.P.excl |= {f"b{i}" for i in range(8)}
        if quarters:
            self.P.quarters = True
            self.P.excl |= {f"b{i}q{q}" for i in range(8) for q in range(4)}


class Glob:
    def __init__(self, nc):
        self.nc = nc
        self.t = {}

    def din(self, name, shape, dt=F32):
        if name not in self.t:
            self.t[name] = self.nc.dram_tensor(name, list(shape), dt, kind="ExternalInput").ap()
        return self.t[name]

    def internal(self, name, shape, dt=F32):
        if name not in self.t:
            self.t[name] = self.nc.dram_tensor(name, list(shape), dt).ap()
        return self.t[name]


GROUPS = [[0, 1, 2, 3], [4, 5, 6, 7]]


def allgather(nc, pairs):
    Prog._uid += 1
    with nc.semaphore(f"cc_{Prog._uid}") as cc:
        with nc.Block() as block:
            @block.gpsimd
            def _(g):
                g.sem_clear(cc)
        with nc.Block() as block:
            @block.gpsimd
            def _(g):
                for src, dst in pairs:
                    g.collective_compute("AllGather", ALU.bypass, replica_groups=GROUPS, ins=[src], outs=[dst]).then_inc(cc, 1)
                g.wait_ge(cc, len(pairs))


def dma(P, q, out, in_, reads=(), writes=()):
    eng = {"sp": P.nc.sync, "actq": P.nc.scalar, "poolq": P.nc.gpsimd}[q]
    return P.op(q, lambda: eng.dma_start(out=out, in_=in_), reads=reads, writes=writes)


def layer_norm_tile(P, nc, src, srck, dst, dstk, g_t, b_t, gk, bk, scr, tag):
    st, mv, rstd, xn = scr["st"], scr["mv"], scr["rstd"], scr["xn"]

    def bs():
        nc.vector.bn_stats(out=st[:, 0, :], in_=src[:, 0:512])
        return nc.vector.bn_stats(out=st[:, 1, :], in_=src[:, 512:1024])
    P.op("dve", bs, reads=[srck], writes=[tag + "st"])
    P.op("dve", lambda: nc.vector.bn_aggr(out=mv[:], in_=st[:].rearrange("p a s -> p (a s)")),
         reads=[tag + "st"], writes=[tag + "mv"])
    P.op("act", lambda: nc.scalar.activation(out=rstd[:], in_=mv[:, 1:2], func=AF.Sqrt, bias=scr["eps_ln"][:, 0:1], scale=1.0),
         reads=[tag + "mv", "eps_ln"], writes=[tag + "rstd"])
    P.op("dve", lambda: nc.vector.reciprocal(out=rstd[:], in_=rstd[:]), reads=[tag + "rstd"], writes=[tag + "rstd"])
    P.op("dve", lambda: nc.vector.tensor_scalar(out=xn[:], in0=src[:], scalar1=mv[:, 0:1], scalar2=rstd[:, 0:1],
                                                op0=ALU.subtract, op1=ALU.mult),
         reads=[srck, tag + "mv", tag + "rstd"], writes=[tag + "xn"])
    P.op("pool", lambda: nc.gpsimd.tensor_tensor(out=xn[:], in0=xn[:], in1=g_t[:], op=ALU.mult),
         reads=[tag + "xn", gk], writes=[tag + "xn"])
    P.op("pool", lambda: nc.gpsimd.tensor_tensor(out=dst[:], in0=xn[:], in1=b_t[:], op=ALU.add),
         reads=[tag + "xn", bk], writes=[dstk])


def ln_scratch(P, tag):
    return dict(st=P.sb([128, 2, 6], F32), mv=P.sb([128, 2], F32), rstd=P.sb([128, 1], F32),
                xn=P.sb([128, 1024], F32))


def const_col(P, nc, val, key):
    t = P.sb([128, 1], F32)
    P.op("pool", lambda: nc.gpsimd.memset(t[:], val), writes=[key])
    return t


def to_featmajor_bf16(P, nc, src, srck, hb, hbk, bank, bankk, dstT, dstk, ident, cast_eng="act"):
    if cast_eng == "act":
        P.op("act", lambda: nc.scalar.copy(out=hb[:], in_=src[:]), reads=[srck], writes=[hbk])
    else:
        P.op("pool", lambda: nc.gpsimd.tensor_copy(out=hb[:], in_=src[:]), reads=[srck], writes=[hbk])
    pv = bank[:].bitcast(BF16).rearrange("p (k t) -> p k t", k=8)

    def tr():
        for kc in range(8):
            ins = nc.tensor.transpose(out=pv[:, kc, :], in_=hb[:, kc * 128:(kc + 1) * 128], identity=ident[:])
        return ins
    P.op("pe", tr, reads=[hbk, "ident"], writes=[bankk])
    P.op("dve", lambda: nc.vector.tensor_copy(out=dstT, in_=pv), reads=[bankk], writes=[dstk])


def stream_ag(P, nc, ag, i, keys):
    pairs, cc = ag
    s_ap, d_ap = pairs[i]
    P.op("pool", lambda: nc.gpsimd.collective_compute("AllGather", ALU.bypass, replica_groups=GROUPS, ins=[s_ap], outs=[d_ap]).then_inc(cc, 1),
         reads=keys)


def phase_pre(nc, G, h, hT_loc, ag, ntok=TOK_CORE):
    C = Ctx(nc)
    P = C.P
    P.pool_post = lambda g: g.wait_ge(ag[1], len(ag[0]))
    nt = ntok // 128
    x = G.din("x", [ntok, D])
    g = G.din("g", [1, D])
    b = G.din("b", [1, D])
    idn = G.din("idn", [128, 128], BF16)
    hT = hT_loc.rearrange("c (k p) t -> c p k t", k=8)
    with P.stack:
        C.alloc_banks()
        gt = P.sb([128, D], F32)
        bt = P.sb([128, D], F32)
        ident = P.sb([128, 128], BF16)
        eps = const_col(P, nc, LN_EPS, "eps_ln")
        xt = [P.sb([128, D], F32) for _ in range(2)]
        ht = [P.sb([128, D], F32) for _ in range(2)]
        hb = P.sb([128, D], BF16)
        hTt = [P.sb([128, 8, 128], BF16) for _ in range(2)]
        scr = ln_scratch(P, "ln")
        scr["eps_ln"] = eps
        dma(P, "sp", gt[:], g.partition_broadcast(128), writes=["g"])
        dma(P, "sp", bt[:], b.partition_broadcast(128), writes=["b"])
        dma(P, "sp", ident[:], idn, writes=["ident"])
        outs = []
        for t in range(nt):
            s = t % 2
            dma(P, "sp", xt[s][:], x[t * 128:(t + 1) * 128, :], writes=[("xt", s)])
            layer_norm_tile(P, nc, xt[s], ("xt", s), ht[s], ("ht", s), gt, bt, "g", "b", scr, "ln")
            dma(P, "poolq", h[t * 128:(t + 1) * 128, :], ht[s][:], reads=[("ht", s)], writes=[("h", t)])
            to_featmajor_bf16(P, nc, ht[s], ("ht", s), hb, "hb", C.banks[s], f"b{s}", hTt[s][:], ("hTt", s), ident)
            dma(P, "sp", hT[t // 4][:, :, (t % 4) * 128:(t % 4 + 1) * 128], hTt[s][:],
                reads=[("hTt", s)], writes=[("hT", t)])
            outs += [("h", t), ("hT", t)]
            if t % 4 == 3:
                stream_ag(P, nc, ag, t // 4, [("hT", t_) for t_ in range(t - 3, t + 1)])
        P.emit(final_wait_keys=outs)


BIG = 1.0e4


def phase_post(nc, G, l, h_in, h_out, hT_loc, y_all, ag=None, ntok=TOK_CORE):
    C = Ctx(nc)
    P = C.P
    nsup = ntok // 512
    pf = f"L{l}_"

    def din(name, shape, dt=F32):
        return G.din(pf + name, shape, dt)
    wout = din("wout", [128, 8, D], BF16)
    wglu = din("wglu", [128, 2, 256], BF16)
    ln1g = din("ln1g", [1, D]); ln1b = din("ln1b", [1, D]); ln2g = din("ln2g", [1, D]); ln2b = din("ln2b", [1, D])
    dng = din("dng", [1, 64]); lamv = din("lamv", [1, 130])
    wr = din("wr", [128, 8, 20]); br = din("br", [1, 20])
    w1 = din("w1", [NEXP, 128, 8, DEXP], BF16); w3 = din("w3", [NEXP, 128, 8, DEXP], BF16)
    w2 = din("w2", [NEXP, 128, 4, D], BF16)
    idn = G.din("idn", [128, 128], BF16); idn32 = G.din("idn32", [128, 128])
    rofs = G.din("rofs", [1, 1], I32)
    hT_out = hT_loc.rearrange("c (k p) t -> c p k t", k=8) if hT_loc is not None else None
    if ag is not None:
        P.pool_post = lambda g: g.wait_ge(ag[1], len(ag[0]))
    ya_all, ybc_all = y_all
    nch = ntok // YCH
    yam = G.internal("ya_mine", [nch, 4 * YCH, 192])
    ybm = G.internal("ybc_mine", [nch, 4 * YCH, 192])
    offh = {}

    from contextlib import contextmanager

    @contextmanager
    def sp_wrap(sp):
        with sp.register(f"rofs{l}") as reg:
            sp.reg_load(reg, rofs[0:1, 0:1])
            offh["v"] = sp.snap(reg)
            yield
    P.sp_wrap = sp_wrap
    for nm_, src_, dst_ in (("a", ya_all, yam), ("b", ybc_all, ybm)):
        P.op("sp", lambda src_=src_, dst_=dst_: nc.sync.dma_start(
            out=dst_.rearrange("i (a b) c -> (i a) (b c)", a=64),
            in_=src_[bass.ds(offh["v"], nch)].rearrange("i (a b) c -> (i a) (b c)", a=64)),
            writes=[("ymine", nm_)])
    yas = yam.rearrange("i (j s) c -> i s j c", j=4)
    ybs = ybm.rearrange("i (j s) c -> i s j c", j=4)

    with P.stack:
        C.alloc_banks()
        B = C.banks
        sb = P.sb
        ident = sb([128, 128], BF16); ident32 = sb([128, 128], F32)
        g1 = sb([128, D], F32); b1 = sb([128, D], F32); g2 = sb([128, D], F32); b2 = sb([128, D], F32)
        wout_t = sb([128, 8, D], BF16); wglu_t = sb([128, 2, 256], BF16)
        wr_t = sb([128, 8, 20], F32); br_t = sb([128, 20], F32)
        gA = sb([128, 64], F32); lam_t = sb([128, 130], F32); lprod = sb([128, 2, 32], F32)
        lsum = sb([128, 2], F32); nlam = sb([128, 1], F32)
        eps_ln = const_col(P, nc, LN_EPS, "eps_ln"); eps_rms = const_col(P, nc, RMS_EPS, "eps_rms")
        scr = ln_scratch(P, "ln"); scr["eps_ln"] = eps_ln
        ht = [sb([128, D], F32) for _ in range(2)]
        ya_t = [sb([128, 768], F32) for _ in range(2)]
        yc_t = [sb([128, 256], F32) for _ in range(2)]
        ymix = [sb([128, D], F32) for _ in range(2)]
        dd = sb([128, 6, 64], F32); sq = sb([128, 6, 64], F32); ss = sb([128, 6], F32)
        c2 = sb([128, 256], F32); c3 = sb([128, 256], F32); ygb = sb([128, 256], BF16)
        ygT = sb([128, 2, 128], BF16); sig = sb([128, 256], F32)
        ymb = sb([128, D], BF16); ymT = sb([128, 8, 128], BF16)
        z = sb([128, D], F32)
        h1 = [sb([128, D], F32) for _ in range(4)]
        h1T32 = sb([128, 8, 128], F32)
        h1T = sb([128, 8, 512], BF16)
        lg = sb([128, 20], F32)
        r = {n: sb([128, s], F32) for n, s in dict(gmax=1, goh=4, ngmax=1, gexp=4, gsum=1, gp=1, em=16, pen=4, m1=1, oh1=16,
                                                   em2=16, m2=1, oh2=16, dl=1, ex=1, den=1, w1=1, w2=1).items()}
        comb = sb([128, 4, 16], F32)
        wA = [sb([128, 8, DEXP], BF16) for _ in range(2)]
        wB = [sb([128, 8, DEXP], BF16) for _ in range(2)]
        wC = [sb([128, 4, D], BF16) for _ in range(2)]
        sil = [sb([128, 512], F32) for _ in range(2)]
        hid = [sb([128, 512], BF16) for _ in range(4)]
        acc = [sb([128, D], F32) for _ in range(4)]
        z2 = sb([128, D], F32)
        ho = [sb([128, D], F32) for _ in range(2)]
        hob = sb([128, D], BF16)
        hoT = [sb([128, 8, 128], BF16) for _ in range(2)]

        dma(P, "sp", ident[:], idn, writes=["ident"]); dma(P, "sp", ident32[:], idn32, writes=["ident32"])
        for t_, s_, k_ in ((g1, ln1g, "g1"), (b1, ln1b, "b1"), (g2, ln2g, "g2"), (b2, ln2b, "b2")):
            dma(P, "sp", t_[:], s_.partition_broadcast(128), writes=[k_])
        dma(P, "sp", wout_t[:], wout, writes=["wout"]); dma(P, "sp", wglu_t[:], wglu, writes=["wglu"])
        dma(P, "sp", wr_t[:], wr, writes=["wr"]); dma(P, "sp", br_t[:], br.partition_broadcast(128), writes=["br"])
        dma(P, "sp", gA[:], dng.partition_broadcast(128), writes=["gA"])
        dma(P, "sp", lam_t[:], lamv.partition_broadcast(128), writes=["lamt"])
        P.op("dve", lambda: nc.vector.tensor_scalar(out=gA[:], in0=gA[:], scalar1=lam_t[:, 128:129], scalar2=None, op0=ALU.mult),
             reads=["gA", "lamt"], writes=["gA"])
        lv = lam_t[:, 0:128].rearrange("p (a b c) -> p a b c", a=2, b=2)
        P.op("dve", lambda: nc.vector.tensor_tensor(out=lprod[:], in0=lv[:, :, 0, :], in1=lv[:, :, 1, :], op=ALU.mult),
             reads=["lamt"], writes=["lprod"])
        P.op("dve", lambda: nc.vector.tensor_reduce(out=lsum[:], in_=lprod[:], axis=AX.X, op=ALU.add), reads=["lprod"], writes=["lsum"])
        P.op("act", lambda: nc.scalar.activation(out=lsum[:], in_=lsum[:], func=AF.Exp), reads=["lsum"], writes=["lsum"])
        P.op("dve", lambda: nc.vector.tensor_tensor(out=nlam[:], in0=lsum[:, 1:2], in1=lsum[:, 0:1], op=ALU.subtract),
             reads=["lsum"], writes=["nlam"])
        P.op("dve", lambda: nc.vector.tensor_scalar(out=nlam[:], in0=nlam[:], scalar1=lam_t[:, 129:130], scalar2=None, op0=ALU.add),
             reads=["nlam", "lamt"], writes=["nlam"])

        outs = []
        for su in range(nsup):
            for tt in range(4):
                t = su * 4 + tt
                s = t % 2
                rows = slice(t * 128, (t + 1) * 128)
                dma(P, "sp", ht[s][:], h_in[rows, :], writes=[("ht", s)])
                ci_, r0_ = (t * 128) // YCH, (t * 128) % YCH
                rw_ = slice(r0_, r0_ + 128)
                dma(P, "sp", ya_t[s][:].rearrange("p (j c) -> p j c", j=4), yas[ci_][rw_, :, :], reads=[("ymine", "a")], writes=[("ya", s)])
                dma(P, "sp", ymix[s][:, 384:768].rearrange("p (j c) -> p j c", j=3), ybs[ci_][rw_, 0:3, 0:128], reads=[("ymine", "b")], writes=[("ymix", s, 1)])
                dma(P, "sp", yc_t[s][:].rearrange("p (j c) -> p j c", j=4), ybs[ci_][rw_, :, 128:192], reads=[("ymine", "b")], writes=[("yc", s)])
                yav = ya_t[s][:].rearrange("p (h m d) -> p h m d", h=6, m=2)
                P.op("dve", lambda yav=yav: nc.vector.scalar_tensor_tensor(out=dd[:], in0=yav[:, :, 1, :], scalar=nlam[:, 0:1],
                                                                          in1=yav[:, :, 0, :], op0=ALU.mult, op1=ALU.add),
                     reads=[("ya", s), "nlam"], writes=["dd"])
                P.op("pool", lambda: nc.gpsimd.tensor_tensor(out=sq[:], in0=dd[:], in1=dd[:], op=ALU.mult), reads=["dd"], writes=["sq"])
                P.op("dve", lambda: nc.vector.tensor_reduce(out=ss[:], in_=sq[:], axis=AX.X, op=ALU.add), reads=["sq"], writes=["ss"])
                P.op("act", lambda: nc.scalar.activation(out=ss[:], in_=ss[:], func=AF.Sqrt, bias=eps_rms[:, 0:1], scale=1.0 / 64),
                     reads=["ss", "eps_rms"], writes=["ss"])
                P.op("dve", lambda: nc.vector.reciprocal(out=ss[:], in_=ss[:]), reads=["ss"], writes=["ss"])
                P.op("dve", lambda: nc.vector.tensor_tensor(out=dd[:], in0=dd[:], in1=ss[:].unsqueeze(2).to_broadcast([128, 6, 64]), op=ALU.mult),
                     reads=["dd", "ss"], writes=["dd"])
                ym0 = ymix[s][:, 0:384].rearrange("p (h d) -> p h d", h=6)
                P.op("pool", lambda ym0=ym0: nc.gpsimd.tensor_tensor(out=ym0, in0=dd[:], in1=gA[:].unsqueeze(1).to_broadcast([128, 6, 64]), op=ALU.mult),
                     reads=["dd", "gA"], writes=[("ymix", s, 0)])
                yct = yc_t[s]
                P.op("pool", lambda yct=yct: nc.gpsimd.tensor_tensor(out=c2[:], in0=yct[:], in1=yct[:], op=ALU.mult), reads=[("yc", s)], writes=["c2"])
                P.op("dve", lambda: nc.vector.tensor_scalar(out=c2[:], in0=c2[:], scalar1=0.044715, scalar2=1.0, op0=ALU.mult, op1=ALU.add),
                     reads=["c2"], writes=["c2"])
                P.op("dve", lambda yct=yct: nc.vector.tensor_tensor(out=c2[:], in0=c2[:], in1=yct[:], op=ALU.mult), reads=["c2", ("yc", s)], writes=["c2"])
                P.op("act", lambda: nc.scalar.activation(out=c2[:], in_=c2[:], func=AF.Sigmoid, scale=1.5957691216057308),
                     reads=["c2"], writes=["c2"])
                P.op("dve", lambda yct=yct: nc.vector.tensor_tensor(out=c3[:], in0=c2[:], in1=yct[:], op=ALU.mult), reads=["c2", ("yc", s)], writes=["c3"])
                P.op("act", lambda: nc.scalar.copy(out=ygb[:], in_=c3[:]), reads=["c3"], writes=["ygb"])
                pv0 = B[0][:].bitcast(BF16)

                def trg(pv0=pv0):
                    for k in range(2):
                        ins = nc.tensor.transpose(out=pv0[:, k * 128:(k + 1) * 128], in_=ygb[:, k * 128:(k + 1) * 128], identity=ident[:])
                    return ins
                P.op("pe", trg, reads=["ygb", "ident"], writes=["b0"])
                P.op("dve", lambda pv0=pv0: nc.vector.tensor_copy(out=ygT[:].rearrange("p k t -> p (k t)"), in_=pv0[:, 0:256]), reads=["b0"], writes=["ygT"])

                def mmg():
                    for k in range(2):
                        ins = nc.tensor.matmul(B[1][:, 0:256], lhsT=ygT[:, k, :], rhs=wglu_t[:, k, :], start=(k == 0), stop=(k == 1))
                    return ins
                P.op("pe", mmg, reads=["ygT", "wglu"], writes=["b1"])
                P.op("act", lambda: nc.scalar.activation(out=sig[:], in_=B[1][:, 0:256], func=AF.Sigmoid), reads=["b1"], writes=["sig"])
                P.op("dve", lambda s=s: nc.vector.tensor_tensor(out=ymix[s][:, 768:1024], in0=c3[:], in1=sig[:], op=ALU.mult),
                     reads=["c3", "sig"], writes=[("ymix", s, 2)])
                ymk = [("ymix", s, 0), ("ymix", s, 1), ("ymix", s, 2)]
                P.op("act", lambda s=s: nc.scalar.copy(out=ymb[:], in_=ymix[s][:]), reads=ymk, writes=["ymb"])
                pvb = B[0][:].bitcast(BF16).rearrange("p (k t) -> p k t", k=8)

                def try_(pvb=pvb):
                    for kc in range(8):
                        ins = nc.tensor.transpose(out=pvb[:, kc, :], in_=ymb[:, kc * 128:(kc + 1) * 128], identity=ident[:])
                    return ins
                P.op("pe", try_, reads=["ymb", "ident"], writes=["b0"])
                P.op("dve", lambda pvb=pvb: nc.vector.tensor_copy(out=ymT[:], in_=pvb), reads=["b0"], writes=["ymT"])
                for half in range(2):
                    def mmo(half=half):
                        for kc in range(8):
                            ins = nc.tensor.matmul(B[2 + half][:], lhsT=ymT[:, kc, :], rhs=wout_t[:, kc, half * 512:(half + 1) * 512],
                                                   start=(kc == 0), stop=(kc == 7))
                        return ins
                    P.op("pe", mmo, reads=["ymT", "wout"], writes=[f"b{2 + half}"])
                    P.op("dve", lambda half=half, s=s: nc.vector.scalar_tensor_tensor(
                        out=z[:, half * 512:(half + 1) * 512], in0=ht[s][:, half * 512:(half + 1) * 512], scalar=float(ALPHA),
                        in1=B[2 + half][:], op0=ALU.mult, op1=ALU.add), reads=[f"b{2 + half}", ("ht", s)], writes=[("z", half)])
                P.op("pool", lambda: nc.gpsimd.tensor_copy(out=z[:, 0:1], in_=z[:, 0:1]), reads=[("z", 0), ("z", 1)], writes=["zz"])
                layer_norm_tile(P, nc, z, "zz", h1[tt], ("h1", tt), g1, b1, "g1", "b1", scr, "ln")
                for half in range(2):
                    pv32 = B[2 + half][:].rearrange("p (k t) -> p k t", k=4)

                    def trh(half=half, pv32=pv32, tt=tt):
                        for k in range(4):
                            kc = half * 4 + k
                            ins = nc.tensor.transpose(out=pv32[:, k, :], in_=h1[tt][:, kc * 128:(kc + 1) * 128], identity=ident32[:])
                        return ins
                    P.op("pe", trh, reads=[("h1", tt), "ident32"], writes=[f"b{2 + half}"])
                    P.op("dve", lambda half=half, pv32=pv32: nc.vector.tensor_copy(out=h1T32[:, half * 4:(half + 1) * 4, :], in_=pv32),
                         reads=[f"b{2 + half}"], writes=[("h1T32", half)])
                    P.op("act", lambda half=half, pv32=pv32, tt=tt: nc.scalar.copy(
                        out=h1T[:, half * 4:(half + 1) * 4, tt * 128:(tt + 1) * 128], in_=pv32),
                        reads=[f"b{2 + half}"], writes=[("h1T", tt, half)])

                def mmr():
                    for kc in range(8):
                        ins = nc.tensor.matmul(B[1][:, 0:20], lhsT=h1T32[:, kc, :], rhs=wr_t[:, kc, :], start=(kc == 0), stop=(kc == 7))
                    return ins
                P.op("pe", mmr, reads=[("h1T32", 0), ("h1T32", 1), "wr"], writes=["b1"])
                P.op("dve", lambda: nc.vector.tensor_tensor(out=lg[:], in0=B[1][:, 0:20], in1=br_t[:], op=ALU.add), reads=["b1", "br"], writes=["lg"])
                V = nc.vector
                glog = lg[:, 0:4]
                elog = lg[:, 4:20]
                seq = [
                    lambda: V.tensor_reduce(out=r["gmax"][:], in_=glog, axis=AX.X, op=ALU.max),
                    lambda: V.tensor_scalar(out=r["goh"][:], in0=glog, scalar1=r["gmax"][:, 0:1], scalar2=None, op0=ALU.is_ge),
                    lambda: V.tensor_scalar(out=r["ngmax"][:], in0=r["gmax"][:], scalar1=-1.0, scalar2=None, op0=ALU.mult),
                ]
                for f_ in seq:
                    P.op("dve", f_, reads=["lg", "rt"], writes=["rt"])
                P.op("act", lambda: nc.scalar.activation(out=r["gexp"][:], in_=glog, func=AF.Exp, bias=r["ngmax"][:, 0:1], scale=1.0),
                     reads=["lg", "rt"], writes=["rt2"])
                seq = [
                    lambda: V.tensor_reduce(out=r["gsum"][:], in_=r["gexp"][:], axis=AX.X, op=ALU.add),
                    lambda: V.reciprocal(out=r["gp"][:], in_=r["gsum"][:]),
                    lambda: V.tensor_tensor(out=r["em"][:].rearrange("p (g e) -> p g e", g=4), in0=elog.rearrange("p (g e) -> p g e", g=4),
                                            in1=r["goh"][:].unsqueeze(2).to_broadcast([128, 4, 4]), op=ALU.mult),
                    lambda: V.tensor_scalar(out=r["pen"][:], in0=r["goh"][:], scalar1=-1.0, scalar2=BIG, op0=ALU.add, op1=ALU.mult),
                    lambda: V.tensor_tensor(out=r["em"][:].rearrange("p (g e) -> p g e", g=4), in0=r["em"][:].rearrange("p (g e) -> p g e", g=4),
                                            in1=r["pen"][:].unsqueeze(2).to_broadcast([128, 4, 4]), op=ALU.add),
                    lambda: V.tensor_reduce(out=r["m1"][:], in_=r["em"][:], axis=AX.X, op=ALU.max),
                    lambda: V.tensor_scalar(out=r["oh1"][:], in0=r["em"][:], scalar1=r["m1"][:, 0:1], scalar2=None, op0=ALU.is_ge),
                    lambda: V.scalar_tensor_tensor(out=r["em2"][:], in0=r["oh1"][:], scalar=-BIG, in1=r["em"][:], op0=ALU.mult, op1=ALU.add),
                    lambda: V.tensor_reduce(out=r["m2"][:], in_=r["em2"][:], axis=AX.X, op=ALU.max),
                    lambda: V.tensor_scalar(out=r["oh2"][:], in0=r["em2"][:], scalar1=r["m2"][:, 0:1], scalar2=None, op0=ALU.is_ge),
                    lambda: V.tensor_tensor(out=r["dl"][:], in0=r["m2"][:], in1=r["m1"][:], op=ALU.subtract),
                ]
                for f_ in seq:
                    P.op("dve", f_, reads=["lg", "rt", "rt2"], writes=["rt"])
                P.op("act", lambda: nc.scalar.activation(out=r["ex"][:], in_=r["dl"][:], func=AF.Exp), reads=["rt"], writes=["rt2"])
                seq = [
                    lambda: V.tensor_scalar(out=r["den"][:], in0=r["ex"][:], scalar1=1.0, scalar2=None, op0=ALU.add),
                    lambda: V.reciprocal(out=r["w1"][:], in_=r["den"][:]),
                    lambda: V.tensor_tensor(out=r["w1"][:], in0=r["w1"][:], in1=r["gp"][:], op=ALU.mult),
                    lambda: V.tensor_tensor(out=r["w2"][:], in0=r["w1"][:], in1=r["ex"][:], op=ALU.mult),
                    lambda tt=tt: V.tensor_scalar(out=comb[:, tt, :], in0=r["oh1"][:], scalar1=r["w1"][:, 0:1], scalar2=None, op0=ALU.mult),
                    lambda tt=tt: V.scalar_tensor_tensor(out=comb[:, tt, :], in0=r["oh2"][:], scalar=r["w2"][:, 0:1], in1=comb[:, tt, :],
                                                         op0=ALU.mult, op1=ALU.add),
                ]
                for i_, f_ in enumerate(seq):
                    P.op("dve", f_, reads=["rt", "rt2"] + ([("comb", tt)] if i_ == 5 else []), writes=["rt"] if i_ < 4 else [("comb", tt)])
            h1Tk = [("h1T", tt, half) for tt in range(4) for half in range(2)]
            for e in range(NEXP):
                ws = e % 2
                dma(P, "sp", wA[ws][:], w1[e], writes=[("wA", ws)])
                dma(P, "poolq", wB[ws][:], w3[e], writes=[("wB", ws)])
                dma(P, "sp", wC[ws][:], w2[e], writes=[("wC", ws)])
                for hc in range(4):
                    ba, bb = 4 + hc % 2, 6 + hc % 2

                    def mma(hc=hc, ba=ba, ws=ws):
                        for kc in range(8):
                            ins = nc.tensor.matmul(B[ba][:], lhsT=wA[ws][:, kc, hc * 128:(hc + 1) * 128], rhs=h1T[:, kc, :],
                                                   start=(kc == 0), stop=(kc == 7))
                        return ins

                    def mmb(hc=hc, bb=bb, ws=ws):
                        for kc in range(8):
                            ins = nc.tensor.matmul(B[bb][:], lhsT=wB[ws][:, kc, hc * 128:(hc + 1) * 128], rhs=h1T[:, kc, :],
                                                   start=(kc == 0), stop=(kc == 7))
                        return ins
                    P.op("pe", mma, reads=h1Tk + [("wA", ws)], writes=[f"b{ba}"])
                    P.op("pe", mmb, reads=h1Tk + [("wB", ws)], writes=[f"b{bb}"])
                    P.op("act", lambda hc=hc, ba=ba: nc.scalar.activation(out=sil[hc % 2][:], in_=B[ba][:], func=AF.Silu),
                         reads=[f"b{ba}"], writes=[("sil", hc % 2)])
                    P.op("dve", lambda hc=hc, bb=bb: nc.vector.tensor_tensor(out=hid[hc][:], in0=sil[hc % 2][:], in1=B[bb][:], op=ALU.mult),
                         reads=[f"b{bb}", ("sil", hc % 2)], writes=[("hid", hc)])
                for tt in range(4):
                    for half in range(2):
                        bo = 2 + half

                        def mm2(tt=tt, half=half, bo=bo, ws=ws):
                            for hc in range(4):
                                ins = nc.tensor.matmul(B[bo][:], lhsT=hid[hc][:, tt * 128:(tt + 1) * 128],
                                                       rhs=wC[ws][:, hc, half * 512:(half + 1) * 512], start=(hc == 0), stop=(hc == 3))
                            return ins
                        P.op("pe", mm2, reads=[("hid", hc) for hc in range(4)] + [("wC", ws)], writes=[f"b{bo}"])
                        av = acc[tt][:, half * 512:(half + 1) * 512]
                        if e == 0:
                            P.op("dve", lambda av=av, bo=bo, tt=tt, e=e: nc.vector.tensor_scalar(
                                out=av, in0=B[bo][:], scalar1=comb[:, tt, e:e + 1], scalar2=None, op0=ALU.mult),
                                reads=[f"b{bo}", ("comb", tt)], writes=[("acc", tt, half)])
                        else:
                            P.op("dve", lambda av=av, bo=bo, tt=tt, e=e: nc.vector.scalar_tensor_tensor(
                                out=av, in0=B[bo][:], scalar=comb[:, tt, e:e + 1], in1=av, op0=ALU.mult, op1=ALU.add),
                                reads=[f"b{bo}", ("comb", tt), ("acc", tt, half)], writes=[("acc", tt, half)])
            for tt in range(4):
                t = su * 4 + tt
                s = t % 2
                rows = slice(t * 128, (t + 1) * 128)
                P.op("dve", lambda tt=tt: nc.vector.scalar_tensor_tensor(out=z2[:], in0=h1[tt][:], scalar=float(ALPHA), in1=acc[tt][:],
                                                                          op0=ALU.mult, op1=ALU.add),
                     reads=[("h1", tt), ("acc", tt, 0), ("acc", tt, 1)], writes=["z2"])
                layer_norm_tile(P, nc, z2, "z2", ho[s], ("ho", s), g2, b2, "g2", "b2", scr, "ln")
                dma(P, "poolq", h_out[rows, :], ho[s][:], reads=[("ho", s)], writes=[("h_out", t)])
                outs.append(("h_out", t))
                if hT_out is not None:
                    to_featmajor_bf16(P, nc, ho[s], ("ho", s), hob, "hob", B[0], "b0", hoT[s][:], ("hoT", s), ident)
                    dma(P, "poolq", hT_out[t // 4][:, :, (t % 4) * 128:(t % 4 + 1) * 128], hoT[s][:], reads=[("hoT", s)], writes=[("hT_out", t)])
                    outs.append(("hT_out", t))
                    if tt == 3:
                        stream_ag(P, nc, ag, su, [("hT_out", t_) for t_ in range(t - 3, t + 1)])
        P.emit(final_wait_keys=outs)


def post_inputs(l, p, lam_init):
    d = {}
    d["wout"] = bf(p["w_out"][l].reshape(8, 128, D).transpose(1, 0, 2))
    d["wglu"] = bf(p["s5_w_glu"][l].reshape(2, 128, 256).transpose(1, 0, 2))
    for n in ("ln1_g", "ln1_b", "ln2_g", "ln2_b"):
        d[n.replace("_", "")] = f32c(p[n][l].reshape(1, D))
    d["dng"] = f32c(p["diff_norm_g"][l].reshape(1, 64))
    d["lamv"] = f32c(np.concatenate([p["lam_q1"][l], p["lam_k1"][l], p["lam_q2"][l], p["lam_k2"][l],
                                     np.array([1.0 - lam_init, -lam_init], np.float32)]).reshape(1, 130))
    wr = np.concatenate([p["moe_w_grp"][l], p["moe_w_exp"][l]], axis=1)
    d["wr"] = f32c(wr.reshape(8, 128, 20).transpose(1, 0, 2))
    d["br"] = f32c(np.concatenate([p["moe_b_grp"][l], p["moe_b_exp"][l]]).reshape(1, 20))
    d["w1"] = bf(p["moe_w1"][l].reshape(NEXP, 8, 128, DEXP).transpose(0, 2, 1, 3))
    d["w3"] = bf(p["moe_w3"][l].reshape(NEXP, 8, 128, DEXP).transpose(0, 2, 1, 3))
    d["w2"] = bf(p["moe_w2"][l].reshape(NEXP, 4, 128, D).transpose(0, 2, 1, 3))
    d["idn"] = bf(np.eye(128)); d["idn32"] = f32c(np.eye(128))
    return d


SBANKS = [0, 1, 2, 6, 7]
NPT = 7
ADEPTH = 4
QUARTERS = False
TWO_PI = 2.0 * math.pi
MAGIC = 12582912.0


def phase_mix(nc, G, l, hT_all, ya_d, ybc_d, S=SEQ, do_attn=True, do_s5=True, do_gdn=True, pool_pre=None, pool_post=None, ag_stream=None):
    debug = False
    C = Ctx(nc)
    P = C.P
    nst = S // 512
    nblk = S // 128
    pf = f"L{l}_"

    def din(name, shape, dt=F32):
        return G.din((pf + name) if name not in ("amask", "idn32", "srow", "cTri", "cSL", "cMask2", "cBones") else name, shape, dt)
    hT4 = hT_all.rearrange("c (r k p) t -> c r p k t", r=4, k=8)
    wq = din("wq", [128, 8, 96], BF16); wk = din("wk", [128, 8, 96], BF16); wv = din("wv", [128, 8, 192], BF16)
    amask = din("amask", [128, 4, 512], BF16)
    idn32 = din("idn32", [128, 128])
    wu = din("wu", [128, 8, 64], BF16)
    s5row = din("s5row", [2, 3, 128])
    s5col = din("s5col", [2, 128, 3])
    s5bT = din("s5bT", [2, 2, 2, 16, 64])
    s5cT = din("s5cT", [2, 2, 2, 64, 16])
    s5d = din("s5d", [64, 1])
    srow = din("srow", [1, 512])
    wg = din("wg", [128, 8, 384], BF16); wt = din("wt", [128, 8, 132], BF16)
    cvw = din("cvw", [128, 3, 4])
    galog = din("galog", [1, 2]); gdtb = din("gdtb", [1, 2]); gng = din("gng", [1, 64])
    cTri = din("cTri", [64, 64]); cSL = din("cSL", [64, 64]); cMask2 = din("cMask2", [64, 2, 64]); cBones = din("cBones", [128, 128])
    ya_o = ya_d.rearrange("s (u d) -> s u d", u=3)
    yb_o = ybc_d[:, 0:128].rearrange("s (h d) -> s h d", h=2)
    yc_o = ybc_d[:, 128:192]
    P.pool_pre, P.pool_post = pool_pre, pool_post
    ag_n = [0]

    def try_ag():
        if ag_stream is None:
            return
        pairs, cc = ag_stream
        per = YCH // 512
        while ag_n[0] < len(pairs):
            sts = range(ag_n[0] * per, (ag_n[0] + 1) * per)
            keys = [("yb_o", s_) for s_ in sts] + [("yc_o", s_) for s_ in sts]
            if not all(k_ in P.last_w for k_ in keys):
                break
            s_ap, d_ap = pairs[ag_n[0]]
            P.op("pool", lambda s_ap=s_ap, d_ap=d_ap: nc.gpsimd.collective_compute(
                "AllGather", ALU.bypass, replica_groups=GROUPS, ins=[s_ap], outs=[d_ap]).then_inc(cc, 1), reads=keys)
            ag_n[0] += 1
    outs = []
    dbg = []
    V = nc.vector
    G = nc.gpsimd
    A = nc.scalar
    T = nc.tensor

    with P.stack:
        C.alloc_banks(quarters=do_gdn and QUARTERS)
        B = C.banks
        sb = P.sb
        ident32 = sb([128, 128], F32)
        dma(P, "sp", ident32[:], idn32, writes=["ident32"])
        hTt = [sb([128, 8, 512], BF16) for _ in range(2)]
        eps_rms = const_col(P, nc, RMS_EPS, "eps_rms")
        if do_attn:
            wq_t = sb([128, 8, 96], BF16); wk_t = sb([128, 8, 96], BF16); wv_t = sb([128, 8, 192], BF16)
            QT = sb([96, S], BF16); KT = sb([96, S], BF16)
            Vall = sb([128, nblk, 3, 65], BF16)
            am_t = sb([128, 4, 512], BF16)
            PT = [sb([128, 512], BF16) for _ in range(NPT)]
            osb = sb([65, 512], F32); rec = sb([128, 4], F32)
            oT = [sb([128, 4, 64], F32) for _ in range(2)]
            dma(P, "sp", wq_t[:], wq, writes=["wq"]); dma(P, "sp", wk_t[:], wk, writes=["wk"]); dma(P, "sp", wv_t[:], wv, writes=["wv"])
            dma(P, "sp", am_t[:], amask, writes=["amask"])
            P.op("pool", lambda: G.memset(Vall[:, :, :, 64:65], 1.0), writes=["Vones"])
        if do_s5:
            wu_t = sb([128, 8, 64], BF16)
            dma(P, "sp", wu_t[:], wu, writes=["wu"])
            uT = [sb([32, 512], F32) for _ in range(2)]
            srow_t = sb([128, 512], F32)
            dma(P, "sp", srow_t[:], srow.partition_broadcast(128), writes=["srow"])
            d_col = [sb([32, 1], F32) for _ in range(2)]
            for pr_ in range(2):
                dma(P, "sp", d_col[pr_][:], s5d[pr_ * 32:(pr_ + 1) * 32, :], writes=[("dcol", pr_)])
            s5 = []
            for pr in range(2):
                t = dict(row=sb([32, 3, 128], F32), col=sb([128, 3], F32),
                         BrBD=sb([32, 128], F32), BiBD=sb([32, 128], F32), CrBD=sb([128, 32], F32), CiBD=sb([128, 32], F32),
                         bbr=sb([32, 128], F32), bbi=sb([32, 128], F32),
                         w=[sb([32, 128], F32) for _ in range(8)],
                         cw=[sb([128, 1], F32) for _ in range(8)],
                         RHO=sb([128, 512], F32), CS=sb([128, 512], F32), SN=sb([128, 512], F32),
                         zi=sb([128, 2], F32), zt=sb([128, 2], F32))
                if pr == 0:
                    for nm_ in ("bre", "bim", "t1", "t2", "zre", "zim", "xre", "xim", "ang", "tmp"):
                        t[nm_] = sb([128, 512], F32)
                if pr == 1:
                    for nm_ in ("bre", "bim", "t1", "t2", "zre", "zim", "xre", "xim", "ang", "tmp"):
                        t[nm_] = s5[0][nm_]
                s5.append(t)
            yT = [sb([32, 512], F32) for _ in range(2)]
            yc_tm = [sb([128, 4, 64], F32) for _ in range(2)]

            def range_reduce(eng_name, x, tmp, key_x, key_t, shape_all=True):
                P.op("dve", lambda: V.tensor_scalar(out=tmp, in0=x, scalar1=1.0 / TWO_PI, scalar2=MAGIC, op0=ALU.mult, op1=ALU.add),
                     reads=[key_x], writes=[key_t])
                P.op("dve", lambda: V.tensor_scalar(out=tmp, in0=tmp, scalar1=-MAGIC, scalar2=-TWO_PI, op0=ALU.add, op1=ALU.mult),
                     reads=[key_t], writes=[key_t])
                P.op("dve", lambda: V.tensor_tensor(out=x, in0=x, in1=tmp, op=ALU.add), reads=[key_x, key_t], writes=[key_x])

            def s5_setup(pr):
                t = s5[pr]
                k = lambda n, pr=pr: ("s5", "sh" if n in ("bre", "bim", "t1", "t2", "zre", "zim", "xre", "xim", "tab", "roww_scratch") else pr, n)
                dma(P, "sp", t["row"][:], s5row[pr:pr + 1].partition_broadcast(32), writes=[k("row")])
                dma(P, "sp", t["col"][:], s5col[pr], writes=[k("col")])
                for nm in ("BrBD", "BiBD", "CrBD", "CiBD"):
                    P.op("pool", lambda nm=nm, t=t: G.memset(t[nm][:], 0.0), writes=[k(nm)])
                for g in range(2):
                    dma(P, "sp", t["BrBD"][g * 16:(g + 1) * 16, g * 64:(g + 1) * 64], s5bT[pr, g, 0], reads=[k("BrBD")], writes=[k("BrBD")])
                    dma(P, "sp", t["BiBD"][g * 16:(g + 1) * 16, g * 64:(g + 1) * 64], s5bT[pr, g, 1], reads=[k("BiBD")], writes=[k("BiBD")])
                    dma(P, "sp", t["CrBD"][g * 64:(g + 1) * 64, g * 16:(g + 1) * 16], s5cT[pr, g, 0], reads=[k("CrBD")], writes=[k("CrBD")])
                    dma(P, "sp", t["CiBD"][g * 64:(g + 1) * 64, g * 16:(g + 1) * 16], s5cT[pr, g, 1], reads=[k("CiBD")], writes=[k("CiBD")])
                P.op("dve", lambda t=t: V.tensor_scalar(out=t["CiBD"][:], in0=t["CiBD"][:], scalar1=-1.0, scalar2=None, op0=ALU.mult),
                     reads=[k("CiBD")], writes=[k("CiBD")])
                lre, lim, ldt = t["row"][:, 0, :], t["row"][:, 1, :], t["row"][:, 2, :]
                dt_, lr_, mag, ang, tmp_, sn, cs, den = [t["w"][i][:] for i in range(8)]
                rk = k("roww")
                steps = [
                    ("act", lambda: A.activation(out=dt_, in_=ldt, func=AF.Exp)),
                    ("dve", lambda: V.tensor_tensor(out=lr_, in0=lre, in1=dt_, op=ALU.mult)),
                    ("act", lambda: A.activation(out=mag, in_=lr_, func=AF.Exp)),
                    ("dve", lambda: V.tensor_tensor(out=ang, in0=lim, in1=dt_, op=ALU.mult)),
                ]
                for e_, f_ in steps:
                    P.op(e_, f_, reads=[k("row"), rk], writes=[rk])
                range_reduce("dve", ang, tmp_, rk, rk)
                P.op("act", lambda: A.activation(out=sn, in_=ang, func=AF.Sin), reads=[rk], writes=[rk])
                P.op("dve", lambda: V.tensor_scalar(out=ang, in0=ang, scalar1=math.pi / 2, scalar2=None, op0=ALU.add), reads=[rk], writes=[rk])
                range_reduce("dve", ang, tmp_, rk, rk)
                P.op("act", lambda: A.activation(out=cs, in_=ang, func=AF.Sin), reads=[rk], writes=[rk])
                steps = [
                    lambda: V.tensor_tensor(out=cs, in0=cs, in1=mag, op=ALU.mult),
                    lambda: V.tensor_scalar(out=cs, in0=cs, scalar1=-1.0, scalar2=None, op0=ALU.add),
                    lambda: V.tensor_tensor(out=sn, in0=sn, in1=mag, op=ALU.mult),
                    lambda: V.tensor_tensor(out=den, in0=lre, in1=lre, op=ALU.mult),
                    lambda: V.tensor_tensor(out=tmp_, in0=lim, in1=lim, op=ALU.mult),
                    lambda: V.tensor_tensor(out=den, in0=den, in1=tmp_, op=ALU.add),
                    lambda: V.reciprocal(out=den, in_=den),
                    lambda: V.tensor_tensor(out=dt_, in0=cs, in1=lre, op=ALU.mult),
                    lambda: V.tensor_tensor(out=tmp_, in0=sn, in1=lim, op=ALU.mult),
                    lambda: V.tensor_tensor(out=dt_, in0=dt_, in1=tmp_, op=ALU.add),
                    lambda: V.tensor_tensor(out=dt_, in0=dt_, in1=den, op=ALU.mult),
                    lambda: V.tensor_tensor(out=lr_, in0=sn, in1=lre, op=ALU.mult),
                    lambda: V.tensor_tensor(out=tmp_, in0=cs, in1=lim, op=ALU.mult),
                    lambda: V.tensor_tensor(out=lr_, in0=lr_, in1=tmp_, op=ALU.subtract),
                    lambda: V.tensor_tensor(out=lr_, in0=lr_, in1=den, op=ALU.mult),
                    lambda t=t: V.tensor_tensor(out=t["bbr"][:], in0=dt_, in1=t["BrBD"][:], op=ALU.mult),
                    lambda t=t: V.tensor_tensor(out=tmp_, in0=lr_, in1=t["BiBD"][:], op=ALU.mult),
                    lambda t=t: V.tensor_tensor(out=t["bbr"][:], in0=t["bbr"][:], in1=tmp_, op=ALU.subtract),
                    lambda t=t: V.tensor_tensor(out=t["bbi"][:], in0=dt_, in1=t["BiBD"][:], op=ALU.mult),
                    lambda t=t: V.tensor_tensor(out=tmp_, in0=lr_, in1=t["BrBD"][:], op=ALU.mult),
                    lambda t=t: V.tensor_tensor(out=t["bbi"][:], in0=t["bbi"][:], in1=tmp_, op=ALU.add),
                ]
                for f_ in steps:
                    P.op("dve", f_, reads=[k("row"), rk, k("BrBD"), k("BiBD")], writes=[rk])
                cdt, cth, crho, ca, ctmp, c512s, c512c, cx = [t["cw"][i][:] for i in range(8)]
                ck = k("colw")
                steps = [
                    ("act", lambda t=t: A.activation(out=cdt, in_=t["col"][:, 2:3], func=AF.Exp)),
                    ("dve", lambda t=t: V.tensor_tensor(out=cth, in0=t["col"][:, 1:2], in1=cdt, op=ALU.mult)),
                    ("dve", lambda t=t: V.tensor_tensor(out=crho, in0=t["col"][:, 0:1], in1=cdt, op=ALU.mult)),
                    ("act", lambda: A.activation(out=crho, in_=crho, func=AF.Exp)),
                    ("dve", lambda: V.tensor_scalar(out=ca, in0=cth, scalar1=512.0, scalar2=None, op0=ALU.mult)),
                ]
                for e_, f_ in steps:
                    P.op(e_, f_, reads=[k("col"), ck], writes=[ck])
                range_reduce("dve", ca, ctmp, ck, ck)
                P.op("act", lambda: A.activation(out=c512s, in_=ca, func=AF.Sin), reads=[ck], writes=[ck])
                P.op("dve", lambda: V.tensor_scalar(out=ca, in0=ca, scalar1=math.pi / 2, scalar2=None, op0=ALU.add), reads=[ck], writes=[ck])
                range_reduce("dve", ca, ctmp, ck, ck)
                P.op("act", lambda: A.activation(out=c512c, in_=ca, func=AF.Sin), reads=[ck], writes=[ck])
                tk = k("tab")
                P.op("dve", lambda t=t: V.tensor_scalar(out=t["ang"][:], in0=srow_t[:], scalar1=cth, scalar2=None, op0=ALU.mult),
                     reads=["srow", ck], writes=[tk])
                range_reduce("dve", t["ang"][:], t["tmp"][:], tk, tk)
                P.op("act", lambda t=t: A.activation(out=t["SN"][:], in_=t["ang"][:], func=AF.Sin), reads=[tk], writes=[tk])
                P.op("dve", lambda t=t: V.tensor_scalar(out=t["ang"][:], in0=t["ang"][:], scalar1=math.pi / 2, scalar2=None, op0=ALU.add), reads=[tk], writes=[tk])
                range_reduce("dve", t["ang"][:], t["tmp"][:], tk, tk)
                P.op("act", lambda t=t: A.activation(out=t["CS"][:], in_=t["ang"][:], func=AF.Sin), reads=[tk], writes=[tk])
                P.op("pool", lambda t=t: G.memset(t["RHO"][:], 1.0), writes=[k("rho")])
                P.op("dve", lambda t=t: V.tensor_scalar(out=t["RHO"][:], in0=t["RHO"][:], scalar1=crho, scalar2=None, op0=ALU.mult),
                     reads=[k("rho"), ck], writes=[k("rho")])
                P.op("pool", lambda t=t: G.memset(t["zi"][:], 0.0), writes=[k("zi")])
                if pr == 0:
                    dbg.extend([("CS", t["CS"][:], [128, 512], [k("tab")]), ("SN", t["SN"][:], [128, 512], [k("tab")]),
                            ("RHO", t["RHO"][:], [128, 512], [k("rho")]), ("bbr", t["bbr"][:], [32, 128], [k("roww")]),
                            ("bbi", t["bbi"][:], [32, 128], [k("roww")]), ("cr", t["w"][0][:], [32, 128], [k("roww")]),
                            ("ci", t["w"][1][:], [32, 128], [k("roww")]), ("c512", t["cw"][5][:], [128, 1], [k("colw")]),
                            ("row", t["row"][:], [32, 3, 128], [k("row")]), ("col", t["col"][:], [128, 3], [k("col")])])
            for pr_ in range(2):
                s5_setup(pr_)
        if do_gdn:
            wg_t = sb([128, 8, 384], BF16); wt_t = sb([128, 8, 132], BF16)
            dma(P, "sp", wg_t[:], wg, writes=["wg"]); dma(P, "sp", wt_t[:], wt, writes=["wt"])
            cvw_t = sb([128, 3, 4], F32); dma(P, "sp", cvw_t[:], cvw, writes=["cvw"])
            alog_t = sb([64, 2], F32); dtb_t = sb([64, 2], F32); ng_t = sb([64, 64], F32)
            dma(P, "sp", alog_t[:], galog.partition_broadcast(64), writes=["alog"])
            dma(P, "sp", dtb_t[:], gdtb.partition_broadcast(64), writes=["dtb"])
            dma(P, "sp", ng_t[:], gng.partition_broadcast(64), writes=["ngt"])
            Tri = sb([64, 64], F32); SL = sb([64, 64], F32); Mask2 = sb([64, 2, 64], F32); Bones = sb([128, 128], F32); ones64 = sb([64, 64], F32)
            dma(P, "sp", Tri[:], cTri, writes=["Tri"]); dma(P, "sp", SL[:], cSL, writes=["SL"])
            dma(P, "sp", Mask2[:], cMask2, writes=["Mask2"]); dma(P, "sp", Bones[:], cBones, writes=["Bones"])
            P.op("pool", lambda: G.memset(ones64[:], 1.0), writes=["ones64"])
            P.op("act", lambda: A.activation(out=alog_t[:], in_=alog_t[:], func=AF.Exp), reads=["alog"], writes=["alog"])
            P.op("dve", lambda: V.tensor_scalar(out=alog_t[:], in0=alog_t[:], scalar1=-1.0, scalar2=None, op0=ALU.mult), reads=["alog"], writes=["alog"])
            xraw = [sb([128, 515], F32) for _ in range(3)]
            for c_ in range(3):
                P.op("pool", lambda c_=c_: G.memset(xraw[c_][:], 0.0), writes=[("xraw", c_)])
            cvt = sb([128, 512], F32)
            qkv = [sb([128, 512], F32) for _ in range(3)]
            sqn = sb([128, 512], F32); rn_ = sb([128, 512], F32)
            Sst = [sb([64, 64], F32) for _ in range(2)]
            for h_ in range(2):
                P.op("pool", lambda h_=h_: G.memset(Sst[h_][:], 0.0), writes=[("S", h_)])
            gd = dict(ch=[], hd=[])
            for sl in range(4):
                gd["ch"].append(dict(gs=sb([64, 128], F32), bg=sb([64, 4], F32), nbeta=sb([64, 2], F32)))
            for sl in range(8):
                gd["hd"].append(dict(qkv_tm=sb([64, 3, 64], F32), gcl=sb([64, 2], F32), ex3=sb([64, 3], F32), Gm=sb([64, 64], F32),
                                     EE=sb([64, 2, 64], F32), AT=sb([64, 64], F32),
                                     W=[sb([64, 256], F32) for _ in range(2)], tb=sb([64, 1], F32),
                                     kdec=sb([64, 64], F32), qdec=sb([64, 64], F32), wqT=sb([64, 2, 64], F32), vnew=sb([64, 64], F32),
                                     osb=sb([64, 64], F32), osq=sb([64, 64], F32), oss=sb([64, 1], F32), ngate=sb([64, 64], F32)))
            ybuf = [sb([64, 8, 2, 64], F32) for _ in range(2)]

        for st in range(nst):
            hs = st % 2
            cols = slice(st * 512, (st + 1) * 512)
            dma(P, "sp", hTt[hs][:], hT4[st % (nst // 4)][st // (nst // 4)], writes=[("hTt", hs)])
            hk = ("hTt", hs)
            if do_attn:
                for (w_t, wkey, dst, dk_, bank) in ((wq_t, "wq", QT, "QT", 0), (wk_t, "wk", KT, "KT", 1)):
                    def mmqk(w_t=w_t, bank=bank, hs=hs):
                        for kc in range(8):
                            ins = T.matmul(B[bank][0:96, :], lhsT=w_t[:, kc, :], rhs=hTt[hs][:, kc, :], start=(kc == 0), stop=(kc == 7))
                        return ins
                    P.op("pe", mmqk, reads=[hk, wkey], writes=[f"b{bank}"])
                    P.op("act", lambda dst=dst, bank=bank, cols=cols: A.copy(out=dst[:, cols], in_=B[bank][0:96, :]),
                         reads=[f"b{bank}"], writes=[(dk_, st)])
                for pair in range(2):
                    bank = 2 + pair
                    pv = B[bank][:, 0:384].rearrange("p (j c) -> p j c", j=2)

                    def mmv(pair=pair, pv=pv, hs=hs):
                        for j in range(2):
                            blk = pair * 2 + j
                            for kc in range(8):
                                ins = T.matmul(pv[:, j, :], lhsT=hTt[hs][:, kc, blk * 128:(blk + 1) * 128], rhs=wv_t[:, kc, :],
                                               start=(kc == 0), stop=(kc == 7))
                        return ins
                    P.op("pe", mmv, reads=[hk, "wv"], writes=[f"b{bank}"])
                    b0 = st * 4 + pair * 2
                    P.op("dve", lambda pv=pv, b0=b0: V.tensor_copy(out=Vall[:, b0:b0 + 2, :, 0:64],
                                                                  in_=pv.rearrange("p j (u d) -> p j u d", u=3)),
                         reads=[f"b{bank}"], writes=[("V", st, pair)])
            if do_s5:
                for pr in range(2):
                    def mmu(hs=hs, pr=pr):
                        for kc in range(8):
                            ins = T.matmul(B[4][0:32, :], lhsT=wu_t[:, kc, pr * 32:(pr + 1) * 32], rhs=hTt[hs][:, kc, :], start=(kc == 0), stop=(kc == 7))
                        return ins
                    P.op("pe", mmu, reads=[hk, "wu"], writes=["b4"])
                    P.op("act", lambda pr=pr: A.copy(out=uT[pr][:], in_=B[4][0:32, :]), reads=["b4"], writes=[("uT", pr)])
                def s5_stream(pr):
                    t = s5[pr]
                    k = lambda n, pr=pr: ("s5", "sh" if n in ("bre", "bim", "t1", "t2", "zre", "zim", "xre", "xim", "tab", "roww_scratch") else pr, n)
                    P.op("pe", lambda t=t, pr=pr: T.matmul(B[5][:], lhsT=t["bbr"][:], rhs=uT[pr][:], start=True, stop=True),
                         reads=[("uT", pr), k("roww")], writes=["b5"])
                    P.op("pe", lambda t=t, pr=pr: T.matmul(B[6][:], lhsT=t["bbi"][:], rhs=uT[pr][:], start=True, stop=True),
                         reads=[("uT", pr), k("roww")], writes=["b6"])
                    P.op("act", lambda t=t: A.copy(out=t["bre"][:], in_=B[5][:]), reads=["b5"], writes=[k("bre")])
                    P.op("act", lambda t=t: A.copy(out=t["bim"][:], in_=B[6][:]), reads=["b6"], writes=[k("bim")])
                    P.op("dve", lambda t=t: V.tensor_tensor(out=t["t1"][:], in0=t["bre"][:], in1=t["CS"][:], op=ALU.mult), reads=[k("bre"), k("tab")], writes=[k("t1")])
                    P.op("pool", lambda t=t: G.tensor_tensor(out=t["t2"][:], in0=t["bim"][:], in1=t["SN"][:], op=ALU.mult), reads=[k("bim"), k("tab")], writes=[k("t2")])
                    P.op("dve", lambda t=t: V.tensor_tensor(out=t["t1"][:], in0=t["t1"][:], in1=t["t2"][:], op=ALU.add), reads=[k("t1"), k("t2")], writes=[k("t1")])
                    P.op("pool", lambda t=t: G.tensor_tensor(out=t["t2"][:], in0=t["bim"][:], in1=t["CS"][:], op=ALU.mult), reads=[k("bim"), k("tab"), k("t1")], writes=[k("t2")])
                    P.op("pool", lambda t=t: G.tensor_tensor(out=t["bre"][:], in0=t["bre"][:], in1=t["SN"][:], op=ALU.mult), reads=[k("bre"), k("tab"), k("t1")], writes=[k("bre")])
                    P.op("pool", lambda t=t: G.tensor_tensor(out=t["t2"][:], in0=t["t2"][:], in1=t["bre"][:], op=ALU.subtract), reads=[k("t2"), k("bre")], writes=[k("t2")])
                    P.op("dve", lambda t=t: V.tensor_tensor_scan(out=t["zre"][:], data0=t["RHO"][:], data1=t["t1"][:], initial=t["zi"][:, 0:1],
                                                                  op0=ALU.mult, op1=ALU.add), reads=[k("t1"), k("rho"), k("zi")], writes=[k("zre")])
                    P.op("dve", lambda t=t: V.tensor_tensor_scan(out=t["zim"][:], data0=t["RHO"][:], data1=t["t2"][:], initial=t["zi"][:, 1:2],
                                                                  op0=ALU.mult, op1=ALU.add), reads=[k("t2"), k("rho"), k("zi")], writes=[k("zim")])
                    cdt, cth, crho, ca, ctmp, c512s, c512c, cx = [t["cw"][i][:] for i in range(8)]
                    zl_re, zl_im = t["zre"][:, 511:512], t["zim"][:, 511:512]
                    P.op("dve", lambda t=t, zl_re=zl_re: V.tensor_tensor(out=t["zt"][:, 0:1], in0=zl_re, in1=c512c, op=ALU.mult), reads=[k("zre"), k("colw")], writes=[k("zt")])
                    P.op("dve", lambda t=t, zl_im=zl_im: V.tensor_tensor(out=t["zt"][:, 1:2], in0=zl_im, in1=c512s, op=ALU.mult), reads=[k("zim"), k("colw")], writes=[k("zt")])
                    P.op("dve", lambda t=t: V.tensor_tensor(out=t["zi"][:, 0:1], in0=t["zt"][:, 0:1], in1=t["zt"][:, 1:2], op=ALU.subtract), reads=[k("zt"), k("zi")], writes=[k("zi")])
                    P.op("dve", lambda t=t, zl_re=zl_re: V.tensor_tensor(out=t["zt"][:, 0:1], in0=zl_re, in1=c512s, op=ALU.mult), reads=[k("zre"), k("colw"), k("zi")], writes=[k("zt")])
                    P.op("dve", lambda t=t, zl_im=zl_im: V.tensor_tensor(out=t["zt"][:, 1:2], in0=zl_im, in1=c512c, op=ALU.mult), reads=[k("zim"), k("colw")], writes=[k("zt")])
                    P.op("dve", lambda t=t: V.tensor_tensor(out=t["zi"][:, 1:2], in0=t["zt"][:, 0:1], in1=t["zt"][:, 1:2], op=ALU.add), reads=[k("zt"), k("zi")], writes=[k("zi")])
                    P.op("dve", lambda t=t: V.tensor_tensor(out=t["xre"][:], in0=t["zre"][:], in1=t["CS"][:], op=ALU.mult), reads=[k("zre"), k("tab")], writes=[k("xre")])
                    P.op("pool", lambda t=t: G.tensor_tensor(out=t["t1"][:], in0=t["zim"][:], in1=t["SN"][:], op=ALU.mult), reads=[k("zim"), k("tab"), k("zre")], writes=[k("t1")])
                    P.op("dve", lambda t=t: V.tensor_tensor(out=t["xre"][:], in0=t["xre"][:], in1=t["t1"][:], op=ALU.subtract), reads=[k("xre"), k("t1")], writes=[k("xre")])
                    P.op("pool", lambda t=t: G.tensor_tensor(out=t["xim"][:], in0=t["zre"][:], in1=t["SN"][:], op=ALU.mult), reads=[k("zre"), k("tab")], writes=[k("xim")])
                    P.op("pool", lambda t=t: G.tensor_tensor(out=t["t2"][:], in0=t["zim"][:], in1=t["CS"][:], op=ALU.mult), reads=[k("zim"), k("tab"), k("zim")], writes=[k("t2")])
                    P.op("pool", lambda t=t: G.tensor_tensor(out=t["xim"][:], in0=t["xim"][:], in1=t["t2"][:], op=ALU.add), reads=[k("xim"), k("t2")], writes=[k("xim")])

                    yb_ = 7 if pr == 0 else 3

                    def mmy(t=t, pr=pr, yb_=yb_):
                        T.matmul(B[yb_][0:32, :], lhsT=t["CrBD"][:], rhs=t["xre"][:], start=True, stop=False)
                        return T.matmul(B[yb_][0:32, :], lhsT=t["CiBD"][:], rhs=t["xim"][:], start=False, stop=True)
                    P.op("pe", mmy, reads=[k("xre"), k("xim"), k("CrBD"), k("CiBD")], writes=[f"b{yb_}"])
                    P.op("dve", lambda pr=pr, yb_=yb_: V.scalar_tensor_tensor(out=yT[pr][:], in0=uT[pr][:], scalar=d_col[pr][:, 0:1], in1=B[yb_][0:32, :],
                                                                             op0=ALU.mult, op1=ALU.add),
                         reads=[f"b{yb_}", ("uT", pr), ("dcol", pr)], writes=[("yT", pr)])
                for pr_ in range(2):
                    s5_stream(pr_)
                pvy = B[4][:, 0:256].rearrange("p (j d) -> p j d", j=4)

                def try4(pvy=pvy):
                    for pr in range(2):
                        for j in range(4):
                            ins = T.transpose(out=pvy[:, j, pr * 32:(pr + 1) * 32], in_=yT[pr][:, j * 128:(j + 1) * 128], identity=ident32[0:32, 0:32])
                    return ins
                P.op("pe", try4, reads=[("yT", 0), ("yT", 1), "ident32"], writes=["b4"])
                P.op("act", lambda pvy=pvy, hs=hs: A.copy(out=yc_tm[hs][:], in_=pvy), reads=["b4"], writes=[("yc_tm", hs)])
                dma(P, "poolq", yc_o[cols, :].rearrange("(j p) d -> p j d", p=128), yc_tm[hs][:], reads=[("yc_tm", hs)], writes=[("yc_o", st)])
                outs.append(("yc_o", st))
            if do_gdn:
                gdn_supertile(P, nc, B, st, hs, hk, hTt, wg_t, wt_t, cvw_t, xraw, cvt, qkv, sqn, rn_, Bones, eps_rms, alog_t, dtb_t, ng_t,
                              Tri, SL, Mask2, ones64, ident32, Sst, gd, ybuf, yb_o, outs)
                try_ag()

        if do_gdn:
            gdn_round(P, gd, [], yb_o, outs)
            try_ag()
            assert ag_stream is None or ag_n[0] == len(ag_stream[0])
        if do_attn:
            scale = 32 ** -0.5
            allqk = [("QT", s_) for s_ in range(nst)] + [("KT", s_) for s_ in range(nst)] + [("V", s_, p_) for s_ in range(nst) for p_ in range(2)] + ["Vones"]
            cnt = 0
            for u in range(3):
                for qt in range(nst):
                    nkb = 4 * (qt + 1)
                    bo = 3 + (qt % 2)
                    pend = []

                    def issue_s(kb, u=u, qt=qt):
                        nonlocal cnt
                        slot = SBANKS[cnt % len(SBANKS)]
                        ps_ = cnt % NPT
                        cnt += 1
                        P.op("pe", lambda: T.matmul(B[slot][:], lhsT=KT[32 * u:32 * u + 32, kb * 128:(kb + 1) * 128],
                                                    rhs=QT[32 * u:32 * u + 32, qt * 512:(qt + 1) * 512], start=True, stop=True),
                             reads=allqk, writes=[f"b{slot}"])
                        P.op("act", lambda: A.activation(out=PT[ps_][:], in_=B[slot][:], func=AF.Exp, scale=scale),
                             reads=[f"b{slot}"], writes=[("PT", ps_)])
                        if kb >= 4 * qt:
                            j = kb - 4 * qt
                            P.op("pool", lambda: G.tensor_tensor(out=PT[ps_][:], in0=PT[ps_][:], in1=am_t[:, j, :], op=ALU.mult),
                                 reads=[("PT", ps_), "amask"], writes=[("PT", ps_)])
                        return ps_

                    def issue_av(kb, ps_, u=u, bo=bo, nkb=nkb):
                        P.op("pe", lambda: T.matmul(B[bo][0:65, :], lhsT=Vall[:, kb, u, :], rhs=PT[ps_][:], start=(kb == 0), stop=(kb == nkb - 1)),
                             reads=[("PT", ps_)] + allqk, writes=[f"b{bo}"])
                    for kb in range(nkb):
                        pend.append((kb, issue_s(kb)))
                        if len(pend) > ADEPTH:
                            issue_av(*pend.pop(0))
                    while pend:
                        issue_av(*pend.pop(0))
                    P.op("act", lambda bo=bo: A.copy(out=osb[:], in_=B[bo][0:65, :]), reads=[f"b{bo}"], writes=["osb"])
                    pvo = B[5][:, 0:260].rearrange("p (j d) -> p j d", j=4)

                    def tro(pvo=pvo):
                        for j in range(4):
                            ins = T.transpose(out=pvo[:, j, :], in_=osb[:, j * 128:(j + 1) * 128], identity=ident32[0:65, 0:65])
                        return ins
                    P.op("pe", tro, reads=["osb", "ident32"], writes=["b5"])
                    P.op("dve", lambda pvo=pvo: V.reciprocal(out=rec[:], in_=pvo[:, :, 64]), reads=["b5"], writes=["rec"])
                    os_ = qt % 2
                    P.op("dve", lambda pvo=pvo, os_=os_: V.tensor_tensor(out=oT[os_][:], in0=pvo[:, :, 0:64],
                                                                        in1=rec[:].unsqueeze(2).to_broadcast([128, 4, 64]), op=ALU.mult),
                         reads=["b5", "rec"], writes=[("oT", os_)])
                    dma(P, "sp", ya_o[qt * 512:(qt + 1) * 512, u, :].rearrange("(j p) d -> p j d", p=128), oT[os_][:],
                        reads=[("oT", os_)], writes=[("ya_o", u, qt)])
                    outs.append(("ya_o", u, qt))
        P.emit(final_wait_keys=outs)


def gdn_supertile(P, nc, B, st, hs, hk, hTt, wg_t, wt_t, cvw_t, xraw, cvt, qkv, sqn, rn_, Bones, eps_rms, nA_t, dtb_t, ng_t,
                  Tri, SL, Mask2, ones64, ident32, Sst, gd, ybuf, yb_o, outs):
    V, G, A, T = nc.vector, nc.gpsimd, nc.scalar, nc.tensor
    for c in range(3):
        def mm(c=c):
            for kc in range(8):
                ins = T.matmul(B[c][:], lhsT=wg_t[:, kc, c * 128:(c + 1) * 128], rhs=hTt[hs][:, kc, :], start=(kc == 0), stop=(kc == 7))
            return ins
        P.op("pe", mm, reads=[hk, "wg"], writes=[f"b{c}"])
        P.op("pool", lambda c=c: G.tensor_copy(out=xraw[c][:, 0:3], in_=xraw[c][:, 512:515]), reads=[("xraw", c)], writes=[("xraw", c)])
        P.op("act", lambda c=c: A.copy(out=xraw[c][:, 3:515], in_=B[c][:]), reads=[f"b{c}", ("xraw", c)], writes=[("xraw", c)])
        P.op("dve", lambda c=c: V.tensor_scalar(out=cvt[:], in0=xraw[c][:, 0:512], scalar1=cvw_t[:, c, 0:1], scalar2=None, op0=ALU.mult),
             reads=[("xraw", c), "cvw"], writes=["cvt"])
        for kk in range(1, 4):
            P.op("dve", lambda c=c, kk=kk: V.scalar_tensor_tensor(out=cvt[:], in0=xraw[c][:, kk:kk + 512], scalar=cvw_t[:, c, kk:kk + 1],
                                                                  in1=cvt[:], op0=ALU.mult, op1=ALU.add),
                 reads=[("xraw", c), "cvw", "cvt"], writes=["cvt"])
        P.op("act", lambda c=c: A.activation(out=qkv[c][:], in_=cvt[:], func=AF.Silu), reads=["cvt"], writes=[("qkv", c)])
    for c in range(2):
        P.op("pool", lambda c=c: G.tensor_tensor(out=sqn[:], in0=qkv[c][:], in1=qkv[c][:], op=ALU.mult), reads=[("qkv", c)], writes=["sqn"])
        P.op("pe", lambda: T.matmul(B[3][:], lhsT=Bones[:], rhs=sqn[:], start=True, stop=True), reads=["sqn", "Bones"], writes=["b3"])
        P.op("act", lambda: A.activation(out=rn_[:], in_=B[3][:], func=AF.Sqrt, bias=eps_rms[:, 0:1], scale=1.0), reads=["b3", "eps_rms"], writes=["rn"])
        P.op("dve", lambda: V.reciprocal(out=rn_[:], in_=rn_[:]), reads=["rn"], writes=["rn"])
        if c == 0:
            P.op("dve", lambda: V.scalar_tensor_tensor(out=qkv[0][:], in0=qkv[0][:], scalar=0.125, in1=rn_[:], op0=ALU.mult, op1=ALU.mult),
                 reads=[("qkv", 0), "rn"], writes=[("qkv", 0)])
        else:
            P.op("dve", lambda: V.tensor_tensor(out=qkv[1][:], in0=qkv[1][:], in1=rn_[:], op=ALU.mult), reads=[("qkv", 1), "rn"], writes=[("qkv", 1)])
    qk_all = [("qkv", 0), ("qkv", 1), ("qkv", 2)]
    yb_s = st % 2
    for cp in range(4):
        new = []
        for c in (2 * cp, 2 * cp + 1):
            cg = st * 8 + c
            cs = slice(c * 64, (c + 1) * 64)
            dch = gd["ch"][cg % 4]
            kch = lambda n, cg=cg: ("gch", cg % 4, n)

            def mmt(cs=cs):
                for kc in range(8):
                    ins = T.matmul(B[0][0:64, 0:132], lhsT=hTt[hs][:, kc, cs], rhs=wt_t[:, kc, :], start=(kc == 0), stop=(kc == 7))
                return ins
            mk = ["b0"]
            P.op("pe", mmt, reads=[hk, "wt"], writes=mk)
            P.op("act", lambda dch=dch: A.activation(out=dch["gs"][:], in_=B[0][0:64, 0:128], func=AF.Silu), reads=mk, writes=[kch("gs")])
            P.op("act", lambda dch=dch: A.activation(out=dch["bg"][:, 0:2], in_=B[0][0:64, 128:130], func=AF.Sigmoid), reads=mk, writes=[kch("bg")])
            P.op("dve", lambda dch=dch: V.tensor_tensor(out=dch["bg"][:, 2:4], in0=B[0][0:64, 130:132], in1=dtb_t[:], op=ALU.add),
                 reads=mk + ["dtb", kch("bg")], writes=[kch("bg")])
            P.op("act", lambda dch=dch: A.activation(out=dch["bg"][:, 2:4], in_=dch["bg"][:, 2:4], func=AF.Exp), reads=[kch("bg")], writes=[kch("bg")])
            P.op("act", lambda dch=dch: A.activation(out=dch["bg"][:, 2:4], in_=dch["bg"][:, 2:4], func=AF.Ln, bias=1.0, scale=1.0), reads=[kch("bg")], writes=[kch("bg")])
            P.op("dve", lambda dch=dch: V.tensor_tensor(out=dch["bg"][:, 2:4], in0=dch["bg"][:, 2:4], in1=nA_t[:], op=ALU.mult),
                 reads=[kch("bg"), "alog"], writes=[kch("bg")])
            P.op("dve", lambda dch=dch: V.tensor_scalar(out=dch["nbeta"][:], in0=dch["bg"][:, 0:2], scalar1=-1.0, scalar2=None, op0=ALU.mult),
                 reads=[kch("bg")], writes=[kch("nbeta")])
            sl0 = (cg % 4) * 2
            new.append([gdn_chunk_head(P, nc, B, h, cs, c, dch, kch, gd["hd"][sl0 + h], sl0 + h, 4 + (cg % 2) * 2 + h, qkv, qk_all, ng_t, Tri, SL, Mask2,
                                       ones64, ident32, Sst, eps_rms, ybuf[yb_s], yb_s) for h in range(2)])
        gdn_round(P, gd, new, yb_o, outs)
        if cp == 3:
            gd["pend_dma"] = (st, yb_s, ybuf[yb_s])


def gdn_round(P, gd, new, yb_o, outs):
    oldg = list(gd.get("pendB", []))
    had_old = bool(oldg)
    actA = [g for grp in new for g in grp]
    curB = oldg.pop(0) if oldg else []
    while actA or curB:
        for g in list(actA):
            try:
                r = next(g)
            except StopIteration:
                raise RuntimeError("chain ended inside stage A")
            if r == "END_A":
                actA.remove(g)
        for g in list(curB):
            try:
                next(g)
            except StopIteration:
                curB.remove(g)
        if not curB and oldg:
            curB = oldg.pop(0)
    gd["pendB"] = [list(grp) for grp in new]
    pd = gd.get("pend_dma")
    if pd is not None and had_old:
        st, yb_s, ybt = pd
        cols = slice(st * 512, (st + 1) * 512)
        dma(P, "poolq", yb_o[cols].rearrange("(c p) h d -> p c h d", p=64), ybt[:], reads=[("ybuf", yb_s, c_, h_) for c_ in range(8) for h_ in range(2)],
            writes=[("yb_o", st)])
        outs.append(("yb_o", st))
        gd["pend_dma"] = None


def gdn_chunk_head(P, nc, B, h, cs, c, dch, kch, d, sl, bank, qkv, qk_all, ng_t, Tri, SL, Mask2, ones64, ident32, Sst, eps_rms, ybuf, yb_s):
    V, G, A, T = nc.vector, nc.gpsimd, nc.scalar, nc.tensor
    hp = slice(h * 64, (h + 1) * 64)
    idh = ident32[hp, hp]
    id0 = ident32[0:64, 0:64]
    k = lambda n: ("ghd", sl, n)
    PA, P3 = B[bank], B[3]
    bk, b3 = [f"b{bank}"], ["b3"]
    o3 = 256 * h
    W = d["W"]
    g_col = dch["bg"][:, 2 + h:3 + h]
    beta_col = dch["bg"][:, h:h + 1]
    nbeta_col = dch["nbeta"][:, h:h + 1]

    def tr1():
        for c3 in range(3):
            ins = T.transpose(out=PA[0:64, c3 * 64:(c3 + 1) * 64], in_=qkv[c3][hp, cs], identity=idh)
        return ins
    P.op("pe", tr1, reads=qk_all + ["ident32"], writes=bk); yield
    P.op("act", lambda: A.copy(out=d["qkv_tm"][:].rearrange("p a b -> p (a b)"), in_=PA[0:64, 0:192]), reads=bk, writes=[k("qkv_tm")]); yield

    def mm2():
        T.matmul(PA[0:64, 256:257], lhsT=Tri[:], rhs=g_col, start=True, stop=True)
        return T.matmul(PA[0:64, 257:258], lhsT=ones64[:], rhs=g_col, start=True, stop=True)
    P.op("pe", mm2, reads=[kch("bg"), "Tri", "ones64"], writes=bk); yield
    P.op("dve", lambda: V.tensor_copy(out=d["gcl"][:], in_=PA[0:64, 256:258]), reads=bk, writes=[k("gcl")]); yield
    P.op("act", lambda: A.activation(out=d["ex3"][:, 0:1], in_=d["gcl"][:, 0:1], func=AF.Exp), reads=[k("gcl")], writes=[k("ex3")]); yield
    P.op("act", lambda: A.activation(out=d["ex3"][:, 1:2], in_=d["gcl"][:, 0:1], func=AF.Exp, bias=d["gcl"][:, 1:2], scale=-1.0),
         reads=[k("gcl"), k("ex3")], writes=[k("ex3")]); yield
    P.op("act", lambda: A.activation(out=d["ex3"][:, 2:3], in_=d["gcl"][:, 1:2], func=AF.Exp), reads=[k("gcl"), k("ex3")], writes=[k("ex3")]); yield
    P.op("dve", lambda: V.tensor_scalar(out=d["Gm"][:], in0=Tri[:], scalar1=g_col, scalar2=None, op0=ALU.mult), reads=["Tri", kch("bg")], writes=[k("Gm")]); yield

    def mm3():
        T.matmul(PA[0:64, 384:448], lhsT=d["Gm"][:], rhs=SL[:], start=True, stop=True)
        return T.matmul(PA[0:64, 448:512], lhsT=SL[:], rhs=d["Gm"][:], start=True, stop=True)
    P.op("pe", mm3, reads=[k("Gm"), "SL"], writes=bk); yield
    P.op("act", lambda: A.activation(out=d["EE"][:].rearrange("p a b -> p (a b)"), in_=PA[0:64, 384:512], func=AF.Exp), reads=bk, writes=[k("EE")]); yield
    P.op("pool", lambda: G.tensor_tensor(out=d["EE"][:], in0=d["EE"][:], in1=Mask2[:], op=ALU.mult), reads=[k("EE"), "Mask2"], writes=[k("EE")]); yield

    def mm4():
        T.matmul(PA[0:64, 0:64], lhsT=qkv[1][hp, cs], rhs=qkv[1][hp, cs], start=True, stop=True)
        return T.matmul(PA[0:64, 64:128], lhsT=qkv[1][hp, cs], rhs=qkv[0][hp, cs], start=True, stop=True)
    P.op("pe", mm4, reads=qk_all, writes=bk); yield
    P.op("dve", lambda: V.scalar_tensor_tensor(out=W[0][:, 128:192], in0=PA[0:64, 0:64], scalar=nbeta_col, in1=d["EE"][:, 0, :], op0=ALU.mult, op1=ALU.mult),
         reads=bk + [kch("nbeta"), k("EE")], writes=[k("W0p")]); yield
    P.op("dve", lambda: V.tensor_tensor(out=d["AT"][:], in0=PA[0:64, 64:128], in1=d["EE"][:, 1, :], op=ALU.mult), reads=bk + [k("EE")], writes=[k("AT")]); yield
    P.op("pe", lambda: T.transpose(out=PA[0:64, 192:256], in_=W[0][:, 128:192], identity=id0), reads=[k("W0p"), "ident32"], writes=bk); yield
    P.op("act", lambda: A.copy(out=W[0][:, 192:256], in_=PA[0:64, 192:256]), reads=bk, writes=[k("W0t")]); yield
    P.op("dve", lambda: V.tensor_tensor(out=d["tb"][:], in0=beta_col, in1=d["ex3"][:, 0:1], op=ALU.mult), reads=[kch("bg"), k("ex3")], writes=[k("tb")]); yield
    P.op("dve", lambda: V.tensor_scalar(out=W[0][:, 0:64], in0=d["qkv_tm"][:, 2, :], scalar1=beta_col, scalar2=None, op0=ALU.mult),
         reads=[k("qkv_tm"), kch("bg")], writes=[k("W0x")]); yield
    P.op("dve", lambda: V.tensor_scalar(out=W[0][:, 64:128], in0=d["qkv_tm"][:, 1, :], scalar1=d["tb"][:, 0:1], scalar2=None, op0=ALU.mult),
         reads=[k("qkv_tm"), k("tb"), k("W0x")], writes=[k("W0x")]); yield
    wk = [[k("W0x"), k("W0p"), k("W0t")], [k("W1")]]
    for lvl in range(6):
        s_, d_ = W[lvl % 2], W[(lvl + 1) % 2]
        last = lvl == 5

        def mml(s_=s_, last=last):
            T.matmul(PA[0:64, 256:384], lhsT=s_[:, 192:256], rhs=s_[:, 0:128], start=True, stop=False)
            ins = T.matmul(PA[0:64, 256:384], lhsT=id0, rhs=s_[:, 0:128], start=False, stop=True)
            if not last:
                T.matmul(PA[0:64, 384:448], lhsT=s_[:, 192:256], rhs=s_[:, 128:192], start=True, stop=True)
                ins = T.matmul(PA[0:64, 448:512], lhsT=s_[:, 128:192], rhs=s_[:, 192:256], start=True, stop=True)
            return ins
        P.op("pe", mml, reads=wk[lvl % 2] + ["ident32"], writes=bk); yield
        n_ = 128 if last else 256
        wkeys = [k("W1")] if (lvl + 1) % 2 == 1 else [k("W0x"), k("W0p"), k("W0t")]
        if lvl % 2 == 0:
            P.op("act", lambda d_=d_, n_=n_: A.copy(out=d_[:, 0:n_], in_=PA[0:64, 256:256 + n_]), reads=bk, writes=wkeys); yield
        else:
            P.op("dve", lambda d_=d_, n_=n_: V.tensor_copy(out=d_[:, 0:n_], in_=PA[0:64, 256:256 + n_]), reads=bk, writes=wkeys); yield
    X = W[0]
    xk = [k("W0x"), k("W0p"), k("W0t")]
    P.op("pool", lambda: G.tensor_scalar(out=d["kdec"][:], in0=d["qkv_tm"][:, 1, :], scalar1=d["ex3"][:, 1:2], scalar2=None, op0=ALU.mult),
         reads=[k("qkv_tm"), k("ex3")], writes=[k("kdec")]); yield
    P.op("pool", lambda: G.tensor_scalar(out=d["qdec"][:], in0=d["qkv_tm"][:, 0, :], scalar1=d["ex3"][:, 0:1], scalar2=None, op0=ALU.mult),
         reads=[k("qkv_tm"), k("ex3")], writes=[k("qdec")]); yield

    def tr8():
        T.transpose(out=PA[0:64, 0:64], in_=X[:, 64:128], identity=id0)
        return T.transpose(out=PA[0:64, 64:128], in_=d["qdec"][:], identity=id0)
    P.op("pe", tr8, reads=xk + [k("qdec"), "ident32"], writes=bk); yield
    P.op("act", lambda: A.copy(out=d["wqT"][:].rearrange("p a b -> p (a b)"), in_=PA[0:64, 0:128]), reads=bk, writes=[k("wqT")]); yield
    P.op("pool", lambda: G.tensor_tensor(out=d["ngate"][:], in0=dch["gs"][:, h * 64:(h + 1) * 64], in1=ng_t[:], op=ALU.mult),
         reads=[kch("gs"), "ngt"], writes=[k("ngate")]); yield
    yield "END_A"
    S_ = Sst[h]
    P.op("pe", lambda: T.matmul(P3[0:64, o3:o3 + 64], lhsT=d["wqT"][:, 0, :], rhs=S_[:], start=True, stop=True), reads=[k("wqT"), ("S", h)], writes=b3); yield
    P.op("dve", lambda: V.tensor_tensor(out=d["vnew"][:], in0=X[:, 0:64], in1=P3[0:64, o3:o3 + 64], op=ALU.subtract),
         reads=b3 + xk, writes=[k("vnew")]); yield

    def mmo():
        T.matmul(P3[0:64, o3 + 64:o3 + 128], lhsT=d["wqT"][:, 1, :], rhs=S_[:], start=True, stop=False)
        T.matmul(P3[0:64, o3 + 64:o3 + 128], lhsT=d["AT"][:], rhs=d["vnew"][:], start=False, stop=True)
        return T.matmul(P3[0:64, o3 + 128:o3 + 192], lhsT=d["kdec"][:], rhs=d["vnew"][:], start=True, stop=True)
    P.op("pe", mmo, reads=[k("wqT"), ("S", h), k("AT"), k("vnew"), k("kdec")], writes=b3); yield
    P.op("dve", lambda: V.scalar_tensor_tensor(out=S_[:], in0=S_[:], scalar=d["ex3"][:, 2:3], in1=P3[0:64, o3 + 128:o3 + 192], op0=ALU.mult, op1=ALU.add),
         reads=b3 + [("S", h), k("ex3")], writes=[("S", h)]); yield
    P.op("act", lambda: A.copy(out=d["osb"][:], in_=P3[0:64, o3 + 64:o3 + 128]), reads=b3, writes=[k("osb")]); yield
    P.op("pool", lambda: G.tensor_tensor(out=d["osq"][:], in0=d["osb"][:], in1=d["osb"][:], op=ALU.mult), reads=[k("osb")], writes=[k("osq")]); yield
    P.op("dve", lambda: V.tensor_reduce(out=d["oss"][:], in_=d["osq"][:], axis=AX.X, op=ALU.add), reads=[k("osq")], writes=[k("oss")]); yield
    P.op("act", lambda: A.activation(out=d["oss"][:], in_=d["oss"][:], func=AF.Sqrt, bias=eps_rms[0:64, 0:1], scale=1.0 / 64),
         reads=[k("oss"), "eps_rms"], writes=[k("oss")]); yield
    P.op("dve", lambda: V.reciprocal(out=d["oss"][:], in_=d["oss"][:]), reads=[k("oss")], writes=[k("oss")]); yield
    P.op("dve", lambda: V.scalar_tensor_tensor(out=ybuf[:, c, h, :], in0=d["osb"][:], scalar=d["oss"][:, 0:1], in1=d["ngate"][:], op0=ALU.mult, op1=ALU.mult),
         reads=[k("osb"), k("oss"), k("ngate")], writes=[("ybuf", yb_s, c, h)]); yield


OFF_AQ, OFF_AK, OFF_AV, OFF_BQKV, OFF_BGATE, OFF_BBETA, OFF_BA, OFF_CU = 0, 384, 768, 1152, 2304, 2688, 2694, 2700
GDN_HEADS_OF = [(0, 1), (2, 3), (4, 5), (4, 5)]


def _wl(w, cols):
    return w[:, cols].reshape(8, 128, len(cols)).transpose(1, 0, 2)


def mix_consts():
    d = {}
    k = np.arange(128)[:, None, None]; j = np.arange(4)[None, :, None]; q = np.arange(512)[None, None, :]
    d["amask"] = bf((q // 64 >= (j * 128 + k) // 64).astype(np.float32))
    d["idn32"] = f32c(np.eye(128))
    d["srow"] = f32c(np.arange(512).reshape(1, 512))
    m = np.arange(64)[:, None]; i = np.arange(64)[None, :]
    d["cTri"] = f32c(m <= i)
    d["cSL"] = f32c(m > i)
    d["cMask2"] = f32c(np.stack([(m > i), (m <= i)], axis=1))
    bo = np.zeros((128, 128), np.float32); bo[:64, :64] = 1; bo[64:, 64:] = 1
    d["cBones"] = bo
    return d


def mix_inputs(l, p, j):
    w = p["w_in"][l]
    d = {}
    units = [3 * j + i for i in range(3)]
    qc, kc_, vc = [], [], []
    for u in units:
        head, mp = u // 2, u % 2
        qc += list(range(OFF_AQ + head * 64 + mp * 32, OFF_AQ + head * 64 + mp * 32 + 32))
        kc_ += list(range(OFF_AK + head * 64 + mp * 32, OFF_AK + head * 64 + mp * 32 + 32))
        vc += list(range(OFF_AV + head * 64, OFF_AV + head * 64 + 64))
    d["wq"] = bf(_wl(w, qc)); d["wk"] = bf(_wl(w, kc_)); d["wv"] = bf(_wl(w, vc))
    gs = [4 * j + i for i in range(4)]
    d["wu"] = bf(_wl(w, list(range(OFF_CU + gs[0] * 16, OFF_CU + gs[0] * 16 + 64))))
    lre, lim, ldt = p["s5_lambda_re"][l], p["s5_lambda_im"][l], p["s5_log_dt"][l]
    row = np.zeros((2, 3, 128), np.float32)
    bT = np.zeros((2, 2, 2, 16, 64), np.float32); cT = np.zeros((2, 2, 2, 64, 16), np.float32)
    for pr in range(2):
        for g in range(2):
            G_ = gs[pr * 2 + g]
            row[pr, 0, g * 64:(g + 1) * 64] = lre[G_]; row[pr, 1, g * 64:(g + 1) * 64] = lim[G_]; row[pr, 2, g * 64:(g + 1) * 64] = ldt[G_]
            bT[pr, g, 0] = p["s5_b_re"][l][G_].T; bT[pr, g, 1] = p["s5_b_im"][l][G_].T
            cT[pr, g, 0] = p["s5_c_re"][l][G_].T; cT[pr, g, 1] = p["s5_c_im"][l][G_].T
    d["s5row"] = row; d["s5col"] = f32c(row.transpose(0, 2, 1)); d["s5bT"] = bT; d["s5cT"] = cT
    d["s5d"] = f32c(p["s5_d"][l][gs[0] * 16:gs[0] * 16 + 64].reshape(64, 1))
    hA, hB = GDN_HEADS_OF[j]
    gcols = []
    for part in range(3):
        for h in (hA, hB):
            gcols += list(range(OFF_BQKV + part * 384 + h * 64, OFF_BQKV + part * 384 + h * 64 + 64))
    d["wg"] = bf(_wl(w, gcols))
    tcols = list(range(OFF_BGATE + hA * 64, OFF_BGATE + hA * 64 + 64)) + list(range(OFF_BGATE + hB * 64, OFF_BGATE + hB * 64 + 64)) \
        + [OFF_BBETA + hA, OFF_BBETA + hB, OFF_BA + hA, OFF_BA + hB]
    d["wt"] = bf(_wl(w, tcols))
    cw = p["dn_conv_w"][l]
    cv = np.zeros((128, 3, 4), np.float32)
    for part in range(3):
        for hi, h in enumerate((hA, hB)):
            cv[hi * 64:(hi + 1) * 64, part, :] = cw[:, part * 384 + h * 64: part * 384 + h * 64 + 64].T
    d["cvw"] = cv
    d["galog"] = f32c(p["dn_a_log"][l][[hA, hB]].reshape(1, 2)); d["gdtb"] = f32c(p["dn_dt_bias"][l][[hA, hB]].reshape(1, 2))
    d["gng"] = f32c(p["dn_norm_g"][l].reshape(1, 64))
    return d


YCH = 1024
NYCH = SEQ // YCH


def build_program(stop=None):
    nc = bass.Bass("TRN2", target_bir_lowering=False)
    G = Glob(nc)
    hbuf = [G.internal(f"hbuf{i}", [TOK_CORE, D]) for i in range(2)]
    hT_loc = G.internal("hT_loc", [TOK_CORE // 512, D, 512], BF16)
    hT_all = G.internal("hT_all", [TOK_CORE // 512, 4 * D, 512], BF16)
    ya_o = G.internal("ya_o", [SEQ, 192])
    ybc_o = G.internal("ybc_o", [SEQ, 192])
    ya_all = G.internal("ya_all", [NYCH, 4 * YCH, 192])
    ybc_all = G.internal("ybc_all", [NYCH, 4 * YCH, 192])
    ag_h = [(hT_loc[c_], hT_all[c_]) for c_ in range(TOK_CORE // 512)]
    ag_ya = [(ya_o[i * YCH:(i + 1) * YCH, :], ya_all[i]) for i in range(NYCH)]
    ag_yb = [(ybc_o[i * YCH:(i + 1) * YCH, :], ybc_all[i]) for i in range(NYCH)]
    out = nc.dram_tensor("out", [TOK_CORE, D], F32, kind="ExternalOutput").ap()
    def with_cc(fn):
        Prog._uid += 1
        with nc.semaphore(f"cch_{Prog._uid}") as cc:
            with nc.Block() as block:
                @block.gpsimd
                def _(g):
                    g.sem_clear(cc)
            fn(cc)
    with_cc(lambda cc: phase_pre(nc, G, hbuf[0], hT_loc, (ag_h, cc)))
    for l in range(DEPTH):
        last = l == DEPTH - 1
        phase_mix(nc, G, l, hT_all, ya_o, ybc_o, do_attn=True, do_s5=False, do_gdn=False)
        Prog._uid += 1
        with nc.semaphore(f"ccy_{Prog._uid}") as cc:
            with nc.Block() as block:
                @block.gpsimd
                def _(g):
                    g.sem_clear(cc)

            def pre(g, cc=cc):
                for s_, d_ in ag_ya:
                    g.collective_compute("AllGather", ALU.bypass, replica_groups=GROUPS, ins=[s_], outs=[d_]).then_inc(cc, 1)

            def post(g, cc=cc):
                g.wait_ge(cc, len(ag_ya) + len(ag_yb))
            phase_mix(nc, G, l, hT_all, ya_o, ybc_o, do_attn=False, do_s5=True, do_gdn=True, pool_pre=pre, pool_post=post, ag_stream=(ag_yb, cc))
        if last:
            phase_post(nc, G, l, hbuf[l % 2], out, None, (ya_all, ybc_all))
        else:
            with_cc(lambda cc: phase_post(nc, G, l, hbuf[l % 2], hbuf[(l + 1) % 2], hT_loc, (ya_all, ybc_all), ag=(ag_h, cc)))
    return nc, G


def kernel(_stop=None, **inputs):
    p = {k: np.asarray(v) for k, v in inputs.items()}
    x = f32c(p["x"]).reshape(BATCH * SEQ, D)
    cores = list(range(NCORES))
    nc, G = build_program()
    shared = dict(mix_consts())
    shared["idn"] = bf(np.eye(128))
    shared["g"] = f32c(p["ln_in_g"].reshape(1, D)); shared["b"] = f32c(p["ln_in_b"].reshape(1, D))
    percore = [dict() for _ in range(4)]
    for l in range(DEPTH):
        lam_init = 0.8 - 0.6 * math.exp(-0.3 * l)
        for k, v in post_inputs(l, p, lam_init).items():
            if k not in ("idn", "idn32"):
                shared[f"L{l}_{k}"] = v
        for j in range(4):
            for k, v in mix_inputs(l, p, j).items():
                percore[j][f"L{l}_{k}"] = v
    ins = []
    for c in cores:
        d = dict(shared); d.update(percore[c % 4])
        d["x"] = x[c * TOK_CORE:(c + 1) * TOK_CORE]
        d["rofs"] = np.array([[(c % 4) * (TOK_CORE // YCH)]], np.int32)
        ins.append({k: v for k, v in d.items() if k in G.t})
    res = run_bass_kernel_spmd(nc, ins, core_ids=cores)
    h = [np.asarray(r["out"]) for r in res.results]
    return np.concatenate(h, axis=0).reshape(BATCH, SEQ, D).astype(np.float32)
```

```python
import math
from contextlib import ExitStack

import numpy as np
import ml_dtypes
import concourse.bass as bass
import concourse.mybir as mybir
from concourse.bass_utils import run_bass_kernel_spmd

F32 = mybir.dt.float32
BF16 = mybir.dt.bfloat16
I32 = mybir.dt.int32
ALU = mybir.AluOpType
AF = mybir.ActivationFunctionType
AX = mybir.AxisListType

NCORES = 8


class Prog:
    COMPUTE = ("pe", "act", "dve", "pool")
    NDMASEM = 6

    _uid = 0
    _phase = 0

    def __init__(self, nc):
        Prog._phase += 1
        self.ph = Prog._phase
        self.nc = nc
        self.ops = []
        self.last_w = {}
        self.readers = {}
        self.dma_count = {"sp": 0, "actq": 0, "poolq": 0}
        self.stack = ExitStack()
        self.nt = 0
        self.excl = set()
        self.quarters = False
        self.bankkeys = {f"b{i}" for i in range(8)}
        self.pool_pre = None
        self.pool_post = None
        self.sp_wrap = None

    def sb(self, shape, dtype, name=None):
        Prog._uid += 1
        return self.stack.enter_context(self.nc.sbuf_tensor(f"{name or 't'}_{Prog._uid}", list(shape), dtype))

    def ps(self, shape, dtype=F32, name=None):
        Prog._uid += 1
        return self.stack.enter_context(self.nc.psum_tensor(f"{name or 'p'}_{Prog._uid}", list(shape), dtype))

    def op(self, eng, fn, reads=(), writes=()):
        idx = len(self.ops)
        isdma = eng in self.dma_count
        issue = {"sp": "sp", "actq": "act", "poolq": "pool"}.get(eng, eng)
        if self.quarters:
            ex = lambda ks: [q for k in ks for q in ([f"{k}q{i}" for i in range(4)] if k in self.bankkeys else [k])]
            reads, writes = ex(reads), ex(writes)
        if self.excl:
            writes = list(writes) + [k for k in reads if k in self.excl]
            reads = [k for k in reads if k not in self.excl]
        deps = set()
        for k in reads:
            w = self.last_w.get(k)
            if w is not None:
                deps.add(w)
        for k in writes:
            w = self.last_w.get(k)
            if w is not None:
                deps.add(w)
            for r in self.readers.get(k, ()):
                deps.add(r)
        o = dict(idx=idx, eng=eng, issue=issue, fn=fn, deps=deps, isdma=isdma, needed=False)
        if isdma:
            n = self.dma_count[eng]
            self.dma_count[eng] = n + 1
            o["dsem"] = n % self.NDMASEM
            o["dtarget"] = 16 * (n // self.NDMASEM + 1)
            o["dprev"] = 16 * (n // self.NDMASEM)
        self.ops.append(o)
        for k in writes:
            self.last_w[k] = idx
            self.readers[k] = []
        for k in reads:
            lst = self.readers.setdefault(k, [])
            if not isdma:
                lst[:] = [r for r in lst if self.ops[r]["isdma"] or self.ops[r]["eng"] != eng]
            lst.append(idx)
        return idx

    def emit(self, final_wait_keys=()):
        nc = self.nc
        ops = self.ops
        for o in ops:
            nd = set()
            for d in o["deps"]:
                p = ops[d]
                if (not p["isdma"]) and (not o["isdma"]) and p["eng"] == o["eng"]:
                    if o["eng"] == "pe":
                        continue
                nd.add(d)
            o["deps"] = nd
            for d in nd:
                ops[d]["needed"] = True
        final = [self.last_w[k] for k in final_wait_keys if k in self.last_w]
        for d in final:
            ops[d]["needed"] = True
        tick = {e: 0 for e in self.COMPUTE}
        for o in ops:
            if not o["isdma"] and o["needed"]:
                tick[o["eng"]] += 1
                o["tick"] = tick[o["eng"]]
        sems = {e: self.stack.enter_context(nc.semaphore(f"s_{e}_{self.ph}")) for e in self.COMPUTE}
        dsems = {q: [self.stack.enter_context(nc.semaphore(f"d_{q}{i}_{self.ph}")) for i in range(self.NDMASEM)]
                 for q in self.dma_count}
        per = {e: [] for e in ("pe", "act", "dve", "pool", "sp")}
        for o in ops:
            per[o["issue"]].append(o)
        engobj = {"pe": nc.tensor, "act": nc.scalar, "dve": nc.vector, "pool": nc.gpsimd, "sp": nc.sync}

        def run(ename, extra_final=False):
            eng = engobj[ename]
            waited = {}

            def wait_for(p):
                if p["isdma"]:
                    key = (p["eng"], p["dsem"])
                    val = p["dtarget"]
                    s = dsems[p["eng"]][p["dsem"]]
                else:
                    key = p["eng"]
                    val = p["tick"]
                    s = sems[p["eng"]]
                if waited.get(key, 0) >= val:
                    return
                waited[key] = val
                eng.wait_ge(s, val)

            for o in per[ename]:
                for d in sorted(o["deps"]):
                    wait_for(ops[d])
                if o["isdma"] and o["dprev"] > 0:
                    key = (o["eng"], o["dsem"])
                    if waited.get(key, 0) < o["dprev"]:
                        waited[key] = o["dprev"]
                        eng.wait_ge(dsems[o["eng"]][o["dsem"]], o["dprev"])
                ins = o["fn"]()
                if o["isdma"]:
                    ins.then_inc(dsems[o["eng"]][o["dsem"]], 16)
                elif o["needed"]:
                    ins.then_inc(sems[o["eng"]], 1)
            if extra_final:
                for d in final:
                    wait_for(ops[d])

        allsems = list(sems.values()) + [s for q in dsems.values() for s in q]
        with nc.Block() as block:
            @block.gpsimd
            def _(e):
                for s in allsems:
                    e.sem_clear(s)

        with nc.Block() as block:
            @block.sync
            def _(e):
                if self.sp_wrap is not None:
                    with self.sp_wrap(e):
                        run("sp", extra_final=True)
                else:
                    run("sp", extra_final=True)

            @block.tensor
            def _(e):
                run("pe")

            @block.scalar
            def _(e):
                run("act")

            @block.vector
            def _(e):
                run("dve")

            @block.gpsimd
            def _(e):
                if self.pool_pre is not None:
                    self.pool_pre(e)
                run("pool")
                if self.pool_post is not None:
                    self.pool_post(e)


D = 1024
SEQ = 16384
BATCH = 2
DEPTH = 2
TOK_CORE = 4096
ALPHA = (2 * DEPTH) ** 0.25
LN_EPS = 1e-5
RMS_EPS = 1e-6
NEXP = 16
DEXP = 512


def bf(a):
    return np.ascontiguousarray(np.asarray(a, np.float32).astype(ml_dtypes.bfloat16))


def f32c(a):
    return np.ascontiguousarray(np.asarray(a, np.float32))


class Ctx:
    def __init__(self, nc):
        self.nc = nc
        self.P = Prog(nc)
        self.banks = None

    def alloc_banks(self, quarters=False):
        self.banks = [self.P.ps([128, 512], F32, name=f"bank{i}") for i in range(8)]
        self.P.excl |= {f"b{i}" for i in range(8)}
        if quarters:
            self.P.quarters = True
            self.P.excl |= {f"b{i}q{q}" for i in range(8) for q in range(4)}


class Glob:
    def __init__(self, nc):
        self.nc = nc
        self.t = {}

    def din(self, name, shape, dt=F32):
        if name not in self.t:
            self.t[name] = self.nc.dram_tensor(name, list(shape), dt, kind="ExternalInput").ap()
        return self.t[name]

    def internal(self, name, shape, dt=F32):
        if name not in self.t:
            self.t[name] = self.nc.dram_tensor(name, list(shape), dt).ap()
        return self.t[name]


GROUPS = [[0, 1, 2, 3], [4, 5, 6, 7]]


def allgather(nc, pairs):
    Prog._uid += 1
    with nc.semaphore(f"cc_{Prog._uid}") as cc:
        with nc.Block() as block:
            @block.gpsimd
            def _(g):
                g.sem_clear(cc)
        with nc.Block() as block:
            @block.gpsimd
            def _(g):
                for src, dst in pairs:
                    g.collective_compute("AllGather", ALU.bypass, replica_groups=GROUPS, ins=[src], outs=[dst]).then_inc(cc, 1)
                g.wait_ge(cc, len(pairs))


def dma(P, q, out, in_, reads=(), writes=()):
    eng = {"sp": P.nc.sync, "actq": P.nc.scalar, "poolq": P.nc.gpsimd}[q]
    return P.op(q, lambda: eng.dma_start(out=out, in_=in_), reads=reads, writes=writes)


def layer_norm_tile(P, nc, src, srck, dst, dstk, g_t, b_t, gk, bk, scr, tag):
    st, mv, rstd, xn = scr["st"], scr["mv"], scr["rstd"], scr["xn"]

    def bs():
        nc.vector.bn_stats(out=st[:, 0, :], in_=src[:, 0:512])
        return nc.vector.bn_stats(out=st[:, 1, :], in_=src[:, 512:1024])
    P.op("dve", bs, reads=[srck], writes=[tag + "st"])
    P.op("dve", lambda: nc.vector.bn_aggr(out=mv[:], in_=st[:].rearrange("p a s -> p (a s)")),
         reads=[tag + "st"], writes=[tag + "mv"])
    P.op("act", lambda: nc.scalar.activation(out=rstd[:], in_=mv[:, 1:2], func=AF.Sqrt, bias=scr["eps_ln"][:, 0:1], scale=1.0),
         reads=[tag + "mv", "eps_ln"], writes=[tag + "rstd"])
    P.op("dve", lambda: nc.vector.reciprocal(out=rstd[:], in_=rstd[:]), reads=[tag + "rstd"], writes=[tag + "rstd"])
    P.op("dve", lambda: nc.vector.tensor_scalar(out=xn[:], in0=src[:], scalar1=mv[:, 0:1], scalar2=rstd[:, 0:1],
                                                op0=ALU.subtract, op1=ALU.mult),
         reads=[srck, tag + "mv", tag + "rstd"], writes=[tag + "xn"])
    P.op("pool", lambda: nc.gpsimd.tensor_tensor(out=xn[:], in0=xn[:], in1=g_t[:], op=ALU.mult),
         reads=[tag + "xn", gk], writes=[tag + "xn"])
    P.op("pool", lambda: nc.gpsimd.tensor_tensor(out=dst[:], in0=xn[:], in1=b_t[:], op=ALU.add),
         reads=[tag + "xn", bk], writes=[dstk])


def ln_scratch(P, tag):
    return dict(st=P.sb([128, 2, 6], F32), mv=P.sb([128, 2], F32), rstd=P.sb([128, 1], F32),
                xn=P.sb([128, 1024], F32))


def const_col(P, nc, val, key):
    t = P.sb([128, 1], F32)
    P.op("pool", lambda: nc.gpsimd.memset(t[:], val), writes=[key])
    return t


def to_featmajor_bf16(P, nc, src, srck, hb, hbk, bank, bankk, dstT, dstk, ident, cast_eng="act"):
    if cast_eng == "act":
        P.op("act", lambda: nc.scalar.copy(out=hb[:], in_=src[:]), reads=[srck], writes=[hbk])
    else:
        P.op("pool", lambda: nc.gpsimd.tensor_copy(out=hb[:], in_=src[:]), reads=[srck], writes=[hbk])
    pv = bank[:].bitcast(BF16).rearrange("p (k t) -> p k t", k=8)

    def tr():
        for kc in range(8):
            ins = nc.tensor.transpose(out=pv[:, kc, :], in_=hb[:, kc * 128:(kc + 1) * 128], identity=ident[:])
        return ins
    P.op("pe", tr, reads=[hbk, "ident"], writes=[bankk])
    P.op("dve", lambda: nc.vector.tensor_copy(out=dstT, in_=pv), reads=[bankk], writes=[dstk])


def stream_ag(P, nc, ag, i, keys):
    pairs, cc = ag
    s_ap, d_ap = pairs[i]
    P.op("pool", lambda: nc.gpsimd.collective_compute("AllGather", ALU.bypass, replica_groups=GROUPS, ins=[s_ap], outs=[d_ap]).then_inc(cc, 1),
         reads=keys)


def phase_pre(nc, G, h, hT_loc, ag, ntok=TOK_CORE):
    C = Ctx(nc)
    P = C.P
    P.pool_post = lambda g: g.wait_ge(ag[1], len(ag[0]))
    nt = ntok // 128
    x = G.din("x", [ntok, D])
    g = G.din("g", [1, D])
    b = G.din("b", [1, D])
    idn = G.din("idn", [128, 128], BF16)
    hT = hT_loc.rearrange("c (k p) t -> c p k t", k=8)
    with P.stack:
        C.alloc_banks()
        gt = P.sb([128, D], F32)
        bt = P.sb([128, D], F32)
        ident = P.sb([128, 128], BF16)
        eps = const_col(P, nc, LN_EPS, "eps_ln")
        xt = [P.sb([128, D], F32) for _ in range(2)]
        ht = [P.sb([128, D], F32) for _ in range(2)]
        hb = P.sb([128, D], BF16)
        hTt = [P.sb([128, 8, 128], BF16) for _ in range(2)]
        scr = ln_scratch(P, "ln")
        scr["eps_ln"] = eps
        dma(P, "sp", gt[:], g.partition_broadcast(128), writes=["g"])
        dma(P, "sp", bt[:], b.partition_broadcast(128), writes=["b"])
        dma(P, "sp", ident[:], idn, writes=["ident"])
        outs = []
        for t in range(nt):
            s = t % 2
            dma(P, "sp", xt[s][:], x[t * 128:(t + 1) * 128, :], writes=[("xt", s)])
            layer_norm_tile(P, nc, xt[s], ("xt", s), ht[s], ("ht", s), gt, bt, "g", "b", scr, "ln")
            dma(P, "poolq", h[t * 128:(t + 1) * 128, :], ht[s][:], reads=[("ht", s)], writes=[("h", t)])
            to_featmajor_bf16(P, nc, ht[s], ("ht", s), hb, "hb", C.banks[s], f"b{s}", hTt[s][:], ("hTt", s), ident)
            dma(P, "sp", hT[t // 4][:, :, (t % 4) * 128:(t % 4 + 1) * 128], hTt[s][:],
                reads=[("hTt", s)], writes=[("hT", t)])
            outs += [("h", t), ("hT", t)]
            if t % 4 == 3:
                stream_ag(P, nc, ag, t // 4, [("hT", t_) for t_ in range(t - 3, t + 1)])
        P.emit(final_wait_keys=outs)


BIG = 1.0e4


def phase_post(nc, G, l, h_in, h_out, hT_loc, y_all, ag=None, ntok=TOK_CORE):
    C = Ctx(nc)
    P = C.P
    nsup = ntok // 512
    pf = f"L{l}_"

    def din(name, shape, dt=F32):
        return G.din(pf + name, shape, dt)
    wout = din("wout", [128, 8, D], BF16)
    wglu = din("wglu", [128, 2, 256], BF16)
    ln1g = din("ln1g", [1, D]); ln1b = din("ln1b", [1, D]); ln2g = din("ln2g", [1, D]); ln2b = din("ln2b", [1, D])
    dng = din("dng", [1, 64]); lamv = din("lamv", [1, 130])
    wr = din("wr", [128, 8, 20]); br = din("br", [1, 20])
    w1 = din("w1", [NEXP, 128, 8, DEXP], BF16); w3 = din("w3", [NEXP, 128, 8, DEXP], BF16)
    w2 = din("w2", [NEXP, 128, 4, D], BF16)
    idn = G.din("idn", [128, 128], BF16); idn32 = G.din("idn32", [128, 128])
    rofs = G.din("rofs", [1, 1], I32)
    hT_out = hT_loc.rearrange("c (k p) t -> c p k t", k=8) if hT_loc is not None else None
    if ag is not None:
        P.pool_post = lambda g: g.wait_ge(ag[1], len(ag[0]))
    ya_all, ybc_all = y_all
    nch = ntok // YCH
    yam = G.internal("ya_mine", [nch, 4 * YCH, 192])
    ybm = G.internal("ybc_mine", [nch, 4 * YCH, 192])
    offh = {}

    from contextlib import contextmanager

    @contextmanager
    def sp_wrap(sp):
        with sp.register(f"rofs{l}") as reg:
            sp.reg_load(reg, rofs[0:1, 0:1])
            offh["v"] = sp.snap(reg)
            yield
    P.sp_wrap = sp_wrap
    for nm_, src_, dst_ in (("a", ya_all, yam), ("b", ybc_all, ybm)):
        P.op("sp", lambda src_=src_, dst_=dst_: nc.sync.dma_start(
            out=dst_.rearrange("i (a b) c -> (i a) (b c)", a=64),
            in_=src_[bass.ds(offh["v"], nch)].rearrange("i (a b) c -> (i a) (b c)", a=64)),
            writes=[("ymine", nm_)])
    yas = yam.rearrange("i (j s) c -> i s j c", j=4)
    ybs = ybm.rearrange("i (j s) c -> i s j c", j=4)

    with P.stack:
        C.alloc_banks()
        B = C.banks
        sb = P.sb
        ident = sb([128, 128], BF16); ident32 = sb([128, 128], F32)
        g1 = sb([128, D], F32); b1 = sb([128, D], F32); g2 = sb([128, D], F32); b2 = sb([128, D], F32)
        wout_t = sb([128, 8, D], BF16); wglu_t = sb([128, 2, 256], BF16)
        wr_t = sb([128, 8, 20], F32); br_t = sb([128, 20], F32)
        gA = sb([128, 64], F32); lam_t = sb([128, 130], F32); lprod = sb([128, 2, 32], F32)
        lsum = sb([128, 2], F32); nlam = sb([128, 1], F32)
        eps_ln = const_col(P, nc, LN_EPS, "eps_ln"); eps_rms = const_col(P, nc, RMS_EPS, "eps_rms")
        scr = ln_scratch(P, "ln"); scr["eps_ln"] = eps_ln
        ht = [sb([128, D], F32) for _ in range(2)]
        ya_t = [sb([128, 768], F32) for _ in range(2)]
        yc_t = [sb([128, 256], F32) for _ in range(2)]
        ymix = [sb([128, D], F32) for _ in range(2)]
        dd = sb([128, 6, 64], F32); sq = sb([128, 6, 64], F32); ss = sb([128, 6], F32)
        c2 = sb([128, 256], F32); c3 = sb([128, 256], F32); ygb = sb([128, 256], BF16)
        ygT = sb([128, 2, 128], BF16); sig = sb([128, 256], F32)
        ymb = sb([128, D], BF16); ymT = sb([128, 8, 128], BF16)
        z = sb([128, D], F32)
        h1 = [sb([128, D], F32) for _ in range(4)]
        h1T32 = sb([128, 8, 128], F32)
        h1T = sb([128, 8, 512], BF16)
        lg = sb([128, 20], F32)
        r = {n: sb([128, s], F32) for n, s in dict(gmax=1, goh=4, ngmax=1, gexp=4, gsum=1, gp=1, em=16, pen=4, m1=1, oh1=16,
                                                   em2=16, m2=1, oh2=16, dl=1, ex=1, den=1, w1=1, w2=1).items()}
        comb = sb([128, 4, 16], F32)
        wA = [sb([128, 8, DEXP], BF16) for _ in range(2)]
        wB = [sb([128, 8, DEXP], BF16) for _ in range(2)]
        wC = [sb([128, 4, D], BF16) for _ in range(2)]
        sil = [sb([128, 512], F32) for _ in range(2)]
        hid = [sb([128, 512], BF16) for _ in range(4)]
        acc = [sb([128, D], F32) for _ in range(4)]
        z2 = sb([128, D], F32)
        ho = [sb([128, D], F32) for _ in range(2)]
        hob = sb([128, D], BF16)
        hoT = [sb([128, 8, 128], BF16) for _ in range(2)]

        dma(P, "sp", ident[:], idn, writes=["ident"]); dma(P, "sp", ident32[:], idn32, writes=["ident32"])
        for t_, s_, k_ in ((g1, ln1g, "g1"), (b1, ln1b, "b1"), (g2, ln2g, "g2"), (b2, ln2b, "b2")):
            dma(P, "sp", t_[:], s_.partition_broadcast(128), writes=[k_])
        dma(P, "sp", wout_t[:], wout, writes=["wout"]); dma(P, "sp", wglu_t[:], wglu, writes=["wglu"])
        dma(P, "sp", wr_t[:], wr, writes=["wr"]); dma(P, "sp", br_t[:], br.partition_broadcast(128), writes=["br"])
        dma(P, "sp", gA[:], dng.partition_broadcast(128), writes=["gA"])
        dma(P, "sp", lam_t[:], lamv.partition_broadcast(128), writes=["lamt"])
        P.op("dve", lambda: nc.vector.tensor_scalar(out=gA[:], in0=gA[:], scalar1=lam_t[:, 128:129], scalar2=None, op0=ALU.mult),
             reads=["gA", "lamt"], writes=["gA"])
        lv = lam_t[:, 0:128].rearrange("p (a b c) -> p a b c", a=2, b=2)
        P.op("dve", lambda: nc.vector.tensor_tensor(out=lprod[:], in0=lv[:, :, 0, :], in1=lv[:, :, 1, :], op=ALU.mult),
             reads=["lamt"], writes=["lprod"])
        P.op("dve", lambda: nc.vector.tensor_reduce(out=lsum[:], in_=lprod[:], axis=AX.X, op=ALU.add), reads=["lprod"], writes=["lsum"])
        P.op("act", lambda: nc.scalar.activation(out=lsum[:], in_=lsum[:], func=AF.Exp), reads=["lsum"], writes=["lsum"])
        P.op("dve", lambda: nc.vector.tensor_tensor(out=nlam[:], in0=lsum[:, 1:2], in1=lsum[:, 0:1], op=ALU.subtract),
             reads=["lsum"], writes=["nlam"])
        P.op("dve", lambda: nc.vector.tensor_scalar(out=nlam[:], in0=nlam[:], scalar1=lam_t[:, 129:130], scalar2=None, op0=ALU.add),
             reads=["nlam", "lamt"], writes=["nlam"])

        outs = []
        for su in range(nsup):
            for tt in range(4):
                t = su * 4 + tt
                s = t % 2
                rows = slice(t * 128, (t + 1) * 128)
                dma(P, "sp", ht[s][:], h_in[rows, :], writes=[("ht", s)])
                ci_, r0_ = (t * 128) // YCH, (t * 128) % YCH
                rw_ = slice(r0_, r0_ + 128)
                dma(P, "sp", ya_t[s][:].rearrange("p (j c) -> p j c", j=4), yas[ci_][rw_, :, :], reads=[("ymine", "a")], writes=[("ya", s)])
                dma(P, "sp", ymix[s][:, 384:768].rearrange("p (j c) -> p j c", j=3), ybs[ci_][rw_, 0:3, 0:128], reads=[("ymine", "b")], writes=[("ymix", s, 1)])
                dma(P, "sp", yc_t[s][:].rearrange("p (j c) -> p j c", j=4), ybs[ci_][rw_, :, 128:192], reads=[("ymine", "b")], writes=[("yc", s)])
                yav = ya_t[s][:].rearrange("p (h m d) -> p h m d", h=6, m=2)
                P.op("dve", lambda yav=yav: nc.vector.scalar_tensor_tensor(out=dd[:], in0=yav[:, :, 1, :], scalar=nlam[:, 0:1],
                                                                          in1=yav[:, :, 0, :], op0=ALU.mult, op1=ALU.add),
                     reads=[("ya", s), "nlam"], writes=["dd"])
                P.op("pool", lambda: nc.gpsimd.tensor_tensor(out=sq[:], in0=dd[:], in1=dd[:], op=ALU.mult), reads=["dd"], writes=["sq"])
                P.op("dve", lambda: nc.vector.tensor_reduce(out=ss[:], in_=sq[:], axis=AX.X, op=ALU.add), reads=["sq"], writes=["ss"])
                P.op("act", lambda: nc.scalar.activation(out=ss[:], in_=ss[:], func=AF.Sqrt, bias=eps_rms[:, 0:1], scale=1.0 / 64),
                     reads=["ss", "eps_rms"], writes=["ss"])
                P.op("dve", lambda: nc.vector.reciprocal(out=ss[:], in_=ss[:]), reads=["ss"], writes=["ss"])
                P.op("dve", lambda: nc.vector.tensor_tensor(out=dd[:], in0=dd[:], in1=ss[:].unsqueeze(2).to_broadcast([128, 6, 64]), op=ALU.mult),
                     reads=["dd", "ss"], writes=["dd"])
                ym0 = ymix[s][:, 0:384].rearrange("p (h d) -> p h d", h=6)
                P.op("pool", lambda ym0=ym0: nc.gpsimd.tensor_tensor(out=ym0, in0=dd[:], in1=gA[:].unsqueeze(1).to_broadcast([128, 6, 64]), op=ALU.mult),
                     reads=["dd", "gA"], writes=[("ymix", s, 0)])
                yct = yc_t[s]
                P.op("pool", lambda yct=yct: nc.gpsimd.tensor_tensor(out=c2[:], in0=yct[:], in1=yct[:], op=ALU.mult), reads=[("yc", s)], writes=["c2"])
                P.op("dve", lambda: nc.vector.tensor_scalar(out=c2[:], in0=c2[:], scalar1=0.044715, scalar2=1.0, op0=ALU.mult, op1=ALU.add),
                     reads=["c2"], writes=["c2"])
                P.op("dve", lambda yct=yct: nc.vector.tensor_tensor(out=c2[:], in0=c2[:], in1=yct[:], op=ALU.mult), reads=["c2", ("yc", s)], writes=["c2"])
                P.op("act", lambda: nc.scalar.activation(out=c2[:], in_=c2[:], func=AF.Sigmoid, scale=1.5957691216057308),
                     reads=["c2"], writes=["c2"])
                P.op("dve", lambda yct=yct: nc.vector.tensor_tensor(out=c3[:], in0=c2[:], in1=yct[:], op=ALU.mult), reads=["c2", ("yc", s)], writes=["c3"])
                P.op("act", lambda: nc.scalar.copy(out=ygb[:], in_=c3[:]), reads=["c3"], writes=["ygb"])
                pv0 = B[0][:].bitcast(BF16)

                def trg(pv0=pv0):
                    for k in range(2):
                        ins = nc.tensor.transpose(out=pv0[:, k * 128:(k + 1) * 128], in_=ygb[:, k * 128:(k + 1) * 128], identity=ident[:])
                    return ins
                P.op("pe", trg, reads=["ygb", "ident"], writes=["b0"])
                P.op("dve", lambda pv0=pv0: nc.vector.tensor_copy(out=ygT[:].rearrange("p k t -> p (k t)"), in_=pv0[:, 0:256]), reads=["b0"], writes=["ygT"])

                def mmg():
                    for k in range(2):
                        ins = nc.tensor.matmul(B[1][:, 0:256], lhsT=ygT[:, k, :], rhs=wglu_t[:, k, :], start=(k == 0), stop=(k == 1))
                    return ins
                P.op("pe", mmg, reads=["ygT", "wglu"], writes=["b1"])
                P.op("act", lambda: nc.scalar.activation(out=sig[:], in_=B[1][:, 0:256], func=AF.Sigmoid), reads=["b1"], writes=["sig"])
                P.op("dve", lambda s=s: nc.vector.tensor_tensor(out=ymix[s][:, 768:1024], in0=c3[:], in1=sig[:], op=ALU.mult),
                     reads=["c3", "sig"], writes=[("ymix", s, 2)])
                ymk = [("ymix", s, 0), ("ymix", s, 1), ("ymix", s, 2)]
                P.op("act", lambda s=s: nc.scalar.copy(out=ymb[:], in_=ymix[s][:]), reads=ymk, writes=["ymb"])
                pvb = B[0][:].bitcast(BF16).rearrange("p (k t) -> p k t", k=8)

                def try_(pvb=pvb):
                    for kc in range(8):
                        ins = nc.tensor.transpose(out=pvb[:, kc, :], in_=ymb[:, kc * 128:(kc + 1) * 128], identity=ident[:])
                    return ins
                P.op("pe", try_, reads=["ymb", "ident"], writes=["b0"])
                P.op("dve", lambda pvb=pvb: nc.vector.tensor_copy(out=ymT[:], in_=pvb), reads=["b0"], writes=["ymT"])
                for half in range(2):
                    def mmo(half=half):
                        for kc in range(8):
                            ins = nc.tensor.matmul(B[2 + half][:], lhsT=ymT[:, kc, :], rhs=wout_t[:, kc, half * 512:(half + 1) * 512],
                                                   start=(kc == 0), stop=(kc == 7))
                        return ins
                    P.op("pe", mmo, reads=["ymT", "wout"], writes=[f"b{2 + half}"])
                    P.op("dve", lambda half=half, s=s: nc.vector.scalar_tensor_tensor(
                        out=z[:, half * 512:(half + 1) * 512], in0=ht[s][:, half * 512:(half + 1) * 512], scalar=float(ALPHA),
                        in1=B[2 + half][:], op0=ALU.mult, op1=ALU.add), reads=[f"b{2 + half}", ("ht", s)], writes=[("z", half)])
                P.op("pool", lambda: nc.gpsimd.tensor_copy(out=z[:, 0:1], in_=z[:, 0:1]), reads=[("z", 0), ("z", 1)], writes=["zz"])
                layer_norm_tile(P, nc, z, "zz", h1[tt], ("h1", tt), g1, b1, "g1", "b1", scr, "ln")
                for half in range(2):
                    pv32 = B[2 + half][:].rearrange("p (k t) -> p k t", k=4)

                    def trh(half=half, pv32=pv32, tt=tt):
                        for k in range(4):
                            kc = half * 4 + k
                            ins = nc.tensor.transpose(out=pv32[:, k, :], in_=h1[tt][:, kc * 128:(kc + 1) * 128], identity=ident32[:])
                        return ins
                    P.op("pe", trh, reads=[("h1", tt), "ident32"], writes=[f"b{2 + half}"])
                    P.op("dve", lambda half=half, pv32=pv32: nc.vector.tensor_copy(out=h1T32[:, half * 4:(half + 1) * 4, :], in_=pv32),
                         reads=[f"b{2 + half}"], writes=[("h1T32", half)])
                    P.op("act", lambda half=half, pv32=pv32, tt=tt: nc.scalar.copy(
                        out=h1T[:, half * 4:(half + 1) * 4, tt * 128:(tt + 1) * 128], in_=pv32),
                        reads=[f"b{2 + half}"], writes=[("h1T", tt, half)])

                def mmr():
                    for kc in range(8):
                        ins = nc.tensor.matmul(B[1][:, 0:20], lhsT=h1T32[:, kc, :], rhs=wr_t[:, kc, :], start=(kc == 0), stop=(kc == 7))
                    return ins
                P.op("pe", mmr, reads=[("h1T32", 0), ("h1T32", 1), "wr"], writes=["b1"])
                P.op("dve", lambda: nc.vector.tensor_tensor(out=lg[:], in0=B[1][:, 0:20], in1=br_t[:], op=ALU.add), reads=["b1", "br"], writes=["lg"])
                V = nc.vector
                glog = lg[:, 0:4]
                elog = lg[:, 4:20]
                seq = [
                    lambda: V.tensor_reduce(out=r["gmax"][:], in_=glog, axis=AX.X, op=ALU.max),
                    lambda: V.tensor_scalar(out=r["goh"][:], in0=glog, scalar1=r["gmax"][:, 0:1], scalar2=None, op0=ALU.is_ge),
                    lambda: V.tensor_scalar(out=r["ngmax"][:], in0=r["gmax"][:], scalar1=-1.0, scalar2=None, op0=ALU.mult),
                ]
                for f_ in seq:
                    P.op("dve", f_, reads=["lg", "rt"], writes=["rt"])
                P.op("act", lambda: nc.scalar.activation(out=r["gexp"][:], in_=glog, func=AF.Exp, bias=r["ngmax"][:, 0:1], scale=1.0),
                     reads=["lg", "rt"], writes=["rt2"])
                seq = [
                    lambda: V.tensor_reduce(out=r["gsum"][:], in_=r["gexp"][:], axis=AX.X, op=ALU.add),
                    lambda: V.reciprocal(out=r["gp"][:], in_=r["gsum"][:]),
                    lambda: V.tensor_tensor(out=r["em"][:].rearrange("p (g e) -> p g e", g=4), in0=elog.rearrange("p (g e) -> p g e", g=4),
                                            in1=r["goh"][:].unsqueeze(2).to_broadcast([128, 4, 4]), op=ALU.mult),
                    lambda: V.tensor_scalar(out=r["pen"][:], in0=r["goh"][:], scalar1=-1.0, scalar2=BIG, op0=ALU.add, op1=ALU.mult),
                    lambda: V.tensor_tensor(out=r["em"][:].rearrange("p (g e) -> p g e", g=4), in0=r["em"][:].rearrange("p (g e) -> p g e", g=4),
                                            in1=r["pen"][:].unsqueeze(2).to_broadcast([128, 4, 4]), op=ALU.add),
                    lambda: V.tensor_reduce(out=r["m1"][:], in_=r["em"][:], axis=AX.X, op=ALU.max),
                    lambda: V.tensor_scalar(out=r["oh1"][:], in0=r["em"][:], scalar1=r["m1"][:, 0:1], scalar2=None, op0=ALU.is_ge),
                    lambda: V.scalar_tensor_tensor(out=r["em2"][:], in0=r["oh1"][:], scalar=-BIG, in1=r["em"][:], op0=ALU.mult, op1=ALU.add),
                    lambda: V.tensor_reduce(out=r["m2"][:], in_=r["em2"][:], axis=AX.X, op=ALU.max),
                    lambda: V.tensor_scalar(out=r["oh2"][:], in0=r["em2"][:], scalar1=r["m2"][:, 0:1], scalar2=None, op0=ALU.is_ge),
                    lambda: V.tensor_tensor(out=r["dl"][:], in0=r["m2"][:], in1=r["m1"][:], op=ALU.subtract),
                ]
                for f_ in seq:
                    P.op("dve", f_, reads=["lg", "rt", "rt2"], writes=["rt"])
                P.op("act", lambda: nc.scalar.activation(out=r["ex"][:], in_=r["dl"][:], func=AF.Exp), reads=["rt"], writes=["rt2"])
                seq = [
                    lambda: V.tensor_scalar(out=r["den"][:], in0=r["ex"][:], scalar1=1.0, scalar2=None, op0=ALU.add),
                    lambda: V.reciprocal(out=r["w1"][:], in_=r["den"][:]),
                    lambda: V.tensor_tensor(out=r["w1"][:], in0=r["w1"][:], in1=r["gp"][:], op=ALU.mult),
                    lambda: V.tensor_tensor(out=r["w2"][:], in0=r["w1"][:], in1=r["ex"][:], op=ALU.mult),
                    lambda tt=tt: V.tensor_scalar(out=comb[:, tt, :], in0=r["oh1"][:], scalar1=r["w1"][:, 0:1], scalar2=None, op0=ALU.mult),
                    lambda tt=tt: V.scalar_tensor_tensor(out=comb[:, tt, :], in0=r["oh2"][:], scalar=r["w2"][:, 0:1], in1=comb[:, tt, :],
                                                         op0=ALU.mult, op1=ALU.add),
                ]
                for i_, f_ in enumerate(seq):
                    P.op("dve", f_, reads=["rt", "rt2"] + ([("comb", tt)] if i_ == 5 else []), writes=["rt"] if i_ < 4 else [("comb", tt)])
            h1Tk = [("h1T", tt, half) for tt in range(4) for half in range(2)]
            for e in range(NEXP):
                ws = e % 2
                dma(P, "sp", wA[ws][:], w1[e], writes=[("wA", ws)])
                dma(P, "poolq", wB[ws][:], w3[e], writes=[("wB", ws)])
                dma(P, "sp", wC[ws][:], w2[e], writes=[("wC", ws)])
                for hc in range(4):
                    ba, bb = 4 + hc % 2, 6 + hc % 2

                    def mma(hc=hc, ba=ba, ws=ws):
                        for kc in range(8):
                            ins = nc.tensor.matmul(B[ba][:], lhsT=wA[ws][:, kc, hc * 128:(hc + 1) * 128], rhs=h1T[:, kc, :],
                                                   start=(kc == 0), stop=(kc == 7))
                        return ins

                    def mmb(hc=hc, bb=bb, ws=ws):
                        for kc in range(8):
                            ins = nc.tensor.matmul(B[bb][:], lhsT=wB[ws][:, kc, hc * 128:(hc + 1) * 128], rhs=h1T[:, kc, :],
                                                   start=(kc == 0), stop=(kc == 7))
                        return ins
                    P.op("pe", mma, reads=h1Tk + [("wA", ws)], writes=[f"b{ba}"])
                    P.op("pe", mmb, reads=h1Tk + [("wB", ws)], writes=[f"b{bb}"])
                    P.op("act", lambda hc=hc, ba=ba: nc.scalar.activation(out=sil[hc % 2][:], in_=B[ba][:], func=AF.Silu),
                         reads=[f"b{ba}"], writes=[("sil", hc % 2)])
                    P.op("dve", lambda hc=hc, bb=bb: nc.vector.tensor_tensor(out=hid[hc][:], in0=sil[hc % 2][:], in1=B[bb][:], op=ALU.mult),
                         reads=[f"b{bb}", ("sil", hc % 2)], writes=[("hid", hc)])
                for tt in range(4):
                    for half in range(2):
                        bo = 2 + half

                        def mm2(tt=tt, half=half, bo=bo, ws=ws):
                            for hc in range(4):
                                ins = nc.tensor.matmul(B[bo][:], lhsT=hid[hc][:, tt * 128:(tt + 1) * 128],
                                                       rhs=wC[ws][:, hc, half * 512:(half + 1) * 512], start=(hc == 0), stop=(hc == 3))
                            return ins
                        P.op("pe", mm2, reads=[("hid", hc) for hc in range(4)] + [("wC", ws)], writes=[f"b{bo}"])
                        av = acc[tt][:, half * 512:(half + 1) * 512]
                        if e == 0:
                            P.op("dve", lambda av=av, bo=bo, tt=tt, e=e: nc.vector.tensor_scalar(
                                out=av, in0=B[bo][:], scalar1=comb[:, tt, e:e + 1], scalar2=None, op0=ALU.mult),
                                reads=[f"b{bo}", ("comb", tt)], writes=[("acc", tt, half)])
                        else:
                            P.op("dve", lambda av=av, bo=bo, tt=tt, e=e: nc.vector.scalar_tensor_tensor(
                                out=av, in0=B[bo][:], scalar=comb[:, tt, e:e + 1], in1=av, op0=ALU.mult, op1=ALU.add),
                                reads=[f"b{bo}", ("comb", tt), ("acc", tt, half)], writes=[("acc", tt, half)])
            for tt in range(4):
                t = su * 4 + tt
                s = t % 2
                rows = slice(t * 128, (t + 1) * 128)
                P.op("dve", lambda tt=tt: nc.vector.scalar_tensor_tensor(out=z2[:], in0=h1[tt][:], scalar=float(ALPHA), in1=acc[tt][:],
                                                                          op0=ALU.mult, op1=ALU.add),
                     reads=[("h1", tt), ("acc", tt, 0), ("acc", tt, 1)], writes=["z2"])
                layer_norm_tile(P, nc, z2, "z2", ho[s], ("ho", s), g2, b2, "g2", "b2", scr, "ln")
                dma(P, "poolq", h_out[rows, :], ho[s][:], reads=[("ho", s)], writes=[("h_out", t)])
                outs.append(("h_out", t))
                if hT_out is not None:
                    to_featmajor_bf16(P, nc, ho[s], ("ho", s), hob, "hob", B[0], "b0", hoT[s][:], ("hoT", s), ident)
                    dma(P, "poolq", hT_out[t // 4][:, :, (t % 4) * 128:(t % 4 + 1) * 128], hoT[s][:], reads=[("hoT", s)], writes=[("hT_out", t)])
                    outs.append(("hT_out", t))
                    if tt == 3:
                        stream_ag(P, nc, ag, su, [("hT_out", t_) for t_ in range(t - 3, t + 1)])
        P.emit(final_wait_keys=outs)


def post_inputs(l, p, lam_init):
    d = {}
    d["wout"] = bf(p["w_out"][l].reshape(8, 128, D).transpose(1, 0, 2))
    d["wglu"] = bf(p["s5_w_glu"][l].reshape(2, 128, 256).transpose(1, 0, 2))
    for n in ("ln1_g", "ln1_b", "ln2_g", "ln2_b"):
        d[n.replace("_", "")] = f32c(p[n][l].reshape(1, D))
    d["dng"] = f32c(p["diff_norm_g"][l].reshape(1, 64))
    d["lamv"] = f32c(np.concatenate([p["lam_q1"][l], p["lam_k1"][l], p["lam_q2"][l], p["lam_k2"][l],
                                     np.array([1.0 - lam_init, -lam_init], np.float32)]).reshape(1, 130))
    wr = np.concatenate([p["moe_w_grp"][l], p["moe_w_exp"][l]], axis=1)
    d["wr"] = f32c(wr.reshape(8, 128, 20).transpose(1, 0, 2))
    d["br"] = f32c(np.concatenate([p["moe_b_grp"][l], p["moe_b_exp"][l]]).reshape(1, 20))
    d["w1"] = bf(p["moe_w1"][l].reshape(NEXP, 8, 128, DEXP).transpose(0, 2, 1, 3))
    d["w3"] = bf(p["moe_w3"][l].reshape(NEXP, 8, 128, DEXP).transpose(0, 2, 1, 3))
    d["w2"] = bf(p["moe_w2"][l].reshape(NEXP, 4, 128, D).transpose(0, 2, 1, 3))
    d["idn"] = bf(np.eye(128)); d["idn32"] = f32c(np.eye(128))
    return d


SBANKS = [0, 1, 2, 6, 7]
NPT = 7
ADEPTH = 4
QUARTERS = False
TWO_PI = 2.0 * math.pi
PI_SAFE = 3.141592
MAGIC = 12582912.0


def phase_mix(nc, G, l, hT_all, ya_d, ybc_d, S=SEQ, do_attn=True, do_s5=True, do_gdn=True, pool_pre=None, pool_post=None, ag_stream=None):
    debug = False
    C = Ctx(nc)
    P = C.P
    nst = S // 512
    nblk = S // 128
    pf = f"L{l}_"

    def din(name, shape, dt=F32):
        return G.din((pf + name) if name not in ("amask", "idn32", "srow", "cTri", "cSL", "cMask2", "cBones") else name, shape, dt)
    hT4 = hT_all.rearrange("c (r k p) t -> c r p k t", r=4, k=8)
    wq = din("wq", [128, 8, 96], BF16); wk = din("wk", [128, 8, 96], BF16); wv = din("wv", [128, 8, 192], BF16)
    amask = din("amask", [128, 4, 512], BF16)
    idn32 = din("idn32", [128, 128])
    wu = din("wu", [128, 8, 64], BF16)
    s5row = din("s5row", [2, 3, 128])
    s5col = din("s5col", [2, 128, 3])
    s5bT = din("s5bT", [2, 2, 2, 16, 64])
    s5cT = din("s5cT", [2, 2, 2, 64, 16])
    s5d = din("s5d", [64, 1])
    srow = din("srow", [1, 512])
    wg = din("wg", [128, 8, 384], BF16); wt = din("wt", [128, 8, 132], BF16)
    cvw = din("cvw", [128, 3, 4])
    galog = din("galog", [1, 2]); gdtb = din("gdtb", [1, 2]); gng = din("gng", [1, 64])
    cTri = din("cTri", [64, 64]); cSL = din("cSL", [64, 64]); cMask2 = din("cMask2", [64, 2, 64]); cBones = din("cBones", [128, 128])
    ya_o = ya_d.rearrange("s (u d) -> s u d", u=3)
    yb_o = ybc_d[:, 0:128].rearrange("s (h d) -> s h d", h=2)
    yc_o = ybc_d[:, 128:192]
    P.pool_pre, P.pool_post = pool_pre, pool_post
    ag_n = [0]

    def try_ag():
        if ag_stream is None:
            return
        pairs, cc = ag_stream
        per = YCH // 512
        while ag_n[0] < len(pairs):
            sts = range(ag_n[0] * per, (ag_n[0] + 1) * per)
            keys = [("yb_o", s_) for s_ in sts] + [("yc_o", s_) for s_ in sts]
            if not all(k_ in P.last_w for k_ in keys):
                break
            s_ap, d_ap = pairs[ag_n[0]]
            P.op("pool", lambda s_ap=s_ap, d_ap=d_ap: nc.gpsimd.collective_compute(
                "AllGather", ALU.bypass, replica_groups=GROUPS, ins=[s_ap], outs=[d_ap]).then_inc(cc, 1), reads=keys)
            ag_n[0] += 1
    outs = []
    dbg = []
    V = nc.vector
    G = nc.gpsimd
    A = nc.scalar
    T = nc.tensor

    with P.stack:
        C.alloc_banks(quarters=do_gdn and QUARTERS)
        B = C.banks
        sb = P.sb
        ident32 = sb([128, 128], F32)
        dma(P, "sp", ident32[:], idn32, writes=["ident32"])
        hTt = [sb([128, 8, 512], BF16) for _ in range(2)]
        eps_rms = const_col(P, nc, RMS_EPS, "eps_rms")
        if do_attn:
            wq_t = sb([128, 8, 96], BF16); wk_t = sb([128, 8, 96], BF16); wv_t = sb([128, 8, 192], BF16)
            QT = sb([96, S], BF16); KT = sb([96, S], BF16)
            Vall = sb([128, nblk, 3, 65], BF16)
            am_t = sb([128, 4, 512], BF16)
            PT = [sb([128, 512], BF16) for _ in range(NPT)]
            osb = sb([65, 512], F32); rec = sb([128, 4], F32)
            oT = [sb([128, 4, 64], F32) for _ in range(2)]
            dma(P, "sp", wq_t[:], wq, writes=["wq"]); dma(P, "sp", wk_t[:], wk, writes=["wk"]); dma(P, "sp", wv_t[:], wv, writes=["wv"])
            dma(P, "sp", am_t[:], amask, writes=["amask"])
            P.op("pool", lambda: G.memset(Vall[:, :, :, 64:65], 1.0), writes=["Vones"])
        if do_s5:
            wu_t = sb([128, 8, 64], BF16)
            dma(P, "sp", wu_t[:], wu, writes=["wu"])
            uT = [sb([32, 512], F32) for _ in range(2)]
            srow_t = sb([128, 512], F32)
            dma(P, "sp", srow_t[:], srow.partition_broadcast(128), writes=["srow"])
            d_col = [sb([32, 1], F32) for _ in range(2)]
            for pr_ in range(2):
                dma(P, "sp", d_col[pr_][:], s5d[pr_ * 32:(pr_ + 1) * 32, :], writes=[("dcol", pr_)])
            s5 = []
            for pr in range(2):
                t = dict(row=sb([32, 3, 128], F32), col=sb([128, 3], F32),
                         BrBD=sb([32, 128], F32), BiBD=sb([32, 128], F32), CrBD=sb([128, 32], F32), CiBD=sb([128, 32], F32),
                         bbr=sb([32, 128], F32), bbi=sb([32, 128], F32),
                         w=[sb([32, 128], F32) for _ in range(8)],
                         cw=[sb([128, 1], F32) for _ in range(8)],
                         RHO=sb([128, 512], F32), CS=sb([128, 512], F32), SN=sb([128, 512], F32),
                         zi=sb([128, 2], F32), zt=sb([128, 2], F32))
                if pr == 0:
                    for nm_ in ("bre", "bim", "t1", "t2", "zre", "zim", "xre", "xim", "ang", "tmp"):
                        t[nm_] = sb([128, 512], F32)
                if pr == 1:
                    for nm_ in ("bre", "bim", "t1", "t2", "zre", "zim", "xre", "xim", "ang", "tmp"):
                        t[nm_] = s5[0][nm_]
                s5.append(t)
            yT = [sb([32, 512], F32) for _ in range(2)]
            yc_tm = [sb([128, 4, 64], F32) for _ in range(2)]

            def range_reduce(eng_name, x, tmp, key_x, key_t, shape_all=True):
                P.op("dve", lambda: V.tensor_scalar(out=tmp, in0=x, scalar1=1.0 / TWO_PI, scalar2=MAGIC, op0=ALU.mult, op1=ALU.add),
                     reads=[key_x], writes=[key_t])
                P.op("dve", lambda: V.tensor_scalar(out=tmp, in0=tmp, scalar1=-MAGIC, scalar2=-TWO_PI, op0=ALU.add, op1=ALU.mult),
                     reads=[key_t], writes=[key_t])
                P.op("dve", lambda: V.tensor_tensor(out=x, in0=x, in1=tmp, op=ALU.add), reads=[key_x, key_t], writes=[key_x])
                P.op("dve", lambda: V.tensor_scalar(out=x, in0=x, scalar1=-PI_SAFE, scalar2=PI_SAFE, op0=ALU.max, op1=ALU.min),
                     reads=[key_x], writes=[key_x])

            def s5_setup(pr):
                t = s5[pr]
                k = lambda n, pr=pr: ("s5", "sh" if n in ("bre", "bim", "t1", "t2", "zre", "zim", "xre", "xim", "tab", "roww_scratch") else pr, n)
                dma(P, "sp", t["row"][:], s5row[pr:pr + 1].partition_broadcast(32), writes=[k("row")])
                dma(P, "sp", t["col"][:], s5col[pr], writes=[k("col")])
                for nm in ("BrBD", "BiBD", "CrBD", "CiBD"):
                    P.op("pool", lambda nm=nm, t=t: G.memset(t[nm][:], 0.0), writes=[k(nm)])
                for g in range(2):
                    dma(P, "sp", t["BrBD"][g * 16:(g + 1) * 16, g * 64:(g + 1) * 64], s5bT[pr, g, 0], reads=[k("BrBD")], writes=[k("BrBD")])
                    dma(P, "sp", t["BiBD"][g * 16:(g + 1) * 16, g * 64:(g + 1) * 64], s5bT[pr, g, 1], reads=[k("BiBD")], writes=[k("BiBD")])
                    dma(P, "sp", t["CrBD"][g * 64:(g + 1) * 64, g * 16:(g + 1) * 16], s5cT[pr, g, 0], reads=[k("CrBD")], writes=[k("CrBD")])
                    dma(P, "sp", t["CiBD"][g * 64:(g + 1) * 64, g * 16:(g + 1) * 16], s5cT[pr, g, 1], reads=[k("CiBD")], writes=[k("CiBD")])
                P.op("dve", lambda t=t: V.tensor_scalar(out=t["CiBD"][:], in0=t["CiBD"][:], scalar1=-1.0, scalar2=None, op0=ALU.mult),
                     reads=[k("CiBD")], writes=[k("CiBD")])
                lre, lim, ldt = t["row"][:, 0, :], t["row"][:, 1, :], t["row"][:, 2, :]
                dt_, lr_, mag, ang, tmp_, sn, cs, den = [t["w"][i][:] for i in range(8)]
                rk = k("roww")
                steps = [
                    ("act", lambda: A.activation(out=dt_, in_=ldt, func=AF.Exp)),
                    ("dve", lambda: V.tensor_tensor(out=lr_, in0=lre, in1=dt_, op=ALU.mult)),
                    ("act", lambda: A.activation(out=mag, in_=lr_, func=AF.Exp)),
                    ("dve", lambda: V.tensor_tensor(out=ang, in0=lim, in1=dt_, op=ALU.mult)),
                ]
                for e_, f_ in steps:
                    P.op(e_, f_, reads=[k("row"), rk], writes=[rk])
                range_reduce("dve", ang, tmp_, rk, rk)
                P.op("act", lambda: A.activation(out=sn, in_=ang, func=AF.Sin), reads=[rk], writes=[rk])
                P.op("dve", lambda: V.tensor_scalar(out=ang, in0=ang, scalar1=math.pi / 2, scalar2=None, op0=ALU.add), reads=[rk], writes=[rk])
                range_reduce("dve", ang, tmp_, rk, rk)
                P.op("act", lambda: A.activation(out=cs, in_=ang, func=AF.Sin), reads=[rk], writes=[rk])
                steps = [
                    lambda: V.tensor_tensor(out=cs, in0=cs, in1=mag, op=ALU.mult),
                    lambda: V.tensor_scalar(out=cs, in0=cs, scalar1=-1.0, scalar2=None, op0=ALU.add),
                    lambda: V.tensor_tensor(out=sn, in0=sn, in1=mag, op=ALU.mult),
                    lambda: V.tensor_tensor(out=den, in0=lre, in1=lre, op=ALU.mult),
                    lambda: V.tensor_tensor(out=tmp_, in0=lim, in1=lim, op=ALU.mult),
                    lambda: V.tensor_tensor(out=den, in0=den, in1=tmp_, op=ALU.add),
                    lambda: V.reciprocal(out=den, in_=den),
                    lambda: V.tensor_tensor(out=dt_, in0=cs, in1=lre, op=ALU.mult),
                    lambda: V.tensor_tensor(out=tmp_, in0=sn, in1=lim, op=ALU.mult),
                    lambda: V.tensor_tensor(out=dt_, in0=dt_, in1=tmp_, op=ALU.add),
                    lambda: V.tensor_tensor(out=dt_, in0=dt_, in1=den, op=ALU.mult),
                    lambda: V.tensor_tensor(out=lr_, in0=sn, in1=lre, op=ALU.mult),
                    lambda: V.tensor_tensor(out=tmp_, in0=cs, in1=lim, op=ALU.mult),
                    lambda: V.tensor_tensor(out=lr_, in0=lr_, in1=tmp_, op=ALU.subtract),
                    lambda: V.tensor_tensor(out=lr_, in0=lr_, in1=den, op=ALU.mult),
                    lambda t=t: V.tensor_tensor(out=t["bbr"][:], in0=dt_, in1=t["BrBD"][:], op=ALU.mult),
                    lambda t=t: V.tensor_tensor(out=tmp_, in0=lr_, in1=t["BiBD"][:], op=ALU.mult),
                    lambda t=t: V.tensor_tensor(out=t["bbr"][:], in0=t["bbr"][:], in1=tmp_, op=ALU.subtract),
                    lambda t=t: V.tensor_tensor(out=t["bbi"][:], in0=dt_, in1=t["BiBD"][:], op=ALU.mult),
                    lambda t=t: V.tensor_tensor(out=tmp_, in0=lr_, in1=t["BrBD"][:], op=ALU.mult),
                    lambda t=t: V.tensor_tensor(out=t["bbi"][:], in0=t["bbi"][:], in1=tmp_, op=ALU.add),
                ]
                for f_ in steps:
                    P.op("dve", f_, reads=[k("row"), rk, k("BrBD"), k("BiBD")], writes=[rk])
                cdt, cth, crho, ca, ctmp, c512s, c512c, cx = [t["cw"][i][:] for i in range(8)]
                ck = k("colw")
                steps = [
                    ("act", lambda t=t: A.activation(out=cdt, in_=t["col"][:, 2:3], func=AF.Exp)),
                    ("dve", lambda t=t: V.tensor_tensor(out=cth, in0=t["col"][:, 1:2], in1=cdt, op=ALU.mult)),
                    ("dve", lambda t=t: V.tensor_tensor(out=crho, in0=t["col"][:, 0:1], in1=cdt, op=ALU.mult)),
                    ("act", lambda: A.activation(out=crho, in_=crho, func=AF.Exp)),
                    ("dve", lambda: V.tensor_scalar(out=ca, in0=cth, scalar1=512.0, scalar2=None, op0=ALU.mult)),
                ]
                for e_, f_ in steps:
                    P.op(e_, f_, reads=[k("col"), ck], writes=[ck])
                range_reduce("dve", ca, ctmp, ck, ck)
                P.op("act", lambda: A.activation(out=c512s, in_=ca, func=AF.Sin), reads=[ck], writes=[ck])
                P.op("dve", lambda: V.tensor_scalar(out=ca, in0=ca, scalar1=math.pi / 2, scalar2=None, op0=ALU.add), reads=[ck], writes=[ck])
                range_reduce("dve", ca, ctmp, ck, ck)
                P.op("act", lambda: A.activation(out=c512c, in_=ca, func=AF.Sin), reads=[ck], writes=[ck])
                tk = k("tab")
                P.op("dve", lambda t=t: V.tensor_scalar(out=t["ang"][:], in0=srow_t[:], scalar1=cth, scalar2=None, op0=ALU.mult),
                     reads=["srow", ck], writes=[tk])
                range_reduce("dve", t["ang"][:], t["tmp"][:], tk, tk)
                P.op("act", lambda t=t: A.activation(out=t["SN"][:], in_=t["ang"][:], func=AF.Sin), reads=[tk], writes=[tk])
                P.op("dve", lambda t=t: V.tensor_scalar(out=t["ang"][:], in0=t["ang"][:], scalar1=math.pi / 2, scalar2=None, op0=ALU.add), reads=[tk], writes=[tk])
                range_reduce("dve", t["ang"][:], t["tmp"][:], tk, tk)
                P.op("act", lambda t=t: A.activation(out=t["CS"][:], in_=t["ang"][:], func=AF.Sin), reads=[tk], writes=[tk])
                P.op("pool", lambda t=t: G.memset(t["RHO"][:], 1.0), writes=[k("rho")])
                P.op("dve", lambda t=t: V.tensor_scalar(out=t["RHO"][:], in0=t["RHO"][:], scalar1=crho, scalar2=None, op0=ALU.mult),
                     reads=[k("rho"), ck], writes=[k("rho")])
                P.op("pool", lambda t=t: G.memset(t["zi"][:], 0.0), writes=[k("zi")])
                if pr == 0:
                    dbg.extend([("CS", t["CS"][:], [128, 512], [k("tab")]), ("SN", t["SN"][:], [128, 512], [k("tab")]),
                            ("RHO", t["RHO"][:], [128, 512], [k("rho")]), ("bbr", t["bbr"][:], [32, 128], [k("roww")]),
                            ("bbi", t["bbi"][:], [32, 128], [k("roww")]), ("cr", t["w"][0][:], [32, 128], [k("roww")]),
                            ("ci", t["w"][1][:], [32, 128], [k("roww")]), ("c512", t["cw"][5][:], [128, 1], [k("colw")]),
                            ("row", t["row"][:], [32, 3, 128], [k("row")]), ("col", t["col"][:], [128, 3], [k("col")])])
            for pr_ in range(2):
                s5_setup(pr_)
        if do_gdn:
            wg_t = sb([128, 8, 384], BF16); wt_t = sb([128, 8, 132], BF16)
            dma(P, "sp", wg_t[:], wg, writes=["wg"]); dma(P, "sp", wt_t[:], wt, writes=["wt"])
            cvw_t = sb([128, 3, 4], F32); dma(P, "sp", cvw_t[:], cvw, writes=["cvw"])
            alog_t = sb([64, 2], F32); dtb_t = sb([64, 2], F32); ng_t = sb([64, 64], F32)
            dma(P, "sp", alog_t[:], galog.partition_broadcast(64), writes=["alog"])
            dma(P, "sp", dtb_t[:], gdtb.partition_broadcast(64), writes=["dtb"])
            dma(P, "sp", ng_t[:], gng.partition_broadcast(64), writes=["ngt"])
            Tri = sb([64, 64], F32); SL = sb([64, 64], F32); Mask2 = sb([64, 2, 64], F32); Bones = sb([128, 128], F32); ones64 = sb([64, 64], F32)
            dma(P, "sp", Tri[:], cTri, writes=["Tri"]); dma(P, "sp", SL[:], cSL, writes=["SL"])
            dma(P, "sp", Mask2[:], cMask2, writes=["Mask2"]); dma(P, "sp", Bones[:], cBones, writes=["Bones"])
            P.op("pool", lambda: G.memset(ones64[:], 1.0), writes=["ones64"])
            P.op("act", lambda: A.activation(out=alog_t[:], in_=alog_t[:], func=AF.Exp), reads=["alog"], writes=["alog"])
            P.op("dve", lambda: V.tensor_scalar(out=alog_t[:], in0=alog_t[:], scalar1=-1.0, scalar2=None, op0=ALU.mult), reads=["alog"], writes=["alog"])
            xraw = [sb([128, 515], F32) for _ in range(3)]
            for c_ in range(3):
                P.op("pool", lambda c_=c_: G.memset(xraw[c_][:], 0.0), writes=[("xraw", c_)])
            cvt = sb([128, 512], F32)
            qkv = [sb([128, 512], F32) for _ in range(3)]
            sqn = sb([128, 512], F32); rn_ = sb([128, 512], F32)
            Sst = [sb([64, 64], F32) for _ in range(2)]
            for h_ in range(2):
                P.op("pool", lambda h_=h_: G.memset(Sst[h_][:], 0.0), writes=[("S", h_)])
            gd = dict(ch=[], hd=[])
            for sl in range(4):
                gd["ch"].append(dict(gs=sb([64, 128], F32), bg=sb([64, 4], F32), nbeta=sb([64, 2], F32)))
            for sl in range(8):
                gd["hd"].append(dict(qkv_tm=sb([64, 3, 64], F32), gcl=sb([64, 2], F32), ex3=sb([64, 3], F32), Gm=sb([64, 64], F32),
                                     EE=sb([64, 2, 64], F32), AT=sb([64, 64], F32),
                                     W=[sb([64, 256], F32) for _ in range(2)], tb=sb([64, 1], F32),
                                     kdec=sb([64, 64], F32), qdec=sb([64, 64], F32), wqT=sb([64, 2, 64], F32), vnew=sb([64, 64], F32),
                                     osb=sb([64, 64], F32), osq=sb([64, 64], F32), oss=sb([64, 1], F32), ngate=sb([64, 64], F32)))
            ybuf = [sb([64, 8, 2, 64], F32) for _ in range(2)]

        for st in range(nst):
            hs = st % 2
            cols = slice(st * 512, (st + 1) * 512)
            dma(P, "sp", hTt[hs][:], hT4[st % (nst // 4)][st // (nst // 4)], writes=[("hTt", hs)])
            hk = ("hTt", hs)
            if do_attn:
                for (w_t, wkey, dst, dk_, bank) in ((wq_t, "wq", QT, "QT", 0), (wk_t, "wk", KT, "KT", 1)):
                    def mmqk(w_t=w_t, bank=bank, hs=hs):
                        for kc in range(8):
                            ins = T.matmul(B[bank][0:96, :], lhsT=w_t[:, kc, :], rhs=hTt[hs][:, kc, :], start=(kc == 0), stop=(kc == 7))
                        return ins
                    P.op("pe", mmqk, reads=[hk, wkey], writes=[f"b{bank}"])
                    P.op("act", lambda dst=dst, bank=bank, cols=cols: A.copy(out=dst[:, cols], in_=B[bank][0:96, :]),
                         reads=[f"b{bank}"], writes=[(dk_, st)])
                for pair in range(2):
                    bank = 2 + pair
                    pv = B[bank][:, 0:384].rearrange("p (j c) -> p j c", j=2)

                    def mmv(pair=pair, pv=pv, hs=hs):
                        for j in range(2):
                            blk = pair * 2 + j
                            for kc in range(8):
                                ins = T.matmul(pv[:, j, :], lhsT=hTt[hs][:, kc, blk * 128:(blk + 1) * 128], rhs=wv_t[:, kc, :],
                                               start=(kc == 0), stop=(kc == 7))
                        return ins
                    P.op("pe", mmv, reads=[hk, "wv"], writes=[f"b{bank}"])
                    b0 = st * 4 + pair * 2
                    P.op("dve", lambda pv=pv, b0=b0: V.tensor_copy(out=Vall[:, b0:b0 + 2, :, 0:64],
                                                                  in_=pv.rearrange("p j (u d) -> p j u d", u=3)),
                         reads=[f"b{bank}"], writes=[("V", st, pair)])
            if do_s5:
                for pr in range(2):
                    def mmu(hs=hs, pr=pr):
                        for kc in range(8):
                            ins = T.matmul(B[4][0:32, :], lhsT=wu_t[:, kc, pr * 32:(pr + 1) * 32], rhs=hTt[hs][:, kc, :], start=(kc == 0), stop=(kc == 7))
                        return ins
                    P.op("pe", mmu, reads=[hk, "wu"], writes=["b4"])
                    P.op("act", lambda pr=pr: A.copy(out=uT[pr][:], in_=B[4][0:32, :]), reads=["b4"], writes=[("uT", pr)])
                def s5_stream(pr):
                    t = s5[pr]
                    k = lambda n, pr=pr: ("s5", "sh" if n in ("bre", "bim", "t1", "t2", "zre", "zim", "xre", "xim", "tab", "roww_scratch") else pr, n)
                    P.op("pe", lambda t=t, pr=pr: T.matmul(B[5][:], lhsT=t["bbr"][:], rhs=uT[pr][:], start=True, stop=True),
                         reads=[("uT", pr), k("roww")], writes=["b5"])
                    P.op("pe", lambda t=t, pr=pr: T.matmul(B[6][:], lhsT=t["bbi"][:], rhs=uT[pr][:], start=True, stop=True),
                         reads=[("uT", pr), k("roww")], writes=["b6"])
                    P.op("act", lambda t=t: A.copy(out=t["bre"][:], in_=B[5][:]), reads=["b5"], writes=[k("bre")])
                    P.op("act", lambda t=t: A.copy(out=t["bim"][:], in_=B[6][:]), reads=["b6"], writes=[k("bim")])
                    P.op("dve", lambda t=t: V.tensor_tensor(out=t["t1"][:], in0=t["bre"][:], in1=t["CS"][:], op=ALU.mult), reads=[k("bre"), k("tab")], writes=[k("t1")])
                    P.op("pool", lambda t=t: G.tensor_tensor(out=t["t2"][:], in0=t["bim"][:], in1=t["SN"][:], op=ALU.mult), reads=[k("bim"), k("tab")], writes=[k("t2")])
                    P.op("dve", lambda t=t: V.tensor_tensor(out=t["t1"][:], in0=t["t1"][:], in1=t["t2"][:], op=ALU.add), reads=[k("t1"), k("t2")], writes=[k("t1")])
                    P.op("pool", lambda t=t: G.tensor_tensor(out=t["t2"][:], in0=t["bim"][:], in1=t["CS"][:], op=ALU.mult), reads=[k("bim"), k("tab"), k("t1")], writes=[k("t2")])
                    P.op("pool", lambda t=t: G.tensor_tensor(out=t["bre"][:], in0=t["bre"][:], in1=t["SN"][:], op=ALU.mult), reads=[k("bre"), k("tab"), k("t1")], writes=[k("bre")])
                    P.op("pool", lambda t=t: G.tensor_tensor(out=t["t2"][:], in0=t["t2"][:], in1=t["bre"][:], op=ALU.subtract), reads=[k("t2"), k("bre")], writes=[k("t2")])
                    P.op("dve", lambda t=t: V.tensor_tensor_scan(out=t["zre"][:], data0=t["RHO"][:], data1=t["t1"][:], initial=t["zi"][:, 0:1],
                                                                  op0=ALU.mult, op1=ALU.add), reads=[k("t1"), k("rho"), k("zi")], writes=[k("zre")])
                    P.op("dve", lambda t=t: V.tensor_tensor_scan(out=t["zim"][:], data0=t["RHO"][:], data1=t["t2"][:], initial=t["zi"][:, 1:2],
                                                                  op0=ALU.mult, op1=ALU.add), reads=[k("t2"), k("rho"), k("zi")], writes=[k("zim")])
                    cdt, cth, crho, ca, ctmp, c512s, c512c, cx = [t["cw"][i][:] for i in range(8)]
                    zl_re, zl_im = t["zre"][:, 511:512], t["zim"][:, 511:512]
                    P.op("dve", lambda t=t, zl_re=zl_re: V.tensor_tensor(out=t["zt"][:, 0:1], in0=zl_re, in1=c512c, op=ALU.mult), reads=[k("zre"), k("colw")], writes=[k("zt")])
                    P.op("dve", lambda t=t, zl_im=zl_im: V.tensor_tensor(out=t["zt"][:, 1:2], in0=zl_im, in1=c512s, op=ALU.mult), reads=[k("zim"), k("colw")], writes=[k("zt")])
                    P.op("dve", lambda t=t: V.tensor_tensor(out=t["zi"][:, 0:1], in0=t["zt"][:, 0:1], in1=t["zt"][:, 1:2], op=ALU.subtract), reads=[k("zt"), k("zi")], writes=[k("zi")])
                    P.op("dve", lambda t=t, zl_re=zl_re: V.tensor_tensor(out=t["zt"][:, 0:1], in0=zl_re, in1=c512s, op=ALU.mult), reads=[k("zre"), k("colw"), k("zi")], writes=[k("zt")])
                    P.op("dve", lambda t=t, zl_im=zl_im: V.tensor_tensor(out=t["zt"][:, 1:2], in0=zl_im, in1=c512c, op=ALU.mult), reads=[k("zim"), k("colw")], writes=[k("zt")])
                    P.op("dve", lambda t=t: V.tensor_tensor(out=t["zi"][:, 1:2], in0=t["zt"][:, 0:1], in1=t["zt"][:, 1:2], op=ALU.add), reads=[k("zt"), k("zi")], writes=[k("zi")])
                    P.op("dve", lambda t=t: V.tensor_tensor(out=t["xre"][:], in0=t["zre"][:], in1=t["CS"][:], op=ALU.mult), reads=[k("zre"), k("tab")], writes=[k("xre")])
                    P.op("pool", lambda t=t: G.tensor_tensor(out=t["t1"][:], in0=t["zim"][:], in1=t["SN"][:], op=ALU.mult), reads=[k("zim"), k("tab"), k("zre")], writes=[k("t1")])
                    P.op("dve", lambda t=t: V.tensor_tensor(out=t["xre"][:], in0=t["xre"][:], in1=t["t1"][:], op=ALU.subtract), reads=[k("xre"), k("t1")], writes=[k("xre")])
                    P.op("pool", lambda t=t: G.tensor_tensor(out=t["xim"][:], in0=t["zre"][:], in1=t["SN"][:], op=ALU.mult), reads=[k("zre"), k("tab")], writes=[k("xim")])
                    P.op("pool", lambda t=t: G.tensor_tensor(out=t["t2"][:], in0=t["zim"][:], in1=t["CS"][:], op=ALU.mult), reads=[k("zim"), k("tab"), k("zim")], writes=[k("t2")])
                    P.op("pool", lambda t=t: G.tensor_tensor(out=t["xim"][:], in0=t["xim"][:], in1=t["t2"][:], op=ALU.add), reads=[k("xim"), k("t2")], writes=[k("xim")])

                    yb_ = 7 if pr == 0 else 3

                    def mmy(t=t, pr=pr, yb_=yb_):
                        T.matmul(B[yb_][0:32, :], lhsT=t["CrBD"][:], rhs=t["xre"][:], start=True, stop=False)
                        return T.matmul(B[yb_][0:32, :], lhsT=t["CiBD"][:], rhs=t["xim"][:], start=False, stop=True)
                    P.op("pe", mmy, reads=[k("xre"), k("xim"), k("CrBD"), k("CiBD")], writes=[f"b{yb_}"])
                    P.op("dve", lambda pr=pr, yb_=yb_: V.scalar_tensor_tensor(out=yT[pr][:], in0=uT[pr][:], scalar=d_col[pr][:, 0:1], in1=B[yb_][0:32, :],
                                                                             op0=ALU.mult, op1=ALU.add),
                         reads=[f"b{yb_}", ("uT", pr), ("dcol", pr)], writes=[("yT", pr)])
                for pr_ in range(2):
                    s5_stream(pr_)
                pvy = B[4][:, 0:256].rearrange("p (j d) -> p j d", j=4)

                def try4(pvy=pvy):
                    for pr in range(2):
                        for j in range(4):
                            ins = T.transpose(out=pvy[:, j, pr * 32:(pr + 1) * 32], in_=yT[pr][:, j * 128:(j + 1) * 128], identity=ident32[0:32, 0:32])
                    return ins
                P.op("pe", try4, reads=[("yT", 0), ("yT", 1), "ident32"], writes=["b4"])
                P.op("act", lambda pvy=pvy, hs=hs: A.copy(out=yc_tm[hs][:], in_=pvy), reads=["b4"], writes=[("yc_tm", hs)])
                dma(P, "poolq", yc_o[cols, :].rearrange("(j p) d -> p j d", p=128), yc_tm[hs][:], reads=[("yc_tm", hs)], writes=[("yc_o", st)])
                outs.append(("yc_o", st))
            if do_gdn:
                gdn_supertile(P, nc, B, st, hs, hk, hTt, wg_t, wt_t, cvw_t, xraw, cvt, qkv, sqn, rn_, Bones, eps_rms, alog_t, dtb_t, ng_t,
                              Tri, SL, Mask2, ones64, ident32, Sst, gd, ybuf, yb_o, outs)
                try_ag()

        if do_gdn:
            gdn_round(P, gd, [], yb_o, outs)
            try_ag()
            assert ag_stream is None or ag_n[0] == len(ag_stream[0])
        if do_attn:
            scale = 32 ** -0.5
            allqk = [("QT", s_) for s_ in range(nst)] + [("KT", s_) for s_ in range(nst)] + [("V", s_, p_) for s_ in range(nst) for p_ in range(2)] + ["Vones"]
            cnt = 0
            for u in range(3):
                for qt in range(nst):
                    nkb = 4 * (qt + 1)
                    bo = 3 + (qt % 2)
                    pend = []

                    def issue_s(kb, u=u, qt=qt):
                        nonlocal cnt
                        slot = SBANKS[cnt % len(SBANKS)]
                        ps_ = cnt % NPT
                        cnt += 1
                        P.op("pe", lambda: T.matmul(B[slot][:], lhsT=KT[32 * u:32 * u + 32, kb * 128:(kb + 1) * 128],
                                                    rhs=QT[32 * u:32 * u + 32, qt * 512:(qt + 1) * 512], start=True, stop=True),
                             reads=allqk, writes=[f"b{slot}"])
                        P.op("act", lambda: A.activation(out=PT[ps_][:], in_=B[slot][:], func=AF.Exp, scale=scale),
                             reads=[f"b{slot}"], writes=[("PT", ps_)])
                        if kb >= 4 * qt:
                            j = kb - 4 * qt
                            P.op("pool", lambda: G.tensor_tensor(out=PT[ps_][:], in0=PT[ps_][:], in1=am_t[:, j, :], op=ALU.mult),
                                 reads=[("PT", ps_), "amask"], writes=[("PT", ps_)])
                        return ps_

                    def issue_av(kb, ps_, u=u, bo=bo, nkb=nkb):
                        P.op("pe", lambda: T.matmul(B[bo][0:65, :], lhsT=Vall[:, kb, u, :], rhs=PT[ps_][:], start=(kb == 0), stop=(kb == nkb - 1)),
                             reads=[("PT", ps_)] + allqk, writes=[f"b{bo}"])
                    for kb in range(nkb):
                        pend.append((kb, issue_s(kb)))
                        if len(pend) > ADEPTH:
                            issue_av(*pend.pop(0))
                    while pend:
                        issue_av(*pend.pop(0))
                    P.op("act", lambda bo=bo: A.copy(out=osb[:], in_=B[bo][0:65, :]), reads=[f"b{bo}"], writes=["osb"])
                    pvo = B[5][:, 0:260].rearrange("p (j d) -> p j d", j=4)

                    def tro(pvo=pvo):
                        for j in range(4):
                            ins = T.transpose(out=pvo[:, j, :], in_=osb[:, j * 128:(j + 1) * 128], identity=ident32[0:65, 0:65])
                        return ins
                    P.op("pe", tro, reads=["osb", "ident32"], writes=["b5"])
                    P.op("dve", lambda pvo=pvo: V.reciprocal(out=rec[:], in_=pvo[:, :, 64]), reads=["b5"], writes=["rec"])
                    os_ = qt % 2
                    P.op("dve", lambda pvo=pvo, os_=os_: V.tensor_tensor(out=oT[os_][:], in0=pvo[:, :, 0:64],
                                                                        in1=rec[:].unsqueeze(2).to_broadcast([128, 4, 64]), op=ALU.mult),
                         reads=["b5", "rec"], writes=[("oT", os_)])
                    dma(P, "sp", ya_o[qt * 512:(qt + 1) * 512, u, :].rearrange("(j p) d -> p j d", p=128), oT[os_][:],
                        reads=[("oT", os_)], writes=[("ya_o", u, qt)])
                    outs.append(("ya_o", u, qt))
        P.emit(final_wait_keys=outs)


def gdn_supertile(P, nc, B, st, hs, hk, hTt, wg_t, wt_t, cvw_t, xraw, cvt, qkv, sqn, rn_, Bones, eps_rms, nA_t, dtb_t, ng_t,
                  Tri, SL, Mask2, ones64, ident32, Sst, gd, ybuf, yb_o, outs):
    V, G, A, T = nc.vector, nc.gpsimd, nc.scalar, nc.tensor
    for c in range(3):
        def mm(c=c):
            for kc in range(8):
                ins = T.matmul(B[c][:], lhsT=wg_t[:, kc, c * 128:(c + 1) * 128], rhs=hTt[hs][:, kc, :], start=(kc == 0), stop=(kc == 7))
            return ins
        P.op("pe", mm, reads=[hk, "wg"], writes=[f"b{c}"])
        P.op("pool", lambda c=c: G.tensor_copy(out=xraw[c][:, 0:3], in_=xraw[c][:, 512:515]), reads=[("xraw", c)], writes=[("xraw", c)])
        P.op("act", lambda c=c: A.copy(out=xraw[c][:, 3:515], in_=B[c][:]), reads=[f"b{c}", ("xraw", c)], writes=[("xraw", c)])
        P.op("dve", lambda c=c: V.tensor_scalar(out=cvt[:], in0=xraw[c][:, 0:512], scalar1=cvw_t[:, c, 0:1], scalar2=None, op0=ALU.mult),
             reads=[("xraw", c), "cvw"], writes=["cvt"])
        for kk in range(1, 4):
            P.op("dve", lambda c=c, kk=kk: V.scalar_tensor_tensor(out=cvt[:], in0=xraw[c][:, kk:kk + 512], scalar=cvw_t[:, c, kk:kk + 1],
                                                                  in1=cvt[:], op0=ALU.mult, op1=ALU.add),
                 reads=[("xraw", c), "cvw", "cvt"], writes=["cvt"])
        P.op("act", lambda c=c: A.activation(out=qkv[c][:], in_=cvt[:], func=AF.Silu), reads=["cvt"], writes=[("qkv", c)])
    for c in range(2):
        P.op("pool", lambda c=c: G.tensor_tensor(out=sqn[:], in0=qkv[c][:], in1=qkv[c][:], op=ALU.mult), reads=[("qkv", c)], writes=["sqn"])
        P.op("pe", lambda: T.matmul(B[3][:], lhsT=Bones[:], rhs=sqn[:], start=True, stop=True), reads=["sqn", "Bones"], writes=["b3"])
        P.op("act", lambda: A.activation(out=rn_[:], in_=B[3][:], func=AF.Sqrt, bias=eps_rms[:, 0:1], scale=1.0), reads=["b3", "eps_rms"], writes=["rn"])
        P.op("dve", lambda: V.reciprocal(out=rn_[:], in_=rn_[:]), reads=["rn"], writes=["rn"])
        if c == 0:
            P.op("dve", lambda: V.scalar_tensor_tensor(out=qkv[0][:], in0=qkv[0][:], scalar=0.125, in1=rn_[:], op0=ALU.mult, op1=ALU.mult),
                 reads=[("qkv", 0), "rn"], writes=[("qkv", 0)])
        else:
            P.op("dve", lambda: V.tensor_tensor(out=qkv[1][:], in0=qkv[1][:], in1=rn_[:], op=ALU.mult), reads=[("qkv", 1), "rn"], writes=[("qkv", 1)])
    qk_all = [("qkv", 0), ("qkv", 1), ("qkv", 2)]
    yb_s = st % 2
    for cp in range(4):
        new = []
        for c in (2 * cp, 2 * cp + 1):
            cg = st * 8 + c
            cs = slice(c * 64, (c + 1) * 64)
            dch = gd["ch"][cg % 4]
            kch = lambda n, cg=cg: ("gch", cg % 4, n)

            def mmt(cs=cs):
                for kc in range(8):
                    ins = T.matmul(B[0][0:64, 0:132], lhsT=hTt[hs][:, kc, cs], rhs=wt_t[:, kc, :], start=(kc == 0), stop=(kc == 7))
                return ins
            mk = ["b0"]
            P.op("pe", mmt, reads=[hk, "wt"], writes=mk)
            P.op("act", lambda dch=dch: A.activation(out=dch["gs"][:], in_=B[0][0:64, 0:128], func=AF.Silu), reads=mk, writes=[kch("gs")])
            P.op("act", lambda dch=dch: A.activation(out=dch["bg"][:, 0:2], in_=B[0][0:64, 128:130], func=AF.Sigmoid), reads=mk, writes=[kch("bg")])
            P.op("dve", lambda dch=dch: V.tensor_tensor(out=dch["bg"][:, 2:4], in0=B[0][0:64, 130:132], in1=dtb_t[:], op=ALU.add),
                 reads=mk + ["dtb", kch("bg")], writes=[kch("bg")])
            P.op("act", lambda dch=dch: A.activation(out=dch["bg"][:, 2:4], in_=dch["bg"][:, 2:4], func=AF.Exp), reads=[kch("bg")], writes=[kch("bg")])
            P.op("act", lambda dch=dch: A.activation(out=dch["bg"][:, 2:4], in_=dch["bg"][:, 2:4], func=AF.Ln, bias=1.0, scale=1.0), reads=[kch("bg")], writes=[kch("bg")])
            P.op("dve", lambda dch=dch: V.tensor_tensor(out=dch["bg"][:, 2:4], in0=dch["bg"][:, 2:4], in1=nA_t[:], op=ALU.mult),
                 reads=[kch("bg"), "alog"], writes=[kch("bg")])
            P.op("dve", lambda dch=dch: V.tensor_scalar(out=dch["nbeta"][:], in0=dch["bg"][:, 0:2], scalar1=-1.0, scalar2=None, op0=ALU.mult),
                 reads=[kch("bg")], writes=[kch("nbeta")])
            sl0 = (cg % 4) * 2
            new.append([gdn_chunk_head(P, nc, B, h, cs, c, dch, kch, gd["hd"][sl0 + h], sl0 + h, 4 + (cg % 2) * 2 + h, qkv, qk_all, ng_t, Tri, SL, Mask2,
                                       ones64, ident32, Sst, eps_rms, ybuf[yb_s], yb_s) for h in range(2)])
        gdn_round(P, gd, new, yb_o, outs)
        if cp == 3:
            gd["pend_dma"] = (st, yb_s, ybuf[yb_s])


def gdn_round(P, gd, new, yb_o, outs):
    oldg = list(gd.get("pendB", []))
    had_old = bool(oldg)
    actA = [g for grp in new for g in grp]
    curB = oldg.pop(0) if oldg else []
    while actA or curB:
        for g in list(actA):
            try:
                r = next(g)
            except StopIteration:
                raise RuntimeError("chain ended inside stage A")
            if r == "END_A":
                actA.remove(g)
        for g in list(curB):
            try:
                next(g)
            except StopIteration:
                curB.remove(g)
        if not curB and oldg:
            curB = oldg.pop(0)
    gd["pendB"] = [list(grp) for grp in new]
    pd = gd.get("pend_dma")
    if pd is not None and had_old:
        st, yb_s, ybt = pd
        cols = slice(st * 512, (st + 1) * 512)
        dma(P, "poolq", yb_o[cols].rearrange("(c p) h d -> p c h d", p=64), ybt[:], reads=[("ybuf", yb_s, c_, h_) for c_ in range(8) for h_ in range(2)],
            writes=[("yb_o", st)])
        outs.append(("yb_o", st))
        gd["pend_dma"] = None


def gdn_chunk_head(P, nc, B, h, cs, c, dch, kch, d, sl, bank, qkv, qk_all, ng_t, Tri, SL, Mask2, ones64, ident32, Sst, eps_rms, ybuf, yb_s):
    V, G, A, T = nc.vector, nc.gpsimd, nc.scalar, nc.tensor
    hp = slice(h * 64, (h + 1) * 64)
    idh = ident32[hp, hp]
    id0 = ident32[0:64, 0:64]
    k = lambda n: ("ghd", sl, n)
    PA, P3 = B[bank], B[3]
    bk, b3 = [f"b{bank}"], ["b3"]
    o3 = 256 * h
    W = d["W"]
    g_col = dch["bg"][:, 2 + h:3 + h]
    beta_col = dch["bg"][:, h:h + 1]
    nbeta_col = dch["nbeta"][:, h:h + 1]

    def tr1():
        for c3 in range(3):
            ins = T.transpose(out=PA[0:64, c3 * 64:(c3 + 1) * 64], in_=qkv[c3][hp, cs], identity=idh)
        return ins
    P.op("pe", tr1, reads=qk_all + ["ident32"], writes=bk); yield
    P.op("act", lambda: A.copy(out=d["qkv_tm"][:].rearrange("p a b -> p (a b)"), in_=PA[0:64, 0:192]), reads=bk, writes=[k("qkv_tm")]); yield

    def mm2():
        T.matmul(PA[0:64, 256:257], lhsT=Tri[:], rhs=g_col, start=True, stop=True)
        return T.matmul(PA[0:64, 257:258], lhsT=ones64[:], rhs=g_col, start=True, stop=True)
    P.op("pe", mm2, reads=[kch("bg"), "Tri", "ones64"], writes=bk); yield
    P.op("dve", lambda: V.tensor_copy(out=d["gcl"][:], in_=PA[0:64, 256:258]), reads=bk, writes=[k("gcl")]); yield
    P.op("act", lambda: A.activation(out=d["ex3"][:, 0:1], in_=d["gcl"][:, 0:1], func=AF.Exp), reads=[k("gcl")], writes=[k("ex3")]); yield
    P.op("act", lambda: A.activation(out=d["ex3"][:, 1:2], in_=d["gcl"][:, 0:1], func=AF.Exp, bias=d["gcl"][:, 1:2], scale=-1.0),
         reads=[k("gcl"), k("ex3")], writes=[k("ex3")]); yield
    P.op("act", lambda: A.activation(out=d["ex3"][:, 2:3], in_=d["gcl"][:, 1:2], func=AF.Exp), reads=[k("gcl"), k("ex3")], writes=[k("ex3")]); yield
    P.op("dve", lambda: V.tensor_scalar(out=d["Gm"][:], in0=Tri[:], scalar1=g_col, scalar2=None, op0=ALU.mult), reads=["Tri", kch("bg")], writes=[k("Gm")]); yield

    def mm3():
        T.matmul(PA[0:64, 384:448], lhsT=d["Gm"][:], rhs=SL[:], start=True, stop=True)
        return T.matmul(PA[0:64, 448:512], lhsT=SL[:], rhs=d["Gm"][:], start=True, stop=True)
    P.op("pe", mm3, reads=[k("Gm"), "SL"], writes=bk); yield
    P.op("act", lambda: A.activation(out=d["EE"][:].rearrange("p a b -> p (a b)"), in_=PA[0:64, 384:512], func=AF.Exp), reads=bk, writes=[k("EE")]); yield
    P.op("pool", lambda: G.tensor_tensor(out=d["EE"][:], in0=d["EE"][:], in1=Mask2[:], op=ALU.mult), reads=[k("EE"), "Mask2"], writes=[k("EE")]); yield

    def mm4():
        T.matmul(PA[0:64, 0:64], lhsT=qkv[1][hp, cs], rhs=qkv[1][hp, cs], start=True, stop=True)
        return T.matmul(PA[0:64, 64:128], lhsT=qkv[1][hp, cs], rhs=qkv[0][hp, cs], start=True, stop=True)
    P.op("pe", mm4, reads=qk_all, writes=bk); yield
    P.op("dve", lambda: V.scalar_tensor_tensor(out=W[0][:, 128:192], in0=PA[0:64, 0:64], scalar=nbeta_col, in1=d["EE"][:, 0, :], op0=ALU.mult, op1=ALU.mult),
         reads=bk + [kch("nbeta"), k("EE")], writes=[k("W0p")]); yield
    P.op("dve", lambda: V.tensor_tensor(out=d["AT"][:], in0=PA[0:64, 64:128], in1=d["EE"][:, 1, :], op=ALU.mult), reads=bk + [k("EE")], writes=[k("AT")]); yield
    P.op("pe", lambda: T.transpose(out=PA[0:64, 192:256], in_=W[0][:, 128:192], identity=id0), reads=[k("W0p"), "ident32"], writes=bk); yield
    P.op("act", lambda: A.copy(out=W[0][:, 192:256], in_=PA[0:64, 192:256]), reads=bk, writes=[k("W0t")]); yield
    P.op("dve", lambda: V.tensor_tensor(out=d["tb"][:], in0=beta_col, in1=d["ex3"][:, 0:1], op=ALU.mult), reads=[kch("bg"), k("ex3")], writes=[k("tb")]); yield
    P.op("dve", lambda: V.tensor_scalar(out=W[0][:, 0:64], in0=d["qkv_tm"][:, 2, :], scalar1=beta_col, scalar2=None, op0=ALU.mult),
         reads=[k("qkv_tm"), kch("bg")], writes=[k("W0x")]); yield
    P.op("dve", lambda: V.tensor_scalar(out=W[0][:, 64:128], in0=d["qkv_tm"][:, 1, :], scalar1=d["tb"][:, 0:1], scalar2=None, op0=ALU.mult),
         reads=[k("qkv_tm"), k("tb"), k("W0x")], writes=[k("W0x")]); yield
    wk = [[k("W0x"), k("W0p"), k("W0t")], [k("W1")]]
    for lvl in range(6):
        s_, d_ = W[lvl % 2], W[(lvl + 1) % 2]
        last = lvl == 5

        def mml(s_=s_, last=last):
            T.matmul(PA[0:64, 256:384], lhsT=s_[:, 192:256], rhs=s_[:, 0:128], start=True, stop=False)
            ins = T.matmul(PA[0:64, 256:384], lhsT=id0, rhs=s_[:, 0:128], start=False, stop=True)
            if not last:
                T.matmul(PA[0:64, 384:448], lhsT=s_[:, 192:256], rhs=s_[:, 128:192], start=True, stop=True)
                ins = T.matmul(PA[0:64, 448:512], lhsT=s_[:, 128:192], rhs=s_[:, 192:256], start=True, stop=True)
            return ins
        P.op("pe", mml, reads=wk[lvl % 2] + ["ident32"], writes=bk); yield
        n_ = 128 if last else 256
        wkeys = [k("W1")] if (lvl + 1) % 2 == 1 else [k("W0x"), k("W0p"), k("W0t")]
        if lvl % 2 == 0:
            P.op("act", lambda d_=d_, n_=n_: A.copy(out=d_[:, 0:n_], in_=PA[0:64, 256:256 + n_]), reads=bk, writes=wkeys); yield
        else:
            P.op("dve", lambda d_=d_, n_=n_: V.tensor_copy(out=d_[:, 0:n_], in_=PA[0:64, 256:256 + n_]), reads=bk, writes=wkeys); yield
    X = W[0]
    xk = [k("W0x"), k("W0p"), k("W0t")]
    P.op("pool", lambda: G.tensor_scalar(out=d["kdec"][:], in0=d["qkv_tm"][:, 1, :], scalar1=d["ex3"][:, 1:2], scalar2=None, op0=ALU.mult),
         reads=[k("qkv_tm"), k("ex3")], writes=[k("kdec")]); yield
    P.op("pool", lambda: G.tensor_scalar(out=d["qdec"][:], in0=d["qkv_tm"][:, 0, :], scalar1=d["ex3"][:, 0:1], scalar2=None, op0=ALU.mult),
         reads=[k("qkv_tm"), k("ex3")], writes=[k("qdec")]); yield

    def tr8():
        T.transpose(out=PA[0:64, 0:64], in_=X[:, 64:128], identity=id0)
        return T.transpose(out=PA[0:64, 64:128], in_=d["qdec"][:], identity=id0)
    P.op("pe", tr8, reads=xk + [k("qdec"), "ident32"], writes=bk); yield
    P.op("act", lambda: A.copy(out=d["wqT"][:].rearrange("p a b -> p (a b)"), in_=PA[0:64, 0:128]), reads=bk, writes=[k("wqT")]); yield
    P.op("pool", lambda: G.tensor_tensor(out=d["ngate"][:], in0=dch["gs"][:, h * 64:(h + 1) * 64], in1=ng_t[:], op=ALU.mult),
         reads=[kch("gs"), "ngt"], writes=[k("ngate")]); yield
    yield "END_A"
    S_ = Sst[h]
    P.op("pe", lambda: T.matmul(P3[0:64, o3:o3 + 64], lhsT=d["wqT"][:, 0, :], rhs=S_[:], start=True, stop=True), reads=[k("wqT"), ("S", h)], writes=b3); yield
    P.op("dve", lambda: V.tensor_tensor(out=d["vnew"][:], in0=X[:, 0:64], in1=P3[0:64, o3:o3 + 64], op=ALU.subtract),
         reads=b3 + xk, writes=[k("vnew")]); yield

    def mmo():
        T.matmul(P3[0:64, o3 + 64:o3 + 128], lhsT=d["wqT"][:, 1, :], rhs=S_[:], start=True, stop=False)
        T.matmul(P3[0:64, o3 + 64:o3 + 128], lhsT=d["AT"][:], rhs=d["vnew"][:], start=False, stop=True)
        return T.matmul(P3[0:64, o3 + 128:o3 + 192], lhsT=d["kdec"][:], rhs=d["vnew"][:], start=True, stop=True)
    P.op("pe", mmo, reads=[k("wqT"), ("S", h), k("AT"), k("vnew"), k("kdec")], writes=b3); yield
    P.op("dve", lambda: V.scalar_tensor_tensor(out=S_[:], in0=S_[:], scalar=d["ex3"][:, 2:3], in1=P3[0:64, o3 + 128:o3 + 192], op0=ALU.mult, op1=ALU.add),
         reads=b3 + [("S", h), k("ex3")], writes=[("S", h)]); yield
    P.op("act", lambda: A.copy(out=d["osb"][:], in_=P3[0:64, o3 + 64:o3 + 128]), reads=b3, writes=[k("osb")]); yield
    P.op("pool", lambda: G.tensor_tensor(out=d["osq"][:], in0=d["osb"][:], in1=d["osb"][:], op=ALU.mult), reads=[k("osb")], writes=[k("osq")]); yield
    P.op("dve", lambda: V.tensor_reduce(out=d["oss"][:], in_=d["osq"][:], axis=AX.X, op=ALU.add), reads=[k("osq")], writes=[k("oss")]); yield
    P.op("act", lambda: A.activation(out=d["oss"][:], in_=d["oss"][:], func=AF.Sqrt, bias=eps_rms[0:64, 0:1], scale=1.0 / 64),
         reads=[k("oss"), "eps_rms"], writes=[k("oss")]); yield
    P.op("dve", lambda: V.reciprocal(out=d["oss"][:], in_=d["oss"][:]), reads=[k("oss")], writes=[k("oss")]); yield
    P.op("dve", lambda: V.scalar_tensor_tensor(out=ybuf[:, c, h, :], in0=d["osb"][:], scalar=d["oss"][:, 0:1], in1=d["ngate"][:], op0=ALU.mult, op1=ALU.mult),
         reads=[k("osb"), k("oss"), k("ngate")], writes=[("ybuf", yb_s, c, h)]); yield


OFF_AQ, OFF_AK, OFF_AV, OFF_BQKV, OFF_BGATE, OFF_BBETA, OFF_BA, OFF_CU = 0, 384, 768, 1152, 2304, 2688, 2694, 2700
GDN_HEADS_OF = [(0, 1), (2, 3), (4, 5), (4, 5)]


def _wl(w, cols):
    return w[:, cols].reshape(8, 128, len(cols)).transpose(1, 0, 2)


def mix_consts():
    d = {}
    k = np.arange(128)[:, None, None]; j = np.arange(4)[None, :, None]; q = np.arange(512)[None, None, :]
    d["amask"] = bf((q // 64 >= (j * 128 + k) // 64).astype(np.float32))
    d["idn32"] = f32c(np.eye(128))
    d["srow"] = f32c(np.arange(512).reshape(1, 512))
    m = np.arange(64)[:, None]; i = np.arange(64)[None, :]
    d["cTri"] = f32c(m <= i)
    d["cSL"] = f32c(m > i)
    d["cMask2"] = f32c(np.stack([(m > i), (m <= i)], axis=1))
    bo = np.zeros((128, 128), np.float32); bo[:64, :64] = 1; bo[64:, 64:] = 1
    d["cBones"] = bo
    return d


def mix_inputs(l, p, j):
    w = p["w_in"][l]
    d = {}
    units = [3 * j + i for i in range(3)]
    qc, kc_, vc = [], [], []
    for u in units:
        head, mp = u // 2, u % 2
        qc += list(range(OFF_AQ + head * 64 + mp * 32, OFF_AQ + head * 64 + mp * 32 + 32))
        kc_ += list(range(OFF_AK + head * 64 + mp * 32, OFF_AK + head * 64 + mp * 32 + 32))
        vc += list(range(OFF_AV + head * 64, OFF_AV + head * 64 + 64))
    d["wq"] = bf(_wl(w, qc)); d["wk"] = bf(_wl(w, kc_)); d["wv"] = bf(_wl(w, vc))
    gs = [4 * j + i for i in range(4)]
    d["wu"] = bf(_wl(w, list(range(OFF_CU + gs[0] * 16, OFF_CU + gs[0] * 16 + 64))))
    lre, lim, ldt = p["s5_lambda_re"][l], p["s5_lambda_im"][l], p["s5_log_dt"][l]
    row = np.zeros((2, 3, 128), np.float32)
    bT = np.zeros((2, 2, 2, 16, 64), np.float32); cT = np.zeros((2, 2, 2, 64, 16), np.float32)
    for pr in range(2):
        for g in range(2):
            G_ = gs[pr * 2 + g]
            row[pr, 0, g * 64:(g + 1) * 64] = lre[G_]; row[pr, 1, g * 64:(g + 1) * 64] = lim[G_]; row[pr, 2, g * 64:(g + 1) * 64] = ldt[G_]
            bT[pr, g, 0] = p["s5_b_re"][l][G_].T; bT[pr, g, 1] = p["s5_b_im"][l][G_].T
            cT[pr, g, 0] = p["s5_c_re"][l][G_].T; cT[pr, g, 1] = p["s5_c_im"][l][G_].T
    d["s5row"] = row; d["s5col"] = f32c(row.transpose(0, 2, 1)); d["s5bT"] = bT; d["s5cT"] = cT
    d["s5d"] = f32c(p["s5_d"][l][gs[0] * 16:gs[0] * 16 + 64].reshape(64, 1))
    hA, hB = GDN_HEADS_OF[j]
    gcols = []
    for part in range(3):
        for h in (hA, hB):
            gcols += list(range(OFF_BQKV + part * 384 + h * 64, OFF_BQKV + part * 384 + h * 64 + 64))
    d["wg"] = bf(_wl(w, gcols))
    tcols = list(range(OFF_BGATE + hA * 64, OFF_BGATE + hA * 64 + 64)) + list(range(OFF_BGATE + hB * 64, OFF_BGATE + hB * 64 + 64)) \
        + [OFF_BBETA + hA, OFF_BBETA + hB, OFF_BA + hA, OFF_BA + hB]
    d["wt"] = bf(_wl(w, tcols))
    cw = p["dn_conv_w"][l]
    cv = np.zeros((128, 3, 4), np.float32)
    for part in range(3):
        for hi, h in enumerate((hA, hB)):
            cv[hi * 64:(hi + 1) * 64, part, :] = cw[:, part * 384 + h * 64: part * 384 + h * 64 + 64].T
    d["cvw"] = cv
    d["galog"] = f32c(p["dn_a_log"][l][[hA, hB]].reshape(1, 2)); d["gdtb"] = f32c(p["dn_dt_bias"][l][[hA, hB]].reshape(1, 2))
    d["gng"] = f32c(p["dn_norm_g"][l].reshape(1, 64))
    return d


YCH = 1024
NYCH = SEQ // YCH


def build_program(stop=None):
    nc = bass.Bass("TRN2", target_bir_lowering=False)
    G = Glob(nc)
    hbuf = [G.internal(f"hbuf{i}", [TOK_CORE, D]) for i in range(2)]
    hT_loc = G.internal("hT_loc", [TOK_CORE // 512, D, 512], BF16)
    hT_all = G.internal("hT_all", [TOK_CORE // 512, 4 * D, 512], BF16)
    ya_o = G.internal("ya_o", [SEQ, 192])
    ybc_o = G.internal("ybc_o", [SEQ, 192])
    ya_all = G.internal("ya_all", [NYCH, 4 * YCH, 192])
    ybc_all = G.internal("ybc_all", [NYCH, 4 * YCH, 192])
    ag_h = [(hT_loc[c_], hT_all[c_]) for c_ in range(TOK_CORE // 512)]
    ag_ya = [(ya_o[i * YCH:(i + 1) * YCH, :], ya_all[i]) for i in range(NYCH)]
    ag_yb = [(ybc_o[i * YCH:(i + 1) * YCH, :], ybc_all[i]) for i in range(NYCH)]
    out = nc.dram_tensor("out", [TOK_CORE, D], F32, kind="ExternalOutput").ap()
    def with_cc(fn):
        Prog._uid += 1
        with nc.semaphore(f"cch_{Prog._uid}") as cc:
            with nc.Block() as block:
                @block.gpsimd
                def _(g):
                    g.sem_clear(cc)
            fn(cc)
    with_cc(lambda cc: phase_pre(nc, G, hbuf[0], hT_loc, (ag_h, cc)))
    for l in range(DEPTH):
        last = l == DEPTH - 1
        phase_mix(nc, G, l, hT_all, ya_o, ybc_o, do_attn=True, do_s5=False, do_gdn=False)
        Prog._uid += 1
        with nc.semaphore(f"ccy_{Prog._uid}") as cc:
            with nc.Block() as block:
                @block.gpsimd
                def _(g):
                    g.sem_clear(cc)

            def pre(g, cc=cc):
                for s_, d_ in ag_ya:
                    g.collective_compute("AllGather", ALU.bypass, replica_groups=GROUPS, ins=[s_], outs=[d_]).then_inc(cc, 1)

            def post(g, cc=cc):
                g.wait_ge(cc, len(ag_ya) + len(ag_yb))
            phase_mix(nc, G, l, hT_all, ya_o, ybc_o, do_attn=False, do_s5=True, do_gdn=True, pool_pre=pre, pool_post=post, ag_stream=(ag_yb, cc))
        if last:
            phase_post(nc, G, l, hbuf[l % 2], out, None, (ya_all, ybc_all))
        else:
            with_cc(lambda cc: phase_post(nc, G, l, hbuf[l % 2], hbuf[(l + 1) % 2], hT_loc, (ya_all, ybc_all), ag=(ag_h, cc)))
    return nc, G


def kernel(_stop=None, **inputs):
    p = {k: np.asarray(v) for k, v in inputs.items()}
    x = f32c(p["x"]).reshape(BATCH * SEQ, D)
    cores = list(range(NCORES))
    nc, G = build_program()
    shared = dict(mix_consts())
    shared["idn"] = bf(np.eye(128))
    shared["g"] = f32c(p["ln_in_g"].reshape(1, D)); shared["b"] = f32c(p["ln_in_b"].reshape(1, D))
    percore = [dict() for _ in range(4)]
    for l in range(DEPTH):
        lam_init = 0.8 - 0.6 * math.exp(-0.3 * l)
        for k, v in post_inputs(l, p, lam_init).items():
            if k not in ("idn", "idn32"):
                shared[f"L{l}_{k}"] = v
        for j in range(4):
            for k, v in mix_inputs(l, p, j).items():
                percore[j][f"L{l}_{k}"] = v
    ins = []
    for c in cores:
        d = dict(shared); d.update(percore[c % 4])
        d["x"] = x[c * TOK_CORE:(c + 1) * TOK_CORE]
        d["rofs"] = np.array([[(c % 4) * (TOK_CORE // YCH)]], np.int32)
        ins.append({k: v for k, v in d.items() if k in G.t})
    res = run_bass_kernel_spmd(nc, ins, core_ids=cores)
    h = [np.asarray(r["out"]) for r in res.results]
    return np.concatenate(h, axis=0).reshape(BATCH, SEQ, D).astype(np.float32)
```
